# Optimizing a Trainium2 kernel written in Bass

```python
import math
import jax, jax.numpy as jnp
from jax import lax
import numpy as np

D_MODEL = 1024
BATCH = 8
SEQ = 4096
DEPTH = 1

EPS = 1e-6
CHUNK = 128
GM_GROUPS = 8
GM_GROUP_DIM = 64
GM_WIDTH = GM_GROUPS * GM_GROUP_DIM
WINDOW = 128
Q_HEADS = 8
KV_HEADS = 2
Q_PER_KV = Q_HEADS // KV_HEADS
HEAD_DIM = 64
ATTN_WIDTH = Q_HEADS * HEAD_DIM
KV_WIDTH = KV_HEADS * HEAD_DIM
REL_BUCKETS = 32
REL_MAX_EXACT = REL_BUCKETS // 2
REL_MAX_DIST = 128
N_GROUPS_MOE = 4
EXPERTS_PER_GROUP = 4
N_EXPERTS = N_GROUPS_MOE * EXPERTS_PER_GROUP
TOP_K = 2
EXPERT_FF = 512
MASK_VALUE = -1e30

SPLIT_SIZES = (GM_WIDTH, GM_WIDTH, ATTN_WIDTH, KV_WIDTH, KV_WIDTH, D_MODEL, D_MODEL)
SPLIT_POINTS = tuple(int(v) for v in np.cumsum(SPLIT_SIZES)[:-1])
IN_WIDTH = int(sum(SPLIT_SIZES))

kernel_name = "hybrid_gmlp_swa_sink_hiermoe_block"


def rms_norm(x, g):
    xf = x.astype(jnp.float32)
    y = xf * lax.rsqrt(jnp.mean(xf * xf, axis=-1, keepdims=True) + EPS)
    return (y * g.astype(jnp.float32)).astype(x.dtype)


def layer_norm(x, g):
    xf = x.astype(jnp.float32)
    mu = jnp.mean(xf, axis=-1, keepdims=True)
    xc = xf - mu
    y = xc * lax.rsqrt(jnp.mean(xc * xc, axis=-1, keepdims=True) + EPS)
    return (y * g.astype(jnp.float32)).astype(x.dtype)


def t5_causal_bucket(dist):
    is_small = dist < REL_MAX_EXACT
    nf = jnp.maximum(dist, 1).astype(jnp.float32)
    large = REL_MAX_EXACT + (
        jnp.log(nf / REL_MAX_EXACT) / math.log(REL_MAX_DIST / REL_MAX_EXACT)
        * (REL_BUCKETS - REL_MAX_EXACT)).astype(jnp.int32)
    large = jnp.minimum(large, REL_BUCKETS - 1)
    return jnp.where(is_small, dist, large)


def gmlp_chunk_mixer(u_raw, v_raw, v_norm_g, w_s, b_s):
    B, S, _ = u_raw.shape
    u = jax.nn.gelu(u_raw)
    v = layer_norm(jax.nn.gelu(v_raw), v_norm_g)
    v = v.reshape(B, S // CHUNK, CHUNK, GM_GROUPS, GM_GROUP_DIM)
    causal = jnp.tril(jnp.ones((CHUNK, CHUNK), dtype=bool))
    ws = jnp.where(causal[None], w_s, jnp.zeros((), w_s.dtype)).astype(v.dtype)
    sv = jnp.einsum("gts,bcsgd->bctgd", ws, v)
    sv = sv + b_s.T.astype(v.dtype)[None, None, :, :, None]
    return u * sv.reshape(B, S, GM_WIDTH)


def swa_sink_attention(q, k, v, sinks, rel_bias):
    B, S, _ = q.shape
    nb = S // WINDOW
    q = q.reshape(B, nb, WINDOW, KV_HEADS, Q_PER_KV, HEAD_DIM)
    k = k.reshape(B, nb, WINDOW, KV_HEADS, HEAD_DIM)
    v = v.reshape(B, nb, WINDOW, KV_HEADS, HEAD_DIM)
    k_prev = jnp.concatenate([jnp.zeros_like(k[:, :1]), k[:, :-1]], axis=1)
    v_prev = jnp.concatenate([jnp.zeros_like(v[:, :1]), v[:, :-1]], axis=1)
    kb = jnp.concatenate([k_prev, k], axis=2)
    vb = jnp.concatenate([v_prev, v], axis=2)

    scores = jnp.einsum("bnqkgd,bnskd->bnkgqs", q, kb,
                        preferred_element_type=jnp.float32) * (HEAD_DIM ** -0.5)

    qi = jnp.arange(WINDOW, dtype=jnp.int32)[:, None]
    sj = jnp.arange(2 * WINDOW, dtype=jnp.int32)[None, :]
    dist = qi + WINDOW - sj
    in_window = (dist >= 0) & (dist < WINDOW)
    bucket = t5_causal_bucket(jnp.clip(dist, 0, WINDOW - 1))
    bias = rel_bias.astype(jnp.float32)[bucket]
    bias = bias.transpose(2, 0, 1).reshape(KV_HEADS, Q_PER_KV, WINDOW, 2 * WINDOW)
    blk = jnp.arange(nb, dtype=jnp.int32)[:, None, None]
    valid = in_window[None] & ((blk > 0) | (sj[None] >= WINDOW))

    scores = scores + bias[None, None]
    scores = jnp.where(valid[None, :, None, None], scores, MASK_VALUE)
    sink = sinks.astype(jnp.float32).reshape(KV_HEADS, Q_PER_KV)[None, None, :, :, None, None]
    m = jnp.maximum(jnp.max(scores, axis=-1, keepdims=True), sink)
    p = jnp.exp(scores - m)
    denom = jnp.sum(p, axis=-1, keepdims=True) + jnp.exp(sink - m)
    probs = (p / denom).astype(vb.dtype)
    out = jnp.einsum("bnkgqs,bnskd->bnqkgd", probs, vb)
    return out.reshape(B, S, ATTN_WIDTH)


def hierarchical_moe(h, rg_w, rg_b, re_w, re_b, w_gate, w_up, w_down):
    B, S, D = h.shape
    t = h.reshape(-1, D)
    g_logits = jnp.dot(t, rg_w, preferred_element_type=jnp.float32) + rg_b.astype(jnp.float32)
    g_probs = jax.nn.softmax(g_logits, axis=-1)
    g_top_p, g_top_i = lax.top_k(g_probs, 1)
    e_all = jnp.einsum("td,gde->tge", t, re_w,
                       preferred_element_type=jnp.float32) + re_b.astype(jnp.float32)
    e_logits = jnp.take_along_axis(e_all, g_top_i[:, :, None], axis=1)[:, 0]
    e_top_v, e_top_i = lax.top_k(e_logits, TOP_K)
    w_top = jax.nn.softmax(e_top_v, axis=-1) * g_top_p
    expert_id = g_top_i * EXPERTS_PER_GROUP + e_top_i
    combine = jnp.einsum("tk,tke->te", w_top,
                         jax.nn.one_hot(expert_id, N_EXPERTS, dtype=jnp.float32))
    combine = combine.astype(t.dtype)
    out = jnp.zeros_like(t)
    for e in range(N_EXPERTS):
        hid = jax.nn.silu(t @ w_gate[e]) * (t @ w_up[e])
        out = out + combine[:, e:e + 1] * (hid @ w_down[e])
    return out.reshape(B, S, D)


def setup_inputs(seed: int = 0) -> dict:
    key = jax.random.key(seed)
    ks = jax.random.split(key, 20)
    f32 = jnp.float32
    nrm = lambda k, shape, scale: jax.random.normal(k, shape, f32) * scale
    L = DEPTH
    return {
        "x": jax.random.normal(ks[0], (BATCH, SEQ, D_MODEL), f32),
        "attn_norm_g": 1.0 + nrm(ks[1], (L, D_MODEL), 0.02),
        "w_in": nrm(ks[2], (L, D_MODEL, IN_WIDTH), D_MODEL ** -0.5),
        "gm_v_norm_g": 1.0 + nrm(ks[3], (L, GM_WIDTH), 0.02),
        "gm_w_spatial": nrm(ks[4], (L, GM_GROUPS, CHUNK, CHUNK), CHUNK ** -0.5),
        "gm_b_spatial": 1.0 + nrm(ks[5], (L, GM_GROUPS, CHUNK), 0.02),
        "attn_sinks": nrm(ks[6], (L, Q_HEADS), 0.5),
        "rel_bias": nrm(ks[7], (REL_BUCKETS, Q_HEADS), 0.5),
        "w_proj_a": nrm(ks[8], (L, GM_WIDTH, D_MODEL), GM_WIDTH ** -0.5),
        "w_proj_b": nrm(ks[9], (L, ATTN_WIDTH, D_MODEL), ATTN_WIDTH ** -0.5),
        "w_out": nrm(ks[10], (L, D_MODEL, D_MODEL), D_MODEL ** -0.5),
        "ffn_norm_g": 1.0 + nrm(ks[11], (L, D_MODEL), 0.02),
        "router_group_w": nrm(ks[12], (L, D_MODEL, N_GROUPS_MOE), D_MODEL ** -0.5),
        "router_group_b": nrm(ks[13], (L, N_GROUPS_MOE), 0.01),
        "router_expert_w": nrm(ks[14], (L, N_GROUPS_MOE, D_MODEL, EXPERTS_PER_GROUP), D_MODEL ** -0.5),
        "router_expert_b": nrm(ks[15], (L, N_GROUPS_MOE, EXPERTS_PER_GROUP), 0.01),
        "expert_w_gate": nrm(ks[16], (L, N_EXPERTS, D_MODEL, EXPERT_FF), D_MODEL ** -0.5),
        "expert_w_up": nrm(ks[17], (L, N_EXPERTS, D_MODEL, EXPERT_FF), D_MODEL ** -0.5),
        "expert_w_down": nrm(ks[18], (L, N_EXPERTS, EXPERT_FF, D_MODEL), EXPERT_FF ** -0.5),
        "final_norm_g": 1.0 + nrm(ks[19], (D_MODEL,), 0.02),
    }


def reference(x, attn_norm_g, w_in, gm_v_norm_g, gm_w_spatial, gm_b_spatial, attn_sinks,
              rel_bias, w_proj_a, w_proj_b, w_out, ffn_norm_g, router_group_w, router_group_b,
              router_expert_w, router_expert_b, expert_w_gate, expert_w_up, expert_w_down,
              final_norm_g):
    h = x
    for l in range(DEPTH):
        hn = rms_norm(h, attn_norm_g[l])
        proj = hn @ w_in[l]
        u_raw, v_raw, q, k, v, gate_a, gate_b = jnp.split(proj, SPLIT_POINTS, axis=-1)
        a = gmlp_chunk_mixer(u_raw, v_raw, gm_v_norm_g[l], gm_w_spatial[l], gm_b_spatial[l])
        b = swa_sink_attention(q, k, v, attn_sinks[l], rel_bias)
        merged = (jax.nn.sigmoid(gate_a) * (a @ w_proj_a[l])
                  + jax.nn.sigmoid(gate_b) * (b @ w_proj_b[l]))
        h = h + merged @ w_out[l]
        hn = rms_norm(h, ffn_norm_g[l])
        h = h + hierarchical_moe(hn, router_group_w[l], router_group_b[l], router_expert_w[l],
                                 router_expert_b[l], expert_w_gate[l], expert_w_up[l],
                                 expert_w_down[l])
    return rms_norm(h, final_norm_g)
```

```python
import math
from contextlib import ExitStack

import numpy as np
import concourse.bass as bass
import concourse.mybir as mybir
from concourse.bass_utils import run_bass_kernel_spmd

F32 = mybir.dt.float32
BF16 = mybir.dt.bfloat16
AF = mybir.ActivationFunctionType
ALU = mybir.AluOpType
AX = mybir.AxisListType

SEQ = 4096
D = 1024
NCORES = 8
PT = 1024
NT = PT // 128
NST = PT // 512
NPASS_FULL = SEQ // PT
EPS = 1e-6
NEG = -30000.0

U_OFF, V_OFF, Q_OFF, K_OFF, VA_OFF, GA_OFF, GB_OFF = 0, 512, 1024, 1536, 1664, 1792, 2816

ENG = ("pe", "act", "dve", "pool", "sp")


class Tok:
    __slots__ = ("name", "last_w", "readers")

    def __init__(self, name):
        self.name = name
        self.last_w = None
        self.readers = []


class Op:
    __slots__ = ("eng", "fn", "deps", "sig", "dma_sem", "dma_cnt", "seq")

    def __init__(self, eng, fn):
        self.eng = eng
        self.fn = fn
        self.deps = set()
        self.sig = False
        self.dma_sem = None
        self.dma_cnt = 0
        self.seq = 0


class Sched:
    def __init__(self):
        self.ops = []
        self.per = {e: [] for e in ENG}
        self.dma_counts = {}
        self.total_keys = set()

    def op(self, eng, fn, reads=(), writes=(), dma=None):
        o = Op(eng, fn)
        for t in list(reads) + list(writes):
            if t.last_w is not None:
                o.deps.add(t.last_w)
        for t in writes:
            for r in t.readers:
                o.deps.add(r)
        o.deps.discard(o)
        for t in reads:
            t.readers.append(o)
        for t in writes:
            t.last_w = o
            t.readers = []
        if dma is not None:
            o.dma_sem = dma
            self.dma_counts[dma] = self.dma_counts.get(dma, 0) + 1
            o.dma_cnt = self.dma_counts[dma]
        self.ops.append(o)
        self.per[eng].append(o)
        return o

    def emit(self, nc, final_wait_ops=()):
        def skip(d, o):
            return d.dma_sem is None and o.dma_sem is None and d.eng == "pe" and o.eng == "pe"

        for o in self.ops:
            for d in o.deps:
                if d.dma_sem is None and not skip(d, o):
                    d.sig = True
        for o in final_wait_ops:
            if o.dma_sem is None:
                o.sig = True
        for e in ENG:
            c = 0
            for o in self.per[e]:
                if o.dma_sem is None and o.sig:
                    c += 1
                    o.seq = c
        with ExitStack() as es:
            esem = {e: es.enter_context(nc.semaphore(f"s_{e}")) for e in ENG}
            dsem = {k: es.enter_context(nc.semaphore(f"d_{k}")) for k in self.dma_counts}
            block = es.enter_context(nc.Block())

            def dval(d):
                if d.dma_sem in self.total_keys:
                    return 16 * self.dma_counts[d.dma_sem]
                return 16 * d.dma_cnt

            def need(o):
                w = {}
                for d in o.deps:
                    if d.dma_sem is not None:
                        key, val = ("d", d.dma_sem), dval(d)
                    else:
                        if skip(d, o):
                            continue
                        key, val = ("e", d.eng), d.seq
                    if w.get(key, 0) < val:
                        w[key] = val
                return w

            def run(ename):
                def body(eng):
                    waited = {}
                    for o in self.per[ename]:
                        for key, val in need(o).items():
                            if waited.get(key, 0) >= val:
                                continue
                            waited[key] = val
                            sem = dsem[key[1]] if key[0] == "d" else esem[key[1]]
                            eng.wait_ge(sem, val)
                        ins = o.fn(eng)
                        if o.dma_sem is not None:
                            ins.then_inc(dsem[o.dma_sem], 16)
                        elif o.sig:
                            ins.then_inc(esem[ename], 1)
                    if ename == "sp":
                        for o in final_wait_ops:
                            if o.dma_sem is not None:
                                eng.wait_ge(dsem[o.dma_sem], dval(o))
                            else:
                                eng.wait_ge(esem[o.eng], o.seq)
                return body

            block.tensor(run("pe"))
            block.scalar(run("act"))
            block.vector(run("dve"))
            block.gpsimd(run("pool"))
            block.sync(run("sp"))


def build(npass=NPASS_FULL, nexp=16, debug=False):
    nc = bass.Bass("TRN2", target_bir_lowering=False)
    ntok = npass * PT

    def din(name, shape):
        return nc.dram_tensor(name, list(shape), F32, kind="ExternalInput").ap()

    x_d = din("x", [SEQ, D])
    win_d = din("w_in", [D, 3840])
    wpa_d = din("wpa", [512, D])
    wpb_d = din("wpb", [512, D])
    wout_d = din("wout", [D, D])
    wg_d = din("wg", [16, D, 512])
    wu_d = din("wu", [16, D, 512])
    wd_d = din("wd", [16, 512, D])
    wr_d = din("wr", [D, 20])
    rb_d = din("rb", [1, 20])
    gacol_d = din("gacol", [128, 8])
    gfcol_d = din("gfcol", [128, 8])
    gvcol_d = din("gvcol", [128, 4])
    gfin_d = din("gfin", [1, D])
    wst_d = din("wst", [128, 8, 128])
    wmask_d = din("wmask", [128, 8, 128])
    bst_d = din("bst", [128, 4, 128])
    sinks_d = din("sinks", [1, 8])
    bias_d = din("bias", [128, 2, 2, 4, 128])
    mask_d = din("maskc", [128, 2, 2, 4, 128])
    idn_d = din("idn", [128, 128])
    y_d = nc.dram_tensor("y", [SEQ, D], F32, kind="ExternalOutput").ap()
    if debug:
        dbg_d = nc.dram_tensor("dbg", [PT, D], F32, kind="ExternalOutput").ap()

    S = Sched()
    S.total_keys.add("const")

    with ExitStack() as es:
        def sb(name, shape, dt):
            return es.enter_context(nc.sbuf_tensor(name, list(shape), dt))

        H = sb("H", [128, NT, D], F32)
        hn2T = sb("hn2T", [128, 8, PT], BF16)
        ident = sb("ident", [128, 128], BF16)
        gacol = sb("gacol_s", [128, 8], F32)
        gfcol = sb("gfcol_s", [128, 8], F32)
        gvcol = sb("gvcol_s", [128, 4], F32)
        gfin = sb("gfin_s", [128, D], F32)
        wstb = sb("wstb", [128, 8, 128], BF16)
        bst = sb("bst_s", [128, 4, 128], F32)
        sinkexp = sb("sinkexp", [64, 8], F32)
        BM = sb("BM", [128, 2, 2, 4, 128], F32)
        ones64 = sb("ones64", [128, 64], BF16)
        wr = sb("wr_s", [128, 8, 20], BF16)
        rb = sb("rb_s", [128, 20], F32)
        wv = sb("wv_s", [128, 8, 512], BF16)
        wva = sb("wva_s", [128, 8, 128], BF16)
        NEB = 2
        wreg = sb("wreg", [128, NEB * 12288], BF16)
        wgs = [wreg[:, b * 12288:b * 12288 + 4096].rearrange("p (c n) -> p c n", c=8) for b in range(NEB)]
        wus = [wreg[:, b * 12288 + 4096:b * 12288 + 8192].rearrange("p (c n) -> p c n", c=8) for b in range(NEB)]
        wds = [wreg[:, b * 12288 + 8192:b * 12288 + 12288].rearrange("p (c n) -> p c n", c=4) for b in range(NEB)]
        wpa = wreg[:, 0:4096].rearrange("p (c n) -> p c n", c=4)
        wout = wreg[:, 4096:12288].rearrange("p (c n) -> p c n", c=8)
        wpb = wreg[0:64, 12288:20480].rearrange("p (c n) -> p c n", c=8)
        NRING = 4
        ring = [sb(f"ring{i}", [128, 8, 128], BF16) for i in range(NRING)]
        hn = sb("hn", [128, D], BF16)
        hnT = sb("hnT", [128, 8, 512], BF16)
        wst = hnT[:, 0:2, :].rearrange("p a (b t) -> p (a b) t", b=4)
        wmask = hnT[:, 2:4, :].rearrange("p a (b t) -> p (a b) t", b=4)
        ss = sb("ss", [128, 8], F32)
        rstd = sb("rstd", [128, 8], F32)
        uT = sb("uT", [128, 4, 512], BF16)
        qT = sb("qT", [128, 4, 512], BF16)
        kT = sb("kT", [128, (NT + 1) * 128], BF16)
        vatt = sb("vatt", [128, NT + 1, 128], BF16)
        gv = sb("gv", [128, 512], F32)
        vnh = sb("vnh", [128, 512], BF16)
        bnst = sb("bnst", [128, 6], F32)
        mv = sb("mv", [128, 2], F32)
        rsv = sb("rsv", [128, 1], F32)
        nmr = sb("nmr", [128, 1], F32)
        gtmp = sb("gtmp", [128, 4, 128], F32)
        sc0_ = sb("sc0", [128, 512], F32)
        sc = [sc0_, sc0_]
        pT = [sb(f"pT{i}", [128, 512], BF16) for i in range(2)]
        den = sb("den", [64, 512], F32)
        aT = sb("aT", [128, 4, 512], BF16)
        bT = sb("bT", [64, 8, 512], BF16)
        sga = sb("sga", [128, 512], F32)
        sgb = sb("sgb", [128, 512], F32)
        mT = sb("mT", [128, 8, 512], BF16)
        maskc = mT[:, 0:4, :].rearrange("p a (b c t) -> p (a b c) t", b=2, c=2).rearrange("p (v k h) t -> p v k h t", v=2, k=2)
        lg = sb("lg", [128, NT, 20], F32)
        r_m = sb("r_m", [128, NT], F32)
        r_og = sb("r_og", [128, NT, 4], F32)
        r_eg = sb("r_eg", [128, NT, 4], F32)
        r_gp = sb("r_gp", [128, NT], F32)
        r_sel = sb("r_sel", [128, NT, 4, 4], F32)
        r_es = sb("r_es", [128, NT, 4], F32)
        r_m1 = sb("r_m1", [128, NT], F32)
        r_o1 = sb("r_o1", [128, NT, 4], F32)
        r_es2 = sb("r_es2", [128, NT, 4], F32)
        r_m2 = sb("r_m2", [128, NT], F32)
        r_o2 = sb("r_o2", [128, NT, 4], F32)
        r_d = sb("r_d", [128, NT], F32)
        r_w1 = sb("r_w1", [128, NT], F32)
        r_w2 = sb("r_w2", [128, NT], F32)
        r_cl = sb("r_cl", [128, NT, 4], F32)
        r_cl2 = sb("r_cl2", [128, NT, 4], F32)
        call = sb("call", [128, NT, 16], F32)
        sgs = [sb(f"sgs{i}", [128, 512], F32) for i in range(2)]
        hid = [sb(f"hid{i}", [128, 4, 512], BF16) for i in range(2)]
        PB = [es.enter_context(nc.psum_tensor(f"pb{i}", [128, 512], F32)) for i in range(8)]

        T = {}

        def tk(n):
            if n not in T:
                T[n] = Tok(n)
            return T[n]

        tH = [tk(f"H{i}") for i in range(NT)]
        tPB = [tk(f"PB{i}") for i in range(8)]
        thn2T = [tk(f"hn2T{g}") for g in range(NST)]
        tE = [[tk(f"wg{i}"), tk(f"wu{i}"), tk(f"wd{i}")] for i in range(NEB)]
        tRing = [tk(f"ring{i}") for i in range(NRING)]

        def op(eng, fn, reads=(), writes=(), dma=None):
            return S.op(eng, fn, [tk(r) if isinstance(r, str) else r for r in reads],
                        [tk(w) if isinstance(w, str) else w for w in writes], dma)

        def cdma(eng, out, in_, w):
            op(eng, lambda e, out=out, in_=in_: e.dma_start(out=out, in_=in_), writes=[w], dma="const")

        cdma("sp", gacol[:], gacol_d, "gacol")
        cdma("sp", gfcol[:], gfcol_d, "gfcol")
        cdma("sp", gvcol[:], gvcol_d, "gvcol")
        cdma("sp", gfin[:], gfin_d.partition_broadcast(128), "gfin")
        cdma("pool", wst, wst_d, "wst_a")
        cdma("sp", bst[:], bst_d, "bst")
        cdma("sp", sinkexp[:], sinks_d.partition_broadcast(64), "sinkexp")
        cdma("sp", BM[:], bias_d, "BM")
        cdma("sp", rb[:], rb_d.partition_broadcast(128), "rb")
        cdma("pool", ident[:], idn_d, "ident")
        cdma("pool", wmask, wmask_d, "wmask_a")
        cdma("pool", maskc, mask_d, "maskc_a")
        cdma("pool", wr[:], wr_d.rearrange("(c p) n -> p c n", p=128), "wr")
        cdma("pool", wv[:], win_d[:, V_OFF:V_OFF + 512].rearrange("(c p) n -> p c n", p=128), "wv")
        cdma("pool", wva[:], win_d[:, VA_OFF:VA_OFF + 128].rearrange("(c p) n -> p c n", p=128), "wva")
        op("dve", lambda e: e.tensor_tensor(out=wstb[:], in0=wst, in1=wmask, op=ALU.mult),
           reads=["wst_a", "wmask_a"], writes=["wstb", "hnT"])
        op("dve", lambda e: e.tensor_tensor(out=BM[:], in0=BM[:], in1=maskc, op=ALU.add),
           reads=["maskc_a"], writes=["BM", "mT"])
        op("act", lambda e: e.activation(out=sinkexp[:], in_=sinkexp[:], func=AF.Exp),
           reads=[], writes=["sinkexp"])
        op("dve", lambda e: e.memset(ones64[:], 1.0), writes=["ones64"])

        out_ops = []

        def rstd_ops(n):
            op("dve", lambda e: e.tensor_scalar(out=rstd[:, 0:n], in0=ss[:, 0:n], scalar1=1.0 / D, scalar2=EPS,
                                                op0=ALU.mult, op1=ALU.add), reads=["ss"], writes=["rstd"])
            op("act", lambda e: e.sqrt(out=rstd[:, 0:n], in_=rstd[:, 0:n]), reads=[], writes=["rstd"])
            op("dve", lambda e: e.reciprocal(out=rstd[:, 0:n], in_=rstd[:, 0:n]), reads=[], writes=["rstd"])

        def norm_transpose(ti_list, src_of, gcol, dstT, dst_col0, dst_tok):
            n = len(ti_list)
            for k, i in enumerate(ti_list):
                op("act", lambda e, i=i, k=k: e.activation(out=hn[:], in_=H[:, i, :], func=AF.Square,
                                                          accum_out=ss[:, k:k + 1]),
                   reads=[tH[i]], writes=["hn", "ss"])
            rstd_ops(n)
            for k, i in enumerate(ti_list):
                op("act", lambda e, i=i, k=k: e.activation(out=hn[:], in_=H[:, i, :], func=AF.Identity,
                                                          scale=rstd[:, k:k + 1]),
                   reads=[tH[i], "rstd"], writes=["hn"])
                pbT = PB[0][:].bitcast(BF16).rearrange("p (c t) -> p c t", c=8)
                for c in range(8):
                    op("pe", lambda e, c=c, pbT=pbT: e.transpose(out=pbT[:, c, :], in_=hn[:, c * 128:(c + 1) * 128],
                                                                 identity=ident[:]),
                       reads=["hn", "ident"], writes=[tPB[0]])
                c0 = dst_col0(i)
                op("dve", lambda e, pbT=pbT, c0=c0: e.tensor_tensor(
                    out=dstT[:, :, c0:c0 + 128], in0=pbT,
                    in1=gcol[:].unsqueeze(2).to_broadcast([128, 8, 128]), op=ALU.mult),
                   reads=[tPB[0], "gacol", "gfcol"], writes=[dst_tok(i)])

        for ps_i in range(npass):
            tok0 = ps_i * PT
            for i in range(NT):
                op("sp", lambda e, i=i, tok0=tok0: e.dma_start(out=H[:, i, :], in_=x_d[tok0 + i * 128: tok0 + (i + 1) * 128, :]),
                   writes=[tH[i]], dma=f"H{i}")
            op("pool", lambda e: e.dma_start(out=wpa, in_=wpa_d.rearrange("(c p) n -> p c n", p=128)),
               writes=[tE[0][0]], dma="wg0")
            op("pool", lambda e: e.dma_start(out=wpb, in_=wpb_d.rearrange("(h d) n -> d h n", d=64)),
               writes=[tE[1][0], tE[1][1]], dma="wg1")
            op("pool", lambda e: e.dma_start(out=wout, in_=wout_d.rearrange("(c p) n -> p c n", p=128)),
               writes=[tE[0][1], tE[0][2]], dma="wu0")
            if ps_i > 0:
                op("dve", lambda e: e.tensor_copy(out=kT[:, 0:128], in_=kT[:, NT * 128:(NT + 1) * 128]),
                   reads=[], writes=["kT"])
                op("dve", lambda e: e.tensor_copy(out=vatt[:, 0, :], in_=vatt[:, NT, :]), reads=[], writes=["vatt"])

            ring_ctr = [0]

            def stream_block(col0):
                r = ring_ctr[0] % NRING
                ring_ctr[0] += 1
                op("pool", lambda e, r=r, col0=col0: e.dma_start(
                    out=ring[r][:], in_=win_d[:, col0:col0 + 128].rearrange("(c p) n -> p c n", p=128)),
                   writes=[tRing[r]], dma=f"ring{r}")
                return ring[r], tRing[r]

            for st in range(NST):
                tiles = [st * 4 + k for k in range(4)]
                norm_transpose(tiles, None, gacol, hnT, lambda i, st=st: (i - st * 4) * 128, lambda i: tk("hnT"))

                def proj_fm(col0, bank, evac):
                    wb_, wt_ = stream_block(col0)
                    for c in range(8):
                        op("pe", lambda e, c=c, wb_=wb_, bank=bank: e.matmul(
                            PB[bank][:], lhsT=wb_[:, c, :], rhs=hnT[:, c, :], start=(c == 0), stop=(c == 7)),
                           reads=[wt_, "hnT"], writes=[tPB[bank]])
                    evac(bank)

                for j in range(4):
                    proj_fm(U_OFF + j * 128, 1 + (j % 2),
                            lambda bank, j=j: op("act", lambda e: e.activation(out=uT[:, j, :], in_=PB[bank][:],
                                                                              func=AF.Gelu_apprx_tanh),
                                                 reads=[tPB[bank]], writes=["uT"]))
                for j in range(4):
                    proj_fm(Q_OFF + j * 128, 1 + (j % 2),
                            lambda bank, j=j: op("act", lambda e: e.activation(out=qT[:, j, :], in_=PB[bank][:],
                                                                              func=AF.Identity, scale=0.125),
                                                 reads=[tPB[bank]], writes=["qT"]))
                kc0 = (1 + st * 4) * 128
                proj_fm(K_OFF, 1,
                        lambda bank, kc0=kc0: op("dve", lambda e: e.tensor_copy(out=kT[:, kc0:kc0 + 512], in_=PB[bank][:]),
                                        reads=[tPB[bank]], writes=["kT"]))

                for i in tiles:
                    il = i - st * 4
                    ts = il * 128
                    slot = 1 + i
                    gblk = ps_i * NT + i
                    for c in range(8):
                        op("pe", lambda e, c=c, ts=ts: e.matmul(PB[3][:], lhsT=hnT[:, c, ts:ts + 128], rhs=wv[:, c, :],
                                                                start=(c == 0), stop=(c == 7)),
                           reads=["hnT", "wv"], writes=[tPB[3]])
                    for c in range(8):
                        op("pe", lambda e, c=c, ts=ts: e.matmul(PB[4][:, 0:128], lhsT=hnT[:, c, ts:ts + 128],
                                                                rhs=wva[:, c, :], start=(c == 0), stop=(c == 7)),
                           reads=["hnT", "wva"], writes=[tPB[4]])
                    op("dve", lambda e, slot=slot: e.tensor_copy(out=vatt[:, slot, :], in_=PB[4][:, 0:128]),
                       reads=[tPB[4]], writes=["vatt"])
                    op("act", lambda e: e.activation(out=gv[:], in_=PB[3][:], func=AF.Gelu_apprx_tanh),
                       reads=[tPB[3]], writes=["gv"])
                    op("dve", lambda e: e.bn_stats(out=bnst[:], in_=gv[:]), reads=["gv"], writes=["bnst"])
                    op("dve", lambda e: e.bn_aggr(out=mv[:], in_=bnst[:]), reads=["bnst"], writes=["mv"])
                    op("dve", lambda e: e.tensor_scalar_add(out=rsv[:], in0=mv[:, 1:2], scalar1=EPS),
                       reads=["mv"], writes=["rsv"])
                    op("act", lambda e: e.sqrt(out=rsv[:], in_=rsv[:]), reads=[], writes=["rsv"])
                    op("dve", lambda e: e.reciprocal(out=rsv[:], in_=rsv[:]), reads=[], writes=["rsv"])
                    op("dve", lambda e: e.tensor_scalar(out=nmr[:], in0=mv[:, 0:1], scalar1=rsv[:], scalar2=-1.0,
                                                        op0=ALU.mult, op1=ALU.mult),
                       reads=["mv", "rsv"], writes=["nmr"])
                    op("act", lambda e: e.activation(out=vnh[:], in_=gv[:], func=AF.Identity, scale=rsv[:], bias=nmr[:]),
                       reads=["gv", "rsv", "nmr"], writes=["vnh"])
                    pbs = PB[5][:].rearrange("p (j t) -> p j t", j=4)
                    for g in range(8):
                        lo = (g % 2) * 64
                        kw = {"tile_position": (0, 64)} if g % 2 else {}
                        op("pe", lambda e, g=g, lo=lo, kw=kw: e.matmul(
                            pbs[lo:lo + 64, g // 2, :], lhsT=vnh[:, g * 64:(g + 1) * 64], rhs=wstb[:, g, :],
                            start=True, stop=True, **kw),
                           reads=["vnh", "wstb"], writes=[tPB[5]])
                    op("dve", lambda e: e.tensor_tensor(out=gtmp[:], in0=pbs,
                                                        in1=gvcol[:].unsqueeze(2).to_broadcast([128, 4, 128]), op=ALU.mult),
                       reads=[tPB[5], "gvcol"], writes=["gtmp"])
                    op("dve", lambda e: e.tensor_tensor(out=gtmp[:], in0=gtmp[:], in1=bst[:], op=ALU.add),
                       reads=["bst"], writes=["gtmp"])
                    op("dve", lambda e, ts=ts: e.tensor_tensor(out=aT[:, :, ts:ts + 128], in0=gtmp[:],
                                                               in1=uT[:, :, ts:ts + 128], op=ALU.mult),
                       reads=["gtmp", "uT"], writes=["aT"])
                    for kh in range(2):
                        pr = slice(kh * 64, (kh + 1) * 64)
                        variants = [(0, slot)] + ([(1, slot - 1)] if gblk > 0 else [])
                        for (vi, ksl) in variants:
                            bank = 6 + vi
                            op("pe", lambda e, pr=pr, ksl=ksl, ts=ts, bank=bank: e.matmul(
                                PB[bank][:].rearrange("p (j t) -> p j t", j=4),
                                lhsT=kT[pr, ksl * 128:(ksl + 1) * 128], rhs=qT[pr, :, ts:ts + 128],
                                start=True, stop=True),
                               reads=["kT", "qT"], writes=[tPB[bank]])
                            op("dve", lambda e, vi=vi, kh=kh, bank=bank: e.tensor_tensor(
                                out=sc[vi][:], in0=PB[bank][:],
                                in1=BM[:, vi, kh, :, :].rearrange("p j t -> p (j t)"), op=ALU.add),
                               reads=[tPB[bank], "BM"], writes=["sc0"])
                            op("act", lambda e, vi=vi: e.activation(out=pT[vi][:], in_=sc[vi][:], func=AF.Exp),
                               reads=["sc0"], writes=[f"pT{vi}"])
                        nv = len(variants)
                        for k, (vi, ksl) in enumerate(variants):
                            op("pe", lambda e, vi=vi, ksl=ksl, kh=kh, k=k, nv=nv: e.matmul(
                                PB[1][0:64, :], lhsT=vatt[:, ksl, kh * 64:(kh + 1) * 64], rhs=pT[vi][:],
                                start=(k == 0), stop=(k == nv - 1)),
                               reads=["vatt", f"pT{vi}"], writes=[tPB[1]])
                        for k, (vi, ksl) in enumerate(variants):
                            op("pe", lambda e, vi=vi, k=k, nv=nv: e.matmul(
                                PB[2][0:64, :], lhsT=ones64[:], rhs=pT[vi][:], start=(k == 0), stop=(k == nv - 1)),
                               reads=["ones64", f"pT{vi}"], writes=[tPB[2]])
                        op("dve", lambda e, kh=kh: e.tensor_tensor(
                            out=den[:].rearrange("p (j t) -> p j t", j=4),
                            in0=PB[2][0:64, :].rearrange("p (j t) -> p j t", j=4),
                            in1=sinkexp[:, kh * 4:(kh + 1) * 4].unsqueeze(2).to_broadcast([64, 4, 128]), op=ALU.add),
                           reads=[tPB[2], "sinkexp"], writes=["den"])
                        op("dve", lambda e: e.reciprocal(out=den[:], in_=den[:]), reads=[], writes=["den"])
                        op("dve", lambda e, kh=kh, ts=ts: e.tensor_tensor(
                            out=bT[:, kh * 4:(kh + 1) * 4, ts:ts + 128],
                            in0=PB[1][0:64, :].rearrange("p (j t) -> p j t", j=4),
                            in1=den[:].rearrange("p (j t) -> p j t", j=4), op=ALU.mult),
                           reads=[tPB[1], "den"], writes=["bT"])

                for j in range(8):
                    wga, tga = stream_block(GA_OFF + j * 128)
                    for c in range(8):
                        op("pe", lambda e, c=c, wga=wga: e.matmul(PB[3][:], lhsT=wga[:, c, :], rhs=hnT[:, c, :],
                                                                  start=(c == 0), stop=(c == 7)),
                           reads=[tga, "hnT"], writes=[tPB[3]])
                    wgb, tgb = stream_block(GB_OFF + j * 128)
                    for c in range(8):
                        op("pe", lambda e, c=c, wgb=wgb: e.matmul(PB[4][:], lhsT=wgb[:, c, :], rhs=hnT[:, c, :],
                                                                  start=(c == 0), stop=(c == 7)),
                           reads=[tgb, "hnT"], writes=[tPB[4]])
                    for c in range(4):
                        op("pe", lambda e, c=c, j=j: e.matmul(PB[5][:], lhsT=wpa[:, c, j * 128:(j + 1) * 128], rhs=aT[:, c, :],
                                                              start=(c == 0), stop=(c == 3)),
                           reads=[tE[0][0], "aT"], writes=[tPB[5]])
                    for h in range(8):
                        op("pe", lambda e, h=h, j=j: e.matmul(PB[6][:], lhsT=wpb[:, h, j * 128:(j + 1) * 128], rhs=bT[:, h, :],
                                                              start=(h == 0), stop=(h == 7)),
                           reads=[tE[1][0], tE[1][1], "bT"], writes=[tPB[6]])
                    op("act", lambda e: e.activation(out=sga[:], in_=PB[3][:], func=AF.Sigmoid),
                       reads=[tPB[3]], writes=["sga"])
                    op("act", lambda e: e.activation(out=sgb[:], in_=PB[4][:], func=AF.Sigmoid),
                       reads=[tPB[4]], writes=["sgb"])
                    op("dve", lambda e: e.tensor_tensor(out=sga[:], in0=PB[5][:], in1=sga[:], op=ALU.mult),
                       reads=[tPB[5]], writes=["sga"])
                    op("dve", lambda e: e.tensor_tensor(out=sgb[:], in0=PB[6][:], in1=sgb[:], op=ALU.mult),
                       reads=[tPB[6]], writes=["sgb"])
                    op("dve", lambda e, j=j: e.tensor_tensor(out=mT[:, j, :], in0=sga[:], in1=sgb[:], op=ALU.add),
                       reads=["sga", "sgb"], writes=["mT"])
                if debug and ps_i == 0 and st == 0:
                    for nm, buf, shp in (("aT", aT, [128, 4, 512]), ("bT", bT, [64, 8, 512]), ("mT", mT, [128, 8, 512]),
                                         ("hnT", hnT, [128, 8, 512]), ("uT", uT, [128, 4, 512]), ("qT", qT, [128, 4, 512]),
                                         ("kT", kT, [128, (NT + 1) * 128]), ("vatt", vatt, [128, NT + 1, 128])):
                        dd = nc.dram_tensor("dbg_" + nm, shp, BF16, kind="ExternalOutput").ap()
                        out_ops.append(op("sp", lambda e, dd=dd, buf=buf: e.dma_start(out=dd, in_=buf[:]),
                                          reads=[nm], dma="dbg_" + nm))
                for i in tiles:
                    ts = (i - st * 4) * 128
                    for hf in range(2):
                        bank = 1 + hf
                        for j in range(8):
                            op("pe", lambda e, j=j, ts=ts, hf=hf, bank=bank: e.matmul(
                                PB[bank][:], lhsT=mT[:, j, ts:ts + 128], rhs=wout[:, j, hf * 512:(hf + 1) * 512],
                                start=(j == 0), stop=(j == 7)),
                               reads=["mT", tE[0][1], tE[0][2]], writes=[tPB[bank]])
                        op("dve", lambda e, i=i, hf=hf, bank=bank: e.tensor_tensor(
                            out=H[:, i, hf * 512:(hf + 1) * 512], in0=PB[bank][:], in1=H[:, i, hf * 512:(hf + 1) * 512],
                            op=ALU.add),
                           reads=[tPB[bank]], writes=[tH[i]])
                norm_transpose(tiles, None, gfcol, hn2T, lambda i: i * 128, lambda i, st=st: thn2T[st])

            if debug and ps_i == 0:
                for i in range(NT):
                    out_ops.append(op("sp", lambda e, i=i: e.dma_start(out=dbg_d[i * 128:(i + 1) * 128, :], in_=H[:, i, :]),
                                      reads=[tH[i]], dma=f"H{i}"))

            pbl = PB[0][:, 0:NT * 20].rearrange("p (t n) -> p t n", t=NT)
            for i in range(NT):
                for c in range(8):
                    op("pe", lambda e, i=i, c=c: e.matmul(pbl[:, i, :], lhsT=hn2T[:, c, i * 128:(i + 1) * 128], rhs=wr[:, c, :],
                                                          start=(c == 0), stop=(c == 7)),
                       reads=[thn2T[i // 4], "wr"], writes=[tPB[0]])
            V = lambda e: e
            op("dve", lambda e: e.tensor_tensor(out=lg[:], in0=pbl, in1=rb[:].unsqueeze(1).to_broadcast([128, NT, 20]),
                                                op=ALU.add), reads=[tPB[0], "rb"], writes=["lg"])
            gl = lg[:, :, 0:4]
            el = lg[:, :, 4:20].rearrange("p t (g x) -> p t g x", g=4)
            bc3 = lambda a: a.unsqueeze(2).to_broadcast([128, NT, 4])
            op("dve", lambda e: e.tensor_reduce(out=r_m[:], in_=gl, axis=AX.X, op=ALU.max), reads=["lg"], writes=["r_m"])
            op("dve", lambda e: e.tensor_tensor(out=r_og[:], in0=gl, in1=bc3(r_m[:]), op=ALU.is_equal),
               reads=["lg", "r_m"], writes=["r_og"])
            op("dve", lambda e: e.tensor_tensor(out=r_eg[:], in0=gl, in1=bc3(r_m[:]), op=ALU.subtract),
               reads=["lg", "r_m"], writes=["r_eg"])
            op("act", lambda e: e.activation(out=r_eg[:], in_=r_eg[:], func=AF.Exp), reads=[], writes=["r_eg"])
            op("dve", lambda e: e.tensor_reduce(out=r_gp[:], in_=r_eg[:], axis=AX.X, op=ALU.add),
               reads=["r_eg"], writes=["r_gp"])
            op("dve", lambda e: e.reciprocal(out=r_gp[:], in_=r_gp[:]), reads=[], writes=["r_gp"])
            op("dve", lambda e: e.tensor_tensor(out=r_sel[:], in0=el,
                                                in1=r_og[:].unsqueeze(3).to_broadcast([128, NT, 4, 4]), op=ALU.mult),
               reads=["lg", "r_og"], writes=["r_sel"])
            op("dve", lambda e: e.tensor_reduce(out=r_es[:], in_=r_sel[:].rearrange("p t g x -> p t x g"),
                                                axis=AX.X, op=ALU.add), reads=["r_sel"], writes=["r_es"])
            op("dve", lambda e: e.tensor_reduce(out=r_m1[:], in_=r_es[:], axis=AX.X, op=ALU.max),
               reads=["r_es"], writes=["r_m1"])
            op("dve", lambda e: e.tensor_tensor(out=r_o1[:], in0=r_es[:], in1=bc3(r_m1[:]), op=ALU.is_equal),
               reads=["r_es", "r_m1"], writes=["r_o1"])
            op("dve", lambda e: e.scalar_tensor_tensor(out=r_es2[:], in0=r_o1[:], scalar=-1e9, in1=r_es[:],
                                                       op0=ALU.mult, op1=ALU.add),
               reads=["r_o1", "r_es"], writes=["r_es2"])
            op("dve", lambda e: e.tensor_reduce(out=r_m2[:], in_=r_es2[:], axis=AX.X, op=ALU.max),
               reads=["r_es2"], writes=["r_m2"])
            op("dve", lambda e: e.tensor_tensor(out=r_o2[:], in0=r_es2[:], in1=bc3(r_m2[:]), op=ALU.is_equal),
               reads=["r_es2", "r_m2"], writes=["r_o2"])
            op("dve", lambda e: e.tensor_tensor(out=r_d[:], in0=r_m2[:], in1=r_m1[:], op=ALU.subtract),
               reads=["r_m1", "r_m2"], writes=["r_d"])
            op("act", lambda e: e.activation(out=r_d[:], in_=r_d[:], func=AF.Exp), reads=[], writes=["r_d"])
            op("dve", lambda e: e.tensor_scalar_add(out=r_w1[:], in0=r_d[:], scalar1=1.0), reads=["r_d"], writes=["r_w1"])
            op("dve", lambda e: e.reciprocal(out=r_w1[:], in_=r_w1[:]), reads=[], writes=["r_w1"])
            op("dve", lambda e: e.tensor_tensor(out=r_w2[:], in0=r_d[:], in1=r_w1[:], op=ALU.mult),
               reads=["r_d", "r_w1"], writes=["r_w2"])
            op("dve", lambda e: e.tensor_tensor(out=r_w1[:], in0=r_w1[:], in1=r_gp[:], op=ALU.mult),
               reads=["r_gp"], writes=["r_w1"])
            op("dve", lambda e: e.tensor_tensor(out=r_w2[:], in0=r_w2[:], in1=r_gp[:], op=ALU.mult),
               reads=["r_gp"], writes=["r_w2"])
            op("dve", lambda e: e.tensor_tensor(out=r_cl[:], in0=r_o1[:], in1=bc3(r_w1[:]), op=ALU.mult),
               reads=["r_o1", "r_w1"], writes=["r_cl"])
            op("dve", lambda e: e.tensor_tensor(out=r_cl2[:], in0=r_o2[:], in1=bc3(r_w2[:]), op=ALU.mult),
               reads=["r_o2", "r_w2"], writes=["r_cl2"])
            op("dve", lambda e: e.tensor_tensor(out=r_cl[:], in0=r_cl[:], in1=r_cl2[:], op=ALU.add),
               reads=["r_cl2"], writes=["r_cl"])
            op("dve", lambda e: e.tensor_tensor(
                out=call[:].rearrange("p t (g x) -> p t g x", g=4),
                in0=r_og[:].unsqueeze(3).to_broadcast([128, NT, 4, 4]),
                in1=r_cl[:].unsqueeze(2).to_broadcast([128, NT, 4, 4]), op=ALU.mult),
               reads=["r_og", "r_cl"], writes=["call"])

            unit = 0
            for ex in range(nexp):
                b = ex % NEB
                op("pool", lambda e, ex=ex, b=b: e.dma_start(out=wgs[b], in_=wg_d[ex].rearrange("(c p) n -> p c n", p=128)),
                   writes=[tE[b][0]], dma=f"wg{b}")
                op("pool", lambda e, ex=ex, b=b: e.dma_start(out=wus[b], in_=wu_d[ex].rearrange("(c p) n -> p c n", p=128)),
                   writes=[tE[b][1]], dma=f"wu{b}")
                op("pool", lambda e, ex=ex, b=b: e.dma_start(out=wds[b], in_=wd_d[ex].rearrange("(c p) n -> p c n", p=128)),
                   writes=[tE[b][2]], dma=f"wd{b}")
                for grp in range(NST):
                    g0 = grp * 512
                    hb = unit % 2
                    unit += 1
                    for j in range(4):
                        bg, bu = (j % 2) * 2, (j % 2) * 2 + 1
                        for c in range(8):
                            op("pe", lambda e, c=c, j=j, b=b, bg=bg, g0=g0: e.matmul(
                                PB[bg][:], lhsT=wgs[b][:, c, j * 128:(j + 1) * 128], rhs=hn2T[:, c, g0:g0 + 512],
                                start=(c == 0), stop=(c == 7)),
                               reads=[tE[b][0], thn2T[grp]], writes=[tPB[bg]])
                        for c in range(8):
                            op("pe", lambda e, c=c, j=j, b=b, bu=bu, g0=g0: e.matmul(
                                PB[bu][:], lhsT=wus[b][:, c, j * 128:(j + 1) * 128], rhs=hn2T[:, c, g0:g0 + 512],
                                start=(c == 0), stop=(c == 7)),
                               reads=[tE[b][1], thn2T[grp]], writes=[tPB[bu]])
                        op("act", lambda e, j=j, bg=bg: e.activation(out=sgs[j % 2][:], in_=PB[bg][:], func=AF.Silu),
                           reads=[tPB[bg]], writes=[f"sgs{j % 2}"])
                        op("dve", lambda e, j=j, bu=bu, hb=hb: e.tensor_tensor(out=hid[hb][:, j, :], in0=PB[bu][:],
                                                                            in1=sgs[j % 2][:], op=ALU.mult),
                           reads=[tPB[bu], f"sgs{j % 2}"], writes=[f"hid{hb}"])
                    for tl in range(4):
                        i = grp * 4 + tl
                        for hf in range(2):
                            bank = 4 + (tl * 2 + hf) % 4
                            for j in range(4):
                                op("pe", lambda e, j=j, tl=tl, hf=hf, b=b, hb=hb, bank=bank: e.matmul(
                                    PB[bank][:], lhsT=hid[hb][:, j, tl * 128:(tl + 1) * 128],
                                    rhs=wds[b][:, j, hf * 512:(hf + 1) * 512], start=(j == 0), stop=(j == 3)),
                                   reads=[f"hid{hb}", tE[b][2]], writes=[tPB[bank]])
                            op("dve", lambda e, i=i, hf=hf, ex=ex, bank=bank: e.scalar_tensor_tensor(
                                out=H[:, i, hf * 512:(hf + 1) * 512], in0=PB[bank][:], scalar=call[:, i, ex:ex + 1],
                                in1=H[:, i, hf * 512:(hf + 1) * 512], op0=ALU.mult, op1=ALU.add),
                               reads=[tPB[bank], "call"], writes=[tH[i]])

            for i in range(NT):
                op("act", lambda e, i=i: e.activation(out=hn[:], in_=H[:, i, :], func=AF.Square, accum_out=ss[:, i:i + 1]),
                   reads=[tH[i]], writes=["hn", "ss"])
            rstd_ops(NT)
            for i in range(NT):
                op("dve", lambda e, i=i: e.scalar_tensor_tensor(out=H[:, i, :], in0=H[:, i, :], scalar=rstd[:, i:i + 1],
                                                                in1=gfin[:], op0=ALU.mult, op1=ALU.mult),
                   reads=["rstd", "gfin"], writes=[tH[i]])
                o = op("sp", lambda e, i=i, tok0=tok0: e.dma_start(out=y_d[tok0 + i * 128: tok0 + (i + 1) * 128, :], in_=H[:, i, :]),
                       reads=[tH[i]], dma=f"H{i}")
                if ps_i == npass - 1:
                    out_ops.append(o)

        S.emit(nc, final_wait_ops=out_ops)
    return nc


def _t5_bucket(dist):
    dist = np.asarray(dist, dtype=np.int64)
    nf = np.maximum(dist, 1).astype(np.float32)
    large = 16 + (np.log(nf / np.float32(16)) / np.float32(math.log(128 / 16)) * np.float32(16)).astype(np.int32)
    large = np.minimum(large, 31)
    return np.where(dist < 16, dist, large)


def prep_shared(inp):
    f = lambda a: np.ascontiguousarray(np.asarray(a, dtype=np.float32))
    w_in = f(inp["w_in"])[0]
    perm = np.arange(3840)
    qcols = []
    for j in range(4):
        qcols += list(range(1024 + j * 64, 1024 + (j + 1) * 64))
        qcols += list(range(1024 + (4 + j) * 64, 1024 + (5 + j) * 64))
    perm[1024:1536] = np.array(qcols)
    w_in = np.ascontiguousarray(w_in[:, perm])
    wr = np.concatenate([f(inp["router_group_w"])[0]] + [f(inp["router_expert_w"])[0, g] for g in range(4)], axis=1)
    rb = np.concatenate([f(inp["router_group_b"])[0], f(inp["router_expert_b"])[0].reshape(-1)])[None, :]
    col = lambda v, c: np.ascontiguousarray(f(v).reshape(c, 128).T)
    ws = f(inp["gm_w_spatial"])[0]
    wst = np.ascontiguousarray(ws.transpose(2, 0, 1))
    s_i = np.arange(128)[:, None, None]
    t_i = np.arange(128)[None, None, :]
    wmask = np.broadcast_to((s_i <= t_i), (128, 8, 128)).astype(np.float32)
    bs = f(inp["gm_b_spatial"])[0]
    bst = np.zeros((128, 4, 128), np.float32)
    for g in range(8):
        bst[(g % 2) * 64:(g % 2) * 64 + 64, g // 2, :] = bs[g][None, :]
    rel = f(inp["rel_bias"])
    s_ = np.arange(128)[:, None]
    q_ = np.arange(128)[None, :]
    d_own = q_ - s_
    d_prev = q_ + 128 - s_
    bias = np.zeros((128, 2, 2, 4, 128), np.float32)
    maskc = np.zeros((128, 2, 2, 4, 128), np.float32)
    b_own = _t5_bucket(np.clip(d_own, 0, 127))
    b_prev = _t5_bucket(np.clip(d_prev, 0, 127))
    for kh in range(2):
        for h4 in range(4):
            h = kh * 4 + h4
            bias[:, 0, kh, h4, :] = rel[b_own, h]
            bias[:, 1, kh, h4, :] = rel[b_prev, h]
            maskc[:, 0, kh, h4, :] = np.where(d_own >= 0, 0.0, NEG)
            maskc[:, 1, kh, h4, :] = np.where(d_prev < 128, 0.0, NEG)
    return {
        "w_in": w_in,
        "wpa": f(inp["w_proj_a"])[0], "wpb": f(inp["w_proj_b"])[0], "wout": f(inp["w_out"])[0],
        "wg": f(inp["expert_w_gate"])[0], "wu": f(inp["expert_w_up"])[0], "wd": f(inp["expert_w_down"])[0],
        "wr": np.ascontiguousarray(wr), "rb": np.ascontiguousarray(rb),
        "gacol": col(inp["attn_norm_g"], 8), "gfcol": col(inp["ffn_norm_g"], 8), "gvcol": col(inp["gm_v_norm_g"], 4),
        "gfin": f(inp["final_norm_g"]).reshape(1, D),
        "wst": wst, "wmask": wmask, "bst": bst,
        "sinks": f(inp["attn_sinks"]).reshape(1, 8),
        "bias": bias, "maskc": maskc,
        "idn": np.eye(128, dtype=np.float32),
    }


def kernel(**inputs):
    shared = prep_shared(inputs)
    x = np.asarray(inputs["x"], dtype=np.float32)
    nc = build()
    in_maps = []
    for c in range(NCORES):
        m = dict(shared)
        m["x"] = np.ascontiguousarray(x[c])
        in_maps.append(m)
    res = run_bass_kernel_spmd(nc, in_maps, core_ids=list(range(NCORES)))
    return np.stack([np.asarray(r["y"], dtype=np.float32) for r in res.results], axis=0)
```

```python
import math
from contextlib import ExitStack

import numpy as np
import concourse.bass as bass
import concourse.mybir as mybir
from concourse.bass_utils import run_bass_kernel_spmd

F32 = mybir.dt.float32
BF16 = mybir.dt.bfloat16
AF = mybir.ActivationFunctionType
ALU = mybir.AluOpType
AX = mybir.AxisListType

SEQ = 4096
D = 1024
NCORES = 8
PT = 1024
NT = PT // 128
NST = PT // 512
NPASS_FULL = SEQ // PT
EPS = 1e-6
NEG = -30000.0
NSLOT = 32
NTT = SEQ // 128
I32 = mybir.dt.int32

U_OFF, V_OFF, Q_OFF, K_OFF, VA_OFF, GA_OFF, GB_OFF = 0, 512, 1024, 1536, 1664, 1792, 2816

ENG = ("pe", "act", "dve", "pool", "sp")


class Tok:
    __slots__ = ("name", "last_w", "readers")

    def __init__(self, name):
        self.name = name
        self.last_w = None
        self.readers = []


class Op:
    __slots__ = ("eng", "fn", "deps", "sig", "dma_sem", "dma_cnt", "seq")

    def __init__(self, eng, fn):
        self.eng = eng
        self.fn = fn
        self.deps = set()
        self.sig = False
        self.dma_sem = None
        self.dma_cnt = 0
        self.seq = 0


class Sched:
    def __init__(self):
        self.ops = []
        self.per = {e: [] for e in ENG}
        self.dma_counts = {}
        self.total_keys = set()
        self.epoch_op = None
        self.last_dma = {}

    def op(self, eng, fn, reads=(), writes=(), dma=None):
        o = Op(eng, fn)
        for t in list(reads) + list(writes):
            if t.last_w is not None:
                o.deps.add(t.last_w)
        for t in writes:
            for r in t.readers:
                o.deps.add(r)
        if self.epoch_op is not None:
            o.deps.add(self.epoch_op)
        o.deps.discard(o)
        for t in reads:
            t.readers.append(o)
        for t in writes:
            t.last_w = o
            t.readers = []
        if dma is not None:
            o.dma_sem = dma
            self.dma_counts[dma] = self.dma_counts.get(dma, 0) + 1
            o.dma_cnt = self.dma_counts[dma]
            self.last_dma[dma] = o
        self.ops.append(o)
        self.per[eng].append(o)
        return o

    def barrier(self, eng, fn):
        o = Op(eng, fn)
        for e in ENG:
            seen_c = False
            for p in reversed(self.per[e]):
                if p.dma_sem is None:
                    o.deps.add(p)
                    break
        for k, p in self.last_dma.items():
            o.deps.add(p)
        if self.epoch_op is not None:
            o.deps.add(self.epoch_op)
        self.ops.append(o)
        self.per[eng].append(o)
        self.epoch_op = o
        return o

    def emit(self, nc, final_wait_ops=()):
        def skip(d, o):
            return d.dma_sem is None and o.dma_sem is None and d.eng == "pe" and o.eng == "pe"

        for o in self.ops:
            for d in o.deps:
                if d.dma_sem is None and not skip(d, o):
                    d.sig = True
        for o in final_wait_ops:
            if o.dma_sem is None:
                o.sig = True
        for e in ENG:
            c = 0
            for o in self.per[e]:
                if o.dma_sem is None and o.sig:
                    c += 1
                    o.seq = c
        with ExitStack() as es:
            esem = {e: es.enter_context(nc.semaphore(f"s_{e}")) for e in ENG}
            dsem = {k: es.enter_context(nc.semaphore(f"d_{k}")) for k in self.dma_counts}
            block = es.enter_context(nc.Block())

            def dval(d):
                if d.dma_sem in self.total_keys:
                    return 16 * self.dma_counts[d.dma_sem]
                return 16 * d.dma_cnt

            def need(o):
                w = {}
                for d in o.deps:
                    if d.dma_sem is not None:
                        key, val = ("d", d.dma_sem), dval(d)
                    else:
                        if skip(d, o):
                            continue
                        key, val = ("e", d.eng), d.seq
                    if w.get(key, 0) < val:
                        w[key] = val
                return w

            def run(ename):
                def body(eng):
                    waited = {}
                    for o in self.per[ename]:
                        for key, val in need(o).items():
                            if waited.get(key, 0) >= val:
                                continue
                            waited[key] = val
                            sem = dsem[key[1]] if key[0] == "d" else esem[key[1]]
                            eng.wait_ge(sem, val)
                        ins = o.fn(eng)
                        if o.dma_sem is not None:
                            ins.then_inc(dsem[o.dma_sem], 16)
                        elif o.sig:
                            ins.then_inc(esem[ename], 1)
                    if ename == "sp":
                        for o in final_wait_ops:
                            if o.dma_sem is not None:
                                eng.wait_ge(dsem[o.dma_sem], dval(o))
                            else:
                                eng.wait_ge(esem[o.eng], o.seq)
                return body

            block.tensor(run("pe"))
            block.scalar(run("act"))
            block.vector(run("dve"))
            block.gpsimd(run("pool"))
            block.sync(run("sp"))


class Arena:
    def __init__(self, nc, es, name, nbytes):
        self.t = es.enter_context(nc.sbuf_tensor(name, [128, nbytes // 2], BF16))
        self.cap = nbytes
        self.off = 0

    def alloc(self, shape, dt):
        n = 1
        for s_ in shape[1:]:
            n *= s_
        esz = 2 if dt == BF16 else 4
        o = self.off
        self.off += (n * esz + 63) // 64 * 64
        assert self.off <= self.cap, (self.off, self.cap)
        ap = self.t[0:shape[0], o // 2:(o + n * esz) // 2]
        if dt != BF16:
            ap = ap.bitcast(dt)
        if len(shape) > 2:
            names = " ".join(f"d{k}" for k in range(len(shape) - 1))
            ap = ap.rearrange(f"p ({names}) -> p {names}", **{f"d{k}": shape[k + 1] for k in range(len(shape) - 1)})
        return ap


def build(npass=NPASS_FULL, debug=False, stages=4):
    nc = bass.Bass("TRN2", target_bir_lowering=False)

    def din(name, shape):
        return nc.dram_tensor(name, list(shape), F32, kind="ExternalInput").ap()

    x_d = din("x", [SEQ, D])
    win_d = din("w_in", [D, 3840])
    wpa_d = din("wpa", [512, D])
    wpb_d = din("wpb", [512, D])
    wout_d = din("wout", [D, D])
    wexp_d = [din("wgp", [16, 128, 4096]), din("wup", [16, 128, 4096]), din("wdp", [16, 128, 4096])]
    wr_d = din("wr", [D, 20])
    rb_d = din("rb", [1, 20])
    gacol_d = din("gacol", [128, 8])
    gfcol_d = din("gfcol", [128, 8])
    gvcol_d = din("gvcol", [128, 4])
    gfin_d = din("gfin", [1, D])
    wst_d = din("wst", [128, 8, 128])
    wmask_d = din("wmask", [128, 8, 128])
    bst_d = din("bst", [128, 4, 128])
    sinks_d = din("sinks", [1, 8])
    bias_d = din("bias", [128, 2, 2, 4, 128])
    mask_d = din("maskc", [128, 2, 2, 4, 128])
    idn_d = din("idn", [128, 128])
    utri_d = din("utri", [128, 128])
    cst_d = din("cst", [1, 8 + 256 + 32])
    iop_d = din("iop", [128, 1])
    y_d = nc.dram_tensor("y", [SEQ, D], F32, kind="ExternalOutput").ap()
    hS_d = nc.dram_tensor("hS", [SEQ, D], F32, kind="Internal").ap()
    hn2S_d = nc.dram_tensor("hn2S", [SEQ, D], BF16, kind="Internal").ap()
    Xs_d = nc.dram_tensor("Xs", [NSLOT * 512, D], BF16, kind="Internal").ap()
    Ys_d = nc.dram_tensor("Ys", [NSLOT * 512, D], F32, kind="Internal").ap()
    wscr_d = [nc.dram_tensor(f"wscr{m}", [16 * 128, 4096], BF16, kind="Internal").ap() for m in range(3)]
    if debug:
        dbg_d = nc.dram_tensor("dbg", [PT, D], F32, kind="ExternalOutput").ap()
        dbgr_d = nc.dram_tensor("dbgr", [128, 6, NTT], F32, kind="ExternalOutput").ap()

    S = Sched()
    S.total_keys.update(["const"])

    with ExitStack() as es:
        def sb(name, shape, dt):
            return es.enter_context(nc.sbuf_tensor(name, list(shape), dt))

        ident = sb("ident", [128, 128], BF16)
        gacol = sb("gacol_s", [128, 8], F32)
        gfcol = sb("gfcol_s", [128, 8], F32)
        gvcol = sb("gvcol_s", [128, 4], F32)
        wstb = sb("wstb", [128, 8, 128], BF16)
        bst = sb("bst_s", [128, 4, 128], F32)
        sinkexp = sb("sinkexp", [64, 8], F32)
        BM = sb("BM", [128, 2, 2, 4, 128], F32)
        ones64 = sb("ones64", [128, 64], BF16)
        ones128 = sb("ones128", [128, 128], BF16)
        utri = sb("utri_s", [128, 128], BF16)
        cst = sb("cst_s", [128, 8 + 256 + 32], F32)
        iop = sb("iop_s", [128, 1], F32)
        wr = sb("wr_s", [128, 8, 20], BF16)
        rb = sb("rb_s", [128, 20], F32)
        lg = sb("lg_all", [128, NTT, 20], F32)
        posA = sb("posA", [128, NTT], I32)
        posB = sb("posB", [128, NTT], I32)
        wA = sb("wA", [128, NTT], F32)
        wB = sb("wB", [128, NTT], F32)
        idxW = sb("idxW", [128, NSLOT], I32)
        dummy = sb("bar_dummy", [128, 1], F32)
        NEB = 2
        wreg = sb("wreg", [128, NEB * 12288], BF16)
        wgs = [wreg[:, b * 12288:b * 12288 + 4096].rearrange("p (c n) -> p c n", c=8) for b in range(NEB)]
        wus = [wreg[:, b * 12288 + 4096:b * 12288 + 8192].rearrange("p (c n) -> p c n", c=8) for b in range(NEB)]
        wds = [wreg[:, b * 12288 + 8192:b * 12288 + 12288].rearrange("p (c n) -> p c n", c=4) for b in range(NEB)]
        wflat = [[wreg[:, b * 12288 + m * 4096:b * 12288 + (m + 1) * 4096] for m in range(3)] for b in range(NEB)]
        wpa = wreg[:, 0:4096].rearrange("p (c n) -> p c n", c=4)
        wout = wreg[:, 4096:12288].rearrange("p (c n) -> p c n", c=8)
        wpb = wreg[0:64, 12288:20480].rearrange("p (c n) -> p c n", c=8)
        stg = wreg[:, 20480:24576]
        AR = Arena(nc, es, "arena", 124 * 1024)
        H = AR.alloc([128, NT, D], F32)
        hn2T = AR.alloc([128, 8, 512], BF16)
        wv = AR.alloc([128, 8, 512], BF16)
        wva = AR.alloc([128, 8, 128], BF16)
        NRING = 4
        ring = [AR.alloc([128, 8, 128], BF16) for _ in range(NRING)]
        hn = AR.alloc([128, D], BF16)
        hnT = AR.alloc([128, 8, 512], BF16)
        wst = hnT[:, 0:2, :].rearrange("p a (b t) -> p (a b) t", b=4)
        wmask = hnT[:, 2:4, :].rearrange("p a (b t) -> p (a b) t", b=4)
        ss = AR.alloc([128, 8], F32)
        rstd = AR.alloc([128, 8], F32)
        uT = AR.alloc([128, 4, 512], BF16)
        qT = AR.alloc([128, 4, 512], BF16)
        kT = AR.alloc([128, (NT + 1) * 128], BF16)
        vatt = AR.alloc([128, NT + 1, 128], BF16)
        gv = AR.alloc([128, 512], F32)
        vnh = AR.alloc([128, 512], BF16)
        bnst = AR.alloc([128, 6], F32)
        mv = AR.alloc([128, 2], F32)
        rsv = AR.alloc([128, 1], F32)
        nmr = AR.alloc([128, 1], F32)
        gtmp = AR.alloc([128, 4, 128], F32)
        sc0_ = AR.alloc([128, 512], F32)
        sc = [sc0_, sc0_]
        pT = [AR.alloc([128, 512], BF16) for _ in range(2)]
        den = AR.alloc([64, 512], F32)
        aT = AR.alloc([128, 4, 512], BF16)
        bT = AR.alloc([64, 8, 512], BF16)
        sga = AR.alloc([128, 512], F32)
        sgb = AR.alloc([128, 512], F32)
        mT = AR.alloc([128, 8, 512], BF16)
        maskc = mT[:, 0:4, :].rearrange("p a (b c t) -> p (a b c) t", b=2, c=2).rearrange("p (v k h) t -> p v k h t", v=2, k=2)
        side1_end = AR.off
        AR.off = 0
        NB_ = NTT
        r_m = AR.alloc([128, NB_], F32)
        r_og = AR.alloc([128, NB_, 4], F32)
        r_eg = AR.alloc([128, NB_, 4], F32)
        r_gp = AR.alloc([128, NB_], F32)
        r_sel = AR.alloc([128, NB_, 4, 4], F32)
        r_es = AR.alloc([128, NB_, 4], F32)
        r_m1 = AR.alloc([128, NB_], F32)
        r_o1 = AR.alloc([128, NB_, 4], F32)
        r_es2 = AR.alloc([128, NB_, 4], F32)
        r_m2 = AR.alloc([128, NB_], F32)
        r_o2 = AR.alloc([128, NB_, 4], F32)
        r_d = AR.alloc([128, NB_], F32)
        r_w1 = AR.alloc([128, NB_], F32)
        M1 = AR.alloc([128, NB_, 4, 4], F32)
        M2 = AR.alloc([128, NB_, 4, 4], F32)
        Mb = AR.alloc([128, NB_ * 16], BF16)
        R1s = AR.alloc([128, NB_, 16], F32)
        Ca = AR.alloc([128, NB_, 16], F32)
        Cb = AR.alloc([128, NB_, 16], F32)
        Tts = AR.alloc([128, NB_, 16], F32)
        r_cmp = AR.alloc([128, 16, 16], F32)
        r_cmp2 = AR.alloc([128, NSLOT, 16], F32)
        r_nb = AR.alloc([128, 16], F32)
        r_ob = AR.alloc([128, 16], F32)
        r_oe = AR.alloc([128, 16], F32)
        r_pf = AR.alloc([128, NB_], F32)
        r_eid = AR.alloc([128, NSLOT], F32)
        gfin = AR.alloc([128, D], F32)
        NXIN = 4
        xin = [AR.alloc([128, D], BF16) for _ in range(NXIN)]
        xs = [AR.alloc([128, 4, D], BF16) for _ in range(2)]
        xT = [AR.alloc([128, 8, 512], BF16) for _ in range(2)]
        sgs = [AR.alloc([128, 512], F32) for _ in range(2)]
        hid = [AR.alloc([128, 4, 512], BF16) for _ in range(2)]
        NYS = 3
        ys = [AR.alloc([128, D], F32) for _ in range(NYS)]
        YA = [AR.alloc([128, D], F32) for _ in range(2)]
        YB = [AR.alloc([128, D], F32) for _ in range(2)]
        hb = [AR.alloc([128, D], F32) for _ in range(2)]
        ss4 = AR.alloc([128, 1], F32)
        rs4 = AR.alloc([128, 1], F32)
        hn4 = AR.alloc([128, D], BF16)
        PB = [es.enter_context(nc.psum_tensor(f"pb{i}", [128, 512], F32)) for i in range(8)]

        T = {}

        def tk(n):
            if n not in T:
                T[n] = Tok(n)
            return T[n]

        tH = [tk(f"H{i}") for i in range(NT)]
        tPB = [tk(f"PB{i}") for i in range(8)]
        tE = [[tk(f"wg{i}"), tk(f"wu{i}"), tk(f"wd{i}")] for i in range(NEB)]
        tRing = [tk(f"ring{i}") for i in range(NRING)]

        def op(eng, fn, reads=(), writes=(), dma=None):
            return S.op(eng, fn, [tk(r) if isinstance(r, str) else r for r in reads],
                        [tk(w) if isinstance(w, str) else w for w in writes], dma)

        def cdma(eng, out, in_, w):
            op(eng, lambda e, out=out, in_=in_: e.dma_start(out=out, in_=in_), writes=[w], dma="const")

        cdma("sp", gacol[:], gacol_d, "gacol")
        cdma("sp", gfcol[:], gfcol_d, "gfcol")
        cdma("sp", gvcol[:], gvcol_d, "gvcol")
        cdma("sp", bst[:], bst_d, "bst")
        cdma("sp", sinkexp[:], sinks_d.partition_broadcast(64), "sinkexp")
        cdma("sp", BM[:], bias_d, "BM")
        cdma("sp", rb[:], rb_d.partition_broadcast(128), "rb")
        cdma("sp", cst[:], cst_d.partition_broadcast(128), "cst")
        cdma("sp", iop[:], iop_d, "iop")
        cdma("pool", ident[:], idn_d, "ident")
        cdma("pool", utri[:], utri_d, "utri")
        cdma("pool", wst, wst_d, "wst_a")
        cdma("pool", wmask, wmask_d, "wmask_a")
        cdma("pool", maskc, mask_d, "maskc_a")
        cdma("pool", wr[:], wr_d.rearrange("(c p) n -> p c n", p=128), "wr")
        cdma("pool", wv, win_d[:, V_OFF:V_OFF + 512].rearrange("(c p) n -> p c n", p=128), "wv")
        cdma("pool", wva, win_d[:, VA_OFF:VA_OFF + 128].rearrange("(c p) n -> p c n", p=128), "wva")
        op("dve", lambda e: e.tensor_tensor(out=wstb[:], in0=wst, in1=wmask, op=ALU.mult),
           reads=["wst_a", "wmask_a"], writes=["wstb", "hnT"])
        op("dve", lambda e: e.tensor_tensor(out=BM[:], in0=BM[:], in1=maskc, op=ALU.add),
           reads=["maskc_a"], writes=["BM", "mT"])
        op("act", lambda e: e.activation(out=sinkexp[:], in_=sinkexp[:], func=AF.Exp), reads=[], writes=["sinkexp"])
        op("dve", lambda e: e.memset(ones64[:], 1.0), writes=["ones64"])
        op("dve", lambda e: e.memset(ones128[:], 1.0), writes=["ones128"])

        out_ops = []

        def rstd_ops(n):
            op("dve", lambda e: e.tensor_scalar(out=rstd[:, 0:n], in0=ss[:, 0:n], scalar1=1.0 / D, scalar2=EPS,
                                                op0=ALU.mult, op1=ALU.add), reads=["ss"], writes=["rstd"])
            op("act", lambda e: e.sqrt(out=rstd[:, 0:n], in_=rstd[:, 0:n]), reads=[], writes=["rstd"])
            op("dve", lambda e: e.reciprocal(out=rstd[:, 0:n], in_=rstd[:, 0:n]), reads=[], writes=["rstd"])

        def norm_transpose(ti_list, gcol, dstT, dst_col0, dst_tok, after_hn=None):
            n = len(ti_list)
            for k, i in enumerate(ti_list):
                op("act", lambda e, i=i, k=k: e.activation(out=hn, in_=H[:, i, :], func=AF.Square,
                                                          accum_out=ss[:, k:k + 1]),
                   reads=[tH[i]], writes=["hn", "ss"])
            rstd_ops(n)
            for k, i in enumerate(ti_list):
                op("act", lambda e, i=i, k=k: e.activation(out=hn, in_=H[:, i, :], func=AF.Identity,
                                                          scale=rstd[:, k:k + 1]),
                   reads=[tH[i], "rstd"], writes=["hn"])
                if after_hn is not None:
                    after_hn(i)
                pbT = PB[0][:].bitcast(BF16).rearrange("p (c t) -> p c t", c=8)
                for c in range(8):
                    op("pe", lambda e, c=c, pbT=pbT: e.transpose(out=pbT[:, c, :], in_=hn[:, c * 128:(c + 1) * 128],
                                                                 identity=ident[:]),
                       reads=["hn", "ident"], writes=[tPB[0]])
                c0 = dst_col0(i)
                op("dve", lambda e, pbT=pbT, c0=c0: e.tensor_tensor(
                    out=dstT[:, :, c0:c0 + 128], in0=pbT,
                    in1=gcol[:].unsqueeze(2).to_broadcast([128, 8, 128]), op=ALU.mult),
                   reads=[tPB[0], "gacol", "gfcol"], writes=[dst_tok(i)])

        precast = [(m, ex) for ex in range(16) for m in range(3)]
        pc_ctr = [0]

        def precast_some(n):
            if stages == 1:
                return
            for _ in range(n):
                if pc_ctr[0] >= len(precast):
                    return
                m, ex = precast[pc_ctr[0]]
                pc_ctr[0] += 1
                op("pool", lambda e, m=m, ex=ex: e.dma_start(
                    out=stg, in_=wexp_d[m][ex]),
                   writes=["stg"], dma="stg")
                op("sp", lambda e, m=m, ex=ex: e.dma_start(out=wscr_d[m][ex * 128:(ex + 1) * 128, :], in_=stg),
                   reads=["stg"], writes=["wscr"], dma="wscr")

        for ps_i in range(npass):
            tok0 = ps_i * PT
            for i in range(NT):
                op("sp", lambda e, i=i, tok0=tok0: e.dma_start(out=H[:, i, :], in_=x_d[tok0 + i * 128: tok0 + (i + 1) * 128, :]),
                   writes=[tH[i]], dma=f"H{i}")
            if ps_i == 0:
                op("pool", lambda e: e.dma_start(out=wpa, in_=wpa_d.rearrange("(c p) n -> p c n", p=128)),
                   writes=[tE[0][0]], dma="wg0")
                op("pool", lambda e: e.dma_start(out=wpb, in_=wpb_d.rearrange("(h d) n -> d h n", d=64)),
                   writes=[tE[1][0], tE[1][1]], dma="wg1")
                op("pool", lambda e: e.dma_start(out=wout, in_=wout_d.rearrange("(c p) n -> p c n", p=128)),
                   writes=[tE[0][1], tE[0][2]], dma="wu0")
            if ps_i > 0:
                op("dve", lambda e: e.tensor_copy(out=kT[:, 0:128], in_=kT[:, NT * 128:(NT + 1) * 128]),
                   reads=[], writes=["kT"])
                op("dve", lambda e: e.tensor_copy(out=vatt[:, 0, :], in_=vatt[:, NT, :]), reads=[], writes=["vatt"])

            ring_ctr = [0]

            def stream_block(col0):
                r = ring_ctr[0] % NRING
                ring_ctr[0] += 1
                op("pool", lambda e, r=r, col0=col0: e.dma_start(
                    out=ring[r], in_=win_d[:, col0:col0 + 128].rearrange("(c p) n -> p c n", p=128)),
                   writes=[tRing[r]], dma=f"ring{r}")
                if ring_ctr[0] % 4 == 0:
                    precast_some(1)
                return ring[r], tRing[r]

            for st in range(NST):
                tiles = [st * 4 + k for k in range(4)]
                norm_transpose(tiles, gacol, hnT, lambda i, st=st: (i - st * 4) * 128, lambda i: tk("hnT"))

                def proj_fm(col0, bank, evac):
                    wb_, wt_ = stream_block(col0)
                    for c in range(8):
                        op("pe", lambda e, c=c, wb_=wb_, bank=bank: e.matmul(
                            PB[bank][:], lhsT=wb_[:, c, :], rhs=hnT[:, c, :], start=(c == 0), stop=(c == 7)),
                           reads=[wt_, "hnT"], writes=[tPB[bank]])
                    evac(bank)

                for j in range(4):
                    proj_fm(U_OFF + j * 128, 1 + (j % 2),
                            lambda bank, j=j: op("act", lambda e: e.activation(out=uT[:, j, :], in_=PB[bank][:],
                                                                              func=AF.Gelu_apprx_tanh),
                                                 reads=[tPB[bank]], writes=["uT"]))
                for j in range(4):
                    proj_fm(Q_OFF + j * 128, 1 + (j % 2),
                            lambda bank, j=j: op("act", lambda e: e.activation(out=qT[:, j, :], in_=PB[bank][:],
                                                                              func=AF.Identity, scale=0.125),
                                                 reads=[tPB[bank]], writes=["qT"]))
                kc0 = (1 + st * 4) * 128
                proj_fm(K_OFF, 1,
                        lambda bank, kc0=kc0: op("dve", lambda e: e.tensor_copy(out=kT[:, kc0:kc0 + 512], in_=PB[bank][:]),
                                                 reads=[tPB[bank]], writes=["kT"]))

                for i in tiles:
                    il = i - st * 4
                    ts = il * 128
                    slot = 1 + i
                    gblk = ps_i * NT + i
                    for c in range(8):
                        op("pe", lambda e, c=c, ts=ts: e.matmul(PB[3][:], lhsT=hnT[:, c, ts:ts + 128], rhs=wv[:, c, :],
                                                                start=(c == 0), stop=(c == 7)),
                           reads=["hnT", "wv"], writes=[tPB[3]])
                    for c in range(8):
                        op("pe", lambda e, c=c, ts=ts: e.matmul(PB[4][:, 0:128], lhsT=hnT[:, c, ts:ts + 128],
                                                                rhs=wva[:, c, :], start=(c == 0), stop=(c == 7)),
                           reads=["hnT", "wva"], writes=[tPB[4]])
                    op("dve", lambda e, slot=slot: e.tensor_copy(out=vatt[:, slot, :], in_=PB[4][:, 0:128]),
                       reads=[tPB[4]], writes=["vatt"])
                    op("act", lambda e: e.activation(out=gv, in_=PB[3][:], func=AF.Gelu_apprx_tanh),
                       reads=[tPB[3]], writes=["gv"])
                    op("dve", lambda e: e.bn_stats(out=bnst, in_=gv), reads=["gv"], writes=["bnst"])
                    op("dve", lambda e: e.bn_aggr(out=mv, in_=bnst), reads=["bnst"], writes=["mv"])
                    op("dve", lambda e: e.tensor_scalar_add(out=rsv, in0=mv[:, 1:2], scalar1=EPS),
                       reads=["mv"], writes=["rsv"])
                    op("act", lambda e: e.sqrt(out=rsv, in_=rsv), reads=[], writes=["rsv"])
                    op("dve", lambda e: e.reciprocal(out=rsv, in_=rsv), reads=[], writes=["rsv"])
                    op("dve", lambda e: e.tensor_scalar(out=nmr, in0=mv[:, 0:1], scalar1=rsv, scalar2=-1.0,
                                                        op0=ALU.mult, op1=ALU.mult),
                       reads=["mv", "rsv"], writes=["nmr"])
                    op("act", lambda e: e.activation(out=vnh, in_=gv, func=AF.Identity, scale=rsv, bias=nmr),
                       reads=["gv", "rsv", "nmr"], writes=["vnh"])
                    pbs = PB[5][:].rearrange("p (j t) -> p j t", j=4)
                    for g in range(8):
                        lo = (g % 2) * 64
                        kw = {"tile_position": (0, 64)} if g % 2 else {}
                        op("pe", lambda e, g=g, lo=lo, kw=kw: e.matmul(
                            pbs[lo:lo + 64, g // 2, :], lhsT=vnh[:, g * 64:(g + 1) * 64], rhs=wstb[:, g, :],
                            start=True, stop=True, **kw),
                           reads=["vnh", "wstb"], writes=[tPB[5]])
                    op("dve", lambda e: e.tensor_tensor(out=gtmp, in0=pbs,
                                                        in1=gvcol[:].unsqueeze(2).to_broadcast([128, 4, 128]), op=ALU.mult),
                       reads=[tPB[5], "gvcol"], writes=["gtmp"])
                    op("dve", lambda e: e.tensor_tensor(out=gtmp, in0=gtmp, in1=bst[:], op=ALU.add),
                       reads=["bst"], writes=["gtmp"])
                    op("dve", lambda e, ts=ts: e.tensor_tensor(out=aT[:, :, ts:ts + 128], in0=gtmp,
                                                               in1=uT[:, :, ts:ts + 128], op=ALU.mult),
                       reads=["gtmp", "uT"], writes=["aT"])
                    for kh in range(2):
                        pr = slice(kh * 64, (kh + 1) * 64)
                        variants = [(0, slot)] + ([(1, slot - 1)] if gblk > 0 else [])
                        for (vi, ksl) in variants:
                            bank = 6 + vi
                            op("pe", lambda e, pr=pr, ksl=ksl, ts=ts, bank=bank: e.matmul(
                                PB[bank][:].rearrange("p (j t) -> p j t", j=4),
                                lhsT=kT[pr, ksl * 128:(ksl + 1) * 128], rhs=qT[pr, :, ts:ts + 128],
                                start=True, stop=True),
                               reads=["kT", "qT"], writes=[tPB[bank]])
                            op("dve", lambda e, vi=vi, kh=kh, bank=bank: e.tensor_tensor(
                                out=sc[vi], in0=PB[bank][:],
                                in1=BM[:, vi, kh, :, :].rearrange("p j t -> p (j t)"), op=ALU.add),
                               reads=[tPB[bank], "BM"], writes=["sc0"])
                            op("act", lambda e, vi=vi: e.activation(out=pT[vi], in_=sc[vi], func=AF.Exp),
                               reads=["sc0"], writes=[f"pT{vi}"])
                        nv = len(variants)
                        for k, (vi, ksl) in enumerate(variants):
                            op("pe", lambda e, vi=vi, ksl=ksl, kh=kh, k=k, nv=nv: e.matmul(
                                PB[1][0:64, :], lhsT=vatt[:, ksl, kh * 64:(kh + 1) * 64], rhs=pT[vi],
                                start=(k == 0), stop=(k == nv - 1)),
                               reads=["vatt", f"pT{vi}"], writes=[tPB[1]])
                        for k, (vi, ksl) in enumerate(variants):
                            op("pe", lambda e, vi=vi, k=k, nv=nv: e.matmul(
                                PB[2][0:64, :], lhsT=ones64[:], rhs=pT[vi], start=(k == 0), stop=(k == nv - 1)),
                               reads=["ones64", f"pT{vi}"], writes=[tPB[2]])
                        op("dve", lambda e, kh=kh: e.tensor_tensor(
                            out=den.rearrange("p (j t) -> p j t", j=4),
                            in0=PB[2][0:64, :].rearrange("p (j t) -> p j t", j=4),
                            in1=sinkexp[:, kh * 4:(kh + 1) * 4].unsqueeze(2).to_broadcast([64, 4, 128]), op=ALU.add),
                           reads=[tPB[2], "sinkexp"], writes=["den"])
                        op("dve", lambda e: e.reciprocal(out=den, in_=den), reads=[], writes=["den"])
                        op("dve", lambda e, kh=kh, ts=ts: e.tensor_tensor(
                            out=bT[:, kh * 4:(kh + 1) * 4, ts:ts + 128],
                            in0=PB[1][0:64, :].rearrange("p (j t) -> p j t", j=4),
                            in1=den.rearrange("p (j t) -> p j t", j=4), op=ALU.mult),
                           reads=[tPB[1], "den"], writes=["bT"])

                for j in range(8):
                    wga, tga = stream_block(GA_OFF + j * 128)
                    for c in range(8):
                        op("pe", lambda e, c=c, wga=wga: e.matmul(PB[3][:], lhsT=wga[:, c, :], rhs=hnT[:, c, :],
                                                                  start=(c == 0), stop=(c == 7)),
                           reads=[tga, "hnT"], writes=[tPB[3]])
                    wgb, tgb = stream_block(GB_OFF + j * 128)
                    for c in range(8):
                        op("pe", lambda e, c=c, wgb=wgb: e.matmul(PB[4][:], lhsT=wgb[:, c, :], rhs=hnT[:, c, :],
                                                                  start=(c == 0), stop=(c == 7)),
                           reads=[tgb, "hnT"], writes=[tPB[4]])
                    for c in range(4):
                        op("pe", lambda e, c=c, j=j: e.matmul(PB[5][:], lhsT=wpa[:, c, j * 128:(j + 1) * 128], rhs=aT[:, c, :],
                                                              start=(c == 0), stop=(c == 3)),
                           reads=[tE[0][0], "aT"], writes=[tPB[5]])
                    for h in range(8):
                        op("pe", lambda e, h=h, j=j: e.matmul(PB[6][:], lhsT=wpb[:, h, j * 128:(j + 1) * 128], rhs=bT[:, h, :],
                                                              start=(h == 0), stop=(h == 7)),
                           reads=[tE[1][0], tE[1][1], "bT"], writes=[tPB[6]])
                    op("act", lambda e: e.activation(out=sga, in_=PB[3][:], func=AF.Sigmoid),
                       reads=[tPB[3]], writes=["sga"])
                    op("act", lambda e: e.activation(out=sgb, in_=PB[4][:], func=AF.Sigmoid),
                       reads=[tPB[4]], writes=["sgb"])
                    op("dve", lambda e: e.tensor_tensor(out=sga, in0=PB[5][:], in1=sga, op=ALU.mult),
                       reads=[tPB[5]], writes=["sga"])
                    op("dve", lambda e: e.tensor_tensor(out=sgb, in0=PB[6][:], in1=sgb, op=ALU.mult),
                       reads=[tPB[6]], writes=["sgb"])
                    op("dve", lambda e, j=j: e.tensor_tensor(out=mT[:, j, :], in0=sga, in1=sgb, op=ALU.add),
                       reads=["sga", "sgb"], writes=["mT"])
                for i in tiles:
                    ts = (i - st * 4) * 128
                    for hf in range(2):
                        bank = 1 + hf
                        for j in range(8):
                            op("pe", lambda e, j=j, ts=ts, hf=hf, bank=bank: e.matmul(
                                PB[bank][:], lhsT=mT[:, j, ts:ts + 128], rhs=wout[:, j, hf * 512:(hf + 1) * 512],
                                start=(j == 0), stop=(j == 7)),
                               reads=["mT", tE[0][1], tE[0][2]], writes=[tPB[bank]])
                        op("dve", lambda e, i=i, hf=hf, bank=bank: e.tensor_tensor(
                            out=H[:, i, hf * 512:(hf + 1) * 512], in0=PB[bank][:], in1=H[:, i, hf * 512:(hf + 1) * 512],
                            op=ALU.add),
                           reads=[tPB[bank]], writes=[tH[i]])
                    op("sp", lambda e, i=i, tok0=tok0: e.dma_start(out=hS_d[tok0 + i * 128: tok0 + (i + 1) * 128, :],
                                                                   in_=H[:, i, :]),
                       reads=[tH[i]], writes=["hS"], dma=f"H{i}")

                def spill_hn2(i, tok0=tok0):
                    op("sp", lambda e, i=i, tok0=tok0: e.dma_start(out=hn2S_d[tok0 + i * 128: tok0 + (i + 1) * 128, :], in_=hn),
                       reads=["hn"], writes=["hn2S"], dma="hn2S")
                norm_transpose(tiles, gfcol, hn2T, lambda i, st=st: (i - st * 4) * 128, lambda i: tk("hn2T"),
                               after_hn=spill_hn2)
                for i in tiles:
                    gi = ps_i * NT + i
                    ts = (i - st * 4) * 128
                    for c in range(8):
                        op("pe", lambda e, c=c, ts=ts: e.matmul(PB[7][:, 0:20], lhsT=hn2T[:, c, ts:ts + 128], rhs=wr[:, c, :],
                                                                start=(c == 0), stop=(c == 7)),
                           reads=["hn2T", "wr"], writes=[tPB[7]])
                    op("dve", lambda e, gi=gi: e.tensor_tensor(out=lg[:, gi, :], in0=PB[7][:, 0:20], in1=rb[:], op=ALU.add),
                       reads=[tPB[7], "rb"], writes=["lg"])

            if debug and ps_i == 0:
                for i in range(NT):
                    out_ops.append(op("sp", lambda e, i=i: e.dma_start(out=dbg_d[i * 128:(i + 1) * 128, :], in_=H[:, i, :]),
                                      reads=[tH[i]], dma=f"H{i}"))
        precast_some(100)

        S.barrier("dve", lambda e: e.memset(dummy[:], 0.0))
        NTK = npass * NT

        cdma2 = op("sp", lambda e: e.dma_start(out=gfin, in_=gfin_d.partition_broadcast(128)), writes=["gfin"], dma="gfin")

        gl = lg[:, :, 0:4]
        el = lg[:, :, 4:20].rearrange("p t (g x) -> p t g x", g=4)
        bc3 = lambda a: a.unsqueeze(2).to_broadcast([128, NTT, 4])
        if npass < NPASS_FULL:
            op("dve", lambda e: e.memset(lg[:, NTK:, :], 0.0), writes=["lg"])
        dv = lambda fn, r, w: op("dve", fn, reads=r, writes=w)
        dv(lambda e: e.tensor_reduce(out=r_m, in_=gl, axis=AX.X, op=ALU.max), ["lg"], ["r_m"])
        dv(lambda e: e.tensor_tensor(out=r_og, in0=gl, in1=bc3(r_m), op=ALU.is_equal), ["lg", "r_m"], ["r_og"])
        dv(lambda e: e.tensor_tensor(out=r_eg, in0=gl, in1=bc3(r_m), op=ALU.subtract), ["lg", "r_m"], ["r_eg"])
        op("act", lambda e: e.activation(out=r_eg, in_=r_eg, func=AF.Exp), reads=[], writes=["r_eg"])
        dv(lambda e: e.tensor_reduce(out=r_gp, in_=r_eg, axis=AX.X, op=ALU.add), ["r_eg"], ["r_gp"])
        dv(lambda e: e.reciprocal(out=r_gp, in_=r_gp), [], ["r_gp"])
        dv(lambda e: e.tensor_tensor(out=r_sel, in0=el, in1=r_og.unsqueeze(3).to_broadcast([128, NTT, 4, 4]), op=ALU.mult),
           ["lg", "r_og"], ["r_sel"])
        dv(lambda e: e.tensor_reduce(out=r_es, in_=r_sel.rearrange("p t g x -> p t x g"), axis=AX.X, op=ALU.add),
           ["r_sel"], ["r_es"])
        dv(lambda e: e.tensor_reduce(out=r_m1, in_=r_es, axis=AX.X, op=ALU.max), ["r_es"], ["r_m1"])
        dv(lambda e: e.tensor_tensor(out=r_o1, in0=r_es, in1=bc3(r_m1), op=ALU.is_equal), ["r_es", "r_m1"], ["r_o1"])
        dv(lambda e: e.scalar_tensor_tensor(out=r_es2, in0=r_o1, scalar=-1e9, in1=r_es, op0=ALU.mult, op1=ALU.add),
           ["r_o1", "r_es"], ["r_es2"])
        dv(lambda e: e.tensor_reduce(out=r_m2, in_=r_es2, axis=AX.X, op=ALU.max), ["r_es2"], ["r_m2"])
        dv(lambda e: e.tensor_tensor(out=r_o2, in0=r_es2, in1=bc3(r_m2), op=ALU.is_equal), ["r_es2", "r_m2"], ["r_o2"])
        dv(lambda e: e.tensor_tensor(out=r_d, in0=r_m2, in1=r_m1, op=ALU.subtract), ["r_m1", "r_m2"], ["r_d"])
        op("act", lambda e: e.activation(out=r_d, in_=r_d, func=AF.Exp), reads=[], writes=["r_d"])
        dv(lambda e: e.tensor_scalar_add(out=r_w1, in0=r_d, scalar1=1.0), ["r_d"], ["r_w1"])
        dv(lambda e: e.reciprocal(out=r_w1, in_=r_w1), [], ["r_w1"])
        dv(lambda e: e.tensor_tensor(out=wB[:], in0=r_d, in1=r_w1, op=ALU.mult), ["r_d", "r_w1"], ["wB"])
        dv(lambda e: e.tensor_tensor(out=wA[:], in0=r_w1, in1=r_gp, op=ALU.mult), ["r_w1", "r_gp"], ["wA"])
        dv(lambda e: e.tensor_tensor(out=wB[:], in0=wB[:], in1=r_gp, op=ALU.mult), ["r_gp"], ["wB"])
        bg = lambda a: a.unsqueeze(3).to_broadcast([128, NTT, 4, 4])
        bx = lambda a: a.unsqueeze(2).to_broadcast([128, NTT, 4, 4])
        dv(lambda e: e.tensor_tensor(out=M1, in0=bg(r_og), in1=bx(r_o1), op=ALU.mult), ["r_og", "r_o1"], ["M1"])
        dv(lambda e: e.tensor_tensor(out=M2, in0=bg(r_og), in1=bx(r_o2), op=ALU.mult), ["r_og", "r_o2"], ["M2"])
        M1f = M1.rearrange("p t g x -> p t (g x)")
        M2f = M2.rearrange("p t g x -> p t (g x)")
        dv(lambda e: e.tensor_tensor(out=Mb.rearrange("p (t n) -> p t n", n=16), in0=M1f, in1=M2f, op=ALU.add),
           ["M1", "M2"], ["Mb"])
        if npass < NPASS_FULL:
            dv(lambda e: e.memset(Mb[:, NTK * 16:], 0.0), [], ["Mb"])
        op("pe", lambda e: e.matmul(PB[0][:], lhsT=utri[:], rhs=Mb, start=True, stop=True),
           reads=["utri", "Mb"], writes=[tPB[0]])
        op("pe", lambda e: e.matmul(PB[1][:], lhsT=ones128[:], rhs=Mb, start=True, stop=True),
           reads=["ones128", "Mb"], writes=[tPB[1]])
        dv(lambda e: e.tensor_copy(out=R1s.rearrange("p t n -> p (t n)"), in_=PB[0][:]), [tPB[0]], ["R1s"])
        dv(lambda e: e.tensor_copy(out=Tts.rearrange("p t n -> p (t n)"), in_=PB[1][:]), [tPB[1]], ["Tts"])
        dv(lambda e: e.tensor_copy(out=Ca, in_=Tts), ["Tts"], ["Ca"])
        cur, nxt, cn, nn = Ca, Cb, "Ca", "Cb"
        sh = 1
        while sh < NTT:
            dv(lambda e, cur=cur, nxt=nxt, sh=sh: e.tensor_copy(out=nxt[:, 0:sh, :], in_=cur[:, 0:sh, :]), [cn], [nn])
            dv(lambda e, cur=cur, nxt=nxt, sh=sh: e.tensor_tensor(out=nxt[:, sh:, :], in0=cur[:, sh:, :],
                                                                  in1=cur[:, 0:NTT - sh, :], op=ALU.add), [cn], [nn])
            cur, nxt, cn, nn = nxt, cur, nn, cn
            sh *= 2
        Cin, cin_n, Cex, cex_n = cur, cn, nxt, nn
        dv(lambda e: e.tensor_tensor(out=Cex, in0=Cin, in1=Tts, op=ALU.subtract), [cin_n, "Tts"], [cex_n])
        ntot = Cin[:, NTT - 1, :]
        thr = cst[:, 0:8]
        tri = cst[:, 8:264].rearrange("p (a b) -> p a b", a=16)
        sio = cst[:, 264:296]
        cmp8 = r_cmp.rearrange("p a b -> p (a b)")[:, 0:128].rearrange("p (a b) -> p a b", b=8)
        dv(lambda e: e.tensor_tensor(out=cmp8, in0=ntot.unsqueeze(2).to_broadcast([128, 16, 8]),
                                     in1=thr.unsqueeze(1).to_broadcast([128, 16, 8]), op=ALU.is_gt),
           [cin_n, "cst"], ["r_cmp"])
        dv(lambda e: e.tensor_reduce(out=r_nb, in_=cmp8, axis=AX.X, op=ALU.add), ["r_cmp"], ["r_nb"])
        dv(lambda e: e.tensor_tensor(out=r_cmp, in0=r_nb.unsqueeze(1).to_broadcast([128, 16, 16]), in1=tri, op=ALU.mult),
           ["r_nb", "cst"], ["r_cmp"])
        dv(lambda e: e.tensor_reduce(out=r_ob, in_=r_cmp, axis=AX.X, op=ALU.add), ["r_cmp"], ["r_ob"])
        dv(lambda e: e.tensor_tensor(out=r_oe, in0=r_ob, in1=r_nb, op=ALU.add), ["r_ob", "r_nb"], ["r_oe"])
        dv(lambda e: e.tensor_tensor(out=R1s, in0=R1s, in1=Cex, op=ALU.add), [cex_n], ["R1s"])
        dv(lambda e: e.scalar_tensor_tensor(out=R1s, in0=r_ob.unsqueeze(1).to_broadcast([128, NTT, 16]), scalar=512.0,
                                            in1=R1s, op0=ALU.mult, op1=ALU.add), ["r_ob"], ["R1s"])
        for (Mf, mn, pos_i, pn) in ((M1f, "M1", posA, "posA"), (M2f, "M2", posB, "posB")):
            dv(lambda e, Mf=Mf: e.tensor_tensor(out=Mf, in0=Mf, in1=R1s, op=ALU.mult), ["R1s"], [mn])
            dv(lambda e, Mf=Mf: e.tensor_reduce(out=r_pf, in_=Mf, axis=AX.X, op=ALU.add), [mn], ["r_pf"])
            dv(lambda e: e.tensor_scalar(out=r_pf, in0=r_pf, scalar1=0.0, scalar2=float(NSLOT * 512 - 1),
                                         op0=ALU.max, op1=ALU.min), [], ["r_pf"])
            dv(lambda e, pos_i=pos_i: e.tensor_copy(out=pos_i[:], in_=r_pf), ["r_pf"], [pn])
        dv(lambda e: e.tensor_tensor(out=r_cmp2, in0=sio.unsqueeze(2).to_broadcast([128, NSLOT, 16]),
                                     in1=r_oe.unsqueeze(1).to_broadcast([128, NSLOT, 16]), op=ALU.is_ge),
           ["cst", "r_oe"], ["r_cmp2"])
        dv(lambda e: e.tensor_reduce(out=r_eid, in_=r_cmp2, axis=AX.X, op=ALU.add), ["r_cmp2"], ["r_eid"])
        dv(lambda e: e.tensor_scalar(out=r_eid, in0=r_eid, scalar1=15.0, scalar2=128.0, op0=ALU.min, op1=ALU.mult),
           [], ["r_eid"])
        dv(lambda e: e.tensor_tensor(out=r_eid, in0=r_eid, in1=iop[:].to_broadcast([128, NSLOT]), op=ALU.add),
           ["iop"], ["r_eid"])
        dv(lambda e: e.tensor_scalar(out=r_eid, in0=r_eid, scalar1=0.0, scalar2=2047.0, op0=ALU.max, op1=ALU.min),
           [], ["r_eid"])
        dv(lambda e: e.tensor_copy(out=idxW[:], in_=r_eid), ["r_eid"], ["idxW"])
        if debug:
            dr = sb("dbgr_s", [128, 6, NTT], F32)
            dv(lambda e: e.tensor_copy(out=dr[:, 0, :], in_=posA[:]), ["posA"], ["dr"])
            dv(lambda e: e.tensor_copy(out=dr[:, 1, :], in_=posB[:]), ["posB"], ["dr"])
            dv(lambda e: e.tensor_copy(out=dr[:, 2, :], in_=wA[:]), ["wA"], ["dr"])
            dv(lambda e: e.tensor_copy(out=dr[:, 3, :], in_=wB[:]), ["wB"], ["dr"])
            dv(lambda e: e.tensor_copy(out=dr[:, 4, :], in_=idxW[:]), ["idxW"], ["dr"])
            dv(lambda e: e.tensor_copy(out=dr[:, 5, 0:16], in_=ntot), [cin_n], ["dr"])
            out_ops.append(op("sp", lambda e: e.dma_start(out=dbgr_d, in_=dr[:]), reads=["dr"], dma="dbgr"))

        for gi in range(NTK if stages >= 3 else 0):
            xb = xin[gi % NXIN]
            xn = f"xin{gi % NXIN}"
            op("sp", lambda e, gi=gi, xb=xb: e.dma_start(out=xb, in_=hn2S_d[gi * 128:(gi + 1) * 128, :]),
               reads=["hn2S"], writes=[xn], dma=xn)
            for (pos_i, pn) in ((posA, "posA"), (posB, "posB")):
                op("pool", lambda e, gi=gi, xb=xb, pos_i=pos_i: e.indirect_dma_start(
                    out=Xs_d, out_offset=bass.IndirectOffsetOnAxis(pos_i[:, gi:gi + 1], 0), in_=xb, in_offset=None),
                   reads=[xn, pn], writes=[f"Xs{gi % NXIN}"], dma=f"scat{gi % NXIN}")

        for s in range(NSLOT if stages >= 3 else 0):
            b = s % NEB
            for m in range(3):
                op("pool", lambda e, s=s, b=b, m=m: e.indirect_dma_start(
                    out=wflat[b][m], out_offset=None, in_=wscr_d[m],
                    in_offset=bass.IndirectOffsetOnAxis(idxW[:, s:s + 1], 0)),
                   reads=["idxW", "wscr"], writes=[tE[b][m]], dma=f"w{'gud'[m]}{b}")
            xb = xs[s % 2]
            xn = f"xs{s % 2}"
            op("sp", lambda e, s=s, xb=xb: e.dma_start(out=xb, in_=Xs_d[s * 512:(s + 1) * 512, :].rearrange("(j p) d -> p j d", p=128)),
               reads=[f"Xs{q}" for q in range(NXIN)], writes=[xn], dma=xn)
            xt = xT[s % 2]
            xtn = f"xT{s % 2}"
            for j in range(4):
                bank = j % 2
                pbT = PB[bank][:].bitcast(BF16).rearrange("p (c t) -> p c t", c=8)
                for c in range(8):
                    op("pe", lambda e, c=c, j=j, pbT=pbT, xb=xb: e.transpose(out=pbT[:, c, :], in_=xb[:, j, c * 128:(c + 1) * 128],
                                                                            identity=ident[:]),
                       reads=[xn, "ident"], writes=[tPB[bank]])
                op("dve", lambda e, j=j, pbT=pbT, xt=xt: e.tensor_tensor(
                    out=xt[:, :, j * 128:(j + 1) * 128], in0=pbT,
                    in1=gfcol[:].unsqueeze(2).to_broadcast([128, 8, 128]), op=ALU.mult),
                   reads=[tPB[bank], "gfcol"], writes=[xtn])
            hb_ = s % 2
            for j in range(4):
                bg_, bu_ = 2 + (j % 2) * 2, 3 + (j % 2) * 2
                for c in range(8):
                    op("pe", lambda e, c=c, j=j, b=b, bg_=bg_, xt=xt: e.matmul(
                        PB[bg_][:], lhsT=wgs[b][:, c, j * 128:(j + 1) * 128], rhs=xt[:, c, :],
                        start=(c == 0), stop=(c == 7)),
                       reads=[tE[b][0], xtn], writes=[tPB[bg_]])
                for c in range(8):
                    op("pe", lambda e, c=c, j=j, b=b, bu_=bu_, xt=xt: e.matmul(
                        PB[bu_][:], lhsT=wus[b][:, c, j * 128:(j + 1) * 128], rhs=xt[:, c, :],
                        start=(c == 0), stop=(c == 7)),
                       reads=[tE[b][1], xtn], writes=[tPB[bu_]])
                op("act", lambda e, j=j, bg_=bg_: e.activation(out=sgs[j % 2], in_=PB[bg_][:], func=AF.Silu),
                   reads=[tPB[bg_]], writes=[f"sgs{j % 2}"])
                op("dve", lambda e, j=j, bu_=bu_, hb_=hb_: e.tensor_tensor(out=hid[hb_][:, j, :], in0=PB[bu_][:],
                                                                         in1=sgs[j % 2], op=ALU.mult),
                   reads=[tPB[bu_], f"sgs{j % 2}"], writes=[f"hid{hb_}"])
            for tl in range(4):
                yi = (s * 4 + tl) % NYS
                yb = ys[yi]
                yn = f"ys{yi}"
                for hf in range(2):
                    bank = 6 + hf
                    for j in range(4):
                        op("pe", lambda e, j=j, tl=tl, hf=hf, b=b, hb_=hb_, bank=bank: e.matmul(
                            PB[bank][:], lhsT=hid[hb_][:, j, tl * 128:(tl + 1) * 128],
                            rhs=wds[b][:, j, hf * 512:(hf + 1) * 512], start=(j == 0), stop=(j == 3)),
                           reads=[f"hid{hb_}", tE[b][2]], writes=[tPB[bank]])
                    op("act", lambda e, hf=hf, bank=bank, yb=yb: e.copy(out=yb[:, hf * 512:(hf + 1) * 512], in_=PB[bank][:]),
                       reads=[tPB[bank]], writes=[yn])
                op("sp", lambda e, s=s, tl=tl, yb=yb: e.dma_start(out=Ys_d[s * 512 + tl * 128: s * 512 + (tl + 1) * 128, :], in_=yb),
                   reads=[yn], writes=[f"Ys{yi}"], dma=f"ysd{yi}")

        for gi in range(NTK if stages >= 4 else 0):
            k2 = gi % 2
            op("sp", lambda e, gi=gi, k2=k2: e.dma_start(out=hb[k2], in_=hS_d[gi * 128:(gi + 1) * 128, :]),
               reads=["hS"], writes=[f"hb{k2}"], dma=f"hb{k2}")
            for (Yb, ynm, pos_i, pn) in ((YA, "YA", posA, "posA"), (YB, "YB", posB, "posB")):
                op("pool", lambda e, gi=gi, k2=k2, Yb=Yb, pos_i=pos_i: e.indirect_dma_start(
                    out=Yb[k2], out_offset=None, in_=Ys_d, in_offset=bass.IndirectOffsetOnAxis(pos_i[:, gi:gi + 1], 0)),
                   reads=[f"Ys{q}" for q in range(NYS)] + [pn], writes=[f"{ynm}{k2}"], dma=f"{ynm}{k2}")
            op("dve", lambda e, gi=gi, k2=k2: e.scalar_tensor_tensor(out=hb[k2], in0=YA[k2], scalar=wA[:, gi:gi + 1], in1=hb[k2],
                                                                     op0=ALU.mult, op1=ALU.add),
               reads=[f"YA{k2}", "wA"], writes=[f"hb{k2}"])
            op("dve", lambda e, gi=gi, k2=k2: e.scalar_tensor_tensor(out=hb[k2], in0=YB[k2], scalar=wB[:, gi:gi + 1], in1=hb[k2],
                                                                     op0=ALU.mult, op1=ALU.add),
               reads=[f"YB{k2}", "wB"], writes=[f"hb{k2}"])
            op("act", lambda e, k2=k2: e.activation(out=hn4, in_=hb[k2], func=AF.Square, accum_out=ss4),
               reads=[f"hb{k2}"], writes=["hn4", "ss4"])
            op("dve", lambda e: e.tensor_scalar(out=rs4, in0=ss4, scalar1=1.0 / D, scalar2=EPS, op0=ALU.mult, op1=ALU.add),
               reads=["ss4"], writes=["rs4"])
            op("act", lambda e: e.sqrt(out=rs4, in_=rs4), reads=[], writes=["rs4"])
            op("dve", lambda e: e.reciprocal(out=rs4, in_=rs4), reads=[], writes=["rs4"])
            op("dve", lambda e, k2=k2: e.scalar_tensor_tensor(out=hb[k2], in0=hb[k2], scalar=rs4, in1=gfin,
                                                              op0=ALU.mult, op1=ALU.mult),
               reads=["rs4", "gfin"], writes=[f"hb{k2}"])
            out_ops.append(op("sp", lambda e, gi=gi, k2=k2: e.dma_start(out=y_d[gi * 128:(gi + 1) * 128, :], in_=hb[k2]),
                              reads=[f"hb{k2}"], dma=f"hb{k2}"))

        S.emit(nc, final_wait_ops=out_ops)
    return nc


def _t5_bucket(dist):
    dist = np.asarray(dist, dtype=np.int64)
    nf = np.maximum(dist, 1).astype(np.float32)
    large = 16 + (np.log(nf / np.float32(16)) / np.float32(math.log(128 / 16)) * np.float32(16)).astype(np.int32)
    large = np.minimum(large, 31)
    return np.where(dist < 16, dist, large)


def prep_shared(inp):
    f = lambda a: np.ascontiguousarray(np.asarray(a, dtype=np.float32))
    w_in = f(inp["w_in"])[0]
    perm = np.arange(3840)
    qcols = []
    for j in range(4):
        qcols += list(range(1024 + j * 64, 1024 + (j + 1) * 64))
        qcols += list(range(1024 + (4 + j) * 64, 1024 + (5 + j) * 64))
    perm[1024:1536] = np.array(qcols)
    w_in = np.ascontiguousarray(w_in[:, perm])
    wr = np.concatenate([f(inp["router_group_w"])[0]] + [f(inp["router_expert_w"])[0, g] for g in range(4)], axis=1)
    rb = np.concatenate([f(inp["router_group_b"])[0], f(inp["router_expert_b"])[0].reshape(-1)])[None, :]
    col = lambda v, c: np.ascontiguousarray(f(v).reshape(c, 128).T)
    ws = f(inp["gm_w_spatial"])[0]
    wst = np.ascontiguousarray(ws.transpose(2, 0, 1))
    s_i = np.arange(128)[:, None, None]
    t_i = np.arange(128)[None, None, :]
    wmask = np.broadcast_to((s_i <= t_i), (128, 8, 128)).astype(np.float32)
    bs = f(inp["gm_b_spatial"])[0]
    bst = np.zeros((128, 4, 128), np.float32)
    for g in range(8):
        bst[(g % 2) * 64:(g % 2) * 64 + 64, g // 2, :] = bs[g][None, :]
    rel = f(inp["rel_bias"])
    s_ = np.arange(128)[:, None]
    q_ = np.arange(128)[None, :]
    d_own = q_ - s_
    d_prev = q_ + 128 - s_
    bias = np.zeros((128, 2, 2, 4, 128), np.float32)
    maskc = np.zeros((128, 2, 2, 4, 128), np.float32)
    b_own = _t5_bucket(np.clip(d_own, 0, 127))
    b_prev = _t5_bucket(np.clip(d_prev, 0, 127))
    for kh in range(2):
        for h4 in range(4):
            h = kh * 4 + h4
            bias[:, 0, kh, h4, :] = rel[b_own, h]
            bias[:, 1, kh, h4, :] = rel[b_prev, h]
            maskc[:, 0, kh, h4, :] = np.where(d_own >= 0, 0.0, NEG)
            maskc[:, 1, kh, h4, :] = np.where(d_prev < 128, 0.0, NEG)
    def pmaj(w, c):
        e_, r_, n_ = w.shape
        return np.ascontiguousarray(w.reshape(e_, c, 128, n_).transpose(0, 2, 1, 3).reshape(e_, 128, c * n_))

    return {
        "w_in": w_in,
        "wpa": f(inp["w_proj_a"])[0], "wpb": f(inp["w_proj_b"])[0], "wout": f(inp["w_out"])[0],
        "wgp": pmaj(f(inp["expert_w_gate"])[0], 8), "wup": pmaj(f(inp["expert_w_up"])[0], 8),
        "wdp": pmaj(f(inp["expert_w_down"])[0], 4),
        "utri": np.triu(np.ones((128, 128), np.float32), 1),
        "cst": np.concatenate([np.arange(8, dtype=np.float32) * 512.0,
                               np.tril(np.ones((16, 16), np.float32), -1).reshape(-1),
                               np.arange(32, dtype=np.float32)])[None, :],
        "iop": np.arange(128, dtype=np.float32)[:, None],
        "wr": np.ascontiguousarray(wr), "rb": np.ascontiguousarray(rb),
        "gacol": col(inp["attn_norm_g"], 8), "gfcol": col(inp["ffn_norm_g"], 8), "gvcol": col(inp["gm_v_norm_g"], 4),
        "gfin": f(inp["final_norm_g"]).reshape(1, D),
        "wst": wst, "wmask": wmask, "bst": bst,
        "sinks": f(inp["attn_sinks"]).reshape(1, 8),
        "bias": bias, "maskc": maskc,
        "idn": np.eye(128, dtype=np.float32),
    }


def kernel(**inputs):
    shared = prep_shared(inputs)
    x = np.asarray(inputs["x"], dtype=np.float32)
    nc = build()
    in_maps = []
    for c in range(NCORES):
        m = dict(shared)
        m["x"] = np.ascontiguousarray(x[c])
        in_maps.append(m)
    res = run_bass_kernel_spmd(nc, in_maps, core_ids=list(range(NCORES)))
    return np.stack([np.asarray(r["y"], dtype=np.float32) for r in res.results], axis=0)
```

```python
import math
from contextlib import ExitStack

import numpy as np
import concourse.bass as bass
import concourse.mybir as mybir
from concourse.bass_utils import run_bass_kernel_spmd

F32 = mybir.dt.float32
BF16 = mybir.dt.bfloat16
AF = mybir.ActivationFunctionType
ALU = mybir.AluOpType
AX = mybir.AxisListType

SEQ = 4096
D = 1024
NCORES = 8
PT = 1024
NT = PT // 128
NST = PT // 512
NPASS_FULL = SEQ // PT
EPS = 1e-6
NEG = -30000.0
NSLOT = 32
NTT = SEQ // 128
I32 = mybir.dt.int32

U_OFF, V_OFF, Q_OFF, K_OFF, VA_OFF, GA_OFF, GB_OFF = 0, 512, 1024, 1536, 1664, 1792, 2816

ENG = ("pe", "act", "dve", "pool", "sp")


class Tok:
    __slots__ = ("name", "last_w", "readers")

    def __init__(self, name):
        self.name = name
        self.last_w = None
        self.readers = []


class Op:
    __slots__ = ("eng", "fn", "deps", "sig", "dma_sem", "dma_cnt", "seq")

    def __init__(self, eng, fn):
        self.eng = eng
        self.fn = fn
        self.deps = set()
        self.sig = False
        self.dma_sem = None
        self.dma_cnt = 0
        self.seq = 0


class Sched:
    def __init__(self):
        self.ops = []
        self.per = {e: [] for e in ENG}
        self.dma_counts = {}
        self.total_keys = set()
        self.epoch_op = None
        self.last_dma = {}

    def op(self, eng, fn, reads=(), writes=(), dma=None):
        o = Op(eng, fn)
        for t in list(reads) + list(writes):
            if t.last_w is not None:
                o.deps.add(t.last_w)
        for t in writes:
            for r in t.readers:
                o.deps.add(r)
        if self.epoch_op is not None:
            o.deps.add(self.epoch_op)
        o.deps.discard(o)
        for t in reads:
            t.readers.append(o)
        for t in writes:
            t.last_w = o
            t.readers = []
        if dma is not None:
            o.dma_sem = dma
            self.dma_counts[dma] = self.dma_counts.get(dma, 0) + 1
            o.dma_cnt = self.dma_counts[dma]
            self.last_dma[dma] = o
        self.ops.append(o)
        self.per[eng].append(o)
        return o

    def barrier(self, eng, fn):
        o = Op(eng, fn)
        for e in ENG:
            seen_c = False
            for p in reversed(self.per[e]):
                if p.dma_sem is None:
                    o.deps.add(p)
                    break
        for k, p in self.last_dma.items():
            o.deps.add(p)
        if self.epoch_op is not None:
            o.deps.add(self.epoch_op)
        self.ops.append(o)
        self.per[eng].append(o)
        self.epoch_op = o
        return o

    def emit(self, nc, final_wait_ops=()):
        def skip(d, o):
            return d.dma_sem is None and o.dma_sem is None and d.eng == "pe" and o.eng == "pe"

        for o in self.ops:
            for d in o.deps:
                if d.dma_sem is None and not skip(d, o):
                    d.sig = True
        for o in final_wait_ops:
            if o.dma_sem is None:
                o.sig = True
        for e in ENG:
            c = 0
            for o in self.per[e]:
                if o.dma_sem is None and o.sig:
                    c += 1
                    o.seq = c
        with ExitStack() as es:
            esem = {e: es.enter_context(nc.semaphore(f"s_{e}")) for e in ENG}
            dsem = {k: es.enter_context(nc.semaphore(f"d_{k}")) for k in self.dma_counts}
            block = es.enter_context(nc.Block())

            def dval(d):
                if d.dma_sem in self.total_keys:
                    return 16 * self.dma_counts[d.dma_sem]
                return 16 * d.dma_cnt

            def need(o):
                w = {}
                for d in o.deps:
                    if d.dma_sem is not None:
                        key, val = ("d", d.dma_sem), dval(d)
                    else:
                        if skip(d, o):
                            continue
                        key, val = ("e", d.eng), d.seq
                    if w.get(key, 0) < val:
                        w[key] = val
                return w

            def run(ename):
                def body(eng):
                    waited = {}
                    for o in self.per[ename]:
                        for key, val in need(o).items():
                            if waited.get(key, 0) >= val:
                                continue
                            waited[key] = val
                            sem = dsem[key[1]] if key[0] == "d" else esem[key[1]]
                            eng.wait_ge(sem, val)
                        ins = o.fn(eng)
                        if o.dma_sem is not None:
                            ins.then_inc(dsem[o.dma_sem], 16)
                        elif o.sig:
                            ins.then_inc(esem[ename], 1)
                    if ename == "sp":
                        for o in final_wait_ops:
                            if o.dma_sem is not None:
                                eng.wait_ge(dsem[o.dma_sem], dval(o))
                            else:
                                eng.wait_ge(esem[o.eng], o.seq)
                return body

            block.tensor(run("pe"))
            block.scalar(run("act"))
            block.vector(run("dve"))
            block.gpsimd(run("pool"))
            block.sync(run("sp"))


class Arena:
    def __init__(self, nc, es, name, nbytes):
        self.t = es.enter_context(nc.sbuf_tensor(name, [128, nbytes // 2], BF16))
        self.cap = nbytes
        self.off = 0

    def alloc(self, shape, dt):
        n = 1
        for s_ in shape[1:]:
            n *= s_
        esz = 2 if dt == BF16 else 4
        o = self.off
        self.off += (n * esz + 63) // 64 * 64
        assert self.off <= self.cap, (self.off, self.cap)
        ap = self.t[0:shape[0], o // 2:(o + n * esz) // 2]
        if dt != BF16:
            ap = ap.bitcast(dt)
        if len(shape) > 2:
            names = " ".join(f"d{k}" for k in range(len(shape) - 1))
            ap = ap.rearrange(f"p ({names}) -> p {names}", **{f"d{k}": shape[k + 1] for k in range(len(shape) - 1)})
        return ap


def build(npass=NPASS_FULL, debug=False, stages=4):
    nc = bass.Bass("TRN2", target_bir_lowering=False)

    def din(name, shape):
        return nc.dram_tensor(name, list(shape), F32, kind="ExternalInput").ap()

    x_d = din("x", [SEQ, D])
    win_d = din("w_in", [D, 3840])
    wpa_d = din("wpa", [512, D])
    wpb_d = din("wpb", [512, D])
    wout_d = din("wout", [D, D])
    wexp_d = [din("wgp", [16, 128, 4096]), din("wup", [16, 128, 4096]), din("wdp", [16, 128, 4096])]
    wr_d = din("wr", [D, 20])
    rb_d = din("rb", [1, 20])
    gacol_d = din("gacol", [128, 8])
    gfcol_d = din("gfcol", [128, 8])
    gvcol_d = din("gvcol", [128, 4])
    gfin_d = din("gfin", [1, D])
    wst_d = din("wst", [128, 8, 128])
    wmask_d = din("wmask", [128, 8, 128])
    bst_d = din("bst", [128, 4, 128])
    sinks_d = din("sinks", [1, 8])
    bias_d = din("bias", [128, 2, 2, 4, 128])
    mask_d = din("maskc", [128, 2, 2, 4, 128])
    idn_d = din("idn", [128, 128])
    utri_d = din("utri", [128, 128])
    cst_d = din("cst", [1, 8 + 256 + 32])
    iop_d = din("iop", [128, 1])
    y_d = nc.dram_tensor("y", [SEQ, D], F32, kind="ExternalOutput").ap()
    hS_d = nc.dram_tensor("hS", [SEQ, D], F32, kind="Internal").ap()
    hn2S_d = nc.dram_tensor("hn2S", [SEQ, D], BF16, kind="Internal").ap()
    Xs_d = nc.dram_tensor("Xs", [NSLOT * 512, D], BF16, kind="Internal").ap()
    Ys_d = nc.dram_tensor("Ys", [NSLOT * 512, D], F32, kind="Internal").ap()
    wscr_d = [nc.dram_tensor(f"wscr{m}", [16 * 128, 4096], BF16, kind="Internal").ap() for m in range(3)]
    if debug:
        dbg_d = nc.dram_tensor("dbg", [PT, D], F32, kind="ExternalOutput").ap()
        dbgr_d = nc.dram_tensor("dbgr", [128, 6, NTT], F32, kind="ExternalOutput").ap()

    S = Sched()
    S.total_keys.update(["const"])

    with ExitStack() as es:
        def sb(name, shape, dt):
            return es.enter_context(nc.sbuf_tensor(name, list(shape), dt))

        ident = sb("ident", [128, 128], BF16)
        gacol = sb("gacol_s", [128, 8], F32)
        gfcol = sb("gfcol_s", [128, 8], F32)
        gvcol = sb("gvcol_s", [128, 4], F32)
        wstb = sb("wstb", [128, 8, 128], BF16)
        bst = sb("bst_s", [128, 4, 128], F32)
        sinkexp = sb("sinkexp", [64, 8], F32)
        BM = sb("BM", [128, 2, 2, 4, 128], F32)
        ones64 = sb("ones64", [128, 64], BF16)
        ones128 = sb("ones128", [128, 128], BF16)
        utri = sb("utri_s", [128, 128], BF16)
        cst = sb("cst_s", [128, 8 + 256 + 32], F32)
        iop = sb("iop_s", [128, 1], F32)
        wr = sb("wr_s", [128, 8, 20], BF16)
        rb = sb("rb_s", [128, 20], F32)
        lg = sb("lg_all", [128, NTT, 20], F32)
        posA = sb("posA", [128, NTT], I32)
        posB = sb("posB", [128, NTT], I32)
        wA = sb("wA", [128, NTT], F32)
        wB = sb("wB", [128, NTT], F32)
        idxW = sb("idxW", [128, NSLOT], I32)
        dummy = sb("bar_dummy", [128, 1], F32)
        NEB = 2
        wreg = sb("wreg", [128, NEB * 12288], BF16)
        wgs = [wreg[:, b * 12288:b * 12288 + 4096].rearrange("p (c n) -> p c n", c=8) for b in range(NEB)]
        wus = [wreg[:, b * 12288 + 4096:b * 12288 + 8192].rearrange("p (c n) -> p c n", c=8) for b in range(NEB)]
        wds = [wreg[:, b * 12288 + 8192:b * 12288 + 12288].rearrange("p (c n) -> p c n", c=4) for b in range(NEB)]
        wflat = [[wreg[:, b * 12288 + m * 4096:b * 12288 + (m + 1) * 4096] for m in range(3)] for b in range(NEB)]
        wpa = wreg[:, 0:4096].rearrange("p (c n) -> p c n", c=4)
        wout = wreg[:, 4096:12288].rearrange("p (c n) -> p c n", c=8)
        wpb = wreg[0:64, 12288:20480].rearrange("p (c n) -> p c n", c=8)
        stg = wreg[:, 20480:24576]
        AR = Arena(nc, es, "arena", 124 * 1024)
        H = AR.alloc([128, NT, D], F32)
        hn2T = AR.alloc([128, 8, 512], BF16)
        wv = AR.alloc([128, 8, 512], BF16)
        wva = AR.alloc([128, 8, 128], BF16)
        NRING = 4
        ring = [AR.alloc([128, 8, 128], BF16) for _ in range(NRING)]
        hn = AR.alloc([128, D], BF16)
        hnT = AR.alloc([128, 8, 512], BF16)
        wst = hnT[:, 0:2, :].rearrange("p a (b t) -> p (a b) t", b=4)
        wmask = hnT[:, 2:4, :].rearrange("p a (b t) -> p (a b) t", b=4)
        ss = AR.alloc([128, 8], F32)
        rstd = AR.alloc([128, 8], F32)
        uT = AR.alloc([128, 4, 512], BF16)
        qT = AR.alloc([128, 4, 512], BF16)
        kT = AR.alloc([128, (NT + 1) * 128], BF16)
        vatt = AR.alloc([128, NT + 1, 128], BF16)
        gv = AR.alloc([128, 512], F32)
        vnh = AR.alloc([128, 512], BF16)
        bnst = AR.alloc([128, 6], F32)
        mv = AR.alloc([128, 2], F32)
        rsv = AR.alloc([128, 1], F32)
        nmr = AR.alloc([128, 1], F32)
        gtmp = AR.alloc([128, 4, 128], F32)
        sc0_ = AR.alloc([128, 512], F32)
        sc = [sc0_, sc0_]
        pT = [AR.alloc([128, 512], BF16) for _ in range(2)]
        den = AR.alloc([64, 512], F32)
        aT = AR.alloc([128, 4, 512], BF16)
        bT = AR.alloc([64, 8, 512], BF16)
        sga = AR.alloc([128, 512], F32)
        sgb = AR.alloc([128, 512], F32)
        mT = AR.alloc([128, 8, 512], BF16)
        maskc = mT[:, 0:4, :].rearrange("p a (b c t) -> p (a b c) t", b=2, c=2).rearrange("p (v k h) t -> p v k h t", v=2, k=2)
        side1_end = AR.off
        AR.off = 0
        NB_ = NTT
        r_m = AR.alloc([128, NB_], F32)
        r_og = AR.alloc([128, NB_, 4], F32)
        r_eg = AR.alloc([128, NB_, 4], F32)
        r_gp = AR.alloc([128, NB_], F32)
        r_sel = AR.alloc([128, NB_, 4, 4], F32)
        r_es = AR.alloc([128, NB_, 4], F32)
        r_m1 = AR.alloc([128, NB_], F32)
        r_o1 = AR.alloc([128, NB_, 4], F32)
        r_es2 = AR.alloc([128, NB_, 4], F32)
        r_m2 = AR.alloc([128, NB_], F32)
        r_o2 = AR.alloc([128, NB_, 4], F32)
        r_d = AR.alloc([128, NB_], F32)
        r_w1 = AR.alloc([128, NB_], F32)
        M1 = AR.alloc([128, NB_, 4, 4], F32)
        M2 = AR.alloc([128, NB_, 4, 4], F32)
        Mb = AR.alloc([128, NB_ * 16], BF16)
        R1s = AR.alloc([128, NB_, 16], F32)
        Ca = AR.alloc([128, NB_, 16], F32)
        Cb = AR.alloc([128, NB_, 16], F32)
        Tts = AR.alloc([128, NB_, 16], F32)
        r_cmp = AR.alloc([128, 16, 16], F32)
        r_cmp2 = AR.alloc([128, NSLOT, 16], F32)
        r_nb = AR.alloc([128, 16], F32)
        r_ob = AR.alloc([128, 16], F32)
        r_oe = AR.alloc([128, 16], F32)
        r_pf = AR.alloc([128, NB_], F32)
        r_eid = AR.alloc([128, NSLOT], F32)
        gfin = AR.alloc([128, D], F32)
        NXIN = 4
        xin = [AR.alloc([128, D], BF16) for _ in range(NXIN)]
        xs = [AR.alloc([128, 4, D], BF16) for _ in range(2)]
        xT = [AR.alloc([128, 8, 512], BF16) for _ in range(2)]
        sgs = [AR.alloc([128, 512], F32) for _ in range(2)]
        hid = [AR.alloc([128, 4, 512], BF16) for _ in range(2)]
        NYS = 3
        ys = [AR.alloc([128, D], F32) for _ in range(NYS)]
        YA = [AR.alloc([128, D], F32) for _ in range(2)]
        YB = [AR.alloc([128, D], F32) for _ in range(2)]
        hb = [AR.alloc([128, D], F32) for _ in range(2)]
        ss4 = AR.alloc([128, 1], F32)
        rs4 = AR.alloc([128, 1], F32)
        hn4 = AR.alloc([128, D], BF16)
        PB = [es.enter_context(nc.psum_tensor(f"pb{i}", [128, 512], F32)) for i in range(8)]

        T = {}

        def tk(n):
            if n not in T:
                T[n] = Tok(n)
            return T[n]

        tH = [tk(f"H{i}") for i in range(NT)]
        tPB = [tk(f"PB{i}") for i in range(8)]
        tE = [[tk(f"wg{i}"), tk(f"wu{i}"), tk(f"wd{i}")] for i in range(NEB)]
        tRing = [tk(f"ring{i}") for i in range(NRING)]

        def op(eng, fn, reads=(), writes=(), dma=None):
            return S.op(eng, fn, [tk(r) if isinstance(r, str) else r for r in reads],
                        [tk(w) if isinstance(w, str) else w for w in writes], dma)

        def cdma(eng, out, in_, w):
            op(eng, lambda e, out=out, in_=in_: e.dma_start(out=out, in_=in_), writes=[w], dma="const")

        cdma("sp", gacol[:], gacol_d, "gacol")
        cdma("sp", gfcol[:], gfcol_d, "gfcol")
        cdma("sp", gvcol[:], gvcol_d, "gvcol")
        cdma("sp", bst[:], bst_d, "bst")
        cdma("sp", sinkexp[:], sinks_d.partition_broadcast(64), "sinkexp")
        cdma("sp", BM[:], bias_d, "BM")
        cdma("sp", rb[:], rb_d.partition_broadcast(128), "rb")
        cdma("sp", cst[:], cst_d.partition_broadcast(128), "cst")
        cdma("sp", iop[:], iop_d, "iop")
        cdma("pool", ident[:], idn_d, "ident")
        cdma("pool", utri[:], utri_d, "utri")
        cdma("pool", wst, wst_d, "wst_a")
        cdma("pool", wmask, wmask_d, "wmask_a")
        cdma("pool", maskc, mask_d, "maskc_a")
        cdma("pool", wr[:], wr_d.rearrange("(c p) n -> p c n", p=128), "wr")
        cdma("pool", wv, win_d[:, V_OFF:V_OFF + 512].rearrange("(c p) n -> p c n", p=128), "wv")
        cdma("pool", wva, win_d[:, VA_OFF:VA_OFF + 128].rearrange("(c p) n -> p c n", p=128), "wva")
        op("dve", lambda e: e.tensor_tensor(out=wstb[:], in0=wst, in1=wmask, op=ALU.mult),
           reads=["wst_a", "wmask_a"], writes=["wstb", "hnT"])
        op("dve", lambda e: e.tensor_tensor(out=BM[:], in0=BM[:], in1=maskc, op=ALU.add),
           reads=["maskc_a"], writes=["BM", "mT"])
        op("act", lambda e: e.activation(out=sinkexp[:], in_=sinkexp[:], func=AF.Exp), reads=[], writes=["sinkexp"])
        op("dve", lambda e: e.memset(ones64[:], 1.0), writes=["ones64"])
        op("dve", lambda e: e.memset(ones128[:], 1.0), writes=["ones128"])

        out_ops = []

        def rstd_ops(n):
            op("dve", lambda e: e.tensor_scalar(out=rstd[:, 0:n], in0=ss[:, 0:n], scalar1=1.0 / D, scalar2=EPS,
                                                op0=ALU.mult, op1=ALU.add), reads=["ss"], writes=["rstd"])
            op("act", lambda e: e.sqrt(out=rstd[:, 0:n], in_=rstd[:, 0:n]), reads=[], writes=["rstd"])
            op("dve", lambda e: e.reciprocal(out=rstd[:, 0:n], in_=rstd[:, 0:n]), reads=[], writes=["rstd"])

        def norm_transpose(ti_list, gcol, dstT, dst_col0, dst_tok, after_hn=None):
            n = len(ti_list)
            for k, i in enumerate(ti_list):
                op("act", lambda e, i=i, k=k: e.activation(out=hn, in_=H[:, i, :], func=AF.Square,
                                                          accum_out=ss[:, k:k + 1]),
                   reads=[tH[i]], writes=["hn", "ss"])
            rstd_ops(n)
            for k, i in enumerate(ti_list):
                op("act", lambda e, i=i, k=k: e.activation(out=hn, in_=H[:, i, :], func=AF.Identity,
                                                          scale=rstd[:, k:k + 1]),
                   reads=[tH[i], "rstd"], writes=["hn"])
                if after_hn is not None:
                    after_hn(i)
                pbT = PB[0][:].bitcast(BF16).rearrange("p (c t) -> p c t", c=8)
                for c in range(8):
                    op("pe", lambda e, c=c, pbT=pbT: e.transpose(out=pbT[:, c, :], in_=hn[:, c * 128:(c + 1) * 128],
                                                                 identity=ident[:]),
                       reads=["hn", "ident"], writes=[tPB[0]])
                c0 = dst_col0(i)
                op("dve", lambda e, pbT=pbT, c0=c0: e.tensor_tensor(
                    out=dstT[:, :, c0:c0 + 128], in0=pbT,
                    in1=gcol[:].unsqueeze(2).to_broadcast([128, 8, 128]), op=ALU.mult),
                   reads=[tPB[0], "gacol", "gfcol"], writes=[dst_tok(i)])

        precast = [(m, ex) for ex in range(16) for m in range(3)]
        pc_ctr = [0]

        def precast_some(n):
            if stages == 1:
                return
            for _ in range(n):
                if pc_ctr[0] >= len(precast):
                    return
                m, ex = precast[pc_ctr[0]]
                pc_ctr[0] += 1
                op("pool", lambda e, m=m, ex=ex: e.dma_start(
                    out=stg, in_=wexp_d[m][ex]),
                   writes=["stg"], dma="stg")
                op("sp", lambda e, m=m, ex=ex: e.dma_start(out=wscr_d[m][ex * 128:(ex + 1) * 128, :], in_=stg),
                   reads=["stg"], writes=["wscr"], dma="wscr")

        for ps_i in range(npass):
            tok0 = ps_i * PT
            for i in range(NT):
                op("sp", lambda e, i=i, tok0=tok0: e.dma_start(out=H[:, i, :], in_=x_d[tok0 + i * 128: tok0 + (i + 1) * 128, :]),
                   writes=[tH[i]], dma=f"H{i}")
            if ps_i == 0:
                op("pool", lambda e: e.dma_start(out=wpa, in_=wpa_d.rearrange("(c p) n -> p c n", p=128)),
                   writes=[tE[0][0]], dma="wg0")
                op("pool", lambda e: e.dma_start(out=wpb, in_=wpb_d.rearrange("(h d) n -> d h n", d=64)),
                   writes=[tE[1][0], tE[1][1]], dma="wg1")
                op("pool", lambda e: e.dma_start(out=wout, in_=wout_d.rearrange("(c p) n -> p c n", p=128)),
                   writes=[tE[0][1], tE[0][2]], dma="wu0")
            if ps_i > 0:
                op("dve", lambda e: e.tensor_copy(out=kT[:, 0:128], in_=kT[:, NT * 128:(NT + 1) * 128]),
                   reads=[], writes=["kT"])
                op("dve", lambda e: e.tensor_copy(out=vatt[:, 0, :], in_=vatt[:, NT, :]), reads=[], writes=["vatt"])

            ring_ctr = [0]

            def stream_block(col0):
                r = ring_ctr[0] % NRING
                ring_ctr[0] += 1
                op("pool", lambda e, r=r, col0=col0: e.dma_start(
                    out=ring[r], in_=win_d[:, col0:col0 + 128].rearrange("(c p) n -> p c n", p=128)),
                   writes=[tRing[r]], dma=f"ring{r}")
                if ring_ctr[0] % 4 == 0:
                    precast_some(1)
                return ring[r], tRing[r]

            for st in range(NST):
                tiles = [st * 4 + k for k in range(4)]
                norm_transpose(tiles, gacol, hnT, lambda i, st=st: (i - st * 4) * 128, lambda i: tk("hnT"))

                def proj_fm(col0, bank, evac):
                    wb_, wt_ = stream_block(col0)
                    for c in range(8):
                        op("pe", lambda e, c=c, wb_=wb_, bank=bank: e.matmul(
                            PB[bank][:], lhsT=wb_[:, c, :], rhs=hnT[:, c, :], start=(c == 0), stop=(c == 7)),
                           reads=[wt_, "hnT"], writes=[tPB[bank]])
                    evac(bank)

                for j in range(4):
                    proj_fm(U_OFF + j * 128, 1 + (j % 2),
                            lambda bank, j=j: op("act", lambda e: e.activation(out=uT[:, j, :], in_=PB[bank][:],
                                                                              func=AF.Gelu_apprx_tanh),
                                                 reads=[tPB[bank]], writes=["uT"]))
                for j in range(4):
                    proj_fm(Q_OFF + j * 128, 1 + (j % 2),
                            lambda bank, j=j: op("act", lambda e: e.activation(out=qT[:, j, :], in_=PB[bank][:],
                                                                              func=AF.Identity, scale=0.125),
                                                 reads=[tPB[bank]], writes=["qT"]))
                kc0 = (1 + st * 4) * 128
                proj_fm(K_OFF, 1,
                        lambda bank, kc0=kc0: op("dve", lambda e: e.tensor_copy(out=kT[:, kc0:kc0 + 512], in_=PB[bank][:]),
                                                 reads=[tPB[bank]], writes=["kT"]))

                for i in tiles:
                    il = i - st * 4
                    ts = il * 128
                    slot = 1 + i
                    gblk = ps_i * NT + i
                    for c in range(8):
                        op("pe", lambda e, c=c, ts=ts: e.matmul(PB[3][:], lhsT=hnT[:, c, ts:ts + 128], rhs=wv[:, c, :],
                                                                start=(c == 0), stop=(c == 7)),
                           reads=["hnT", "wv"], writes=[tPB[3]])
                    for c in range(8):
                        op("pe", lambda e, c=c, ts=ts: e.matmul(PB[4][:, 0:128], lhsT=hnT[:, c, ts:ts + 128],
                                                                rhs=wva[:, c, :], start=(c == 0), stop=(c == 7)),
                           reads=["hnT", "wva"], writes=[tPB[4]])
                    op("dve", lambda e, slot=slot: e.tensor_copy(out=vatt[:, slot, :], in_=PB[4][:, 0:128]),
                       reads=[tPB[4]], writes=["vatt"])
                    op("act", lambda e: e.activation(out=gv, in_=PB[3][:], func=AF.Gelu_apprx_tanh),
                       reads=[tPB[3]], writes=["gv"])
                    op("dve", lambda e: e.bn_stats(out=bnst, in_=gv), reads=["gv"], writes=["bnst"])
                    op("dve", lambda e: e.bn_aggr(out=mv, in_=bnst), reads=["bnst"], writes=["mv"])
                    op("dve", lambda e: e.tensor_scalar_add(out=rsv, in0=mv[:, 1:2], scalar1=EPS),
                       reads=["mv"], writes=["rsv"])
                    op("act", lambda e: e.sqrt(out=rsv, in_=rsv), reads=[], writes=["rsv"])
                    op("dve", lambda e: e.reciprocal(out=rsv, in_=rsv), reads=[], writes=["rsv"])
                    op("dve", lambda e: e.tensor_scalar(out=nmr, in0=mv[:, 0:1], scalar1=rsv, scalar2=-1.0,
                                                        op0=ALU.mult, op1=ALU.mult),
                       reads=["mv", "rsv"], writes=["nmr"])
                    op("act", lambda e: e.activation(out=vnh, in_=gv, func=AF.Identity, scale=rsv, bias=nmr),
                       reads=["gv", "rsv", "nmr"], writes=["vnh"])
                    pbs = PB[5][:].rearrange("p (j t) -> p j t", j=4)
                    for g in range(8):
                        lo = (g % 2) * 64
                        kw = {"tile_position": (0, 64)} if g % 2 else {}
                        op("pe", lambda e, g=g, lo=lo, kw=kw: e.matmul(
                            pbs[lo:lo + 64, g // 2, :], lhsT=vnh[:, g * 64:(g + 1) * 64], rhs=wstb[:, g, :],
                            start=True, stop=True, **kw),
                           reads=["vnh", "wstb"], writes=[tPB[5]])
                    op("dve", lambda e: e.tensor_tensor(out=gtmp, in0=pbs,
                                                        in1=gvcol[:].unsqueeze(2).to_broadcast([128, 4, 128]), op=ALU.mult),
                       reads=[tPB[5], "gvcol"], writes=["gtmp"])
                    op("dve", lambda e: e.tensor_tensor(out=gtmp, in0=gtmp, in1=bst[:], op=ALU.add),
                       reads=["bst"], writes=["gtmp"])
                    op("dve", lambda e, ts=ts: e.tensor_tensor(out=aT[:, :, ts:ts + 128], in0=gtmp,
                                                               in1=uT[:, :, ts:ts + 128], op=ALU.mult),
                       reads=["gtmp", "uT"], writes=["aT"])
                    for kh in range(2):
                        pr = slice(kh * 64, (kh + 1) * 64)
                        variants = [(0, slot)] + ([(1, slot - 1)] if gblk > 0 else [])
                        for (vi, ksl) in variants:
                            bank = 6 + vi
                            op("pe", lambda e, pr=pr, ksl=ksl, ts=ts, bank=bank: e.matmul(
                                PB[bank][:].rearrange("p (j t) -> p j t", j=4),
                                lhsT=kT[pr, ksl * 128:(ksl + 1) * 128], rhs=qT[pr, :, ts:ts + 128],
                                start=True, stop=True),
                               reads=["kT", "qT"], writes=[tPB[bank]])
                            op("dve", lambda e, vi=vi, kh=kh, bank=bank: e.tensor_tensor(
                                out=sc[vi], in0=PB[bank][:],
                                in1=BM[:, vi, kh, :, :].rearrange("p j t -> p (j t)"), op=ALU.add),
                               reads=[tPB[bank], "BM"], writes=["sc0"])
                            op("act", lambda e, vi=vi: e.activation(out=pT[vi], in_=sc[vi], func=AF.Exp),
                               reads=["sc0"], writes=[f"pT{vi}"])
                        nv = len(variants)
                        for k, (vi, ksl) in enumerate(variants):
                            op("pe", lambda e, vi=vi, ksl=ksl, kh=kh, k=k, nv=nv: e.matmul(
                                PB[1][0:64, :], lhsT=vatt[:, ksl, kh * 64:(kh + 1) * 64], rhs=pT[vi],
                                start=(k == 0), stop=(k == nv - 1)),
                               reads=["vatt", f"pT{vi}"], writes=[tPB[1]])
                        for k, (vi, ksl) in enumerate(variants):
                            op("pe", lambda e, vi=vi, k=k, nv=nv: e.matmul(
                                PB[2][0:64, :], lhsT=ones64[:], rhs=pT[vi], start=(k == 0), stop=(k == nv - 1)),
                               reads=["ones64", f"pT{vi}"], writes=[tPB[2]])
                        op("dve", lambda e, kh=kh: e.tensor_tensor(
                            out=den.rearrange("p (j t) -> p j t", j=4),
                            in0=PB[2][0:64, :].rearrange("p (j t) -> p j t", j=4),
                            in1=sinkexp[:, kh * 4:(kh + 1) * 4].unsqueeze(2).to_broadcast([64, 4, 128]), op=ALU.add),
                           reads=[tPB[2], "sinkexp"], writes=["den"])
                        op("dve", lambda e: e.reciprocal(out=den, in_=den), reads=[], writes=["den"])
                        op("dve", lambda e, kh=kh, ts=ts: e.tensor_tensor(
                            out=bT[:, kh * 4:(kh + 1) * 4, ts:ts + 128],
                            in0=PB[1][0:64, :].rearrange("p (j t) -> p j t", j=4),
                            in1=den.rearrange("p (j t) -> p j t", j=4), op=ALU.mult),
                           reads=[tPB[1], "den"], writes=["bT"])

                for j in range(8):
                    wga, tga = stream_block(GA_OFF + j * 128)
                    for c in range(8):
                        op("pe", lambda e, c=c, wga=wga: e.matmul(PB[3][:], lhsT=wga[:, c, :], rhs=hnT[:, c, :],
                                                                  start=(c == 0), stop=(c == 7)),
                           reads=[tga, "hnT"], writes=[tPB[3]])
                    wgb, tgb = stream_block(GB_OFF + j * 128)
                    for c in range(8):
                        op("pe", lambda e, c=c, wgb=wgb: e.matmul(PB[4][:], lhsT=wgb[:, c, :], rhs=hnT[:, c, :],
                                                                  start=(c == 0), stop=(c == 7)),
                           reads=[tgb, "hnT"], writes=[tPB[4]])
                    for c in range(4):
                        op("pe", lambda e, c=c, j=j: e.matmul(PB[5][:], lhsT=wpa[:, c, j * 128:(j + 1) * 128], rhs=aT[:, c, :],
                                                              start=(c == 0), stop=(c == 3)),
                           reads=[tE[0][0], "aT"], writes=[tPB[5]])
                    for h in range(8):
                        op("pe", lambda e, h=h, j=j: e.matmul(PB[6][:], lhsT=wpb[:, h, j * 128:(j + 1) * 128], rhs=bT[:, h, :],
                                                              start=(h == 0), stop=(h == 7)),
                           reads=[tE[1][0], tE[1][1], "bT"], writes=[tPB[6]])
                    op("act", lambda e: e.activation(out=sga, in_=PB[3][:], func=AF.Sigmoid),
                       reads=[tPB[3]], writes=["sga"])
                    op("act", lambda e: e.activation(out=sgb, in_=PB[4][:], func=AF.Sigmoid),
                       reads=[tPB[4]], writes=["sgb"])
                    op("dve", lambda e: e.tensor_tensor(out=sga, in0=PB[5][:], in1=sga, op=ALU.mult),
                       reads=[tPB[5]], writes=["sga"])
                    op("dve", lambda e: e.tensor_tensor(out=sgb, in0=PB[6][:], in1=sgb, op=ALU.mult),
                       reads=[tPB[6]], writes=["sgb"])
                    op("dve", lambda e, j=j: e.tensor_tensor(out=mT[:, j, :], in0=sga, in1=sgb, op=ALU.add),
                       reads=["sga", "sgb"], writes=["mT"])
                for i in tiles:
                    ts = (i - st * 4) * 128
                    for hf in range(2):
                        bank = 1 + hf
                        for j in range(8):
                            op("pe", lambda e, j=j, ts=ts, hf=hf, bank=bank: e.matmul(
                                PB[bank][:], lhsT=mT[:, j, ts:ts + 128], rhs=wout[:, j, hf * 512:(hf + 1) * 512],
                                start=(j == 0), stop=(j == 7)),
                               reads=["mT", tE[0][1], tE[0][2]], writes=[tPB[bank]])
                        op("dve", lambda e, i=i, hf=hf, bank=bank: e.tensor_tensor(
                            out=H[:, i, hf * 512:(hf + 1) * 512], in0=PB[bank][:], in1=H[:, i, hf * 512:(hf + 1) * 512],
                            op=ALU.add),
                           reads=[tPB[bank]], writes=[tH[i]])
                    op("sp", lambda e, i=i, tok0=tok0: e.dma_start(out=hS_d[tok0 + i * 128: tok0 + (i + 1) * 128, :],
                                                                   in_=H[:, i, :]),
                       reads=[tH[i]], writes=["hS"], dma=f"H{i}")

                def spill_hn2(i, tok0=tok0):
                    op("sp", lambda e, i=i, tok0=tok0: e.dma_start(out=hn2S_d[tok0 + i * 128: tok0 + (i + 1) * 128, :], in_=hn),
                       reads=["hn"], writes=["hn2S"], dma="hn2S")
                norm_transpose(tiles, gfcol, hn2T, lambda i, st=st: (i - st * 4) * 128, lambda i: tk("hn2T"),
                               after_hn=spill_hn2)
                for i in tiles:
                    gi = ps_i * NT + i
                    ts = (i - st * 4) * 128
                    for c in range(8):
                        op("pe", lambda e, c=c, ts=ts: e.matmul(PB[7][:, 0:20], lhsT=hn2T[:, c, ts:ts + 128], rhs=wr[:, c, :],
                                                                start=(c == 0), stop=(c == 7)),
                           reads=["hn2T", "wr"], writes=[tPB[7]])
                    op("dve", lambda e, gi=gi: e.tensor_tensor(out=lg[:, gi, :], in0=PB[7][:, 0:20], in1=rb[:], op=ALU.add),
                       reads=[tPB[7], "rb"], writes=["lg"])

            if debug and ps_i == 0:
                for i in range(NT):
                    out_ops.append(op("sp", lambda e, i=i: e.dma_start(out=dbg_d[i * 128:(i + 1) * 128, :], in_=H[:, i, :]),
                                      reads=[tH[i]], dma=f"H{i}"))
        precast_some(100)

        S.barrier("dve", lambda e: e.memset(dummy[:], 0.0))
        NTK = npass * NT

        cdma2 = op("sp", lambda e: e.dma_start(out=gfin, in_=gfin_d.partition_broadcast(128)), writes=["gfin"], dma="gfin")

        gl = lg[:, :, 0:4]
        el = lg[:, :, 4:20].rearrange("p t (g x) -> p t g x", g=4)
        bc3 = lambda a: a.unsqueeze(2).to_broadcast([128, NTT, 4])
        if npass < NPASS_FULL:
            op("dve", lambda e: e.memset(lg[:, NTK:, :], 0.0), writes=["lg"])
        dv = lambda fn, r, w: op("dve", fn, reads=r, writes=w)
        dv(lambda e: e.tensor_reduce(out=r_m, in_=gl, axis=AX.X, op=ALU.max), ["lg"], ["r_m"])
        dv(lambda e: e.tensor_tensor(out=r_og, in0=gl, in1=bc3(r_m), op=ALU.is_equal), ["lg", "r_m"], ["r_og"])
        dv(lambda e: e.tensor_tensor(out=r_eg, in0=gl, in1=bc3(r_m), op=ALU.subtract), ["lg", "r_m"], ["r_eg"])
        op("act", lambda e: e.activation(out=r_eg, in_=r_eg, func=AF.Exp), reads=[], writes=["r_eg"])
        dv(lambda e: e.tensor_reduce(out=r_gp, in_=r_eg, axis=AX.X, op=ALU.add), ["r_eg"], ["r_gp"])
        dv(lambda e: e.reciprocal(out=r_gp, in_=r_gp), [], ["r_gp"])
        dv(lambda e: e.tensor_tensor(out=r_sel, in0=el, in1=r_og.unsqueeze(3).to_broadcast([128, NTT, 4, 4]), op=ALU.mult),
           ["lg", "r_og"], ["r_sel"])
        dv(lambda e: e.tensor_reduce(out=r_es, in_=r_sel.rearrange("p t g x -> p t x g"), axis=AX.X, op=ALU.add),
           ["r_sel"], ["r_es"])
        dv(lambda e: e.tensor_reduce(out=r_m1, in_=r_es, axis=AX.X, op=ALU.max), ["r_es"], ["r_m1"])
        dv(lambda e: e.tensor_tensor(out=r_o1, in0=r_es, in1=bc3(r_m1), op=ALU.is_equal), ["r_es", "r_m1"], ["r_o1"])
        dv(lambda e: e.scalar_tensor_tensor(out=r_es2, in0=r_o1, scalar=-1e9, in1=r_es, op0=ALU.mult, op1=ALU.add),
           ["r_o1", "r_es"], ["r_es2"])
        dv(lambda e: e.tensor_reduce(out=r_m2, in_=r_es2, axis=AX.X, op=ALU.max), ["r_es2"], ["r_m2"])
        dv(lambda e: e.tensor_tensor(out=r_o2, in0=r_es2, in1=bc3(r_m2), op=ALU.is_equal), ["r_es2", "r_m2"], ["r_o2"])
        dv(lambda e: e.tensor_tensor(out=r_d, in0=r_m2, in1=r_m1, op=ALU.subtract), ["r_m1", "r_m2"], ["r_d"])
        op("act", lambda e: e.activation(out=r_d, in_=r_d, func=AF.Exp), reads=[], writes=["r_d"])
        dv(lambda e: e.tensor_scalar_add(out=r_w1, in0=r_d, scalar1=1.0), ["r_d"], ["r_w1"])
        dv(lambda e: e.reciprocal(out=r_w1, in_=r_w1), [], ["r_w1"])
        dv(lambda e: e.tensor_tensor(out=wB[:], in0=r_d, in1=r_w1, op=ALU.mult), ["r_d", "r_w1"], ["wB"])
        dv(lambda e: e.tensor_tensor(out=wA[:], in0=r_w1, in1=r_gp, op=ALU.mult), ["r_w1", "r_gp"], ["wA"])
        dv(lambda e: e.tensor_tensor(out=wB[:], in0=wB[:], in1=r_gp, op=ALU.mult), ["r_gp"], ["wB"])
        bg = lambda a: a.unsqueeze(3).to_broadcast([128, NTT, 4, 4])
        bx = lambda a: a.unsqueeze(2).to_broadcast([128, NTT, 4, 4])
        dv(lambda e: e.tensor_tensor(out=M1, in0=bg(r_og), in1=bx(r_o1), op=ALU.mult), ["r_og", "r_o1"], ["M1"])
        dv(lambda e: e.tensor_tensor(out=M2, in0=bg(r_og), in1=bx(r_o2), op=ALU.mult), ["r_og", "r_o2"], ["M2"])
        M1f = M1.rearrange("p t g x -> p t (g x)")
        M2f = M2.rearrange("p t g x -> p t (g x)")
        dv(lambda e: e.tensor_tensor(out=Mb.rearrange("p (t n) -> p t n", n=16), in0=M1f, in1=M2f, op=ALU.add),
           ["M1", "M2"], ["Mb"])
        if npass < NPASS_FULL:
            dv(lambda e: e.memset(Mb[:, NTK * 16:], 0.0), [], ["Mb"])
        op("pe", lambda e: e.matmul(PB[0][:], lhsT=utri[:], rhs=Mb, start=True, stop=True),
           reads=["utri", "Mb"], writes=[tPB[0]])
        op("pe", lambda e: e.matmul(PB[1][:], lhsT=ones128[:], rhs=Mb, start=True, stop=True),
           reads=["ones128", "Mb"], writes=[tPB[1]])
        dv(lambda e: e.tensor_copy(out=R1s.rearrange("p t n -> p (t n)"), in_=PB[0][:]), [tPB[0]], ["R1s"])
        dv(lambda e: e.tensor_copy(out=Tts.rearrange("p t n -> p (t n)"), in_=PB[1][:]), [tPB[1]], ["Tts"])
        dv(lambda e: e.tensor_copy(out=Ca, in_=Tts), ["Tts"], ["Ca"])
        cur, nxt, cn, nn = Ca, Cb, "Ca", "Cb"
        sh = 1
        while sh < NTT:
            dv(lambda e, cur=cur, nxt=nxt, sh=sh: e.tensor_copy(out=nxt[:, 0:sh, :], in_=cur[:, 0:sh, :]), [cn], [nn])
            dv(lambda e, cur=cur, nxt=nxt, sh=sh: e.tensor_tensor(out=nxt[:, sh:, :], in0=cur[:, sh:, :],
                                                                  in1=cur[:, 0:NTT - sh, :], op=ALU.add), [cn], [nn])
            cur, nxt, cn, nn = nxt, cur, nn, cn
            sh *= 2
        Cin, cin_n, Cex, cex_n = cur, cn, nxt, nn
        dv(lambda e: e.tensor_tensor(out=Cex, in0=Cin, in1=Tts, op=ALU.subtract), [cin_n, "Tts"], [cex_n])
        ntot = Cin[:, NTT - 1, :]
        thr = cst[:, 0:8]
        tri = cst[:, 8:264].rearrange("p (a b) -> p a b", a=16)
        sio = cst[:, 264:296]
        cmp8 = r_cmp.rearrange("p a b -> p (a b)")[:, 0:128].rearrange("p (a b) -> p a b", b=8)
        dv(lambda e: e.tensor_tensor(out=cmp8, in0=ntot.unsqueeze(2).to_broadcast([128, 16, 8]),
                                     in1=thr.unsqueeze(1).to_broadcast([128, 16, 8]), op=ALU.is_gt),
           [cin_n, "cst"], ["r_cmp"])
        dv(lambda e: e.tensor_reduce(out=r_nb, in_=cmp8, axis=AX.X, op=ALU.add), ["r_cmp"], ["r_nb"])
        dv(lambda e: e.tensor_tensor(out=r_cmp, in0=r_nb.unsqueeze(1).to_broadcast([128, 16, 16]), in1=tri, op=ALU.mult),
           ["r_nb", "cst"], ["r_cmp"])
        dv(lambda e: e.tensor_reduce(out=r_ob, in_=r_cmp, axis=AX.X, op=ALU.add), ["r_cmp"], ["r_ob"])
        dv(lambda e: e.tensor_tensor(out=r_oe, in0=r_ob, in1=r_nb, op=ALU.add), ["r_ob", "r_nb"], ["r_oe"])
        dv(lambda e: e.tensor_tensor(out=R1s, in0=R1s, in1=Cex, op=ALU.add), [cex_n], ["R1s"])
        dv(lambda e: e.scalar_tensor_tensor(out=R1s, in0=r_ob.unsqueeze(1).to_broadcast([128, NTT, 16]), scalar=512.0,
                                            in1=R1s, op0=ALU.mult, op1=ALU.add), ["r_ob"], ["R1s"])
        for (Mf, mn, pos_i, pn) in ((M1f, "M1", posA, "posA"), (M2f, "M2", posB, "posB")):
            dv(lambda e, Mf=Mf: e.tensor_tensor(out=Mf, in0=Mf, in1=R1s, op=ALU.mult), ["R1s"], [mn])
            dv(lambda e, Mf=Mf: e.tensor_reduce(out=r_pf, in_=Mf, axis=AX.X, op=ALU.add), [mn], ["r_pf"])
            dv(lambda e: e.tensor_scalar(out=r_pf, in0=r_pf, scalar1=0.0, scalar2=float(NSLOT * 512 - 1),
                                         op0=ALU.max, op1=ALU.min), [], ["r_pf"])
            dv(lambda e, pos_i=pos_i: e.tensor_copy(out=pos_i[:], in_=r_pf), ["r_pf"], [pn])
        dv(lambda e: e.tensor_tensor(out=r_cmp2, in0=sio.unsqueeze(2).to_broadcast([128, NSLOT, 16]),
                                     in1=r_oe.unsqueeze(1).to_broadcast([128, NSLOT, 16]), op=ALU.is_ge),
           ["cst", "r_oe"], ["r_cmp2"])
        dv(lambda e: e.tensor_reduce(out=r_eid, in_=r_cmp2, axis=AX.X, op=ALU.add), ["r_cmp2"], ["r_eid"])
        dv(lambda e: e.tensor_scalar(out=r_eid, in0=r_eid, scalar1=15.0, scalar2=128.0, op0=ALU.min, op1=ALU.mult),
           [], ["r_eid"])
        dv(lambda e: e.tensor_tensor(out=r_eid, in0=r_eid, in1=iop[:].to_broadcast([128, NSLOT]), op=ALU.add),
           ["iop"], ["r_eid"])
        dv(lambda e: e.tensor_scalar(out=r_eid, in0=r_eid, scalar1=0.0, scalar2=2047.0, op0=ALU.max, op1=ALU.min),
           [], ["r_eid"])
        dv(lambda e: e.tensor_copy(out=idxW[:], in_=r_eid), ["r_eid"], ["idxW"])
        if debug:
            dr = sb("dbgr_s", [128, 6, NTT], F32)
            dv(lambda e: e.tensor_copy(out=dr[:, 0, :], in_=posA[:]), ["posA"], ["dr"])
            dv(lambda e: e.tensor_copy(out=dr[:, 1, :], in_=posB[:]), ["posB"], ["dr"])
            dv(lambda e: e.tensor_copy(out=dr[:, 2, :], in_=wA[:]), ["wA"], ["dr"])
            dv(lambda e: e.tensor_copy(out=dr[:, 3, :], in_=wB[:]), ["wB"], ["dr"])
            dv(lambda e: e.tensor_copy(out=dr[:, 4, :], in_=idxW[:]), ["idxW"], ["dr"])
            dv(lambda e: e.tensor_copy(out=dr[:, 5, 0:16], in_=ntot), [cin_n], ["dr"])
            out_ops.append(op("sp", lambda e: e.dma_start(out=dbgr_d, in_=dr[:]), reads=["dr"], dma="dbgr"))

        for gi in range(NTK if stages >= 3 else 0):
            xb = xin[gi % NXIN]
            xn = f"xin{gi % NXIN}"
            op("sp", lambda e, gi=gi, xb=xb: e.dma_start(out=xb, in_=hn2S_d[gi * 128:(gi + 1) * 128, :]),
               reads=["hn2S"], writes=[xn], dma=xn)
            for (pos_i, pn) in ((posA, "posA"), (posB, "posB")):
                op("pool", lambda e, gi=gi, xb=xb, pos_i=pos_i: e.indirect_dma_start(
                    out=Xs_d, out_offset=bass.IndirectOffsetOnAxis(pos_i[:, gi:gi + 1], 0), in_=xb, in_offset=None),
                   reads=[xn, pn], writes=[f"Xs{gi % NXIN}"], dma=f"scat{gi % NXIN}")

        NS3 = NSLOT if stages >= 3 else 0

        def slot_loads(s):
            b = s % NEB
            for m in range(3):
                op("pool", lambda e, s=s, b=b, m=m: e.indirect_dma_start(
                    out=wflat[b][m], out_offset=None, in_=wscr_d[m],
                    in_offset=bass.IndirectOffsetOnAxis(idxW[:, s:s + 1], 0)),
                   reads=["idxW", "wscr"], writes=[tE[b][m]], dma=f"w{'gud'[m]}{b}")
            xb = xs[s % 2]
            xn = f"xs{s % 2}"
            op("sp", lambda e, s=s, xb=xb: e.dma_start(out=xb, in_=Xs_d[s * 512:(s + 1) * 512, :].rearrange("(j p) d -> p j d", p=128)),
               reads=[f"Xs{q}" for q in range(NXIN)], writes=[xn], dma=xn)

        def slot_transposes(s):
            xb = xs[s % 2]
            xn = f"xs{s % 2}"
            xt = xT[s % 2]
            xtn = f"xT{s % 2}"
            for j in range(4):
                bank = j % 2
                pbT = PB[bank][:].bitcast(BF16).rearrange("p (c t) -> p c t", c=8)
                for c in range(8):
                    op("pe", lambda e, c=c, j=j, pbT=pbT, xb=xb: e.transpose(out=pbT[:, c, :], in_=xb[:, j, c * 128:(c + 1) * 128],
                                                                            identity=ident[:]),
                       reads=[xn, "ident"], writes=[tPB[bank]])
                op("dve", lambda e, j=j, pbT=pbT, xt=xt: e.tensor_tensor(
                    out=xt[:, :, j * 128:(j + 1) * 128], in0=pbT,
                    in1=gfcol[:].unsqueeze(2).to_broadcast([128, 8, 128]), op=ALU.mult),
                   reads=[tPB[bank], "gfcol"], writes=[xtn])

        def slot_gateup(s):
            b = s % NEB
            xt = xT[s % 2]
            xtn = f"xT{s % 2}"
            hb_ = s % 2
            for j in range(4):
                bg_, bu_ = 2 + (j % 2) * 2, 3 + (j % 2) * 2
                for c in range(8):
                    op("pe", lambda e, c=c, j=j, b=b, bg_=bg_, xt=xt: e.matmul(
                        PB[bg_][:], lhsT=wgs[b][:, c, j * 128:(j + 1) * 128], rhs=xt[:, c, :],
                        start=(c == 0), stop=(c == 7)),
                       reads=[tE[b][0], xtn], writes=[tPB[bg_]])
                for c in range(8):
                    op("pe", lambda e, c=c, j=j, b=b, bu_=bu_, xt=xt: e.matmul(
                        PB[bu_][:], lhsT=wus[b][:, c, j * 128:(j + 1) * 128], rhs=xt[:, c, :],
                        start=(c == 0), stop=(c == 7)),
                       reads=[tE[b][1], xtn], writes=[tPB[bu_]])
                op("act", lambda e, j=j, bg_=bg_: e.activation(out=sgs[j % 2], in_=PB[bg_][:], func=AF.Silu),
                   reads=[tPB[bg_]], writes=[f"sgs{j % 2}"])
                op("dve", lambda e, j=j, bu_=bu_, hb_=hb_: e.tensor_tensor(out=hid[hb_][:, j, :], in0=PB[bu_][:],
                                                                         in1=sgs[j % 2], op=ALU.mult),
                   reads=[tPB[bu_], f"sgs{j % 2}"], writes=[f"hid{hb_}"])

        def slot_down(s):
            b = s % NEB
            hb_ = s % 2
            for tl in range(4):
                yi = (s * 4 + tl) % NYS
                yb = ys[yi]
                yn = f"ys{yi}"
                for hf in range(2):
                    bank = 6 + hf
                    for j in range(4):
                        op("pe", lambda e, j=j, tl=tl, hf=hf, b=b, hb_=hb_, bank=bank: e.matmul(
                            PB[bank][:], lhsT=hid[hb_][:, j, tl * 128:(tl + 1) * 128],
                            rhs=wds[b][:, j, hf * 512:(hf + 1) * 512], start=(j == 0), stop=(j == 3)),
                           reads=[f"hid{hb_}", tE[b][2]], writes=[tPB[bank]])
                    op("act", lambda e, hf=hf, bank=bank, yb=yb: e.copy(out=yb[:, hf * 512:(hf + 1) * 512], in_=PB[bank][:]),
                       reads=[tPB[bank]], writes=[yn])
                op("sp", lambda e, s=s, tl=tl, yb=yb: e.dma_start(out=Ys_d[s * 512 + tl * 128: s * 512 + (tl + 1) * 128, :], in_=yb),
                   reads=[yn], writes=[f"Ys{yi}"], dma=f"ysd{yi}")

        if NS3:
            slot_loads(0)
            slot_transposes(0)
            slot_loads(1)
        for s in range(NS3):
            slot_gateup(s)
            if s + 1 < NS3:
                slot_transposes(s + 1)
            slot_down(s)
            if s + 2 < NS3:
                slot_loads(s + 2)

        for gi in range(NTK if stages >= 4 else 0):
            k2 = gi % 2
            op("sp", lambda e, gi=gi, k2=k2: e.dma_start(out=hb[k2], in_=hS_d[gi * 128:(gi + 1) * 128, :]),
               reads=["hS"], writes=[f"hb{k2}"], dma=f"hb{k2}")
            for (Yb, ynm, pos_i, pn) in ((YA, "YA", posA, "posA"), (YB, "YB", posB, "posB")):
                op("pool", lambda e, gi=gi, k2=k2, Yb=Yb, pos_i=pos_i: e.indirect_dma_start(
                    out=Yb[k2], out_offset=None, in_=Ys_d, in_offset=bass.IndirectOffsetOnAxis(pos_i[:, gi:gi + 1], 0)),
                   reads=[f"Ys{q}" for q in range(NYS)] + [pn], writes=[f"{ynm}{k2}"], dma=f"{ynm}{k2}")
            op("dve", lambda e, gi=gi, k2=k2: e.scalar_tensor_tensor(out=hb[k2], in0=YA[k2], scalar=wA[:, gi:gi + 1], in1=hb[k2],
                                                                     op0=ALU.mult, op1=ALU.add),
               reads=[f"YA{k2}", "wA"], writes=[f"hb{k2}"])
            op("dve", lambda e, gi=gi, k2=k2: e.scalar_tensor_tensor(out=hb[k2], in0=YB[k2], scalar=wB[:, gi:gi + 1], in1=hb[k2],
                                                                     op0=ALU.mult, op1=ALU.add),
               reads=[f"YB{k2}", "wB"], writes=[f"hb{k2}"])
            op("act", lambda e, k2=k2: e.activation(out=hn4, in_=hb[k2], func=AF.Square, accum_out=ss4),
               reads=[f"hb{k2}"], writes=["hn4", "ss4"])
            op("dve", lambda e: e.tensor_scalar(out=rs4, in0=ss4, scalar1=1.0 / D, scalar2=EPS, op0=ALU.mult, op1=ALU.add),
               reads=["ss4"], writes=["rs4"])
            op("act", lambda e: e.sqrt(out=rs4, in_=rs4), reads=[], writes=["rs4"])
            op("dve", lambda e: e.reciprocal(out=rs4, in_=rs4), reads=[], writes=["rs4"])
            op("dve", lambda e, k2=k2: e.scalar_tensor_tensor(out=hb[k2], in0=hb[k2], scalar=rs4, in1=gfin,
                                                              op0=ALU.mult, op1=ALU.mult),
               reads=["rs4", "gfin"], writes=[f"hb{k2}"])
            out_ops.append(op("sp", lambda e, gi=gi, k2=k2: e.dma_start(out=y_d[gi * 128:(gi + 1) * 128, :], in_=hb[k2]),
                              reads=[f"hb{k2}"], dma=f"hb{k2}"))

        S.emit(nc, final_wait_ops=out_ops)
    return nc


def _t5_bucket(dist):
    dist = np.asarray(dist, dtype=np.int64)
    nf = np.maximum(dist, 1).astype(np.float32)
    large = 16 + (np.log(nf / np.float32(16)) / np.float32(math.log(128 / 16)) * np.float32(16)).astype(np.int32)
    large = np.minimum(large, 31)
    return np.where(dist < 16, dist, large)


def prep_shared(inp):
    f = lambda a: np.ascontiguousarray(np.asarray(a, dtype=np.float32))
    w_in = f(inp["w_in"])[0]
    perm = np.arange(3840)
    qcols = []
    for j in range(4):
        qcols += list(range(1024 + j * 64, 1024 + (j + 1) * 64))
        qcols += list(range(1024 + (4 + j) * 64, 1024 + (5 + j) * 64))
    perm[1024:1536] = np.array(qcols)
    w_in = np.ascontiguousarray(w_in[:, perm])
    wr = np.concatenate([f(inp["router_group_w"])[0]] + [f(inp["router_expert_w"])[0, g] for g in range(4)], axis=1)
    rb = np.concatenate([f(inp["router_group_b"])[0], f(inp["router_expert_b"])[0].reshape(-1)])[None, :]
    col = lambda v, c: np.ascontiguousarray(f(v).reshape(c, 128).T)
    ws = f(inp["gm_w_spatial"])[0]
    wst = np.ascontiguousarray(ws.transpose(2, 0, 1))
    s_i = np.arange(128)[:, None, None]
    t_i = np.arange(128)[None, None, :]
    wmask = np.broadcast_to((s_i <= t_i), (128, 8, 128)).astype(np.float32)
    bs = f(inp["gm_b_spatial"])[0]
    bst = np.zeros((128, 4, 128), np.float32)
    for g in range(8):
        bst[(g % 2) * 64:(g % 2) * 64 + 64, g // 2, :] = bs[g][None, :]
    rel = f(inp["rel_bias"])
    s_ = np.arange(128)[:, None]
    q_ = np.arange(128)[None, :]
    d_own = q_ - s_
    d_prev = q_ + 128 - s_
    bias = np.zeros((128, 2, 2, 4, 128), np.float32)
    maskc = np.zeros((128, 2, 2, 4, 128), np.float32)
    b_own = _t5_bucket(np.clip(d_own, 0, 127))
    b_prev = _t5_bucket(np.clip(d_prev, 0, 127))
    for kh in range(2):
        for h4 in range(4):
            h = kh * 4 + h4
            bias[:, 0, kh, h4, :] = rel[b_own, h]
            bias[:, 1, kh, h4, :] = rel[b_prev, h]
            maskc[:, 0, kh, h4, :] = np.where(d_own >= 0, 0.0, NEG)
            maskc[:, 1, kh, h4, :] = np.where(d_prev < 128, 0.0, NEG)
    def pmaj(w, c):
        e_, r_, n_ = w.shape
        return np.ascontiguousarray(w.reshape(e_, c, 128, n_).transpose(0, 2, 1, 3).reshape(e_, 128, c * n_))

    return {
        "w_in": w_in,
        "wpa": f(inp["w_proj_a"])[0], "wpb": f(inp["w_proj_b"])[0], "wout": f(inp["w_out"])[0],
        "wgp": pmaj(f(inp["expert_w_gate"])[0], 8), "wup": pmaj(f(inp["expert_w_up"])[0], 8),
        "wdp": pmaj(f(inp["expert_w_down"])[0], 4),
        "utri": np.triu(np.ones((128, 128), np.float32), 1),
        "cst": np.concatenate([np.arange(8, dtype=np.float32) * 512.0,
                               np.tril(np.ones((16, 16), np.float32), -1).reshape(-1),
                               np.arange(32, dtype=np.float32)])[None, :],
        "iop": np.arange(128, dtype=np.float32)[:, None],
        "wr": np.ascontiguousarray(wr), "rb": np.ascontiguousarray(rb),
        "gacol": col(inp["attn_norm_g"], 8), "gfcol": col(inp["ffn_norm_g"], 8), "gvcol": col(inp["gm_v_norm_g"], 4),
        "gfin": f(inp["final_norm_g"]).reshape(1, D),
        "wst": wst, "wmask": wmask, "bst": bst,
        "sinks": f(inp["attn_sinks"]).reshape(1, 8),
        "bias": bias, "maskc": maskc,
        "idn": np.eye(128, dtype=np.float32),
    }


def kernel(**inputs):
    shared = prep_shared(inputs)
    x = np.asarray(inputs["x"], dtype=np.float32)
    nc = build()
    in_maps = []
    for c in range(NCORES):
        m = dict(shared)
        m["x"] = np.ascontiguousarray(x[c])
        in_maps.append(m)
    res = run_bass_kernel_spmd(nc, in_maps, core_ids=list(range(NCORES)))
    return np.stack([np.asarray(r["y"], dtype=np.float32) for r in res.results], axis=0)
```

```python
import math
from contextlib import ExitStack

import numpy as np
import concourse.bass as bass
import concourse.mybir as mybir
from concourse.bass_utils import run_bass_kernel_spmd

F32 = mybir.dt.float32
BF16 = mybir.dt.bfloat16
AF = mybir.ActivationFunctionType
ALU = mybir.AluOpType
AX = mybir.AxisListType

SEQ = 4096
D = 1024
NCORES = 8
PT = 1024
NT = PT // 128
NST = PT // 512
NPASS_FULL = SEQ // PT
EPS = 1e-6
NEG = -30000.0
NSLOT = 32
NTT = SEQ // 128
I32 = mybir.dt.int32

U_OFF, V_OFF, Q_OFF, K_OFF, VA_OFF, GA_OFF, GB_OFF = 0, 512, 1024, 1536, 1664, 1792, 2816

ENG = ("pe", "act", "dve", "pool", "sp")


class Tok:
    __slots__ = ("name", "last_w", "readers")

    def __init__(self, name):
        self.name = name
        self.last_w = None
        self.readers = []


class Op:
    __slots__ = ("eng", "fn", "deps", "sig", "dma_sem", "dma_cnt", "seq")

    def __init__(self, eng, fn):
        self.eng = eng
        self.fn = fn
        self.deps = set()
        self.sig = False
        self.dma_sem = None
        self.dma_cnt = 0
        self.seq = 0


class Sched:
    def __init__(self):
        self.ops = []
        self.per = {e: [] for e in ENG}
        self.dma_counts = {}
        self.total_keys = set()
        self.epoch_op = None
        self.last_dma = {}

    def op(self, eng, fn, reads=(), writes=(), dma=None):
        o = Op(eng, fn)
        for t in list(reads) + list(writes):
            if t.last_w is not None:
                o.deps.add(t.last_w)
        for t in writes:
            for r in t.readers:
                o.deps.add(r)
        if self.epoch_op is not None:
            o.deps.add(self.epoch_op)
        o.deps.discard(o)
        for t in reads:
            t.readers.append(o)
        for t in writes:
            t.last_w = o
            t.readers = []
        if dma is not None:
            o.dma_sem = dma
            self.dma_counts[dma] = self.dma_counts.get(dma, 0) + 1
            o.dma_cnt = self.dma_counts[dma]
            self.last_dma[dma] = o
        self.ops.append(o)
        self.per[eng].append(o)
        return o

    def barrier(self, eng, fn):
        o = Op(eng, fn)
        for e in ENG:
            seen_c = False
            for p in reversed(self.per[e]):
                if p.dma_sem is None:
                    o.deps.add(p)
                    break
        for k, p in self.last_dma.items():
            o.deps.add(p)
        if self.epoch_op is not None:
            o.deps.add(self.epoch_op)
        self.ops.append(o)
        self.per[eng].append(o)
        self.epoch_op = o
        return o

    def emit(self, nc, final_wait_ops=()):
        def skip(d, o):
            return d.dma_sem is None and o.dma_sem is None and d.eng == "pe" and o.eng == "pe"

        for o in self.ops:
            for d in o.deps:
                if d.dma_sem is None and not skip(d, o):
                    d.sig = True
        for o in final_wait_ops:
            if o.dma_sem is None:
                o.sig = True
        for e in ENG:
            c = 0
            for o in self.per[e]:
                if o.dma_sem is None and o.sig:
                    c += 1
                    o.seq = c
        with ExitStack() as es:
            esem = {e: es.enter_context(nc.semaphore(f"s_{e}")) for e in ENG}
            dsem = {k: es.enter_context(nc.semaphore(f"d_{k}")) for k in self.dma_counts}
            block = es.enter_context(nc.Block())

            def dval(d):
                if d.dma_sem in self.total_keys:
                    return 16 * self.dma_counts[d.dma_sem]
                return 16 * d.dma_cnt

            def need(o):
                w = {}
                for d in o.deps:
                    if d.dma_sem is not None:
                        key, val = ("d", d.dma_sem), dval(d)
                    else:
                        if skip(d, o):
                            continue
                        key, val = ("e", d.eng), d.seq
                    if w.get(key, 0) < val:
                        w[key] = val
                return w

            def run(ename):
                def body(eng):
                    waited = {}
                    for o in self.per[ename]:
                        for key, val in need(o).items():
                            if waited.get(key, 0) >= val:
                                continue
                            waited[key] = val
                            sem = dsem[key[1]] if key[0] == "d" else esem[key[1]]
                            eng.wait_ge(sem, val)
                        ins = o.fn(eng)
                        if o.dma_sem is not None:
                            ins.then_inc(dsem[o.dma_sem], 16)
                        elif o.sig:
                            ins.then_inc(esem[ename], 1)
                    if ename == "sp":
                        for o in final_wait_ops:
                            if o.dma_sem is not None:
                                eng.wait_ge(dsem[o.dma_sem], dval(o))
                            else:
                                eng.wait_ge(esem[o.eng], o.seq)
                return body

            block.tensor(run("pe"))
            block.scalar(run("act"))
            block.vector(run("dve"))
            block.gpsimd(run("pool"))
            block.sync(run("sp"))


class Arena:
    def __init__(self, nc, es, name, nbytes):
        self.t = es.enter_context(nc.sbuf_tensor(name, [128, nbytes // 2], BF16))
        self.cap = nbytes
        self.off = 0

    def alloc(self, shape, dt):
        n = 1
        for s_ in shape[1:]:
            n *= s_
        esz = 2 if dt == BF16 else 4
        o = self.off
        self.off += (n * esz + 63) // 64 * 64
        assert self.off <= self.cap, (self.off, self.cap)
        ap = self.t[0:shape[0], o // 2:(o + n * esz) // 2]
        if dt != BF16:
            ap = ap.bitcast(dt)
        if len(shape) > 2:
            names = " ".join(f"d{k}" for k in range(len(shape) - 1))
            ap = ap.rearrange(f"p ({names}) -> p {names}", **{f"d{k}": shape[k + 1] for k in range(len(shape) - 1)})
        return ap


def build(npass=NPASS_FULL, debug=False, stages=4):
    nc = bass.Bass("TRN2", target_bir_lowering=False)

    def din(name, shape):
        return nc.dram_tensor(name, list(shape), F32, kind="ExternalInput").ap()

    x_d = din("x", [SEQ, D])
    win_d = din("w_in", [D, 3840])
    wpa_d = din("wpa", [512, D])
    wpb_d = din("wpb", [512, D])
    wout_d = din("wout", [D, D])
    wexp_d = [din("wgp", [16, 128, 4096]), din("wup", [16, 128, 4096]), din("wdp", [16, 128, 4096])]
    wr_d = din("wr", [D, 20])
    rb_d = din("rb", [1, 20])
    gacol_d = din("gacol", [128, 8])
    gfcol_d = din("gfcol", [128, 8])
    gvcol_d = din("gvcol", [128, 4])
    gfin_d = din("gfin", [1, D])
    wst_d = din("wst", [128, 8, 128])
    wmask_d = din("wmask", [128, 8, 128])
    bst_d = din("bst", [128, 4, 128])
    sinks_d = din("sinks", [128, 4])
    bias_d = din("bias", [128, 2, 2, 4, 128])
    mask_d = din("maskc", [128, 2, 2, 4, 128])
    idn_d = din("idn", [128, 128])
    utri_d = din("utri", [128, 128])
    cst_d = din("cst", [1, 8 + 256 + 32])
    iop_d = din("iop", [128, 1])
    y_d = nc.dram_tensor("y", [SEQ, D], F32, kind="ExternalOutput").ap()
    hS_d = nc.dram_tensor("hS", [SEQ, D], F32, kind="Internal").ap()
    hn2S_d = nc.dram_tensor("hn2S", [SEQ, D], BF16, kind="Internal").ap()
    Xs_d = nc.dram_tensor("Xs", [NSLOT * 512, D], BF16, kind="Internal").ap()
    Ys_d = nc.dram_tensor("Ys", [NSLOT * 512, D], F32, kind="Internal").ap()
    wscr_d = [nc.dram_tensor(f"wscr{m}", [16 * 128, 4096], BF16, kind="Internal").ap() for m in range(3)]
    if debug:
        dbg_d = nc.dram_tensor("dbg", [PT, D], F32, kind="ExternalOutput").ap()
        dbgr_d = nc.dram_tensor("dbgr", [128, 6, NTT], F32, kind="ExternalOutput").ap()

    S = Sched()
    S.total_keys.update(["const"])

    with ExitStack() as es:
        def sb(name, shape, dt):
            return es.enter_context(nc.sbuf_tensor(name, list(shape), dt))

        ident = sb("ident", [128, 128], BF16)
        gacol = sb("gacol_s", [128, 8], F32)
        gfcol = sb("gfcol_s", [128, 8], F32)
        gvcol = sb("gvcol_s", [128, 4], F32)
        wstb = sb("wstb", [128, 8, 128], BF16)
        bst = sb("bst_s", [128, 4, 128], F32)
        sinkexp = sb("sinkexp", [128, 4], F32)
        BM = sb("BM", [128, 2, 2, 4, 128], F32)
        ones64 = sb("ones64", [128, 64], BF16)
        ones128 = sb("ones128", [128, 128], BF16)
        utri = sb("utri_s", [128, 128], BF16)
        cst = sb("cst_s", [128, 8 + 256 + 32], F32)
        iop = sb("iop_s", [128, 1], F32)
        wr = sb("wr_s", [128, 8, 20], BF16)
        rb = sb("rb_s", [128, 20], F32)
        lg = sb("lg_all", [128, NTT, 20], F32)
        posA = sb("posA", [128, NTT], I32)
        posB = sb("posB", [128, NTT], I32)
        wA = sb("wA", [128, NTT], F32)
        wB = sb("wB", [128, NTT], F32)
        idxW = sb("idxW", [128, NSLOT], I32)
        dummy = sb("bar_dummy", [128, 1], F32)
        NEB = 2
        wreg = sb("wreg", [128, NEB * 12288], BF16)
        wgs = [wreg[:, b * 12288:b * 12288 + 4096].rearrange("p (c n) -> p c n", c=8) for b in range(NEB)]
        wus = [wreg[:, b * 12288 + 4096:b * 12288 + 8192].rearrange("p (c n) -> p c n", c=8) for b in range(NEB)]
        wds = [wreg[:, b * 12288 + 8192:b * 12288 + 12288].rearrange("p (c n) -> p c n", c=4) for b in range(NEB)]
        wflat = [[wreg[:, b * 12288 + m * 4096:b * 12288 + (m + 1) * 4096] for m in range(3)] for b in range(NEB)]
        wpa = wreg[:, 0:4096].rearrange("p (c n) -> p c n", c=4)
        wout = wreg[:, 4096:12288].rearrange("p (c n) -> p c n", c=8)
        wpb = wreg[:, 12288:16384].rearrange("p (c n) -> p c n", c=4)
        stg = wreg[:, 20480:24576]
        AR = Arena(nc, es, "arena", 124 * 1024)
        H = AR.alloc([128, NT, D], F32)
        hn2T = AR.alloc([128, 8, 512], BF16)
        wv = AR.alloc([128, 8, 512], BF16)
        wva = AR.alloc([128, 8, 128], BF16)
        NRING = 6
        ring = [AR.alloc([128, 8, 128], BF16) for _ in range(NRING)]
        hn2 = [AR.alloc([128, D], BF16) for _ in range(2)]
        hn = hn2[0]
        hnT = AR.alloc([128, 8, 512], BF16)
        wst = hnT[:, 0:2, :].rearrange("p a (b t) -> p (a b) t", b=4)
        wmask = hnT[:, 2:4, :].rearrange("p a (b t) -> p (a b) t", b=4)
        ss = AR.alloc([128, 8], F32)
        rstd = AR.alloc([128, 8], F32)
        uT = AR.alloc([128, 4, 512], BF16)
        qT = AR.alloc([128, 4, 512], BF16)
        kT = AR.alloc([128, (NT + 1) * 128], BF16)
        vatt = AR.alloc([128, NT + 1, 128], BF16)
        gv2 = [AR.alloc([128, 512], F32) for _ in range(2)]
        vnh2 = [AR.alloc([128, 512], BF16) for _ in range(2)]
        bnst2 = [AR.alloc([128, 6], F32) for _ in range(2)]
        mv2 = [AR.alloc([128, 2], F32) for _ in range(2)]
        rsv2 = [AR.alloc([128, 1], F32) for _ in range(2)]
        nmr2 = [AR.alloc([128, 1], F32) for _ in range(2)]
        gtmp = AR.alloc([128, 4, 128], F32)
        pT = AR.alloc([128, 2048], BF16)
        den = AR.alloc([128, 512], F32)
        aT = AR.alloc([128, 4, 512], BF16)
        bT = AR.alloc([128, 4, 512], BF16)
        sga = AR.alloc([128, 512], F32)
        sgb = AR.alloc([128, 512], F32)
        mT = AR.alloc([128, 8, 512], BF16)
        maskc = mT[:, 0:4, :].rearrange("p a (b c t) -> p (a b c) t", b=2, c=2).rearrange("p (v k h) t -> p v k h t", v=2, k=2)
        side1_end = AR.off
        AR.off = 0
        NB_ = NTT
        r_m = AR.alloc([128, NB_], F32)
        r_og = AR.alloc([128, NB_, 4], F32)
        r_eg = AR.alloc([128, NB_, 4], F32)
        r_gp = AR.alloc([128, NB_], F32)
        r_sel = AR.alloc([128, NB_, 4, 4], F32)
        r_es = AR.alloc([128, NB_, 4], F32)
        r_m1 = AR.alloc([128, NB_], F32)
        r_o1 = AR.alloc([128, NB_, 4], F32)
        r_es2 = AR.alloc([128, NB_, 4], F32)
        r_m2 = AR.alloc([128, NB_], F32)
        r_o2 = AR.alloc([128, NB_, 4], F32)
        r_d = AR.alloc([128, NB_], F32)
        r_w1 = AR.alloc([128, NB_], F32)
        M1 = AR.alloc([128, NB_, 4, 4], F32)
        M2 = AR.alloc([128, NB_, 4, 4], F32)
        Mb = AR.alloc([128, NB_ * 16], BF16)
        R1s = AR.alloc([128, NB_, 16], F32)
        Ca = AR.alloc([128, NB_, 16], F32)
        Cb = AR.alloc([128, NB_, 16], F32)
        Tts = AR.alloc([128, NB_, 16], F32)
        r_cmp = AR.alloc([128, 16, 16], F32)
        r_cmp2 = AR.alloc([128, NSLOT, 16], F32)
        r_nb = AR.alloc([128, 16], F32)
        r_ob = AR.alloc([128, 16], F32)
        r_oe = AR.alloc([128, 16], F32)
        r_pf = AR.alloc([128, NB_], F32)
        r_eid = AR.alloc([128, NSLOT], F32)
        gfin = AR.alloc([128, D], F32)
        NXIN = 4
        xin = [AR.alloc([128, D], BF16) for _ in range(NXIN)]
        xs = [AR.alloc([128, 4, D], BF16) for _ in range(2)]
        xT = [AR.alloc([128, 8, 512], BF16) for _ in range(2)]
        sgs = [AR.alloc([128, 512], F32) for _ in range(2)]
        hid = [AR.alloc([128, 4, 512], BF16) for _ in range(2)]
        NYS = 3
        ys = [AR.alloc([128, D], F32) for _ in range(NYS)]
        YA = [AR.alloc([128, D], F32) for _ in range(2)]
        YB = [AR.alloc([128, D], F32) for _ in range(2)]
        hb = [AR.alloc([128, D], F32) for _ in range(2)]
        ss4 = AR.alloc([128, 1], F32)
        rs4 = AR.alloc([128, 1], F32)
        hn4 = AR.alloc([128, D], BF16)
        PQ = es.enter_context(nc.psum_tensor("pq", [128, 4096], F32))
        PB = [PQ[:, i * 512:(i + 1) * 512] for i in range(8)]

        T = {}

        def tk(n):
            if n not in T:
                T[n] = Tok(n)
            return T[n]

        tH = [tk(f"H{i}") for i in range(NT)]
        tPB = [tk(f"PB{i}") for i in range(8)]
        tE = [[tk(f"wg{i}"), tk(f"wu{i}"), tk(f"wd{i}")] for i in range(NEB)]
        tRing = [tk(f"ring{i}") for i in range(NRING)]

        def op(eng, fn, reads=(), writes=(), dma=None):
            return S.op(eng, fn, [tk(r) if isinstance(r, str) else r for r in reads],
                        [tk(w) if isinstance(w, str) else w for w in writes], dma)

        def cdma(eng, out, in_, w):
            op(eng, lambda e, out=out, in_=in_: e.dma_start(out=out, in_=in_), writes=[w], dma="const")

        cdma("sp", gacol[:], gacol_d, "gacol")
        cdma("sp", gfcol[:], gfcol_d, "gfcol")
        cdma("sp", gvcol[:], gvcol_d, "gvcol")
        cdma("sp", bst[:], bst_d, "bst")
        cdma("sp", sinkexp[:], sinks_d, "sinkexp")
        cdma("sp", BM[:], bias_d, "BM")
        cdma("sp", rb[:], rb_d.partition_broadcast(128), "rb")
        cdma("sp", cst[:], cst_d.partition_broadcast(128), "cst")
        cdma("sp", iop[:], iop_d, "iop")
        cdma("pool", ident[:], idn_d, "ident")
        cdma("pool", utri[:], utri_d, "utri")
        cdma("pool", wst, wst_d, "wst_a")
        cdma("pool", wmask, wmask_d, "wmask_a")
        cdma("pool", maskc, mask_d, "maskc_a")
        cdma("pool", wr[:], wr_d.rearrange("(c p) n -> p c n", p=128), "wr")
        cdma("pool", wv, win_d[:, V_OFF:V_OFF + 512].rearrange("(c p) n -> p c n", p=128), "wv")
        cdma("pool", wva, win_d[:, VA_OFF:VA_OFF + 128].rearrange("(c p) n -> p c n", p=128), "wva")
        op("dve", lambda e: e.tensor_tensor(out=wstb[:], in0=wst, in1=wmask, op=ALU.mult),
           reads=["wst_a", "wmask_a"], writes=["wstb", "hnT"])
        op("dve", lambda e: e.tensor_tensor(out=BM[:], in0=BM[:], in1=maskc, op=ALU.add),
           reads=["maskc_a"], writes=["BM", "mT"])
        op("act", lambda e: e.activation(out=sinkexp[:], in_=sinkexp[:], func=AF.Exp), reads=[], writes=["sinkexp"])
        op("dve", lambda e: e.memset(ones64[:], 1.0), writes=["ones64"])
        op("dve", lambda e: e.memset(ones128[:], 1.0), writes=["ones128"])

        out_ops = []

        def rstd_ops(n):
            op("dve", lambda e: e.tensor_scalar(out=rstd[:, 0:n], in0=ss[:, 0:n], scalar1=1.0 / D, scalar2=EPS,
                                                op0=ALU.mult, op1=ALU.add), reads=["ss"], writes=["rstd"])
            op("act", lambda e: e.sqrt(out=rstd[:, 0:n], in_=rstd[:, 0:n]), reads=[], writes=["rstd"])
            op("dve", lambda e: e.reciprocal(out=rstd[:, 0:n], in_=rstd[:, 0:n]), reads=[], writes=["rstd"])

        def norm_transpose(ti_list, gcol, dstT, dst_col0, dst_tok, after_hn=None):
            n = len(ti_list)
            for k, i in enumerate(ti_list):
                hb2 = hn2[k % 2]
                op("act", lambda e, i=i, k=k, hb2=hb2: e.activation(out=hb2, in_=H[:, i, :], func=AF.Square,
                                                                   accum_out=ss[:, k:k + 1]),
                   reads=[tH[i]], writes=[f"hn{k % 2}", "ss"])
            rstd_ops(n)
            for k, i in enumerate(ti_list):
                hb2 = hn2[k % 2]
                hnn = f"hn{k % 2}"
                op("act", lambda e, i=i, k=k, hb2=hb2: e.activation(out=hb2, in_=H[:, i, :], func=AF.Identity,
                                                                   scale=rstd[:, k:k + 1]),
                   reads=[tH[i], "rstd"], writes=[hnn])
                if after_hn is not None:
                    after_hn(i, hb2, hnn)
                bank = k % 2
                pbT = PB[bank].bitcast(BF16).rearrange("p (c t) -> p c t", c=8)
                for c in range(8):
                    op("pe", lambda e, c=c, pbT=pbT, hb2=hb2: e.transpose(out=pbT[:, c, :], in_=hb2[:, c * 128:(c + 1) * 128],
                                                                         identity=ident[:]),
                       reads=[hnn, "ident"], writes=[tPB[bank]])
                c0 = dst_col0(i)
                op("dve", lambda e, pbT=pbT, c0=c0: e.tensor_tensor(
                    out=dstT[:, :, c0:c0 + 128], in0=pbT,
                    in1=gcol[:].unsqueeze(2).to_broadcast([128, 8, 128]), op=ALU.mult),
                   reads=[tPB[bank], "gacol", "gfcol"], writes=[dst_tok(i)])

        precast = [(m, ex) for ex in range(16) for m in range(3)]
        pc_ctr = [0]

        def precast_some(n):
            if stages == 1:
                return
            for _ in range(n):
                if pc_ctr[0] >= len(precast):
                    return
                m, ex = precast[pc_ctr[0]]
                pc_ctr[0] += 1
                op("pool", lambda e, m=m, ex=ex: e.dma_start(
                    out=stg, in_=wexp_d[m][ex]),
                   writes=["stg"], dma="stg")
                op("sp", lambda e, m=m, ex=ex: e.dma_start(out=wscr_d[m][ex * 128:(ex + 1) * 128, :], in_=stg),
                   reads=["stg"], writes=["wscr"], dma="wscr")

        for ps_i in range(npass):
            tok0 = ps_i * PT
            for i in range(NT):
                op("sp", lambda e, i=i, tok0=tok0: e.dma_start(out=H[:, i, :], in_=x_d[tok0 + i * 128: tok0 + (i + 1) * 128, :]),
                   writes=[tH[i]], dma=f"H{i}")
            if ps_i == 0:
                op("pool", lambda e: e.dma_start(out=wpa, in_=wpa_d.rearrange("(c p) n -> p c n", p=128)),
                   writes=[tE[0][0]], dma="wg0")
                for a_ in range(2):
                    op("pool", lambda e, a_=a_: e.dma_start(
                        out=wpb[a_ * 64:(a_ + 1) * 64, :, :],
                        in_=wpb_d.rearrange("(a j d) n -> a d j n", a=2, j=4)[a_]),
                       writes=[tk(f"wpb{a_}")], dma=f"wpbd{a_}")
                op("pool", lambda e: e.dma_start(out=wout, in_=wout_d.rearrange("(c p) n -> p c n", p=128)),
                   writes=[tE[0][1], tE[0][2]], dma="wu0")
            if ps_i > 0:
                op("dve", lambda e: e.tensor_copy(out=kT[:, 0:128], in_=kT[:, NT * 128:(NT + 1) * 128]),
                   reads=[], writes=["kT"])
                op("dve", lambda e: e.tensor_copy(out=vatt[:, 0, :], in_=vatt[:, NT, :]), reads=[], writes=["vatt"])

            ring_ctr = [0]

            def stream_block(col0):
                r = ring_ctr[0] % NRING
                ring_ctr[0] += 1
                op("pool", lambda e, r=r, col0=col0: e.dma_start(
                    out=ring[r], in_=win_d[:, col0:col0 + 128].rearrange("(c p) n -> p c n", p=128)),
                   writes=[tRing[r]], dma=f"ring{r}")
                if ring_ctr[0] % 4 == 0:
                    precast_some(1)
                return ring[r], tRing[r]

            for st in range(NST):
                tiles = [st * 4 + k for k in range(4)]
                norm_transpose(tiles, gacol, hnT, lambda i, st=st: (i - st * 4) * 128, lambda i: tk("hnT"))

                def proj_fm(col0, bank, evac):
                    wb_, wt_ = stream_block(col0)
                    for c in range(8):
                        op("pe", lambda e, c=c, wb_=wb_, bank=bank: e.matmul(
                            PB[bank][:], lhsT=wb_[:, c, :], rhs=hnT[:, c, :], start=(c == 0), stop=(c == 7)),
                           reads=[wt_, "hnT"], writes=[tPB[bank]])
                    evac(bank)

                for j in range(4):
                    proj_fm(U_OFF + j * 128, 1 + (j % 2),
                            lambda bank, j=j: op("act", lambda e: e.activation(out=uT[:, j, :], in_=PB[bank][:],
                                                                              func=AF.Gelu_apprx_tanh),
                                                 reads=[tPB[bank]], writes=["uT"]))
                for j in range(4):
                    proj_fm(Q_OFF + j * 128, 1 + (j % 2),
                            lambda bank, j=j: op("act", lambda e: e.activation(out=qT[:, j, :], in_=PB[bank][:],
                                                                              func=AF.Identity, scale=0.125),
                                                 reads=[tPB[bank]], writes=["qT"]))
                kc0 = (1 + st * 4) * 128
                proj_fm(K_OFF, 1,
                        lambda bank, kc0=kc0: op("dve", lambda e: e.tensor_copy(out=kT[:, kc0:kc0 + 512], in_=PB[bank][:]),
                                                 reads=[tPB[bank]], writes=["kT"]))

                def t_v(i):
                    ts = (i - st * 4) * 128
                    slot = 1 + i
                    q2 = i % 2
                    gv, vnh, bnst, mv, rsv, nmr = gv2[q2], vnh2[q2], bnst2[q2], mv2[q2], rsv2[q2], nmr2[q2]
                    nm = lambda x: f"{x}{q2}"
                    for c in range(8):
                        op("pe", lambda e, c=c, ts=ts: e.matmul(PB[0], lhsT=hnT[:, c, ts:ts + 128], rhs=wv[:, c, :],
                                                                start=(c == 0), stop=(c == 7)),
                           reads=["hnT", "wv"], writes=[tPB[0]])
                    for c in range(8):
                        op("pe", lambda e, c=c, ts=ts: e.matmul(PB[1][:, 0:128], lhsT=hnT[:, c, ts:ts + 128],
                                                                rhs=wva[:, c, :], start=(c == 0), stop=(c == 7)),
                           reads=["hnT", "wva"], writes=[tPB[1]])
                    op("act", lambda e, gv=gv: e.activation(out=gv, in_=PB[0], func=AF.Gelu_apprx_tanh),
                       reads=[tPB[0]], writes=[nm("gv")])
                    op("dve", lambda e, slot=slot: e.tensor_copy(out=vatt[:, slot, :], in_=PB[1][:, 0:128]),
                       reads=[tPB[1]], writes=["vatt"])
                    op("dve", lambda e, gv=gv, bnst=bnst: e.bn_stats(out=bnst, in_=gv), reads=[nm("gv")], writes=[nm("bnst")])
                    op("dve", lambda e, mv=mv, bnst=bnst: e.bn_aggr(out=mv, in_=bnst), reads=[nm("bnst")], writes=[nm("mv")])
                    op("dve", lambda e, mv=mv, rsv=rsv: e.tensor_scalar_add(out=rsv, in0=mv[:, 1:2], scalar1=EPS),
                       reads=[nm("mv")], writes=[nm("rsv")])
                    op("act", lambda e, rsv=rsv: e.sqrt(out=rsv, in_=rsv), reads=[], writes=[nm("rsv")])
                    op("dve", lambda e, rsv=rsv: e.reciprocal(out=rsv, in_=rsv), reads=[], writes=[nm("rsv")])
                    op("dve", lambda e, mv=mv, rsv=rsv, nmr=nmr: e.tensor_scalar(out=nmr, in0=mv[:, 0:1], scalar1=rsv, scalar2=-1.0,
                                                                               op0=ALU.mult, op1=ALU.mult),
                       reads=[nm("mv"), nm("rsv")], writes=[nm("nmr")])
                    op("act", lambda e, gv=gv, vnh=vnh, rsv=rsv, nmr=nmr: e.activation(out=vnh, in_=gv, func=AF.Identity,
                                                                                     scale=rsv, bias=nmr),
                       reads=[nm("gv"), nm("rsv"), nm("nmr")], writes=[nm("vnh")])

                def t_sc(i):
                    ts = (i - st * 4) * 128
                    slot = 1 + i
                    gblk = ps_i * NT + i
                    for kh in range(2):
                        pr = slice(kh * 64, (kh + 1) * 64)
                        for vi in range(2 if gblk > 0 else 1):
                            bank = 4 + kh * 2 + vi
                            ksl = slot - vi
                            op("pe", lambda e, pr=pr, ksl=ksl, ts=ts, bank=bank: e.matmul(
                                PB[bank].rearrange("p (j t) -> p j t", j=4),
                                lhsT=kT[pr, ksl * 128:(ksl + 1) * 128], rhs=qT[pr, :, ts:ts + 128],
                                start=True, stop=True),
                               reads=["kT", "qT"], writes=[tPB[bank]])
                    scb = PQ[:, 2048:4096]
                    op("dve", lambda e, scb=scb: e.tensor_tensor(out=scb, in0=scb, in1=BM[:].rearrange("p k v j t -> p (k v j t)"),
                                                                 op=ALU.add),
                       reads=["BM"], writes=[tPB[4], tPB[5], tPB[6], tPB[7]])
                    op("act", lambda e, scb=scb: e.activation(out=pT, in_=scb, func=AF.Exp),
                       reads=[tPB[4], tPB[5], tPB[6], tPB[7]], writes=["pT"])

                def t_pv(i):
                    ts = (i - st * 4) * 128
                    slot = 1 + i
                    gblk = ps_i * NT + i
                    nv = 2 if gblk > 0 else 1
                    for (bank, use_v) in ((3, True), (0, False)):
                        for kh in range(2):
                            kw = {"tile_position": (0, 64)} if kh else {}
                            for vi in range(nv):
                                ksl = slot - vi
                                lhs = vatt[:, ksl, kh * 64:(kh + 1) * 64] if use_v else ones64[:]
                                pc = (kh * 2 + vi) * 512
                                op("pe", lambda e, bank=bank, kh=kh, vi=vi, lhs=lhs, pc=pc, kw=kw, nv=nv: e.matmul(
                                    PB[bank][kh * 64:(kh + 1) * 64, :], lhsT=lhs, rhs=pT[:, pc:pc + 512],
                                    start=(vi == 0), stop=(vi == nv - 1), **kw),
                                   reads=["vatt", "ones64", "pT"], writes=[tPB[bank]])
                    op("dve", lambda e: e.tensor_tensor(
                        out=den.rearrange("p (j t) -> p j t", j=4), in0=PB[0].rearrange("p (j t) -> p j t", j=4),
                        in1=sinkexp[:].unsqueeze(2).to_broadcast([128, 4, 128]), op=ALU.add),
                       reads=[tPB[0], "sinkexp"], writes=["den"])
                    op("dve", lambda e: e.reciprocal(out=den, in_=den), reads=[], writes=["den"])
                    op("dve", lambda e, ts=ts: e.tensor_tensor(
                        out=bT[:, :, ts:ts + 128], in0=PB[3].rearrange("p (j t) -> p j t", j=4),
                        in1=den.rearrange("p (j t) -> p j t", j=4), op=ALU.mult),
                       reads=[tPB[3], "den"], writes=["bT"])

                def t_sp(i):
                    ts = (i - st * 4) * 128
                    q2 = i % 2
                    vnh = vnh2[q2]
                    pbs = PB[2].rearrange("p (j t) -> p j t", j=4)
                    for g in range(8):
                        lo = (g % 2) * 64
                        kw = {"tile_position": (0, 64)} if g % 2 else {}
                        op("pe", lambda e, g=g, lo=lo, kw=kw, vnh=vnh: e.matmul(
                            pbs[lo:lo + 64, g // 2, :], lhsT=vnh[:, g * 64:(g + 1) * 64], rhs=wstb[:, g, :],
                            start=True, stop=True, **kw),
                           reads=[f"vnh{q2}", "wstb"], writes=[tPB[2]])
                    op("dve", lambda e: e.tensor_tensor(out=gtmp, in0=pbs,
                                                        in1=gvcol[:].unsqueeze(2).to_broadcast([128, 4, 128]), op=ALU.mult),
                       reads=[tPB[2], "gvcol"], writes=["gtmp"])
                    op("dve", lambda e: e.tensor_tensor(out=gtmp, in0=gtmp, in1=bst[:], op=ALU.add),
                       reads=["bst"], writes=["gtmp"])
                    op("dve", lambda e, ts=ts: e.tensor_tensor(out=aT[:, :, ts:ts + 128], in0=gtmp,
                                                               in1=uT[:, :, ts:ts + 128], op=ALU.mult),
                       reads=["gtmp", "uT"], writes=["aT"])

                t_v(tiles[0])
                t_sc(tiles[0])
                for k in range(1, 4):
                    t_v(tiles[k])
                    t_pv(tiles[k - 1])
                    t_sc(tiles[k])
                    t_sp(tiles[k - 1])
                t_pv(tiles[3])
                t_sp(tiles[3])

                for j in range(8):
                    b0 = (j % 2) * 4
                    wga, tga = stream_block(GA_OFF + j * 128)
                    for c in range(8):
                        op("pe", lambda e, c=c, wga=wga, b0=b0: e.matmul(PB[b0], lhsT=wga[:, c, :], rhs=hnT[:, c, :],
                                                                         start=(c == 0), stop=(c == 7)),
                           reads=[tga, "hnT"], writes=[tPB[b0]])
                    wgb, tgb = stream_block(GB_OFF + j * 128)
                    for c in range(8):
                        op("pe", lambda e, c=c, wgb=wgb, b0=b0: e.matmul(PB[b0 + 1], lhsT=wgb[:, c, :], rhs=hnT[:, c, :],
                                                                         start=(c == 0), stop=(c == 7)),
                           reads=[tgb, "hnT"], writes=[tPB[b0 + 1]])
                    for c in range(4):
                        op("pe", lambda e, c=c, j=j, b0=b0: e.matmul(PB[b0 + 2], lhsT=wpa[:, c, j * 128:(j + 1) * 128], rhs=aT[:, c, :],
                                                                     start=(c == 0), stop=(c == 3)),
                           reads=[tE[0][0], "aT"], writes=[tPB[b0 + 2]])
                    for c in range(4):
                        op("pe", lambda e, c=c, j=j, b0=b0: e.matmul(PB[b0 + 3], lhsT=wpb[:, c, j * 128:(j + 1) * 128], rhs=bT[:, c, :],
                                                                     start=(c == 0), stop=(c == 3)),
                           reads=["wpb0", "wpb1", "bT"], writes=[tPB[b0 + 3]])
                    op("act", lambda e, b0=b0: e.activation(out=sga, in_=PB[b0], func=AF.Sigmoid),
                       reads=[tPB[b0]], writes=["sga"])
                    op("act", lambda e, b0=b0: e.activation(out=sgb, in_=PB[b0 + 1], func=AF.Sigmoid),
                       reads=[tPB[b0 + 1]], writes=["sgb"])
                    op("dve", lambda e, b0=b0: e.tensor_tensor(out=sga, in0=PB[b0 + 2], in1=sga, op=ALU.mult),
                       reads=[tPB[b0 + 2]], writes=["sga"])
                    op("dve", lambda e, b0=b0: e.tensor_tensor(out=sgb, in0=PB[b0 + 3], in1=sgb, op=ALU.mult),
                       reads=[tPB[b0 + 3]], writes=["sgb"])
                    op("dve", lambda e, j=j: e.tensor_tensor(out=mT[:, j, :], in0=sga, in1=sgb, op=ALU.add),
                       reads=["sga", "sgb"], writes=["mT"])
                for i in tiles:
                    ts = (i - st * 4) * 128
                    for hf in range(2):
                        bank = 1 + hf
                        for j in range(8):
                            op("pe", lambda e, j=j, ts=ts, hf=hf, bank=bank: e.matmul(
                                PB[bank][:], lhsT=mT[:, j, ts:ts + 128], rhs=wout[:, j, hf * 512:(hf + 1) * 512],
                                start=(j == 0), stop=(j == 7)),
                               reads=["mT", tE[0][1], tE[0][2]], writes=[tPB[bank]])
                        op("dve", lambda e, i=i, hf=hf, bank=bank: e.tensor_tensor(
                            out=H[:, i, hf * 512:(hf + 1) * 512], in0=PB[bank][:], in1=H[:, i, hf * 512:(hf + 1) * 512],
                            op=ALU.add),
                           reads=[tPB[bank]], writes=[tH[i]])
                    op("sp", lambda e, i=i, tok0=tok0: e.dma_start(out=hS_d[tok0 + i * 128: tok0 + (i + 1) * 128, :],
                                                                   in_=H[:, i, :]),
                       reads=[tH[i]], writes=["hS"], dma=f"H{i}")

                def spill_hn2(i, hb2, hnn, tok0=tok0):
                    op("sp", lambda e, i=i, tok0=tok0, hb2=hb2: e.dma_start(out=hn2S_d[tok0 + i * 128: tok0 + (i + 1) * 128, :], in_=hb2),
                       reads=[hnn], writes=["hn2S"], dma="hn2S" + hnn)
                norm_transpose(tiles, gfcol, hn2T, lambda i, st=st: (i - st * 4) * 128, lambda i: tk("hn2T"),
                               after_hn=spill_hn2)
                for i in tiles:
                    gi = ps_i * NT + i
                    ts = (i - st * 4) * 128
                    for c in range(8):
                        op("pe", lambda e, c=c, ts=ts: e.matmul(PB[7][:, 0:20], lhsT=hn2T[:, c, ts:ts + 128], rhs=wr[:, c, :],
                                                                start=(c == 0), stop=(c == 7)),
                           reads=["hn2T", "wr"], writes=[tPB[7]])
                    op("dve", lambda e, gi=gi: e.tensor_tensor(out=lg[:, gi, :], in0=PB[7][:, 0:20], in1=rb[:], op=ALU.add),
                       reads=[tPB[7], "rb"], writes=["lg"])

            if debug and ps_i == 0:
                for i in range(NT):
                    out_ops.append(op("sp", lambda e, i=i: e.dma_start(out=dbg_d[i * 128:(i + 1) * 128, :], in_=H[:, i, :]),
                                      reads=[tH[i]], dma=f"H{i}"))
        precast_some(100)

        S.barrier("dve", lambda e: e.memset(dummy[:], 0.0))
        NTK = npass * NT

        cdma2 = op("sp", lambda e: e.dma_start(out=gfin, in_=gfin_d.partition_broadcast(128)), writes=["gfin"], dma="gfin")

        gl = lg[:, :, 0:4]
        el = lg[:, :, 4:20].rearrange("p t (g x) -> p t g x", g=4)
        bc3 = lambda a: a.unsqueeze(2).to_broadcast([128, NTT, 4])
        if npass < NPASS_FULL:
            op("dve", lambda e: e.memset(lg[:, NTK:, :], 0.0), writes=["lg"])
        dv = lambda fn, r, w: op("dve", fn, reads=r, writes=w)
        dv(lambda e: e.tensor_reduce(out=r_m, in_=gl, axis=AX.X, op=ALU.max), ["lg"], ["r_m"])
        dv(lambda e: e.tensor_tensor(out=r_og, in0=gl, in1=bc3(r_m), op=ALU.is_equal), ["lg", "r_m"], ["r_og"])
        dv(lambda e: e.tensor_tensor(out=r_eg, in0=gl, in1=bc3(r_m), op=ALU.subtract), ["lg", "r_m"], ["r_eg"])
        op("act", lambda e: e.activation(out=r_eg, in_=r_eg, func=AF.Exp), reads=[], writes=["r_eg"])
        dv(lambda e: e.tensor_reduce(out=r_gp, in_=r_eg, axis=AX.X, op=ALU.add), ["r_eg"], ["r_gp"])
        dv(lambda e: e.reciprocal(out=r_gp, in_=r_gp), [], ["r_gp"])
        dv(lambda e: e.tensor_tensor(out=r_sel, in0=el, in1=r_og.unsqueeze(3).to_broadcast([128, NTT, 4, 4]), op=ALU.mult),
           ["lg", "r_og"], ["r_sel"])
        dv(lambda e: e.tensor_reduce(out=r_es, in_=r_sel.rearrange("p t g x -> p t x g"), axis=AX.X, op=ALU.add),
           ["r_sel"], ["r_es"])
        dv(lambda e: e.tensor_reduce(out=r_m1, in_=r_es, axis=AX.X, op=ALU.max), ["r_es"], ["r_m1"])
        dv(lambda e: e.tensor_tensor(out=r_o1, in0=r_es, in1=bc3(r_m1), op=ALU.is_equal), ["r_es", "r_m1"], ["r_o1"])
        dv(lambda e: e.scalar_tensor_tensor(out=r_es2, in0=r_o1, scalar=-1e9, in1=r_es, op0=ALU.mult, op1=ALU.add),
           ["r_o1", "r_es"], ["r_es2"])
        dv(lambda e: e.tensor_reduce(out=r_m2, in_=r_es2, axis=AX.X, op=ALU.max), ["r_es2"], ["r_m2"])
        dv(lambda e: e.tensor_tensor(out=r_o2, in0=r_es2, in1=bc3(r_m2), op=ALU.is_equal), ["r_es2", "r_m2"], ["r_o2"])
        dv(lambda e: e.tensor_tensor(out=r_d, in0=r_m2, in1=r_m1, op=ALU.subtract), ["r_m1", "r_m2"], ["r_d"])
        op("act", lambda e: e.activation(out=r_d, in_=r_d, func=AF.Exp), reads=[], writes=["r_d"])
        dv(lambda e: e.tensor_scalar_add(out=r_w1, in0=r_d, scalar1=1.0), ["r_d"], ["r_w1"])
        dv(lambda e: e.reciprocal(out=r_w1, in_=r_w1), [], ["r_w1"])
        dv(lambda e: e.tensor_tensor(out=wB[:], in0=r_d, in1=r_w1, op=ALU.mult), ["r_d", "r_w1"], ["wB"])
        dv(lambda e: e.tensor_tensor(out=wA[:], in0=r_w1, in1=r_gp, op=ALU.mult), ["r_w1", "r_gp"], ["wA"])
        dv(lambda e: e.tensor_tensor(out=wB[:], in0=wB[:], in1=r_gp, op=ALU.mult), ["r_gp"], ["wB"])
        bg = lambda a: a.unsqueeze(3).to_broadcast([128, NTT, 4, 4])
        bx = lambda a: a.unsqueeze(2).to_broadcast([128, NTT, 4, 4])
        dv(lambda e: e.tensor_tensor(out=M1, in0=bg(r_og), in1=bx(r_o1), op=ALU.mult), ["r_og", "r_o1"], ["M1"])
        dv(lambda e: e.tensor_tensor(out=M2, in0=bg(r_og), in1=bx(r_o2), op=ALU.mult), ["r_og", "r_o2"], ["M2"])
        M1f = M1.rearrange("p t g x -> p t (g x)")
        M2f = M2.rearrange("p t g x -> p t (g x)")
        dv(lambda e: e.tensor_tensor(out=Mb.rearrange("p (t n) -> p t n", n=16), in0=M1f, in1=M2f, op=ALU.add),
           ["M1", "M2"], ["Mb"])
        if npass < NPASS_FULL:
            dv(lambda e: e.memset(Mb[:, NTK * 16:], 0.0), [], ["Mb"])
        op("pe", lambda e: e.matmul(PB[0][:], lhsT=utri[:], rhs=Mb, start=True, stop=True),
           reads=["utri", "Mb"], writes=[tPB[0]])
        op("pe", lambda e: e.matmul(PB[1][:], lhsT=ones128[:], rhs=Mb, start=True, stop=True),
           reads=["ones128", "Mb"], writes=[tPB[1]])
        dv(lambda e: e.tensor_copy(out=R1s.rearrange("p t n -> p (t n)"), in_=PB[0][:]), [tPB[0]], ["R1s"])
        dv(lambda e: e.tensor_copy(out=Tts.rearrange("p t n -> p (t n)"), in_=PB[1][:]), [tPB[1]], ["Tts"])
        dv(lambda e: e.tensor_copy(out=Ca, in_=Tts), ["Tts"], ["Ca"])
        cur, nxt, cn, nn = Ca, Cb, "Ca", "Cb"
        sh = 1
        while sh < NTT:
            dv(lambda e, cur=cur, nxt=nxt, sh=sh: e.tensor_copy(out=nxt[:, 0:sh, :], in_=cur[:, 0:sh, :]), [cn], [nn])
            dv(lambda e, cur=cur, nxt=nxt, sh=sh: e.tensor_tensor(out=nxt[:, sh:, :], in0=cur[:, sh:, :],
                                                                  in1=cur[:, 0:NTT - sh, :], op=ALU.add), [cn], [nn])
            cur, nxt, cn, nn = nxt, cur, nn, cn
            sh *= 2
        Cin, cin_n, Cex, cex_n = cur, cn, nxt, nn
        dv(lambda e: e.tensor_tensor(out=Cex, in0=Cin, in1=Tts, op=ALU.subtract), [cin_n, "Tts"], [cex_n])
        ntot = Cin[:, NTT - 1, :]
        thr = cst[:, 0:8]
        tri = cst[:, 8:264].rearrange("p (a b) -> p a b", a=16)
        sio = cst[:, 264:296]
        cmp8 = r_cmp.rearrange("p a b -> p (a b)")[:, 0:128].rearrange("p (a b) -> p a b", b=8)
        dv(lambda e: e.tensor_tensor(out=cmp8, in0=ntot.unsqueeze(2).to_broadcast([128, 16, 8]),
                                     in1=thr.unsqueeze(1).to_broadcast([128, 16, 8]), op=ALU.is_gt),
           [cin_n, "cst"], ["r_cmp"])
        dv(lambda e: e.tensor_reduce(out=r_nb, in_=cmp8, axis=AX.X, op=ALU.add), ["r_cmp"], ["r_nb"])
        dv(lambda e: e.tensor_tensor(out=r_cmp, in0=r_nb.unsqueeze(1).to_broadcast([128, 16, 16]), in1=tri, op=ALU.mult),
           ["r_nb", "cst"], ["r_cmp"])
        dv(lambda e: e.tensor_reduce(out=r_ob, in_=r_cmp, axis=AX.X, op=ALU.add), ["r_cmp"], ["r_ob"])
        dv(lambda e: e.tensor_tensor(out=r_oe, in0=r_ob, in1=r_nb, op=ALU.add), ["r_ob", "r_nb"], ["r_oe"])
        dv(lambda e: e.tensor_tensor(out=R1s, in0=R1s, in1=Cex, op=ALU.add), [cex_n], ["R1s"])
        dv(lambda e: e.scalar_tensor_tensor(out=R1s, in0=r_ob.unsqueeze(1).to_broadcast([128, NTT, 16]), scalar=512.0,
                                            in1=R1s, op0=ALU.mult, op1=ALU.add), ["r_ob"], ["R1s"])
        for (Mf, mn, pos_i, pn) in ((M1f, "M1", posA, "posA"), (M2f, "M2", posB, "posB")):
            dv(lambda e, Mf=Mf: e.tensor_tensor(out=Mf, in0=Mf, in1=R1s, op=ALU.mult), ["R1s"], [mn])
            dv(lambda e, Mf=Mf: e.tensor_reduce(out=r_pf, in_=Mf, axis=AX.X, op=ALU.add), [mn], ["r_pf"])
            dv(lambda e: e.tensor_scalar(out=r_pf, in0=r_pf, scalar1=0.0, scalar2=float(NSLOT * 512 - 1),
                                         op0=ALU.max, op1=ALU.min), [], ["r_pf"])
            dv(lambda e, pos_i=pos_i: e.tensor_copy(out=pos_i[:], in_=r_pf), ["r_pf"], [pn])
        dv(lambda e: e.tensor_tensor(out=r_cmp2, in0=sio.unsqueeze(2).to_broadcast([128, NSLOT, 16]),
                                     in1=r_oe.unsqueeze(1).to_broadcast([128, NSLOT, 16]), op=ALU.is_ge),
           ["cst", "r_oe"], ["r_cmp2"])
        dv(lambda e: e.tensor_reduce(out=r_eid, in_=r_cmp2, axis=AX.X, op=ALU.add), ["r_cmp2"], ["r_eid"])
        dv(lambda e: e.tensor_scalar(out=r_eid, in0=r_eid, scalar1=15.0, scalar2=128.0, op0=ALU.min, op1=ALU.mult),
           [], ["r_eid"])
        dv(lambda e: e.tensor_tensor(out=r_eid, in0=r_eid, in1=iop[:].to_broadcast([128, NSLOT]), op=ALU.add),
           ["iop"], ["r_eid"])
        dv(lambda e: e.tensor_scalar(out=r_eid, in0=r_eid, scalar1=0.0, scalar2=2047.0, op0=ALU.max, op1=ALU.min),
           [], ["r_eid"])
        dv(lambda e: e.tensor_copy(out=idxW[:], in_=r_eid), ["r_eid"], ["idxW"])
        if debug:
            dr = sb("dbgr_s", [128, 6, NTT], F32)
            dv(lambda e: e.tensor_copy(out=dr[:, 0, :], in_=posA[:]), ["posA"], ["dr"])
            dv(lambda e: e.tensor_copy(out=dr[:, 1, :], in_=posB[:]), ["posB"], ["dr"])
            dv(lambda e: e.tensor_copy(out=dr[:, 2, :], in_=wA[:]), ["wA"], ["dr"])
            dv(lambda e: e.tensor_copy(out=dr[:, 3, :], in_=wB[:]), ["wB"], ["dr"])
            dv(lambda e: e.tensor_copy(out=dr[:, 4, :], in_=idxW[:]), ["idxW"], ["dr"])
            dv(lambda e: e.tensor_copy(out=dr[:, 5, 0:16], in_=ntot), [cin_n], ["dr"])
            out_ops.append(op("sp", lambda e: e.dma_start(out=dbgr_d, in_=dr[:]), reads=["dr"], dma="dbgr"))

        for gi in range(NTK if stages >= 3 else 0):
            xb = xin[gi % NXIN]
            xn = f"xin{gi % NXIN}"
            op("sp", lambda e, gi=gi, xb=xb: e.dma_start(out=xb, in_=hn2S_d[gi * 128:(gi + 1) * 128, :]),
               reads=["hn2S"], writes=[xn], dma=xn)
            for (pos_i, pn) in ((posA, "posA"), (posB, "posB")):
                op("pool", lambda e, gi=gi, xb=xb, pos_i=pos_i: e.indirect_dma_start(
                    out=Xs_d, out_offset=bass.IndirectOffsetOnAxis(pos_i[:, gi:gi + 1], 0), in_=xb, in_offset=None),
                   reads=[xn, pn], writes=[f"Xs{gi % NXIN}"], dma=f"scat{gi % NXIN}")

        NS3 = NSLOT if stages >= 3 else 0

        def slot_loads(s):
            b = s % NEB
            for m in range(3):
                op("pool", lambda e, s=s, b=b, m=m: e.indirect_dma_start(
                    out=wflat[b][m], out_offset=None, in_=wscr_d[m],
                    in_offset=bass.IndirectOffsetOnAxis(idxW[:, s:s + 1], 0)),
                   reads=["idxW", "wscr"], writes=[tE[b][m]], dma=f"w{'gud'[m]}{b}")
            xb = xs[s % 2]
            xn = f"xs{s % 2}"
            op("sp", lambda e, s=s, xb=xb: e.dma_start(out=xb, in_=Xs_d[s * 512:(s + 1) * 512, :].rearrange("(j p) d -> p j d", p=128)),
               reads=[f"Xs{q}" for q in range(NXIN)], writes=[xn], dma=xn)

        def slot_transposes(s):
            xb = xs[s % 2]
            xn = f"xs{s % 2}"
            xt = xT[s % 2]
            xtn = f"xT{s % 2}"
            for j in range(4):
                bank = j % 2
                pbT = PB[bank][:].bitcast(BF16).rearrange("p (c t) -> p c t", c=8)
                for c in range(8):
                    op("pe", lambda e, c=c, j=j, pbT=pbT, xb=xb: e.transpose(out=pbT[:, c, :], in_=xb[:, j, c * 128:(c + 1) * 128],
                                                                            identity=ident[:]),
                       reads=[xn, "ident"], writes=[tPB[bank]])
                op("dve", lambda e, j=j, pbT=pbT, xt=xt: e.tensor_tensor(
                    out=xt[:, :, j * 128:(j + 1) * 128], in0=pbT,
                    in1=gfcol[:].unsqueeze(2).to_broadcast([128, 8, 128]), op=ALU.mult),
                   reads=[tPB[bank], "gfcol"], writes=[xtn])

        def slot_gateup(s):
            b = s % NEB
            xt = xT[s % 2]
            xtn = f"xT{s % 2}"
            hb_ = s % 2
            for j in range(4):
                bg_, bu_ = 2 + (j % 2) * 2, 3 + (j % 2) * 2
                for c in range(8):
                    op("pe", lambda e, c=c, j=j, b=b, bg_=bg_, xt=xt: e.matmul(
                        PB[bg_][:], lhsT=wgs[b][:, c, j * 128:(j + 1) * 128], rhs=xt[:, c, :],
                        start=(c == 0), stop=(c == 7)),
                       reads=[tE[b][0], xtn], writes=[tPB[bg_]])
                for c in range(8):
                    op("pe", lambda e, c=c, j=j, b=b, bu_=bu_, xt=xt: e.matmul(
                        PB[bu_][:], lhsT=wus[b][:, c, j * 128:(j + 1) * 128], rhs=xt[:, c, :],
                        start=(c == 0), stop=(c == 7)),
                       reads=[tE[b][1], xtn], writes=[tPB[bu_]])
                op("act", lambda e, j=j, bg_=bg_: e.activation(out=sgs[j % 2], in_=PB[bg_][:], func=AF.Silu),
                   reads=[tPB[bg_]], writes=[f"sgs{j % 2}"])
                op("dve", lambda e, j=j, bu_=bu_, hb_=hb_: e.tensor_tensor(out=hid[hb_][:, j, :], in0=PB[bu_][:],
                                                                         in1=sgs[j % 2], op=ALU.mult),
                   reads=[tPB[bu_], f"sgs{j % 2}"], writes=[f"hid{hb_}"])

        def slot_down(s):
            b = s % NEB
            hb_ = s % 2
            for tl in range(4):
                yi = (s * 4 + tl) % NYS
                yb = ys[yi]
                yn = f"ys{yi}"
                for hf in range(2):
                    bank = 6 + hf
                    for j in range(4):
                        op("pe", lambda e, j=j, tl=tl, hf=hf, b=b, hb_=hb_, bank=bank: e.matmul(
                            PB[bank][:], lhsT=hid[hb_][:, j, tl * 128:(tl + 1) * 128],
                            rhs=wds[b][:, j, hf * 512:(hf + 1) * 512], start=(j == 0), stop=(j == 3)),
                           reads=[f"hid{hb_}", tE[b][2]], writes=[tPB[bank]])
                    op("act", lambda e, hf=hf, bank=bank, yb=yb: e.copy(out=yb[:, hf * 512:(hf + 1) * 512], in_=PB[bank][:]),
                       reads=[tPB[bank]], writes=[yn])
                op("sp", lambda e, s=s, tl=tl, yb=yb: e.dma_start(out=Ys_d[s * 512 + tl * 128: s * 512 + (tl + 1) * 128, :], in_=yb),
                   reads=[yn], writes=[f"Ys{yi}"], dma=f"ysd{yi}")

        if NS3:
            slot_loads(0)
            slot_transposes(0)
            slot_loads(1)
        for s in range(NS3):
            slot_gateup(s)
            if s + 1 < NS3:
                slot_transposes(s + 1)
            slot_down(s)
            if s + 2 < NS3:
                slot_loads(s + 2)

        for gi in range(NTK if stages >= 4 else 0):
            k2 = gi % 2
            op("sp", lambda e, gi=gi, k2=k2: e.dma_start(out=hb[k2], in_=hS_d[gi * 128:(gi + 1) * 128, :]),
               reads=["hS"], writes=[f"hb{k2}"], dma=f"hb{k2}")
            for (Yb, ynm, pos_i, pn) in ((YA, "YA", posA, "posA"), (YB, "YB", posB, "posB")):
                op("pool", lambda e, gi=gi, k2=k2, Yb=Yb, pos_i=pos_i: e.indirect_dma_start(
                    out=Yb[k2], out_offset=None, in_=Ys_d, in_offset=bass.IndirectOffsetOnAxis(pos_i[:, gi:gi + 1], 0)),
                   reads=[f"Ys{q}" for q in range(NYS)] + [pn], writes=[f"{ynm}{k2}"], dma=f"{ynm}{k2}")
            op("dve", lambda e, gi=gi, k2=k2: e.scalar_tensor_tensor(out=hb[k2], in0=YA[k2], scalar=wA[:, gi:gi + 1], in1=hb[k2],
                                                                     op0=ALU.mult, op1=ALU.add),
               reads=[f"YA{k2}", "wA"], writes=[f"hb{k2}"])
            op("dve", lambda e, gi=gi, k2=k2: e.scalar_tensor_tensor(out=hb[k2], in0=YB[k2], scalar=wB[:, gi:gi + 1], in1=hb[k2],
                                                                     op0=ALU.mult, op1=ALU.add),
               reads=[f"YB{k2}", "wB"], writes=[f"hb{k2}"])
            op("act", lambda e, k2=k2: e.activation(out=hn4, in_=hb[k2], func=AF.Square, accum_out=ss4),
               reads=[f"hb{k2}"], writes=["hn4", "ss4"])
            op("dve", lambda e: e.tensor_scalar(out=rs4, in0=ss4, scalar1=1.0 / D, scalar2=EPS, op0=ALU.mult, op1=ALU.add),
               reads=["ss4"], writes=["rs4"])
            op("act", lambda e: e.sqrt(out=rs4, in_=rs4), reads=[], writes=["rs4"])
            op("dve", lambda e: e.reciprocal(out=rs4, in_=rs4), reads=[], writes=["rs4"])
            op("dve", lambda e, k2=k2: e.scalar_tensor_tensor(out=hb[k2], in0=hb[k2], scalar=rs4, in1=gfin,
                                                              op0=ALU.mult, op1=ALU.mult),
               reads=["rs4", "gfin"], writes=[f"hb{k2}"])
            out_ops.append(op("sp", lambda e, gi=gi, k2=k2: e.dma_start(out=y_d[gi * 128:(gi + 1) * 128, :], in_=hb[k2]),
                              reads=[f"hb{k2}"], dma=f"hb{k2}"))

        S.emit(nc, final_wait_ops=out_ops)
    return nc


def _t5_bucket(dist):
    dist = np.asarray(dist, dtype=np.int64)
    nf = np.maximum(dist, 1).astype(np.float32)
    large = 16 + (np.log(nf / np.float32(16)) / np.float32(math.log(128 / 16)) * np.float32(16)).astype(np.int32)
    large = np.minimum(large, 31)
    return np.where(dist < 16, dist, large)


def prep_shared(inp):
    f = lambda a: np.ascontiguousarray(np.asarray(a, dtype=np.float32))
    w_in = f(inp["w_in"])[0]
    perm = np.arange(3840)
    qcols = []
    for j in range(4):
        qcols += list(range(1024 + j * 64, 1024 + (j + 1) * 64))
        qcols += list(range(1024 + (4 + j) * 64, 1024 + (5 + j) * 64))
    perm[1024:1536] = np.array(qcols)
    w_in = np.ascontiguousarray(w_in[:, perm])
    wr = np.concatenate([f(inp["router_group_w"])[0]] + [f(inp["router_expert_w"])[0, g] for g in range(4)], axis=1)
    rb = np.concatenate([f(inp["router_group_b"])[0], f(inp["router_expert_b"])[0].reshape(-1)])[None, :]
    col = lambda v, c: np.ascontiguousarray(f(v).reshape(c, 128).T)
    ws = f(inp["gm_w_spatial"])[0]
    wst = np.ascontiguousarray(ws.transpose(2, 0, 1))
    s_i = np.arange(128)[:, None, None]
    t_i = np.arange(128)[None, None, :]
    wmask = np.broadcast_to((s_i <= t_i), (128, 8, 128)).astype(np.float32)
    bs = f(inp["gm_b_spatial"])[0]
    bst = np.zeros((128, 4, 128), np.float32)
    for g in range(8):
        bst[(g % 2) * 64:(g % 2) * 64 + 64, g // 2, :] = bs[g][None, :]
    rel = f(inp["rel_bias"])
    s_ = np.arange(128)[:, None]
    q_ = np.arange(128)[None, :]
    d_own = q_ - s_
    d_prev = q_ + 128 - s_
    bias = np.zeros((128, 2, 2, 4, 128), np.float32)
    maskc = np.zeros((128, 2, 2, 4, 128), np.float32)
    b_own = _t5_bucket(np.clip(d_own, 0, 127))
    b_prev = _t5_bucket(np.clip(d_prev, 0, 127))
    for kh in range(2):
        for h4 in range(4):
            h = kh * 4 + h4
            bias[:, kh, 0, h4, :] = rel[b_own, h]
            bias[:, kh, 1, h4, :] = rel[b_prev, h]
            maskc[:, kh, 0, h4, :] = np.where(d_own >= 0, 0.0, NEG)
            maskc[:, kh, 1, h4, :] = np.where(d_prev < 128, 0.0, NEG)
    def pmaj(w, c):
        e_, r_, n_ = w.shape
        return np.ascontiguousarray(w.reshape(e_, c, 128, n_).transpose(0, 2, 1, 3).reshape(e_, 128, c * n_))

    return {
        "w_in": w_in,
        "wpa": f(inp["w_proj_a"])[0], "wpb": f(inp["w_proj_b"])[0], "wout": f(inp["w_out"])[0],
        "wgp": pmaj(f(inp["expert_w_gate"])[0], 8), "wup": pmaj(f(inp["expert_w_up"])[0], 8),
        "wdp": pmaj(f(inp["expert_w_down"])[0], 4),
        "utri": np.triu(np.ones((128, 128), np.float32), 1),
        "cst": np.concatenate([np.arange(8, dtype=np.float32) * 512.0,
                               np.tril(np.ones((16, 16), np.float32), -1).reshape(-1),
                               np.arange(32, dtype=np.float32)])[None, :],
        "iop": np.arange(128, dtype=np.float32)[:, None],
        "wr": np.ascontiguousarray(wr), "rb": np.ascontiguousarray(rb),
        "gacol": col(inp["attn_norm_g"], 8), "gfcol": col(inp["ffn_norm_g"], 8), "gvcol": col(inp["gm_v_norm_g"], 4),
        "gfin": f(inp["final_norm_g"]).reshape(1, D),
        "wst": wst, "wmask": wmask, "bst": bst,
        "sinks": np.ascontiguousarray(np.repeat(f(inp["attn_sinks"]).reshape(2, 4), 64, axis=0)),
        "bias": bias, "maskc": maskc,
        "idn": np.eye(128, dtype=np.float32),
    }


def kernel(**inputs):
    shared = prep_shared(inputs)
    x = np.asarray(inputs["x"], dtype=np.float32)
    nc = build()
    in_maps = []
    for c in range(NCORES):
        m = dict(shared)
        m["x"] = np.ascontiguousarray(x[c])
        in_maps.append(m)
    res = run_bass_kernel_spmd(nc, in_maps, core_ids=list(range(NCORES)))
    return np.stack([np.asarray(r["y"], dtype=np.float32) for r in res.results], axis=0)
```

```python
import math
from contextlib import ExitStack

import numpy as np
import concourse.bass as bass
import concourse.mybir as mybir
from concourse.bass_utils import run_bass_kernel_spmd

F32 = mybir.dt.float32
BF16 = mybir.dt.bfloat16
AF = mybir.ActivationFunctionType
ALU = mybir.AluOpType
AX = mybir.AxisListType

SEQ = 4096
D = 1024
NCORES = 8
PT = 1024
NT = PT // 128
NST = PT // 512
NPASS_FULL = SEQ // PT
EPS = 1e-6
NEG = -30000.0
NSLOT = 31
NTT = SEQ // 128
I32 = mybir.dt.int32

U_OFF, V_OFF, Q_OFF, K_OFF, VA_OFF, GA_OFF, GB_OFF = 0, 512, 1024, 1536, 1664, 1792, 2816

ENG = ("pe", "act", "dve", "pool", "sp")


class Tok:
    __slots__ = ("name", "last_w", "readers")

    def __init__(self, name):
        self.name = name
        self.last_w = None
        self.readers = []


class Op:
    __slots__ = ("eng", "fn", "deps", "sig", "dma_sem", "dma_cnt", "seq")

    def __init__(self, eng, fn):
        self.eng = eng
        self.fn = fn
        self.deps = set()
        self.sig = False
        self.dma_sem = None
        self.dma_cnt = 0
        self.seq = 0


class Sched:
    def __init__(self):
        self.ops = []
        self.per = {e: [] for e in ENG}
        self.dma_counts = {}
        self.total_keys = set()
        self.epoch_op = None
        self.last_dma = {}

    def op(self, eng, fn, reads=(), writes=(), dma=None):
        o = Op(eng, fn)
        for t in list(reads) + list(writes):
            if t.last_w is not None:
                o.deps.add(t.last_w)
        for t in writes:
            for r in t.readers:
                o.deps.add(r)
        if self.epoch_op is not None:
            o.deps.add(self.epoch_op)
        o.deps.discard(o)
        for t in reads:
            t.readers.append(o)
        for t in writes:
            t.last_w = o
            t.readers = []
        if dma is not None:
            o.dma_sem = dma
            self.dma_counts[dma] = self.dma_counts.get(dma, 0) + 1
            o.dma_cnt = self.dma_counts[dma]
            self.last_dma[dma] = o
        self.ops.append(o)
        self.per[eng].append(o)
        return o

    def barrier(self, eng, fn):
        o = Op(eng, fn)
        for e in ENG:
            seen_c = False
            for p in reversed(self.per[e]):
                if p.dma_sem is None:
                    o.deps.add(p)
                    break
        for k, p in self.last_dma.items():
            o.deps.add(p)
        if self.epoch_op is not None:
            o.deps.add(self.epoch_op)
        self.ops.append(o)
        self.per[eng].append(o)
        self.epoch_op = o
        return o

    def emit(self, nc, final_wait_ops=()):
        def skip(d, o):
            return d.dma_sem is None and o.dma_sem is None and d.eng == "pe" and o.eng == "pe"

        for o in self.ops:
            for d in o.deps:
                if d.dma_sem is None and not skip(d, o):
                    d.sig = True
        for o in final_wait_ops:
            if o.dma_sem is None:
                o.sig = True
        for e in ENG:
            c = 0
            for o in self.per[e]:
                if o.dma_sem is None and o.sig:
                    c += 1
                    o.seq = c
        with ExitStack() as es:
            esem = {e: es.enter_context(nc.semaphore(f"s_{e}")) for e in ENG}
            dsem = {k: es.enter_context(nc.semaphore(f"d_{k}")) for k in self.dma_counts}
            block = es.enter_context(nc.Block())

            def dval(d):
                if d.dma_sem in self.total_keys:
                    return 16 * self.dma_counts[d.dma_sem]
                return 16 * d.dma_cnt

            def need(o):
                w = {}
                for d in o.deps:
                    if d.dma_sem is not None:
                        key, val = ("d", d.dma_sem), dval(d)
                    else:
                        if skip(d, o):
                            continue
                        key, val = ("e", d.eng), d.seq
                    if w.get(key, 0) < val:
                        w[key] = val
                return w

            def run(ename):
                def body(eng):
                    waited = {}
                    for o in self.per[ename]:
                        for key, val in need(o).items():
                            if waited.get(key, 0) >= val:
                                continue
                            waited[key] = val
                            sem = dsem[key[1]] if key[0] == "d" else esem[key[1]]
                            eng.wait_ge(sem, val)
                        ins = o.fn(eng)
                        if o.dma_sem is not None:
                            ins.then_inc(dsem[o.dma_sem], 16)
                        elif o.sig:
                            ins.then_inc(esem[ename], 1)
                    if ename == "sp":
                        for o in final_wait_ops:
                            if o.dma_sem is not None:
                                eng.wait_ge(dsem[o.dma_sem], dval(o))
                            else:
                                eng.wait_ge(esem[o.eng], o.seq)
                return body

            block.tensor(run("pe"))
            block.scalar(run("act"))
            block.vector(run("dve"))
            block.gpsimd(run("pool"))
            block.sync(run("sp"))


class Arena:
    def __init__(self, nc, es, name, nbytes):
        self.t = es.enter_context(nc.sbuf_tensor(name, [128, nbytes // 2], BF16))
        self.cap = nbytes
        self.off = 0

    def alloc(self, shape, dt):
        n = 1
        for s_ in shape[1:]:
            n *= s_
        esz = 2 if dt == BF16 else 4
        o = self.off
        self.off += (n * esz + 63) // 64 * 64
        assert self.off <= self.cap, (self.off, self.cap)
        ap = self.t[0:shape[0], o // 2:(o + n * esz) // 2]
        if dt != BF16:
            ap = ap.bitcast(dt)
        if len(shape) > 2:
            names = " ".join(f"d{k}" for k in range(len(shape) - 1))
            ap = ap.rearrange(f"p ({names}) -> p {names}", **{f"d{k}": shape[k + 1] for k in range(len(shape) - 1)})
        return ap


def build(npass=NPASS_FULL, debug=False, stages=4):
    nc = bass.Bass("TRN2", target_bir_lowering=False)

    def din(name, shape):
        return nc.dram_tensor(name, list(shape), F32, kind="ExternalInput").ap()

    x_d = din("x", [SEQ, D])
    win_d = din("w_in", [D, 3840])
    wpa_d = din("wpa", [512, D])
    wpb_d = din("wpb", [512, D])
    wout_d = din("wout", [D, D])
    wexp_d = [din("wgp", [16, 128, 4096]), din("wup", [16, 128, 4096]), din("wdp", [16, 128, 4096])]
    wr_d = din("wr", [D, 20])
    rb_d = din("rb", [1, 20])
    gacol_d = din("gacol", [128, 8])
    gfcol_d = din("gfcol", [128, 8])
    gvcol_d = din("gvcol", [128, 4])
    gfin_d = din("gfin", [1, D])
    wst_d = din("wst", [128, 8, 128])
    wmask_d = din("wmask", [128, 8, 128])
    bst_d = din("bst", [128, 4, 128])
    sinks_d = din("sinks", [128, 4])
    bias_d = din("bias", [128, 2, 2, 4, 128])
    mask_d = din("maskc", [128, 2, 2, 4, 128])
    idn_d = din("idn", [128, 128])
    utri_d = din("utri", [128, 128])
    cst_d = din("cst", [1, 8 + 256 + 32])
    iop_d = din("iop", [128, 1])
    y_d = nc.dram_tensor("y", [SEQ, D], F32, kind="ExternalOutput").ap()
    hS_d = nc.dram_tensor("hS", [SEQ, D], F32, kind="Internal").ap()
    hn2S_d = nc.dram_tensor("hn2S", [SEQ, D], BF16, kind="Internal").ap()
    Xs_d = nc.dram_tensor("Xs", [NSLOT * 512, D], BF16, kind="Internal").ap()
    Ys_d = nc.dram_tensor("Ys", [NSLOT * 512, D], F32, kind="Internal").ap()
    wscr_d = [nc.dram_tensor(f"wscr{m}", [16 * 128, 4096], BF16, kind="Internal").ap() for m in range(3)]
    if debug:
        dbg_d = nc.dram_tensor("dbg", [PT, D], F32, kind="ExternalOutput").ap()
        dbgr_d = nc.dram_tensor("dbgr", [128, 6, NTT], F32, kind="ExternalOutput").ap()

    S = Sched()
    S.total_keys.update(["const"])

    with ExitStack() as es:
        def sb(name, shape, dt):
            return es.enter_context(nc.sbuf_tensor(name, list(shape), dt))

        ident = sb("ident", [128, 128], BF16)
        gacol = sb("gacol_s", [128, 8], F32)
        gfcol = sb("gfcol_s", [128, 8], F32)
        gvcol = sb("gvcol_s", [128, 4], F32)
        wstb = sb("wstb", [128, 8, 128], BF16)
        bst = sb("bst_s", [128, 4, 128], F32)
        sinkexp = sb("sinkexp", [128, 4], F32)
        BM = sb("BM", [128, 2, 2, 4, 128], F32)
        ones64 = sb("ones64", [128, 64], BF16)
        ones128 = sb("ones128", [128, 128], BF16)
        utri = sb("utri_s", [128, 128], BF16)
        cst = sb("cst_s", [128, 8 + 256 + 32], F32)
        iop = sb("iop_s", [128, 1], F32)
        wr = sb("wr_s", [128, 8, 20], BF16)
        rb = sb("rb_s", [128, 20], F32)
        lg = sb("lg_all", [128, NTT, 20], F32)
        posA = sb("posA", [128, NTT], I32)
        posB = sb("posB", [128, NTT], I32)
        wA = sb("wA", [128, NTT], F32)
        wB = sb("wB", [128, NTT], F32)
        idxW = sb("idxW", [128, NSLOT], I32)
        dummy = sb("bar_dummy", [128, 1], F32)
        gfin = sb("gfin_s", [128, D], F32)
        NEB = 2
        wreg = sb("wreg", [128, NEB * 12288], BF16)
        wgs = [wreg[:, b * 12288:b * 12288 + 4096].rearrange("p (c n) -> p c n", c=8) for b in range(NEB)]
        wus = [wreg[:, b * 12288 + 4096:b * 12288 + 8192].rearrange("p (c n) -> p c n", c=8) for b in range(NEB)]
        wds = [wreg[:, b * 12288 + 8192:b * 12288 + 12288].rearrange("p (c n) -> p c n", c=4) for b in range(NEB)]
        wflat = [[wreg[:, b * 12288 + m * 4096:b * 12288 + (m + 1) * 4096] for m in range(3)] for b in range(NEB)]
        wpa = wreg[:, 0:4096].rearrange("p (c n) -> p c n", c=4)
        wout = wreg[:, 4096:12288].rearrange("p (c n) -> p c n", c=8)
        wpb = wreg[:, 12288:16384].rearrange("p (c n) -> p c n", c=4)
        stg = wreg[:, 20480:24576]
        AR = Arena(nc, es, "arena", 124 * 1024)
        H = AR.alloc([128, NT, D], F32)
        hn2T = AR.alloc([128, 8, 512], BF16)
        wv = AR.alloc([128, 8, 512], BF16)
        wva = AR.alloc([128, 8, 128], BF16)
        NRING = 6
        ring = [AR.alloc([128, 8, 128], BF16) for _ in range(NRING)]
        hn2 = [AR.alloc([128, D], BF16) for _ in range(2)]
        hn = hn2[0]
        hnT = AR.alloc([128, 8, 512], BF16)
        wst = hnT[:, 0:2, :].rearrange("p a (b t) -> p (a b) t", b=4)
        wmask = hnT[:, 2:4, :].rearrange("p a (b t) -> p (a b) t", b=4)
        ss = AR.alloc([128, 8], F32)
        rstd = AR.alloc([128, 8], F32)
        uT = AR.alloc([128, 4, 512], BF16)
        qT = AR.alloc([128, 4, 512], BF16)
        kT = AR.alloc([128, (NT + 1) * 128], BF16)
        vatt = AR.alloc([128, NT + 1, 128], BF16)
        gv2 = [AR.alloc([128, 512], F32) for _ in range(2)]
        vnh2 = [AR.alloc([128, 512], BF16) for _ in range(2)]
        bnst2 = [AR.alloc([128, 6], F32) for _ in range(2)]
        mv2 = [AR.alloc([128, 2], F32) for _ in range(2)]
        rsv2 = [AR.alloc([128, 1], F32) for _ in range(2)]
        nmr2 = [AR.alloc([128, 1], F32) for _ in range(2)]
        gtmp = AR.alloc([128, 4, 128], F32)
        pT = AR.alloc([128, 2048], BF16)
        den = AR.alloc([128, 512], F32)
        aT = AR.alloc([128, 4, 512], BF16)
        bT = AR.alloc([128, 4, 512], BF16)
        sga = AR.alloc([128, 512], F32)
        sgb = AR.alloc([128, 512], F32)
        mT = AR.alloc([128, 8, 512], BF16)
        maskc = mT[:, 0:4, :].rearrange("p a (b c t) -> p (a b c) t", b=2, c=2).rearrange("p (v k h) t -> p v k h t", v=2, k=2)
        side1_end = AR.off
        AR.off = 0
        NB_ = NTT
        r_m = AR.alloc([128, NB_], F32)
        r_og = AR.alloc([128, NB_, 4], F32)
        r_eg = AR.alloc([128, NB_, 4], F32)
        r_gp = AR.alloc([128, NB_], F32)
        r_sel = AR.alloc([128, NB_, 4, 4], F32)
        r_es = AR.alloc([128, NB_, 4], F32)
        r_m1 = AR.alloc([128, NB_], F32)
        r_o1 = AR.alloc([128, NB_, 4], F32)
        r_es2 = AR.alloc([128, NB_, 4], F32)
        r_m2 = AR.alloc([128, NB_], F32)
        r_o2 = AR.alloc([128, NB_, 4], F32)
        r_d = AR.alloc([128, NB_], F32)
        r_w1 = AR.alloc([128, NB_], F32)
        M1 = AR.alloc([128, NB_, 4, 4], F32)
        M2 = AR.alloc([128, NB_, 4, 4], F32)
        Mb = AR.alloc([128, NB_ * 16], BF16)
        R1s = AR.alloc([128, NB_, 16], F32)
        Ca = AR.alloc([128, NB_, 16], F32)
        Cb = AR.alloc([128, NB_, 16], F32)
        Tts = AR.alloc([128, NB_, 16], F32)
        r_cmp = AR.alloc([128, 16, 16], F32)
        r_cmp2 = AR.alloc([128, NSLOT, 16], F32)
        r_nb = AR.alloc([128, 16], F32)
        r_ob = AR.alloc([128, 16], F32)
        r_oe = AR.alloc([128, 16], F32)
        r_pf = AR.alloc([128, NB_], F32)
        r_eid = AR.alloc([128, NSLOT], F32)
        NXIN = 12
        xin = [AR.alloc([128, D], BF16) for _ in range(NXIN)]
        xs = [AR.alloc([128, 4, D], BF16) for _ in range(2)]
        xT = [AR.alloc([128, 8, 512], BF16) for _ in range(2)]
        sgs = [AR.alloc([128, 512], F32) for _ in range(2)]
        hid = [AR.alloc([128, 4, 512], BF16) for _ in range(2)]
        NYS = 3
        ys = [AR.alloc([128, D], F32) for _ in range(NYS)]
        side2_end = AR.off
        AR.off = 0
        N4 = 6
        YA = [AR.alloc([128, D], F32) for _ in range(N4)]
        YB = [AR.alloc([128, D], F32) for _ in range(N4)]
        hb = [AR.alloc([128, D], F32) for _ in range(N4)]
        ss4 = [AR.alloc([128, 1], F32) for _ in range(N4)]
        rs4 = [AR.alloc([128, 1], F32) for _ in range(N4)]
        hn4 = AR.alloc([128, D], BF16)
        PQ = es.enter_context(nc.psum_tensor("pq", [128, 4096], F32))
        PB = [PQ[:, i * 512:(i + 1) * 512] for i in range(8)]

        T = {}

        def tk(n):
            if n not in T:
                T[n] = Tok(n)
            return T[n]

        tH = [tk(f"H{i}") for i in range(NT)]
        tPB = [tk(f"PB{i}") for i in range(8)]
        tE = [[tk(f"wg{i}"), tk(f"wu{i}"), tk(f"wd{i}")] for i in range(NEB)]
        tRing = [tk(f"ring{i}") for i in range(NRING)]

        def op(eng, fn, reads=(), writes=(), dma=None):
            return S.op(eng, fn, [tk(r) if isinstance(r, str) else r for r in reads],
                        [tk(w) if isinstance(w, str) else w for w in writes], dma)

        def cdma(eng, out, in_, w):
            op(eng, lambda e, out=out, in_=in_: e.dma_start(out=out, in_=in_), writes=[w], dma="const")

        cdma("sp", gacol[:], gacol_d, "gacol")
        cdma("sp", gfcol[:], gfcol_d, "gfcol")
        cdma("sp", gvcol[:], gvcol_d, "gvcol")
        cdma("sp", bst[:], bst_d, "bst")
        cdma("sp", sinkexp[:], sinks_d, "sinkexp")
        cdma("sp", BM[:], bias_d, "BM")
        cdma("sp", rb[:], rb_d.partition_broadcast(128), "rb")
        cdma("sp", cst[:], cst_d.partition_broadcast(128), "cst")
        cdma("sp", iop[:], iop_d, "iop")
        cdma("pool", ident[:], idn_d, "ident")
        cdma("pool", utri[:], utri_d, "utri")
        cdma("pool", wst, wst_d, "wst_a")
        cdma("pool", wmask, wmask_d, "wmask_a")
        cdma("pool", maskc, mask_d, "maskc_a")
        cdma("pool", wr[:], wr_d.rearrange("(c p) n -> p c n", p=128), "wr")
        cdma("pool", wv, win_d[:, V_OFF:V_OFF + 512].rearrange("(c p) n -> p c n", p=128), "wv")
        cdma("pool", wva, win_d[:, VA_OFF:VA_OFF + 128].rearrange("(c p) n -> p c n", p=128), "wva")
        op("dve", lambda e: e.tensor_tensor(out=wstb[:], in0=wst, in1=wmask, op=ALU.mult),
           reads=["wst_a", "wmask_a"], writes=["wstb", "hnT"])
        op("dve", lambda e: e.tensor_tensor(out=BM[:], in0=BM[:], in1=maskc, op=ALU.add),
           reads=["maskc_a"], writes=["BM", "mT"])
        op("act", lambda e: e.activation(out=sinkexp[:], in_=sinkexp[:], func=AF.Exp), reads=[], writes=["sinkexp"])
        op("dve", lambda e: e.memset(ones64[:], 1.0), writes=["ones64"])
        op("dve", lambda e: e.memset(ones128[:], 1.0), writes=["ones128"])

        out_ops = []

        def rstd_ops(n):
            op("dve", lambda e: e.tensor_scalar(out=rstd[:, 0:n], in0=ss[:, 0:n], scalar1=1.0 / D, scalar2=EPS,
                                                op0=ALU.mult, op1=ALU.add), reads=["ss"], writes=["rstd"])
            op("act", lambda e: e.sqrt(out=rstd[:, 0:n], in_=rstd[:, 0:n]), reads=[], writes=["rstd"])
            op("dve", lambda e: e.reciprocal(out=rstd[:, 0:n], in_=rstd[:, 0:n]), reads=[], writes=["rstd"])

        def norm_transpose(ti_list, gcol, dstT, dst_col0, dst_tok, after_hn=None):
            n = len(ti_list)
            for k, i in enumerate(ti_list):
                hb2 = hn2[k % 2]
                op("act", lambda e, i=i, k=k, hb2=hb2: e.activation(out=hb2, in_=H[:, i, :], func=AF.Square,
                                                                   accum_out=ss[:, k:k + 1]),
                   reads=[tH[i]], writes=[f"hn{k % 2}", "ss"])
            rstd_ops(n)
            for k, i in enumerate(ti_list):
                hb2 = hn2[k % 2]
                hnn = f"hn{k % 2}"
                op("act", lambda e, i=i, k=k, hb2=hb2: e.activation(out=hb2, in_=H[:, i, :], func=AF.Identity,
                                                                   scale=rstd[:, k:k + 1]),
                   reads=[tH[i], "rstd"], writes=[hnn])
                if after_hn is not None:
                    after_hn(i, hb2, hnn)
                bank = k % 2
                pbT = PB[bank].bitcast(BF16).rearrange("p (c t) -> p c t", c=8)
                for c in range(8):
                    op("pe", lambda e, c=c, pbT=pbT, hb2=hb2: e.transpose(out=pbT[:, c, :], in_=hb2[:, c * 128:(c + 1) * 128],
                                                                         identity=ident[:]),
                       reads=[hnn, "ident"], writes=[tPB[bank]])
                c0 = dst_col0(i)
                op("dve", lambda e, pbT=pbT, c0=c0: e.tensor_tensor(
                    out=dstT[:, :, c0:c0 + 128], in0=pbT,
                    in1=gcol[:].unsqueeze(2).to_broadcast([128, 8, 128]), op=ALU.mult),
                   reads=[tPB[bank], "gacol", "gfcol"], writes=[dst_tok(i)])

        precast = [(m, ex) for ex in range(16) for m in range(3)]
        pc_ctr = [0]

        def precast_some(n):
            if stages == 1:
                return
            for _ in range(n):
                if pc_ctr[0] >= len(precast):
                    return
                m, ex = precast[pc_ctr[0]]
                pc_ctr[0] += 1
                op("pool", lambda e, m=m, ex=ex: e.dma_start(
                    out=stg, in_=wexp_d[m][ex]),
                   writes=["stg"], dma="stg")
                op("sp", lambda e, m=m, ex=ex: e.dma_start(out=wscr_d[m][ex * 128:(ex + 1) * 128, :], in_=stg),
                   reads=["stg"], writes=["wscr"], dma="wscr")

        for ps_i in range(npass):
            tok0 = ps_i * PT
            for i in range(NT):
                op("sp", lambda e, i=i, tok0=tok0: e.dma_start(out=H[:, i, :], in_=x_d[tok0 + i * 128: tok0 + (i + 1) * 128, :]),
                   writes=[tH[i]], dma=f"H{i}")
            if ps_i == 0:
                op("pool", lambda e: e.dma_start(out=wpa, in_=wpa_d.rearrange("(c p) n -> p c n", p=128)),
                   writes=[tE[0][0]], dma="wg0")
                for a_ in range(2):
                    op("pool", lambda e, a_=a_: e.dma_start(
                        out=wpb[a_ * 64:(a_ + 1) * 64, :, :],
                        in_=wpb_d.rearrange("(a j d) n -> a d j n", a=2, j=4)[a_]),
                       writes=[tk(f"wpb{a_}")], dma=f"wpbd{a_}")
                op("pool", lambda e: e.dma_start(out=wout, in_=wout_d.rearrange("(c p) n -> p c n", p=128)),
                   writes=[tE[0][1], tE[0][2]], dma="wu0")
            if ps_i > 0:
                op("dve", lambda e: e.tensor_copy(out=kT[:, 0:128], in_=kT[:, NT * 128:(NT + 1) * 128]),
                   reads=[], writes=["kT"])
                op("dve", lambda e: e.tensor_copy(out=vatt[:, 0, :], in_=vatt[:, NT, :]), reads=[], writes=["vatt"])

            ring_ctr = [0]

            def stream_block(col0):
                r = ring_ctr[0] % NRING
                ring_ctr[0] += 1
                op("pool", lambda e, r=r, col0=col0: e.dma_start(
                    out=ring[r], in_=win_d[:, col0:col0 + 128].rearrange("(c p) n -> p c n", p=128)),
                   writes=[tRing[r]], dma=f"ring{r}")
                if ring_ctr[0] % 4 == 0:
                    precast_some(1)
                return ring[r], tRing[r]

            for st in range(NST):
                tiles = [st * 4 + k for k in range(4)]
                norm_transpose(tiles, gacol, hnT, lambda i, st=st: (i - st * 4) * 128, lambda i: tk("hnT"))

                def proj_fm(col0, bank, evac):
                    wb_, wt_ = stream_block(col0)
                    for c in range(8):
                        op("pe", lambda e, c=c, wb_=wb_, bank=bank: e.matmul(
                            PB[bank][:], lhsT=wb_[:, c, :], rhs=hnT[:, c, :], start=(c == 0), stop=(c == 7)),
                           reads=[wt_, "hnT"], writes=[tPB[bank]])
                    evac(bank)

                for j in range(4):
                    proj_fm(U_OFF + j * 128, 1 + (j % 2),
                            lambda bank, j=j: op("act", lambda e: e.activation(out=uT[:, j, :], in_=PB[bank][:],
                                                                              func=AF.Gelu_apprx_tanh),
                                                 reads=[tPB[bank]], writes=["uT"]))
                for j in range(4):
                    proj_fm(Q_OFF + j * 128, 1 + (j % 2),
                            lambda bank, j=j: op("act", lambda e: e.activation(out=qT[:, j, :], in_=PB[bank][:],
                                                                              func=AF.Identity, scale=0.125),
                                                 reads=[tPB[bank]], writes=["qT"]))
                kc0 = (1 + st * 4) * 128
                proj_fm(K_OFF, 1,
                        lambda bank, kc0=kc0: op("dve", lambda e: e.tensor_copy(out=kT[:, kc0:kc0 + 512], in_=PB[bank][:]),
                                                 reads=[tPB[bank]], writes=["kT"]))

                def t_v(i):
                    ts = (i - st * 4) * 128
                    slot = 1 + i
                    q2 = i % 2
                    gv, vnh, bnst, mv, rsv, nmr = gv2[q2], vnh2[q2], bnst2[q2], mv2[q2], rsv2[q2], nmr2[q2]
                    nm = lambda x: f"{x}{q2}"
                    for c in range(8):
                        op("pe", lambda e, c=c, ts=ts: e.matmul(PB[0], lhsT=hnT[:, c, ts:ts + 128], rhs=wv[:, c, :],
                                                                start=(c == 0), stop=(c == 7)),
                           reads=["hnT", "wv"], writes=[tPB[0]])
                    for c in range(8):
                        op("pe", lambda e, c=c, ts=ts: e.matmul(PB[1][:, 0:128], lhsT=hnT[:, c, ts:ts + 128],
                                                                rhs=wva[:, c, :], start=(c == 0), stop=(c == 7)),
                           reads=["hnT", "wva"], writes=[tPB[1]])
                    op("act", lambda e, gv=gv: e.activation(out=gv, in_=PB[0], func=AF.Gelu_apprx_tanh),
                       reads=[tPB[0]], writes=[nm("gv")])
                    op("dve", lambda e, slot=slot: e.tensor_copy(out=vatt[:, slot, :], in_=PB[1][:, 0:128]),
                       reads=[tPB[1]], writes=["vatt"])
                    op("dve", lambda e, gv=gv, bnst=bnst: e.bn_stats(out=bnst, in_=gv), reads=[nm("gv")], writes=[nm("bnst")])
                    op("dve", lambda e, mv=mv, bnst=bnst: e.bn_aggr(out=mv, in_=bnst), reads=[nm("bnst")], writes=[nm("mv")])
                    op("dve", lambda e, mv=mv, rsv=rsv: e.tensor_scalar_add(out=rsv, in0=mv[:, 1:2], scalar1=EPS),
                       reads=[nm("mv")], writes=[nm("rsv")])
                    op("act", lambda e, rsv=rsv: e.sqrt(out=rsv, in_=rsv), reads=[], writes=[nm("rsv")])
                    op("dve", lambda e, rsv=rsv: e.reciprocal(out=rsv, in_=rsv), reads=[], writes=[nm("rsv")])
                    op("dve", lambda e, mv=mv, rsv=rsv, nmr=nmr: e.tensor_scalar(out=nmr, in0=mv[:, 0:1], scalar1=rsv, scalar2=-1.0,
                                                                               op0=ALU.mult, op1=ALU.mult),
                       reads=[nm("mv"), nm("rsv")], writes=[nm("nmr")])
                    op("act", lambda e, gv=gv, vnh=vnh, rsv=rsv, nmr=nmr: e.activation(out=vnh, in_=gv, func=AF.Identity,
                                                                                     scale=rsv, bias=nmr),
                       reads=[nm("gv"), nm("rsv"), nm("nmr")], writes=[nm("vnh")])

                def t_sc(i):
                    ts = (i - st * 4) * 128
                    slot = 1 + i
                    gblk = ps_i * NT + i
                    for kh in range(2):
                        pr = slice(kh * 64, (kh + 1) * 64)
                        for vi in range(2 if gblk > 0 else 1):
                            bank = 4 + kh * 2 + vi
                            ksl = slot - vi
                            op("pe", lambda e, pr=pr, ksl=ksl, ts=ts, bank=bank: e.matmul(
                                PB[bank].rearrange("p (j t) -> p j t", j=4),
                                lhsT=kT[pr, ksl * 128:(ksl + 1) * 128], rhs=qT[pr, :, ts:ts + 128],
                                start=True, stop=True),
                               reads=["kT", "qT"], writes=[tPB[bank]])
                    scb = PQ[:, 2048:4096]
                    op("dve", lambda e, scb=scb: e.tensor_tensor(out=scb, in0=scb, in1=BM[:].rearrange("p k v j t -> p (k v j t)"),
                                                                 op=ALU.add),
                       reads=["BM"], writes=[tPB[4], tPB[5], tPB[6], tPB[7]])
                    op("act", lambda e, scb=scb: e.activation(out=pT, in_=scb, func=AF.Exp),
                       reads=[tPB[4], tPB[5], tPB[6], tPB[7]], writes=["pT"])

                def t_pv(i):
                    ts = (i - st * 4) * 128
                    slot = 1 + i
                    gblk = ps_i * NT + i
                    nv = 2 if gblk > 0 else 1
                    for (bank, use_v) in ((3, True), (0, False)):
                        for kh in range(2):
                            kw = {"tile_position": (0, 64)} if kh else {}
                            for vi in range(nv):
                                ksl = slot - vi
                                lhs = vatt[:, ksl, kh * 64:(kh + 1) * 64] if use_v else ones64[:]
                                pc = (kh * 2 + vi) * 512
                                op("pe", lambda e, bank=bank, kh=kh, vi=vi, lhs=lhs, pc=pc, kw=kw, nv=nv: e.matmul(
                                    PB[bank][kh * 64:(kh + 1) * 64, :], lhsT=lhs, rhs=pT[:, pc:pc + 512],
                                    start=(vi == 0), stop=(vi == nv - 1), **kw),
                                   reads=["vatt", "ones64", "pT"], writes=[tPB[bank]])
                    op("dve", lambda e: e.tensor_tensor(
                        out=den.rearrange("p (j t) -> p j t", j=4), in0=PB[0].rearrange("p (j t) -> p j t", j=4),
                        in1=sinkexp[:].unsqueeze(2).to_broadcast([128, 4, 128]), op=ALU.add),
                       reads=[tPB[0], "sinkexp"], writes=["den"])
                    op("dve", lambda e: e.reciprocal(out=den, in_=den), reads=[], writes=["den"])
                    op("dve", lambda e, ts=ts: e.tensor_tensor(
                        out=bT[:, :, ts:ts + 128], in0=PB[3].rearrange("p (j t) -> p j t", j=4),
                        in1=den.rearrange("p (j t) -> p j t", j=4), op=ALU.mult),
                       reads=[tPB[3], "den"], writes=["bT"])

                def t_sp(i):
                    ts = (i - st * 4) * 128
                    q2 = i % 2
                    vnh = vnh2[q2]
                    pbs = PB[2].rearrange("p (j t) -> p j t", j=4)
                    for g in range(8):
                        lo = (g % 2) * 64
                        kw = {"tile_position": (0, 64)} if g % 2 else {}
                        op("pe", lambda e, g=g, lo=lo, kw=kw, vnh=vnh: e.matmul(
                            pbs[lo:lo + 64, g // 2, :], lhsT=vnh[:, g * 64:(g + 1) * 64], rhs=wstb[:, g, :],
                            start=True, stop=True, **kw),
                           reads=[f"vnh{q2}", "wstb"], writes=[tPB[2]])
                    op("dve", lambda e: e.tensor_tensor(out=gtmp, in0=pbs,
                                                        in1=gvcol[:].unsqueeze(2).to_broadcast([128, 4, 128]), op=ALU.mult),
                       reads=[tPB[2], "gvcol"], writes=["gtmp"])
                    op("dve", lambda e: e.tensor_tensor(out=gtmp, in0=gtmp, in1=bst[:], op=ALU.add),
                       reads=["bst"], writes=["gtmp"])
                    op("dve", lambda e, ts=ts: e.tensor_tensor(out=aT[:, :, ts:ts + 128], in0=gtmp,
                                                               in1=uT[:, :, ts:ts + 128], op=ALU.mult),
                       reads=["gtmp", "uT"], writes=["aT"])

                t_v(tiles[0])
                t_sc(tiles[0])
                for k in range(1, 4):
                    t_v(tiles[k])
                    t_pv(tiles[k - 1])
                    t_sc(tiles[k])
                    t_sp(tiles[k - 1])
                t_pv(tiles[3])
                t_sp(tiles[3])

                for j in range(8):
                    b0 = (j % 2) * 4
                    wga, tga = stream_block(GA_OFF + j * 128)
                    for c in range(8):
                        op("pe", lambda e, c=c, wga=wga, b0=b0: e.matmul(PB[b0], lhsT=wga[:, c, :], rhs=hnT[:, c, :],
                                                                         start=(c == 0), stop=(c == 7)),
                           reads=[tga, "hnT"], writes=[tPB[b0]])
                    wgb, tgb = stream_block(GB_OFF + j * 128)
                    for c in range(8):
                        op("pe", lambda e, c=c, wgb=wgb, b0=b0: e.matmul(PB[b0 + 1], lhsT=wgb[:, c, :], rhs=hnT[:, c, :],
                                                                         start=(c == 0), stop=(c == 7)),
                           reads=[tgb, "hnT"], writes=[tPB[b0 + 1]])
                    for c in range(4):
                        op("pe", lambda e, c=c, j=j, b0=b0: e.matmul(PB[b0 + 2], lhsT=wpa[:, c, j * 128:(j + 1) * 128], rhs=aT[:, c, :],
                                                                     start=(c == 0), stop=(c == 3)),
                           reads=[tE[0][0], "aT"], writes=[tPB[b0 + 2]])
                    for c in range(4):
                        op("pe", lambda e, c=c, j=j, b0=b0: e.matmul(PB[b0 + 3], lhsT=wpb[:, c, j * 128:(j + 1) * 128], rhs=bT[:, c, :],
                                                                     start=(c == 0), stop=(c == 3)),
                           reads=["wpb0", "wpb1", "bT"], writes=[tPB[b0 + 3]])
                    op("act", lambda e, b0=b0: e.activation(out=sga, in_=PB[b0], func=AF.Sigmoid),
                       reads=[tPB[b0]], writes=["sga"])
                    op("act", lambda e, b0=b0: e.activation(out=sgb, in_=PB[b0 + 1], func=AF.Sigmoid),
                       reads=[tPB[b0 + 1]], writes=["sgb"])
                    op("dve", lambda e, b0=b0: e.tensor_tensor(out=sga, in0=PB[b0 + 2], in1=sga, op=ALU.mult),
                       reads=[tPB[b0 + 2]], writes=["sga"])
                    op("dve", lambda e, b0=b0: e.tensor_tensor(out=sgb, in0=PB[b0 + 3], in1=sgb, op=ALU.mult),
                       reads=[tPB[b0 + 3]], writes=["sgb"])
                    op("dve", lambda e, j=j: e.tensor_tensor(out=mT[:, j, :], in0=sga, in1=sgb, op=ALU.add),
                       reads=["sga", "sgb"], writes=["mT"])
                for i in tiles:
                    ts = (i - st * 4) * 128
                    for hf in range(2):
                        bank = 1 + hf
                        for j in range(8):
                            op("pe", lambda e, j=j, ts=ts, hf=hf, bank=bank: e.matmul(
                                PB[bank][:], lhsT=mT[:, j, ts:ts + 128], rhs=wout[:, j, hf * 512:(hf + 1) * 512],
                                start=(j == 0), stop=(j == 7)),
                               reads=["mT", tE[0][1], tE[0][2]], writes=[tPB[bank]])
                        op("dve", lambda e, i=i, hf=hf, bank=bank: e.tensor_tensor(
                            out=H[:, i, hf * 512:(hf + 1) * 512], in0=PB[bank][:], in1=H[:, i, hf * 512:(hf + 1) * 512],
                            op=ALU.add),
                           reads=[tPB[bank]], writes=[tH[i]])
                    op("sp", lambda e, i=i, tok0=tok0: e.dma_start(out=hS_d[tok0 + i * 128: tok0 + (i + 1) * 128, :],
                                                                   in_=H[:, i, :]),
                       reads=[tH[i]], writes=["hS"], dma=f"H{i}")

                def spill_hn2(i, hb2, hnn, tok0=tok0):
                    op("sp", lambda e, i=i, tok0=tok0, hb2=hb2: e.dma_start(out=hn2S_d[tok0 + i * 128: tok0 + (i + 1) * 128, :], in_=hb2),
                       reads=[hnn], writes=["hn2S"], dma="hn2S" + hnn)
                norm_transpose(tiles, gfcol, hn2T, lambda i, st=st: (i - st * 4) * 128, lambda i: tk("hn2T"),
                               after_hn=spill_hn2)
                for i in tiles:
                    gi = ps_i * NT + i
                    ts = (i - st * 4) * 128
                    for c in range(8):
                        op("pe", lambda e, c=c, ts=ts: e.matmul(PB[7][:, 0:20], lhsT=hn2T[:, c, ts:ts + 128], rhs=wr[:, c, :],
                                                                start=(c == 0), stop=(c == 7)),
                           reads=["hn2T", "wr"], writes=[tPB[7]])
                    op("dve", lambda e, gi=gi: e.tensor_tensor(out=lg[:, gi, :], in0=PB[7][:, 0:20], in1=rb[:], op=ALU.add),
                       reads=[tPB[7], "rb"], writes=["lg"])

            if debug and ps_i == 0:
                for i in range(NT):
                    out_ops.append(op("sp", lambda e, i=i: e.dma_start(out=dbg_d[i * 128:(i + 1) * 128, :], in_=H[:, i, :]),
                                      reads=[tH[i]], dma=f"H{i}"))
        precast_some(100)

        S.barrier("dve", lambda e: e.memset(dummy[:], 0.0))
        NTK = npass * NT

        cdma2 = op("sp", lambda e: e.dma_start(out=gfin[:], in_=gfin_d.partition_broadcast(128)), writes=["gfin"], dma="gfin")

        gl = lg[:, :, 0:4]
        el = lg[:, :, 4:20].rearrange("p t (g x) -> p t g x", g=4)
        bc3 = lambda a: a.unsqueeze(2).to_broadcast([128, NTT, 4])
        if npass < NPASS_FULL:
            op("dve", lambda e: e.memset(lg[:, NTK:, :], 0.0), writes=["lg"])
        dv = lambda fn, r, w: op("dve", fn, reads=r, writes=w)
        dv(lambda e: e.tensor_reduce(out=r_m, in_=gl, axis=AX.X, op=ALU.max), ["lg"], ["r_m"])
        dv(lambda e: e.tensor_tensor(out=r_og, in0=gl, in1=bc3(r_m), op=ALU.is_equal), ["lg", "r_m"], ["r_og"])
        dv(lambda e: e.tensor_tensor(out=r_eg, in0=gl, in1=bc3(r_m), op=ALU.subtract), ["lg", "r_m"], ["r_eg"])
        op("act", lambda e: e.activation(out=r_eg, in_=r_eg, func=AF.Exp), reads=[], writes=["r_eg"])
        dv(lambda e: e.tensor_reduce(out=r_gp, in_=r_eg, axis=AX.X, op=ALU.add), ["r_eg"], ["r_gp"])
        dv(lambda e: e.reciprocal(out=r_gp, in_=r_gp), [], ["r_gp"])
        dv(lambda e: e.tensor_tensor(out=r_sel, in0=el, in1=r_og.unsqueeze(3).to_broadcast([128, NTT, 4, 4]), op=ALU.mult),
           ["lg", "r_og"], ["r_sel"])
        dv(lambda e: e.tensor_reduce(out=r_es, in_=r_sel.rearrange("p t g x -> p t x g"), axis=AX.X, op=ALU.add),
           ["r_sel"], ["r_es"])
        dv(lambda e: e.tensor_reduce(out=r_m1, in_=r_es, axis=AX.X, op=ALU.max), ["r_es"], ["r_m1"])
        dv(lambda e: e.tensor_tensor(out=r_o1, in0=r_es, in1=bc3(r_m1), op=ALU.is_equal), ["r_es", "r_m1"], ["r_o1"])
        dv(lambda e: e.scalar_tensor_tensor(out=r_es2, in0=r_o1, scalar=-1e9, in1=r_es, op0=ALU.mult, op1=ALU.add),
           ["r_o1", "r_es"], ["r_es2"])
        dv(lambda e: e.tensor_reduce(out=r_m2, in_=r_es2, axis=AX.X, op=ALU.max), ["r_es2"], ["r_m2"])
        dv(lambda e: e.tensor_tensor(out=r_o2, in0=r_es2, in1=bc3(r_m2), op=ALU.is_equal), ["r_es2", "r_m2"], ["r_o2"])
        dv(lambda e: e.tensor_tensor(out=r_d, in0=r_m2, in1=r_m1, op=ALU.subtract), ["r_m1", "r_m2"], ["r_d"])
        op("act", lambda e: e.activation(out=r_d, in_=r_d, func=AF.Exp), reads=[], writes=["r_d"])
        dv(lambda e: e.tensor_scalar_add(out=r_w1, in0=r_d, scalar1=1.0), ["r_d"], ["r_w1"])
        dv(lambda e: e.reciprocal(out=r_w1, in_=r_w1), [], ["r_w1"])
        dv(lambda e: e.tensor_tensor(out=wB[:], in0=r_d, in1=r_w1, op=ALU.mult), ["r_d", "r_w1"], ["wB"])
        dv(lambda e: e.tensor_tensor(out=wA[:], in0=r_w1, in1=r_gp, op=ALU.mult), ["r_w1", "r_gp"], ["wA"])
        dv(lambda e: e.tensor_tensor(out=wB[:], in0=wB[:], in1=r_gp, op=ALU.mult), ["r_gp"], ["wB"])
        bg = lambda a: a.unsqueeze(3).to_broadcast([128, NTT, 4, 4])
        bx = lambda a: a.unsqueeze(2).to_broadcast([128, NTT, 4, 4])
        dv(lambda e: e.tensor_tensor(out=M1, in0=bg(r_og), in1=bx(r_o1), op=ALU.mult), ["r_og", "r_o1"], ["M1"])
        dv(lambda e: e.tensor_tensor(out=M2, in0=bg(r_og), in1=bx(r_o2), op=ALU.mult), ["r_og", "r_o2"], ["M2"])
        M1f = M1.rearrange("p t g x -> p t (g x)")
        M2f = M2.rearrange("p t g x -> p t (g x)")
        dv(lambda e: e.tensor_tensor(out=Mb.rearrange("p (t n) -> p t n", n=16), in0=M1f, in1=M2f, op=ALU.add),
           ["M1", "M2"], ["Mb"])
        if npass < NPASS_FULL:
            dv(lambda e: e.memset(Mb[:, NTK * 16:], 0.0), [], ["Mb"])
        op("pe", lambda e: e.matmul(PB[0][:], lhsT=utri[:], rhs=Mb, start=True, stop=True),
           reads=["utri", "Mb"], writes=[tPB[0]])
        op("pe", lambda e: e.matmul(PB[1][:], lhsT=ones128[:], rhs=Mb, start=True, stop=True),
           reads=["ones128", "Mb"], writes=[tPB[1]])
        dv(lambda e: e.tensor_copy(out=R1s.rearrange("p t n -> p (t n)"), in_=PB[0][:]), [tPB[0]], ["R1s"])
        dv(lambda e: e.tensor_copy(out=Tts.rearrange("p t n -> p (t n)"), in_=PB[1][:]), [tPB[1]], ["Tts"])
        dv(lambda e: e.tensor_copy(out=Ca, in_=Tts), ["Tts"], ["Ca"])
        cur, nxt, cn, nn = Ca, Cb, "Ca", "Cb"
        sh = 1
        while sh < NTT:
            dv(lambda e, cur=cur, nxt=nxt, sh=sh: e.tensor_copy(out=nxt[:, 0:sh, :], in_=cur[:, 0:sh, :]), [cn], [nn])
            dv(lambda e, cur=cur, nxt=nxt, sh=sh: e.tensor_tensor(out=nxt[:, sh:, :], in0=cur[:, sh:, :],
                                                                  in1=cur[:, 0:NTT - sh, :], op=ALU.add), [cn], [nn])
            cur, nxt, cn, nn = nxt, cur, nn, cn
            sh *= 2
        Cin, cin_n, Cex, cex_n = cur, cn, nxt, nn
        dv(lambda e: e.tensor_tensor(out=Cex, in0=Cin, in1=Tts, op=ALU.subtract), [cin_n, "Tts"], [cex_n])
        ntot = Cin[:, NTT - 1, :]
        thr = cst[:, 0:8]
        tri = cst[:, 8:264].rearrange("p (a b) -> p a b", a=16)
        sio = cst[:, 264:264 + NSLOT]
        cmp8 = r_cmp.rearrange("p a b -> p (a b)")[:, 0:128].rearrange("p (a b) -> p a b", b=8)
        dv(lambda e: e.tensor_tensor(out=cmp8, in0=ntot.unsqueeze(2).to_broadcast([128, 16, 8]),
                                     in1=thr.unsqueeze(1).to_broadcast([128, 16, 8]), op=ALU.is_gt),
           [cin_n, "cst"], ["r_cmp"])
        dv(lambda e: e.tensor_reduce(out=r_nb, in_=cmp8, axis=AX.X, op=ALU.add), ["r_cmp"], ["r_nb"])
        dv(lambda e: e.tensor_tensor(out=r_cmp, in0=r_nb.unsqueeze(1).to_broadcast([128, 16, 16]), in1=tri, op=ALU.mult),
           ["r_nb", "cst"], ["r_cmp"])
        dv(lambda e: e.tensor_reduce(out=r_ob, in_=r_cmp, axis=AX.X, op=ALU.add), ["r_cmp"], ["r_ob"])
        dv(lambda e: e.tensor_tensor(out=r_oe, in0=r_ob, in1=r_nb, op=ALU.add), ["r_ob", "r_nb"], ["r_oe"])
        dv(lambda e: e.tensor_tensor(out=R1s, in0=R1s, in1=Cex, op=ALU.add), [cex_n], ["R1s"])
        dv(lambda e: e.scalar_tensor_tensor(out=R1s, in0=r_ob.unsqueeze(1).to_broadcast([128, NTT, 16]), scalar=512.0,
                                            in1=R1s, op0=ALU.mult, op1=ALU.add), ["r_ob"], ["R1s"])
        for (Mf, mn, pos_i, pn) in ((M1f, "M1", posA, "posA"), (M2f, "M2", posB, "posB")):
            dv(lambda e, Mf=Mf: e.tensor_tensor(out=Mf, in0=Mf, in1=R1s, op=ALU.mult), ["R1s"], [mn])
            dv(lambda e, Mf=Mf: e.tensor_reduce(out=r_pf, in_=Mf, axis=AX.X, op=ALU.add), [mn], ["r_pf"])
            dv(lambda e: e.tensor_scalar(out=r_pf, in0=r_pf, scalar1=0.0, scalar2=float(NSLOT * 512 - 1),
                                         op0=ALU.max, op1=ALU.min), [], ["r_pf"])
            dv(lambda e, pos_i=pos_i: e.tensor_copy(out=pos_i[:], in_=r_pf), ["r_pf"], [pn])
        dv(lambda e: e.tensor_tensor(out=r_cmp2, in0=sio.unsqueeze(2).to_broadcast([128, NSLOT, 16]),
                                     in1=r_oe.unsqueeze(1).to_broadcast([128, NSLOT, 16]), op=ALU.is_ge),
           ["cst", "r_oe"], ["r_cmp2"])
        dv(lambda e: e.tensor_reduce(out=r_eid, in_=r_cmp2, axis=AX.X, op=ALU.add), ["r_cmp2"], ["r_eid"])
        dv(lambda e: e.tensor_scalar(out=r_eid, in0=r_eid, scalar1=15.0, scalar2=128.0, op0=ALU.min, op1=ALU.mult),
           [], ["r_eid"])
        dv(lambda e: e.tensor_tensor(out=r_eid, in0=r_eid, in1=iop[:].to_broadcast([128, NSLOT]), op=ALU.add),
           ["iop"], ["r_eid"])
        dv(lambda e: e.tensor_scalar(out=r_eid, in0=r_eid, scalar1=0.0, scalar2=2047.0, op0=ALU.max, op1=ALU.min),
           [], ["r_eid"])
        dv(lambda e: e.tensor_copy(out=idxW[:], in_=r_eid), ["r_eid"], ["idxW"])
        if debug:
            dr = sb("dbgr_s", [128, 6, NTT], F32)
            dv(lambda e: e.tensor_copy(out=dr[:, 0, :], in_=posA[:]), ["posA"], ["dr"])
            dv(lambda e: e.tensor_copy(out=dr[:, 1, :], in_=posB[:]), ["posB"], ["dr"])
            dv(lambda e: e.tensor_copy(out=dr[:, 2, :], in_=wA[:]), ["wA"], ["dr"])
            dv(lambda e: e.tensor_copy(out=dr[:, 3, :], in_=wB[:]), ["wB"], ["dr"])
            dv(lambda e: e.tensor_copy(out=dr[:, 4, 0:NSLOT], in_=idxW[:]), ["idxW"], ["dr"])
            dv(lambda e: e.tensor_copy(out=dr[:, 5, 0:16], in_=ntot), [cin_n], ["dr"])
            out_ops.append(op("sp", lambda e: e.dma_start(out=dbgr_d, in_=dr[:]), reads=["dr"], dma="dbgr"))

        for gi in range(NTK if stages >= 3 else 0):
            xb = xin[gi % NXIN]
            xn = f"xin{gi % NXIN}"
            op("sp", lambda e, gi=gi, xb=xb: e.dma_start(out=xb, in_=hn2S_d[gi * 128:(gi + 1) * 128, :]),
               reads=["hn2S"], writes=[xn], dma=xn)
            for (pos_i, pn) in ((posA, "posA"), (posB, "posB")):
                op("pool", lambda e, gi=gi, xb=xb, pos_i=pos_i: e.indirect_dma_start(
                    out=Xs_d, out_offset=bass.IndirectOffsetOnAxis(pos_i[:, gi:gi + 1], 0), in_=xb, in_offset=None),
                   reads=[xn, pn], writes=[f"Xs{gi % NXIN}"], dma=f"scat{gi % NXIN}")

        NS3 = NSLOT if stages >= 3 else 0

        def slot_loads(s):
            b = s % NEB
            for m in range(3):
                op("pool", lambda e, s=s, b=b, m=m: e.indirect_dma_start(
                    out=wflat[b][m], out_offset=None, in_=wscr_d[m],
                    in_offset=bass.IndirectOffsetOnAxis(idxW[:, s:s + 1], 0)),
                   reads=["idxW", "wscr"], writes=[tE[b][m]], dma=f"w{'gud'[m]}{b}")
            xb = xs[s % 2]
            xn = f"xs{s % 2}"
            op("sp", lambda e, s=s, xb=xb: e.dma_start(out=xb, in_=Xs_d[s * 512:(s + 1) * 512, :].rearrange("(j p) d -> p j d", p=128)),
               reads=[f"Xs{q}" for q in range(NXIN)], writes=[xn], dma=xn)

        def slot_transposes(s):
            xb = xs[s % 2]
            xn = f"xs{s % 2}"
            xt = xT[s % 2]
            xtn = f"xT{s % 2}"
            for j in range(4):
                bank = j % 2
                pbT = PB[bank][:].bitcast(BF16).rearrange("p (c t) -> p c t", c=8)
                for c in range(8):
                    op("pe", lambda e, c=c, j=j, pbT=pbT, xb=xb: e.transpose(out=pbT[:, c, :], in_=xb[:, j, c * 128:(c + 1) * 128],
                                                                            identity=ident[:]),
                       reads=[xn, "ident"], writes=[tPB[bank]])
                op("dve", lambda e, j=j, pbT=pbT, xt=xt: e.tensor_tensor(
                    out=xt[:, :, j * 128:(j + 1) * 128], in0=pbT,
                    in1=gfcol[:].unsqueeze(2).to_broadcast([128, 8, 128]), op=ALU.mult),
                   reads=[tPB[bank], "gfcol"], writes=[xtn])

        def slot_gateup(s):
            b = s % NEB
            xt = xT[s % 2]
            xtn = f"xT{s % 2}"
            hb_ = s % 2
            for j in range(4):
                bg_, bu_ = 2 + (j % 2) * 2, 3 + (j % 2) * 2
                for c in range(8):
                    op("pe", lambda e, c=c, j=j, b=b, bg_=bg_, xt=xt: e.matmul(
                        PB[bg_][:], lhsT=wgs[b][:, c, j * 128:(j + 1) * 128], rhs=xt[:, c, :],
                        start=(c == 0), stop=(c == 7)),
                       reads=[tE[b][0], xtn], writes=[tPB[bg_]])
                for c in range(8):
                    op("pe", lambda e, c=c, j=j, b=b, bu_=bu_, xt=xt: e.matmul(
                        PB[bu_][:], lhsT=wus[b][:, c, j * 128:(j + 1) * 128], rhs=xt[:, c, :],
                        start=(c == 0), stop=(c == 7)),
                       reads=[tE[b][1], xtn], writes=[tPB[bu_]])
                op("act", lambda e, j=j, bg_=bg_: e.activation(out=sgs[j % 2], in_=PB[bg_][:], func=AF.Silu),
                   reads=[tPB[bg_]], writes=[f"sgs{j % 2}"])
                op("dve", lambda e, j=j, bu_=bu_, hb_=hb_: e.tensor_tensor(out=hid[hb_][:, j, :], in0=PB[bu_][:],
                                                                         in1=sgs[j % 2], op=ALU.mult),
                   reads=[tPB[bu_], f"sgs{j % 2}"], writes=[f"hid{hb_}"])

        def slot_down(s):
            b = s % NEB
            hb_ = s % 2
            for tl in range(4):
                yi = (s * 4 + tl) % NYS
                yb = ys[yi]
                yn = f"ys{yi}"
                for hf in range(2):
                    bank = 6 + hf
                    for j in range(4):
                        op("pe", lambda e, j=j, tl=tl, hf=hf, b=b, hb_=hb_, bank=bank: e.matmul(
                            PB[bank][:], lhsT=hid[hb_][:, j, tl * 128:(tl + 1) * 128],
                            rhs=wds[b][:, j, hf * 512:(hf + 1) * 512], start=(j == 0), stop=(j == 3)),
                           reads=[f"hid{hb_}", tE[b][2]], writes=[tPB[bank]])
                    op("act", lambda e, hf=hf, bank=bank, yb=yb: e.copy(out=yb[:, hf * 512:(hf + 1) * 512], in_=PB[bank][:]),
                       reads=[tPB[bank]], writes=[yn])
                op("sp", lambda e, s=s, tl=tl, yb=yb: e.dma_start(out=Ys_d[s * 512 + tl * 128: s * 512 + (tl + 1) * 128, :], in_=yb),
                   reads=[yn], writes=[f"Ys{yi}"], dma=f"ysd{yi}")

        if NS3:
            slot_loads(0)
            slot_transposes(0)
            slot_loads(1)
        for s in range(NS3):
            slot_gateup(s)
            if s + 1 < NS3:
                slot_transposes(s + 1)
            slot_down(s)
            if s + 2 < NS3:
                slot_loads(s + 2)

        if stages >= 4:
            S.barrier("dve", lambda e: e.memset(dummy[:], 0.0))
        NT4 = NTK if stages >= 4 else 0

        def loads4(gi):
            k2 = gi % N4
            op("sp", lambda e, gi=gi, k2=k2: e.dma_start(out=hb[k2], in_=hS_d[gi * 128:(gi + 1) * 128, :]),
               reads=["hS"], writes=[f"hb{k2}"], dma=f"hb{k2}")
            for (Yb, ynm, pos_i, pn) in ((YA, "YA", posA, "posA"), (YB, "YB", posB, "posB")):
                op("pool", lambda e, gi=gi, k2=k2, Yb=Yb, pos_i=pos_i: e.indirect_dma_start(
                    out=Yb[k2], out_offset=None, in_=Ys_d, in_offset=bass.IndirectOffsetOnAxis(pos_i[:, gi:gi + 1], 0)),
                   reads=[pn], writes=[f"{ynm}{k2}"], dma=f"{ynm}{k2}")

        for gi in range(min(N4, NT4)):
            loads4(gi)
        for gi in range(NT4):
            k2 = gi % N4
            op("dve", lambda e, gi=gi, k2=k2: e.scalar_tensor_tensor(out=hb[k2], in0=YA[k2], scalar=wA[:, gi:gi + 1], in1=hb[k2],
                                                                     op0=ALU.mult, op1=ALU.add),
               reads=[f"YA{k2}", "wA"], writes=[f"hb{k2}"])
            op("dve", lambda e, gi=gi, k2=k2: e.scalar_tensor_tensor(out=hb[k2], in0=YB[k2], scalar=wB[:, gi:gi + 1], in1=hb[k2],
                                                                     op0=ALU.mult, op1=ALU.add),
               reads=[f"YB{k2}", "wB"], writes=[f"hb{k2}"])
            op("act", lambda e, k2=k2: e.activation(out=hn4, in_=hb[k2], func=AF.Square, accum_out=ss4[k2]),
               reads=[f"hb{k2}"], writes=["hn4", f"ss4{k2}"])
            op("dve", lambda e, k2=k2: e.tensor_scalar(out=rs4[k2], in0=ss4[k2], scalar1=1.0 / D, scalar2=EPS, op0=ALU.mult, op1=ALU.add),
               reads=[f"ss4{k2}"], writes=[f"rs4{k2}"])
            op("act", lambda e, k2=k2: e.sqrt(out=rs4[k2], in_=rs4[k2]), reads=[], writes=[f"rs4{k2}"])
            op("dve", lambda e, k2=k2: e.reciprocal(out=rs4[k2], in_=rs4[k2]), reads=[], writes=[f"rs4{k2}"])
            op("dve", lambda e, k2=k2: e.scalar_tensor_tensor(out=hb[k2], in0=hb[k2], scalar=rs4[k2], in1=gfin[:],
                                                              op0=ALU.mult, op1=ALU.mult),
               reads=[f"rs4{k2}", "gfin"], writes=[f"hb{k2}"])
            out_ops.append(op("sp", lambda e, gi=gi, k2=k2: e.dma_start(out=y_d[gi * 128:(gi + 1) * 128, :], in_=hb[k2]),
                              reads=[f"hb{k2}"], dma=f"hb{k2}"))
            if gi + N4 < NT4:
                loads4(gi + N4)

        S.emit(nc, final_wait_ops=out_ops)
    return nc


def _t5_bucket(dist):
    dist = np.asarray(dist, dtype=np.int64)
    nf = np.maximum(dist, 1).astype(np.float32)
    large = 16 + (np.log(nf / np.float32(16)) / np.float32(math.log(128 / 16)) * np.float32(16)).astype(np.int32)
    large = np.minimum(large, 31)
    return np.where(dist < 16, dist, large)


def prep_shared(inp):
    f = lambda a: np.ascontiguousarray(np.asarray(a, dtype=np.float32))
    w_in = f(inp["w_in"])[0]
    perm = np.arange(3840)
    qcols = []
    for j in range(4):
        qcols += list(range(1024 + j * 64, 1024 + (j + 1) * 64))
        qcols += list(range(1024 + (4 + j) * 64, 1024 + (5 + j) * 64))
    perm[1024:1536] = np.array(qcols)
    w_in = np.ascontiguousarray(w_in[:, perm])
    wr = np.concatenate([f(inp["router_group_w"])[0]] + [f(inp["router_expert_w"])[0, g] for g in range(4)], axis=1)
    rb = np.concatenate([f(inp["router_group_b"])[0], f(inp["router_expert_b"])[0].reshape(-1)])[None, :]
    col = lambda v, c: np.ascontiguousarray(f(v).reshape(c, 128).T)
    ws = f(inp["gm_w_spatial"])[0]
    wst = np.ascontiguousarray(ws.transpose(2, 0, 1))
    s_i = np.arange(128)[:, None, None]
    t_i = np.arange(128)[None, None, :]
    wmask = np.broadcast_to((s_i <= t_i), (128, 8, 128)).astype(np.float32)
    bs = f(inp["gm_b_spatial"])[0]
    bst = np.zeros((128, 4, 128), np.float32)
    for g in range(8):
        bst[(g % 2) * 64:(g % 2) * 64 + 64, g // 2, :] = bs[g][None, :]
    rel = f(inp["rel_bias"])
    s_ = np.arange(128)[:, None]
    q_ = np.arange(128)[None, :]
    d_own = q_ - s_
    d_prev = q_ + 128 - s_
    bias = np.zeros((128, 2, 2, 4, 128), np.float32)
    maskc = np.zeros((128, 2, 2, 4, 128), np.float32)
    b_own = _t5_bucket(np.clip(d_own, 0, 127))
    b_prev = _t5_bucket(np.clip(d_prev, 0, 127))
    for kh in range(2):
        for h4 in range(4):
            h = kh * 4 + h4
            bias[:, kh, 0, h4, :] = rel[b_own, h]
            bias[:, kh, 1, h4, :] = rel[b_prev, h]
            maskc[:, kh, 0, h4, :] = np.where(d_own >= 0, 0.0, NEG)
            maskc[:, kh, 1, h4, :] = np.where(d_prev < 128, 0.0, NEG)
    def pmaj(w, c):
        e_, r_, n_ = w.shape
        return np.ascontiguousarray(w.reshape(e_, c, 128, n_).transpose(0, 2, 1, 3).reshape(e_, 128, c * n_))

    return {
        "w_in": w_in,
        "wpa": f(inp["w_proj_a"])[0], "wpb": f(inp["w_proj_b"])[0], "wout": f(inp["w_out"])[0],
        "wgp": pmaj(f(inp["expert_w_gate"])[0], 8), "wup": pmaj(f(inp["expert_w_up"])[0], 8),
        "wdp": pmaj(f(inp["expert_w_down"])[0], 4),
        "utri": np.triu(np.ones((128, 128), np.float32), 1),
        "cst": np.concatenate([np.arange(8, dtype=np.float32) * 512.0,
                               np.tril(np.ones((16, 16), np.float32), -1).reshape(-1),
                               np.arange(32, dtype=np.float32)])[None, :],
        "iop": np.arange(128, dtype=np.float32)[:, None],
        "wr": np.ascontiguousarray(wr), "rb": np.ascontiguousarray(rb),
        "gacol": col(inp["attn_norm_g"], 8), "gfcol": col(inp["ffn_norm_g"], 8), "gvcol": col(inp["gm_v_norm_g"], 4),
        "gfin": f(inp["final_norm_g"]).reshape(1, D),
        "wst": wst, "wmask": wmask, "bst": bst,
        "sinks": np.ascontiguousarray(np.repeat(f(inp["attn_sinks"]).reshape(2, 4), 64, axis=0)),
        "bias": bias, "maskc": maskc,
        "idn": np.eye(128, dtype=np.float32),
    }


def kernel(**inputs):
    shared = prep_shared(inputs)
    x = np.asarray(inputs["x"], dtype=np.float32)
    nc = build()
    in_maps = []
    for c in range(NCORES):
        m = dict(shared)
        m["x"] = np.ascontiguousarray(x[c])
        in_maps.append(m)
    res = run_bass_kernel_spmd(nc, in_maps, core_ids=list(range(NCORES)))
    return np.stack([np.asarray(r["y"], dtype=np.float32) for r in res.results], axis=0)
```

```python
import math
from contextlib import ExitStack

import numpy as np
import concourse.bass as bass
import concourse.mybir as mybir
from concourse.bass_utils import run_bass_kernel_spmd

F32 = mybir.dt.float32
BF16 = mybir.dt.bfloat16
AF = mybir.ActivationFunctionType
ALU = mybir.AluOpType
AX = mybir.AxisListType

SEQ = 4096
D = 1024
NCORES = 8
PT = 1024
NT = PT // 128
NST = PT // 512
NPASS_FULL = SEQ // PT
EPS = 1e-6
NEG = -30000.0
NSLOT = 31
NTT = SEQ // 128
I32 = mybir.dt.int32

U_OFF, V_OFF, Q_OFF, K_OFF, VA_OFF, GA_OFF, GB_OFF = 0, 512, 1024, 1536, 1664, 1792, 2816

ENG = ("pe", "act", "dve", "pool", "sp")


class Tok:
    __slots__ = ("name", "last_w", "readers")

    def __init__(self, name):
        self.name = name
        self.last_w = None
        self.readers = []


class Op:
    __slots__ = ("eng", "fn", "deps", "sig", "dma_sem", "dma_cnt", "seq")

    def __init__(self, eng, fn):
        self.eng = eng
        self.fn = fn
        self.deps = set()
        self.sig = False
        self.dma_sem = None
        self.dma_cnt = 0
        self.seq = 0


class Sched:
    def __init__(self):
        self.ops = []
        self.per = {e: [] for e in ENG}
        self.dma_counts = {}
        self.total_keys = set()
        self.epoch_op = None
        self.last_dma = {}

    def op(self, eng, fn, reads=(), writes=(), dma=None):
        o = Op(eng, fn)
        for t in list(reads) + list(writes):
            if t.last_w is not None:
                o.deps.add(t.last_w)
        for t in writes:
            for r in t.readers:
                o.deps.add(r)
        if self.epoch_op is not None:
            o.deps.add(self.epoch_op)
        o.deps.discard(o)
        for t in reads:
            t.readers.append(o)
        for t in writes:
            t.last_w = o
            t.readers = []
        if dma is not None:
            o.dma_sem = dma
            self.dma_counts[dma] = self.dma_counts.get(dma, 0) + 1
            o.dma_cnt = self.dma_counts[dma]
            self.last_dma[dma] = o
        self.ops.append(o)
        self.per[eng].append(o)
        return o

    def barrier(self, eng, fn):
        o = Op(eng, fn)
        for e in ENG:
            seen_c = False
            for p in reversed(self.per[e]):
                if p.dma_sem is None:
                    o.deps.add(p)
                    break
        for k, p in self.last_dma.items():
            o.deps.add(p)
        if self.epoch_op is not None:
            o.deps.add(self.epoch_op)
        self.ops.append(o)
        self.per[eng].append(o)
        self.epoch_op = o
        return o

    def emit(self, nc, final_wait_ops=()):
        def skip(d, o):
            return d.dma_sem is None and o.dma_sem is None and d.eng == "pe" and o.eng == "pe"

        for o in self.ops:
            for d in o.deps:
                if d.dma_sem is None and not skip(d, o):
                    d.sig = True
        for o in final_wait_ops:
            if o.dma_sem is None:
                o.sig = True
        for e in ENG:
            c = 0
            for o in self.per[e]:
                if o.dma_sem is None and o.sig:
                    c += 1
                    o.seq = c
        with ExitStack() as es:
            esem = {e: es.enter_context(nc.semaphore(f"s_{e}")) for e in ENG}
            dsem = {k: es.enter_context(nc.semaphore(f"d_{k}")) for k in self.dma_counts}
            block = es.enter_context(nc.Block())

            def dval(d):
                if d.dma_sem in self.total_keys:
                    return 16 * self.dma_counts[d.dma_sem]
                return 16 * d.dma_cnt

            def need(o):
                w = {}
                for d in o.deps:
                    if d.dma_sem is not None:
                        key, val = ("d", d.dma_sem), dval(d)
                    else:
                        if skip(d, o):
                            continue
                        key, val = ("e", d.eng), d.seq
                    if w.get(key, 0) < val:
                        w[key] = val
                return w

            def run(ename):
                def body(eng):
                    waited = {}
                    for o in self.per[ename]:
                        for key, val in need(o).items():
                            if waited.get(key, 0) >= val:
                                continue
                            waited[key] = val
                            sem = dsem[key[1]] if key[0] == "d" else esem[key[1]]
                            eng.wait_ge(sem, val)
                        ins = o.fn(eng)
                        if o.dma_sem is not None:
                            ins.then_inc(dsem[o.dma_sem], 16)
                        elif o.sig:
                            ins.then_inc(esem[ename], 1)
                    if ename == "sp":
                        for o in final_wait_ops:
                            if o.dma_sem is not None:
                                eng.wait_ge(dsem[o.dma_sem], dval(o))
                            else:
                                eng.wait_ge(esem[o.eng], o.seq)
                return body

            block.tensor(run("pe"))
            block.scalar(run("act"))
            block.vector(run("dve"))
            block.gpsimd(run("pool"))
            block.sync(run("sp"))


class Arena:
    def __init__(self, nc, es, name, nbytes):
        self.t = es.enter_context(nc.sbuf_tensor(name, [128, nbytes // 2], BF16))
        self.cap = nbytes
        self.off = 0

    def alloc(self, shape, dt):
        n = 1
        for s_ in shape[1:]:
            n *= s_
        esz = 2 if dt == BF16 else 4
        o = self.off
        self.off += (n * esz + 63) // 64 * 64
        assert self.off <= self.cap, (self.off, self.cap)
        ap = self.t[0:shape[0], o // 2:(o + n * esz) // 2]
        if dt != BF16:
            ap = ap.bitcast(dt)
        if len(shape) > 2:
            names = " ".join(f"d{k}" for k in range(len(shape) - 1))
            ap = ap.rearrange(f"p ({names}) -> p {names}", **{f"d{k}": shape[k + 1] for k in range(len(shape) - 1)})
        return ap


def build(npass=NPASS_FULL, debug=False, stages=4):
    nc = bass.Bass("TRN2", target_bir_lowering=False)

    def din(name, shape):
        return nc.dram_tensor(name, list(shape), F32, kind="ExternalInput").ap()

    x_d = din("x", [SEQ, D])
    win_d = din("w_in", [D, 3840])
    wpa_d = din("wpa", [512, D])
    wpb_d = din("wpb", [512, D])
    wout_d = din("wout", [D, D])
    wexp_d = [din("wgp", [16, 128, 4096]), din("wup", [16, 128, 4096]), din("wdp", [16, 128, 4096])]
    wr_d = din("wr", [D, 20])
    rb_d = din("rb", [1, 20])
    gacol_d = din("gacol", [128, 8])
    gfcol_d = din("gfcol", [128, 8])
    gvcol_d = din("gvcol", [128, 4])
    gfin_d = din("gfin", [1, D])
    wst_d = din("wst", [128, 8, 128])
    wmask_d = din("wmask", [128, 8, 128])
    bst_d = din("bst", [128, 4, 128])
    sinks_d = din("sinks", [128, 4])
    bias_d = din("bias", [128, 2, 2, 4, 128])
    mask_d = din("maskc", [128, 2, 2, 4, 128])
    idn_d = din("idn", [128, 128])
    utri_d = din("utri", [128, 128])
    cst_d = din("cst", [1, 8 + 256 + 32])
    iop_d = din("iop", [128, 1])
    y_d = nc.dram_tensor("y", [SEQ, D], F32, kind="ExternalOutput").ap()
    hS_d = nc.dram_tensor("hS", [SEQ, D], F32, kind="Internal").ap()
    hn2S_d = nc.dram_tensor("hn2S", [SEQ, D], BF16, kind="Internal").ap()
    Xs_d = nc.dram_tensor("Xs", [NSLOT * 512, D], BF16, kind="Internal").ap()
    Ys_d = nc.dram_tensor("Ys", [NSLOT * 512, D], F32, kind="Internal").ap()
    wscr_d = [nc.dram_tensor(f"wscr{m}", [16 * 128, 4096], BF16, kind="Internal").ap() for m in range(3)]
    if debug:
        dbg_d = nc.dram_tensor("dbg", [PT, D], F32, kind="ExternalOutput").ap()
        dbgr_d = nc.dram_tensor("dbgr", [128, 6, NTT], F32, kind="ExternalOutput").ap()

    S = Sched()
    S.total_keys.update(["const"])

    with ExitStack() as es:
        def sb(name, shape, dt):
            return es.enter_context(nc.sbuf_tensor(name, list(shape), dt))

        ident = sb("ident", [128, 128], BF16)
        gacol = sb("gacol_s", [128, 8], F32)
        gfcol = sb("gfcol_s", [128, 8], F32)
        gvcol = sb("gvcol_s", [128, 4], F32)
        wstb = sb("wstb", [128, 8, 128], BF16)
        bst = sb("bst_s", [128, 4, 128], F32)
        sinkexp = sb("sinkexp", [128, 4], F32)
        BMhi = sb("BMhi", [128, 2048], BF16)
        BMlo = sb("BMlo", [128, 2048], BF16)
        ones64 = sb("ones64", [128, 64], BF16)
        ones128 = sb("ones128", [128, 128], BF16)
        utri = sb("utri_s", [128, 128], BF16)
        cst = sb("cst_s", [128, 8 + 256 + 32], F32)
        iop = sb("iop_s", [128, 1], F32)
        wr = sb("wr_s", [128, 8, 20], BF16)
        rb = sb("rb_s", [128, 20], F32)
        lg = sb("lg_all", [128, NTT, 20], F32)
        posA = sb("posA", [128, NTT], I32)
        posB = sb("posB", [128, NTT], I32)
        wA = sb("wA", [128, NTT], F32)
        wB = sb("wB", [128, NTT], F32)
        idxW = sb("idxW", [128, NSLOT], I32)
        dummy = sb("bar_dummy", [128, 1], F32)
        gfin = sb("gfin_s", [128, D], F32)
        NEB = 2
        wreg = sb("wreg", [128, NEB * 12288], BF16)
        wgs = [wreg[:, b * 12288:b * 12288 + 4096].rearrange("p (c n) -> p c n", c=8) for b in range(NEB)]
        wus = [wreg[:, b * 12288 + 4096:b * 12288 + 8192].rearrange("p (c n) -> p c n", c=8) for b in range(NEB)]
        wds = [wreg[:, b * 12288 + 8192:b * 12288 + 12288].rearrange("p (c n) -> p c n", c=4) for b in range(NEB)]
        wflat = [[wreg[:, b * 12288 + m * 4096:b * 12288 + (m + 1) * 4096] for m in range(3)] for b in range(NEB)]
        wpa = wreg[:, 0:4096].rearrange("p (c n) -> p c n", c=4)
        wout = wreg[:, 4096:12288].rearrange("p (c n) -> p c n", c=8)
        wpb = wreg[:, 12288:16384].rearrange("p (c n) -> p c n", c=4)
        stg = wreg[:, 20480:24576]
        AR = Arena(nc, es, "arena", 133 * 1024)
        H = AR.alloc([128, NT, D], F32)
        BM = H[:, 6:8, :].rearrange("p a (k v j t) -> p (a k) v j t", k=1, v=2, j=4)
        hn2T = AR.alloc([128, 8, 512], BF16)
        wv = AR.alloc([128, 8, 512], BF16)
        wva = AR.alloc([128, 8, 128], BF16)
        NRING = 6
        ring = [AR.alloc([128, 8, 128], BF16) for _ in range(NRING)]
        hn2 = [AR.alloc([128, D], BF16) for _ in range(2)]
        hn = hn2[0]
        hnT = AR.alloc([128, 8, 512], BF16)
        hnTb = AR.alloc([128, 8, 512], BF16)
        ssA = AR.alloc([128, 4], F32)
        rsA = AR.alloc([128, 4], F32)
        ssF = AR.alloc([128, 4], F32)
        rsF = AR.alloc([128, 4], F32)
        wst = hnT[:, 0:2, :].rearrange("p a (b t) -> p (a b) t", b=4)
        wmask = hnT[:, 2:4, :].rearrange("p a (b t) -> p (a b) t", b=4)
        ss = AR.alloc([128, 8], F32)
        rstd = AR.alloc([128, 8], F32)
        uT = AR.alloc([128, 4, 512], BF16)
        qT = AR.alloc([128, 4, 512], BF16)
        kT = AR.alloc([128, (NT + 1) * 128], BF16)
        vatt = AR.alloc([128, NT + 1, 128], BF16)
        gv2 = [AR.alloc([128, 512], F32) for _ in range(2)]
        vnh2 = [AR.alloc([128, 512], BF16) for _ in range(2)]
        bnst2 = [AR.alloc([128, 6], F32) for _ in range(2)]
        mv2 = [AR.alloc([128, 2], F32) for _ in range(2)]
        rsv2 = [AR.alloc([128, 1], F32) for _ in range(2)]
        nmr2 = [AR.alloc([128, 1], F32) for _ in range(2)]
        gtmp = AR.alloc([128, 4, 128], F32)
        pT = AR.alloc([128, 2048], BF16)
        den = AR.alloc([128, 512], F32)
        aT = AR.alloc([128, 4, 512], BF16)
        bT = AR.alloc([128, 4, 512], BF16)
        sga = AR.alloc([128, 512], F32)
        sgb = AR.alloc([128, 512], F32)
        mT = AR.alloc([128, 8, 512], BF16)
        maskc = mT[:, 0:4, :].rearrange("p a (b c t) -> p (a b c) t", b=2, c=2).rearrange("p (v k h) t -> p v k h t", v=2, k=2)
        side1_end = AR.off
        AR.off = 0
        NB_ = NTT
        r_m = AR.alloc([128, NB_], F32)
        r_og = AR.alloc([128, NB_, 4], F32)
        r_eg = AR.alloc([128, NB_, 4], F32)
        r_gp = AR.alloc([128, NB_], F32)
        r_sel = AR.alloc([128, NB_, 4, 4], F32)
        r_es = AR.alloc([128, NB_, 4], F32)
        r_m1 = AR.alloc([128, NB_], F32)
        r_o1 = AR.alloc([128, NB_, 4], F32)
        r_es2 = AR.alloc([128, NB_, 4], F32)
        r_m2 = AR.alloc([128, NB_], F32)
        r_o2 = AR.alloc([128, NB_, 4], F32)
        r_d = AR.alloc([128, NB_], F32)
        r_w1 = AR.alloc([128, NB_], F32)
        M1 = AR.alloc([128, NB_, 4, 4], F32)
        M2 = AR.alloc([128, NB_, 4, 4], F32)
        Mb = AR.alloc([128, NB_ * 16], BF16)
        R1s = AR.alloc([128, NB_, 16], F32)
        Ca = AR.alloc([128, NB_, 16], F32)
        Cb = AR.alloc([128, NB_, 16], F32)
        Tts = AR.alloc([128, NB_, 16], F32)
        r_cmp = AR.alloc([128, 16, 16], F32)
        r_cmp2 = AR.alloc([128, NSLOT, 16], F32)
        r_nb = AR.alloc([128, 16], F32)
        r_ob = AR.alloc([128, 16], F32)
        r_oe = AR.alloc([128, 16], F32)
        r_pf = AR.alloc([128, NB_], F32)
        r_eid = AR.alloc([128, NSLOT], F32)
        NXIN = 12
        xin = [AR.alloc([128, D], BF16) for _ in range(NXIN)]
        xs = [AR.alloc([128, 4, D], BF16) for _ in range(2)]
        xT = [AR.alloc([128, 8, 512], BF16) for _ in range(2)]
        sgs = [AR.alloc([128, 512], F32) for _ in range(2)]
        hid = [AR.alloc([128, 4, 512], BF16) for _ in range(2)]
        NYS = 3
        ys = [AR.alloc([128, D], F32) for _ in range(NYS)]
        side2_end = AR.off
        AR.off = 0
        N4 = 6
        YA = [AR.alloc([128, D], F32) for _ in range(N4)]
        YB = [AR.alloc([128, D], F32) for _ in range(N4)]
        hb = [AR.alloc([128, D], F32) for _ in range(N4)]
        ss4 = [AR.alloc([128, 1], F32) for _ in range(N4)]
        rs4 = [AR.alloc([128, 1], F32) for _ in range(N4)]
        hn4 = AR.alloc([128, D], BF16)
        PQ = es.enter_context(nc.psum_tensor("pq", [128, 4096], F32))
        PB = [PQ[:, i * 512:(i + 1) * 512] for i in range(8)]

        T = {}

        def tk(n):
            if n not in T:
                T[n] = Tok(n)
            return T[n]

        tH = [tk(f"H{i}") for i in range(NT)]
        tPB = [tk(f"PB{i}") for i in range(8)]
        tE = [[tk(f"wg{i}"), tk(f"wu{i}"), tk(f"wd{i}")] for i in range(NEB)]
        tRing = [tk(f"ring{i}") for i in range(NRING)]

        def op(eng, fn, reads=(), writes=(), dma=None):
            return S.op(eng, fn, [tk(r) if isinstance(r, str) else r for r in reads],
                        [tk(w) if isinstance(w, str) else w for w in writes], dma)

        def cdma(eng, out, in_, w):
            op(eng, lambda e, out=out, in_=in_: e.dma_start(out=out, in_=in_), writes=[w], dma="const")

        cdma("sp", gacol[:], gacol_d, "gacol")
        cdma("sp", gfcol[:], gfcol_d, "gfcol")
        cdma("sp", gvcol[:], gvcol_d, "gvcol")
        cdma("sp", bst[:], bst_d, "bst")
        cdma("sp", sinkexp[:], sinks_d, "sinkexp")
        cdma("sp", BM, bias_d, "BM")
        cdma("sp", rb[:], rb_d.partition_broadcast(128), "rb")
        cdma("sp", cst[:], cst_d.partition_broadcast(128), "cst")
        cdma("sp", iop[:], iop_d, "iop")
        cdma("pool", ident[:], idn_d, "ident")
        cdma("pool", utri[:], utri_d, "utri")
        cdma("pool", wst, wst_d, "wst_a")
        cdma("pool", wmask, wmask_d, "wmask_a")
        cdma("pool", maskc, mask_d, "maskc_a")
        cdma("pool", wr[:], wr_d.rearrange("(c p) n -> p c n", p=128), "wr")
        cdma("pool", wv, win_d[:, V_OFF:V_OFF + 512].rearrange("(c p) n -> p c n", p=128), "wv")
        cdma("pool", wva, win_d[:, VA_OFF:VA_OFF + 128].rearrange("(c p) n -> p c n", p=128), "wva")
        op("dve", lambda e: e.tensor_tensor(out=wstb[:], in0=wst, in1=wmask, op=ALU.mult),
           reads=["wst_a", "wmask_a"], writes=["wstb", "hnT"])
        BMf = BM.rearrange("p k v j t -> p (k v j t)")
        op("dve", lambda e: e.tensor_tensor(out=BMf, in0=BMf, in1=maskc.rearrange("p k v j t -> p (k v j t)"), op=ALU.add),
           reads=["maskc_a"], writes=["BM", "mT"])
        op("dve", lambda e: e.tensor_copy(out=BMhi[:], in_=BMf), reads=["BM"], writes=["BMhi"])
        op("dve", lambda e: e.tensor_tensor(out=BMlo[:], in0=BMf, in1=BMhi[:], op=ALU.subtract),
           reads=["BM", "BMhi"], writes=["BMlo", tH[6], tH[7]])
        op("act", lambda e: e.activation(out=sinkexp[:], in_=sinkexp[:], func=AF.Exp), reads=[], writes=["sinkexp"])
        op("dve", lambda e: e.memset(ones64[:], 1.0), writes=["ones64"])
        op("dve", lambda e: e.memset(ones128[:], 1.0), writes=["ones128"])

        out_ops = []

        def rstd_ops(n):
            op("dve", lambda e: e.tensor_scalar(out=rstd[:, 0:n], in0=ss[:, 0:n], scalar1=1.0 / D, scalar2=EPS,
                                                op0=ALU.mult, op1=ALU.add), reads=["ss"], writes=["rstd"])
            op("act", lambda e: e.sqrt(out=rstd[:, 0:n], in_=rstd[:, 0:n]), reads=[], writes=["rstd"])
            op("dve", lambda e: e.reciprocal(out=rstd[:, 0:n], in_=rstd[:, 0:n]), reads=[], writes=["rstd"])

        def norm_transpose(ti_list, gcol, dstT, dst_col0, dst_tok, after_hn=None):
            n = len(ti_list)
            for k, i in enumerate(ti_list):
                hb2 = hn2[k % 2]
                op("act", lambda e, i=i, k=k, hb2=hb2: e.activation(out=hb2, in_=H[:, i, :], func=AF.Square,
                                                                   accum_out=ss[:, k:k + 1]),
                   reads=[tH[i]], writes=[f"hn{k % 2}", "ss"])
            rstd_ops(n)
            for k, i in enumerate(ti_list):
                hb2 = hn2[k % 2]
                hnn = f"hn{k % 2}"
                op("act", lambda e, i=i, k=k, hb2=hb2: e.activation(out=hb2, in_=H[:, i, :], func=AF.Identity,
                                                                   scale=rstd[:, k:k + 1]),
                   reads=[tH[i], "rstd"], writes=[hnn])
                if after_hn is not None:
                    after_hn(i, hb2, hnn)
                bank = k % 2
                pbT = PB[bank].bitcast(BF16).rearrange("p (c t) -> p c t", c=8)
                for c in range(8):
                    op("pe", lambda e, c=c, pbT=pbT, hb2=hb2: e.transpose(out=pbT[:, c, :], in_=hb2[:, c * 128:(c + 1) * 128],
                                                                         identity=ident[:]),
                       reads=[hnn, "ident"], writes=[tPB[bank]])
                c0 = dst_col0(i)
                op("dve", lambda e, pbT=pbT, c0=c0: e.tensor_tensor(
                    out=dstT[:, :, c0:c0 + 128], in0=pbT,
                    in1=gcol[:].unsqueeze(2).to_broadcast([128, 8, 128]), op=ALU.mult),
                   reads=[tPB[bank], "gacol", "gfcol"], writes=[dst_tok(i)])

        precast = [(m, ex) for ex in range(16) for m in range(3)]
        pc_ctr = [0]

        def precast_some(n):
            if stages == 1:
                return
            for _ in range(n):
                if pc_ctr[0] >= len(precast):
                    return
                m, ex = precast[pc_ctr[0]]
                pc_ctr[0] += 1
                op("pool", lambda e, m=m, ex=ex: e.dma_start(
                    out=stg, in_=wexp_d[m][ex]),
                   writes=["stg"], dma="stg")
                op("sp", lambda e, m=m, ex=ex: e.dma_start(out=wscr_d[m][ex * 128:(ex + 1) * 128, :], in_=stg),
                   reads=["stg"], writes=["wscr"], dma="wscr")

        hnTs = [hnT, hnTb]
        hnTn = ["hnT", "hnTb"]
        ring_ctr = [0]

        def stream_block(col0):
            r = ring_ctr[0] % NRING
            ring_ctr[0] += 1
            op("pool", lambda e, r=r, col0=col0: e.dma_start(
                out=ring[r], in_=win_d[:, col0:col0 + 128].rearrange("(c p) n -> p c n", p=128)),
               writes=[tRing[r]], dma=f"ring{r}")
            if ring_ctr[0] % 4 == 0:
                precast_some(1)
            return ring[r], tRing[r]

        def load_x(g):
            for k in range(4):
                i = (g % 2) * 4 + k
                t0_ = g * 512 + k * 128
                op("sp", lambda e, i=i, t0_=t0_: e.dma_start(out=H[:, i, :], in_=x_d[t0_:t0_ + 128, :]),
                   writes=[tH[i]], dma=f"H{i}")

        def norm_pieces(tiles, gcol, dstT, dst_tok, ssb, rsb, ssn, rsn, banks, after_hn=None):
            def sq(k):
                i = tiles[k]
                hb2 = hn2[k % 2]
                op("act", lambda e: e.activation(out=hb2, in_=H[:, i, :], func=AF.Square, accum_out=ssb[:, k:k + 1]),
                   reads=[tH[i]], writes=[f"hn{k % 2}", ssn])

            def rs():
                op("dve", lambda e: e.tensor_scalar(out=rsb, in0=ssb, scalar1=1.0 / D, scalar2=EPS,
                                                    op0=ALU.mult, op1=ALU.add), reads=[ssn], writes=[rsn])
                op("act", lambda e: e.sqrt(out=rsb, in_=rsb), reads=[], writes=[rsn])
                op("dve", lambda e: e.reciprocal(out=rsb, in_=rsb), reads=[], writes=[rsn])

            def tr(k):
                i = tiles[k]
                hb2 = hn2[k % 2]
                hnn = f"hn{k % 2}"
                op("act", lambda e: e.activation(out=hb2, in_=H[:, i, :], func=AF.Identity, scale=rsb[:, k:k + 1]),
                   reads=[tH[i], rsn], writes=[hnn])
                if after_hn is not None:
                    after_hn(i, hb2, hnn)
                bank = banks[k % 2]
                pbT = PB[bank].bitcast(BF16).rearrange("p (c t) -> p c t", c=8)
                for c in range(8):
                    op("pe", lambda e, c=c: e.transpose(out=pbT[:, c, :], in_=hb2[:, c * 128:(c + 1) * 128], identity=ident[:]),
                       reads=[hnn, "ident"], writes=[tPB[bank]])
                op("dve", lambda e: e.tensor_tensor(out=dstT[:, :, k * 128:(k + 1) * 128], in0=pbT,
                                                    in1=gcol[:].unsqueeze(2).to_broadcast([128, 8, 128]), op=ALU.mult),
                   reads=[tPB[bank], "gacol", "gfcol"], writes=[dst_tok])
            return sq, rs, tr

        def make_st(g):
            ps_i, st = g // 2, g % 2
            tok0 = ps_i * PT
            tiles = [st * 4 + k for k in range(4)]
            hT, hTn = hnTs[g % 2], hnTn[g % 2]

            def A_pieces():
                return norm_pieces(tiles, gacol, hT, hTn, ssA, rsA, "ssA", "rsA", (0, 1))

            def proj_fm(col0, bank, evac):
                wb_, wt_ = stream_block(col0)
                for c in range(8):
                    op("pe", lambda e, c=c: e.matmul(PB[bank], lhsT=wb_[:, c, :], rhs=hT[:, c, :], start=(c == 0), stop=(c == 7)),
                       reads=[wt_, hTn], writes=[tPB[bank]])
                evac(bank)

            def B(hook):
                if st == 0 and ps_i > 0:
                    op("dve", lambda e: e.tensor_copy(out=kT[:, 0:128], in_=kT[:, NT * 128:(NT + 1) * 128]),
                       reads=[], writes=["kT"])
                    op("dve", lambda e: e.tensor_copy(out=vatt[:, 0, :], in_=vatt[:, NT, :]), reads=[], writes=["vatt"])
                blk = 0
                for j in range(4):
                    proj_fm(U_OFF + j * 128, 1 + (j % 2),
                            lambda bank, j=j: op("act", lambda e: e.activation(out=uT[:, j, :], in_=PB[bank],
                                                                              func=AF.Gelu_apprx_tanh),
                                                 reads=[tPB[bank]], writes=["uT"]))
                    if hook is not None:
                        hook(blk)
                    blk += 1
                for j in range(4):
                    proj_fm(Q_OFF + j * 128, 1 + (j % 2),
                            lambda bank, j=j: op("act", lambda e: e.activation(out=qT[:, j, :], in_=PB[bank],
                                                                              func=AF.Identity, scale=0.125),
                                                 reads=[tPB[bank]], writes=["qT"]))
                    if hook is not None:
                        hook(blk)
                    blk += 1
                kc0 = (1 + st * 4) * 128
                proj_fm(K_OFF, 1,
                        lambda bank: op("dve", lambda e: e.tensor_copy(out=kT[:, kc0:kc0 + 512], in_=PB[bank]),
                                        reads=[tPB[bank]], writes=["kT"]))
                if hook is not None:
                    hook(blk)

            def t_v(i):
                ts = (i - st * 4) * 128
                slot = 1 + i
                q2 = i % 2
                gv, vnh, bnst, mv, rsv, nmr = gv2[q2], vnh2[q2], bnst2[q2], mv2[q2], rsv2[q2], nmr2[q2]
                nm = lambda x: f"{x}{q2}"
                for c in range(8):
                    op("pe", lambda e, c=c, ts=ts: e.matmul(PB[0], lhsT=hT[:, c, ts:ts + 128], rhs=wv[:, c, :],
                                                            start=(c == 0), stop=(c == 7)),
                       reads=[hTn, "wv"], writes=[tPB[0]])
                for c in range(8):
                    op("pe", lambda e, c=c, ts=ts: e.matmul(PB[1][:, 0:128], lhsT=hT[:, c, ts:ts + 128],
                                                            rhs=wva[:, c, :], start=(c == 0), stop=(c == 7)),
                       reads=[hTn, "wva"], writes=[tPB[1]])
                op("act", lambda e, gv=gv: e.activation(out=gv, in_=PB[0], func=AF.Gelu_apprx_tanh),
                   reads=[tPB[0]], writes=[nm("gv")])
                op("dve", lambda e, slot=slot: e.tensor_copy(out=vatt[:, slot, :], in_=PB[1][:, 0:128]),
                   reads=[tPB[1]], writes=["vatt"])
                op("dve", lambda e, gv=gv, bnst=bnst: e.bn_stats(out=bnst, in_=gv), reads=[nm("gv")], writes=[nm("bnst")])
                op("dve", lambda e, mv=mv, bnst=bnst: e.bn_aggr(out=mv, in_=bnst), reads=[nm("bnst")], writes=[nm("mv")])
                op("dve", lambda e, mv=mv, rsv=rsv: e.tensor_scalar_add(out=rsv, in0=mv[:, 1:2], scalar1=EPS),
                   reads=[nm("mv")], writes=[nm("rsv")])
                op("act", lambda e, rsv=rsv: e.sqrt(out=rsv, in_=rsv), reads=[], writes=[nm("rsv")])
                op("dve", lambda e, rsv=rsv: e.reciprocal(out=rsv, in_=rsv), reads=[], writes=[nm("rsv")])
                op("dve", lambda e, mv=mv, rsv=rsv, nmr=nmr: e.tensor_scalar(out=nmr, in0=mv[:, 0:1], scalar1=rsv, scalar2=-1.0,
                                                                           op0=ALU.mult, op1=ALU.mult),
                   reads=[nm("mv"), nm("rsv")], writes=[nm("nmr")])
                op("act", lambda e, gv=gv, vnh=vnh, rsv=rsv, nmr=nmr: e.activation(out=vnh, in_=gv, func=AF.Identity,
                                                                                 scale=rsv, bias=nmr),
                   reads=[nm("gv"), nm("rsv"), nm("nmr")], writes=[nm("vnh")])

            def t_sc(i):
                ts = (i - st * 4) * 128
                slot = 1 + i
                gblk = ps_i * NT + i
                for kh in range(2):
                    pr = slice(kh * 64, (kh + 1) * 64)
                    for vi in range(2 if gblk > 0 else 1):
                        bank = 4 + kh * 2 + vi
                        ksl = slot - vi
                        pc = (kh * 2 + vi) * 512
                        op("pe", lambda e, pr=pr, ksl=ksl, ts=ts, bank=bank: e.matmul(
                            PB[bank].rearrange("p (j t) -> p j t", j=4),
                            lhsT=kT[pr, ksl * 128:(ksl + 1) * 128], rhs=qT[pr, :, ts:ts + 128],
                            start=True, stop=False),
                           reads=["kT", "qT"], writes=[tPB[bank]])
                        op("pe", lambda e, bank=bank, pc=pc: e.matmul(PB[bank], lhsT=ident[:], rhs=BMhi[:, pc:pc + 512],
                                                                      start=False, stop=False),
                           reads=["ident", "BMhi"], writes=[tPB[bank]])
                        op("pe", lambda e, bank=bank, pc=pc: e.matmul(PB[bank], lhsT=ident[:], rhs=BMlo[:, pc:pc + 512],
                                                                      start=False, stop=True),
                           reads=["ident", "BMlo"], writes=[tPB[bank]])
                scb = PQ[:, 2048:4096]
                op("act", lambda e, scb=scb: e.activation(out=pT, in_=scb, func=AF.Exp),
                   reads=[tPB[4], tPB[5], tPB[6], tPB[7]], writes=["pT"])

            def t_pv(i):
                ts = (i - st * 4) * 128
                slot = 1 + i
                gblk = ps_i * NT + i
                nv = 2 if gblk > 0 else 1
                for (bank, use_v) in ((3, True), (0, False)):
                    for kh in range(2):
                        kw = {"tile_position": (0, 64)} if kh else {}
                        for vi in range(nv):
                            ksl = slot - vi
                            lhs = vatt[:, ksl, kh * 64:(kh + 1) * 64] if use_v else ones64[:]
                            pc = (kh * 2 + vi) * 512
                            op("pe", lambda e, bank=bank, kh=kh, vi=vi, lhs=lhs, pc=pc, kw=kw, nv=nv: e.matmul(
                                PB[bank][kh * 64:(kh + 1) * 64, :], lhsT=lhs, rhs=pT[:, pc:pc + 512],
                                start=(vi == 0), stop=(vi == nv - 1), **kw),
                               reads=["vatt", "ones64", "pT"], writes=[tPB[bank]])
                op("dve", lambda e: e.tensor_tensor(
                    out=den.rearrange("p (j t) -> p j t", j=4), in0=PB[0].rearrange("p (j t) -> p j t", j=4),
                    in1=sinkexp[:].unsqueeze(2).to_broadcast([128, 4, 128]), op=ALU.add),
                   reads=[tPB[0], "sinkexp"], writes=["den"])
                op("dve", lambda e: e.reciprocal(out=den, in_=den), reads=[], writes=["den"])
                op("dve", lambda e, ts=ts: e.tensor_tensor(
                    out=bT[:, :, ts:ts + 128], in0=PB[3].rearrange("p (j t) -> p j t", j=4),
                    in1=den.rearrange("p (j t) -> p j t", j=4), op=ALU.mult),
                   reads=[tPB[3], "den"], writes=["bT"])

            def t_sp(i):
                ts = (i - st * 4) * 128
                q2 = i % 2
                vnh = vnh2[q2]
                pbs = PB[2].rearrange("p (j t) -> p j t", j=4)
                for g in range(8):
                    lo = (g % 2) * 64
                    kw = {"tile_position": (0, 64)} if g % 2 else {}
                    op("pe", lambda e, g=g, lo=lo, kw=kw, vnh=vnh: e.matmul(
                        pbs[lo:lo + 64, g // 2, :], lhsT=vnh[:, g * 64:(g + 1) * 64], rhs=wstb[:, g, :],
                        start=True, stop=True, **kw),
                       reads=[f"vnh{q2}", "wstb"], writes=[tPB[2]])
                op("dve", lambda e: e.tensor_tensor(out=gtmp, in0=pbs,
                                                    in1=gvcol[:].unsqueeze(2).to_broadcast([128, 4, 128]), op=ALU.mult),
                   reads=[tPB[2], "gvcol"], writes=["gtmp"])
                op("dve", lambda e: e.tensor_tensor(out=gtmp, in0=gtmp, in1=bst[:], op=ALU.add),
                   reads=["bst"], writes=["gtmp"])
                op("dve", lambda e, ts=ts: e.tensor_tensor(out=aT[:, :, ts:ts + 128], in0=gtmp,
                                                           in1=uT[:, :, ts:ts + 128], op=ALU.mult),
                   reads=["gtmp", "uT"], writes=["aT"])

            def C():
                t_v(tiles[0])
                t_sc(tiles[0])
                for k in range(1, 4):
                    t_v(tiles[k])
                    t_pv(tiles[k - 1])
                    t_sc(tiles[k])
                    t_sp(tiles[k - 1])
                t_pv(tiles[3])
                t_sp(tiles[3])

            def Dg(hook):
                for j in range(8):
                    b0 = (j % 2) * 4
                    wga, tga = stream_block(GA_OFF + j * 128)
                    for c in range(8):
                        op("pe", lambda e, c=c, wga=wga, b0=b0: e.matmul(PB[b0], lhsT=wga[:, c, :], rhs=hT[:, c, :],
                                                                         start=(c == 0), stop=(c == 7)),
                           reads=[tga, hTn], writes=[tPB[b0]])
                    wgb, tgb = stream_block(GB_OFF + j * 128)
                    for c in range(8):
                        op("pe", lambda e, c=c, wgb=wgb, b0=b0: e.matmul(PB[b0 + 1], lhsT=wgb[:, c, :], rhs=hT[:, c, :],
                                                                         start=(c == 0), stop=(c == 7)),
                           reads=[tgb, hTn], writes=[tPB[b0 + 1]])
                    for c in range(4):
                        op("pe", lambda e, c=c, j=j, b0=b0: e.matmul(PB[b0 + 2], lhsT=wpa[:, c, j * 128:(j + 1) * 128], rhs=aT[:, c, :],
                                                                     start=(c == 0), stop=(c == 3)),
                           reads=[tE[0][0], "aT"], writes=[tPB[b0 + 2]])
                    for c in range(4):
                        op("pe", lambda e, c=c, j=j, b0=b0: e.matmul(PB[b0 + 3], lhsT=wpb[:, c, j * 128:(j + 1) * 128], rhs=bT[:, c, :],
                                                                     start=(c == 0), stop=(c == 3)),
                           reads=["wpb0", "wpb1", "bT"], writes=[tPB[b0 + 3]])
                    op("act", lambda e, b0=b0: e.activation(out=sga, in_=PB[b0], func=AF.Sigmoid),
                       reads=[tPB[b0]], writes=["sga"])
                    op("act", lambda e, b0=b0: e.activation(out=sgb, in_=PB[b0 + 1], func=AF.Sigmoid),
                       reads=[tPB[b0 + 1]], writes=["sgb"])
                    op("dve", lambda e, b0=b0: e.tensor_tensor(out=sga, in0=PB[b0 + 2], in1=sga, op=ALU.mult),
                       reads=[tPB[b0 + 2]], writes=["sga"])
                    op("dve", lambda e, b0=b0: e.tensor_tensor(out=sgb, in0=PB[b0 + 3], in1=sgb, op=ALU.mult),
                       reads=[tPB[b0 + 3]], writes=["sgb"])
                    op("dve", lambda e, j=j: e.tensor_tensor(out=mT[:, j, :], in0=sga, in1=sgb, op=ALU.add),
                       reads=["sga", "sgb"], writes=["mT"])
                    if hook is not None:
                        hook(j)

            def E():
                for i in tiles:
                    ts = (i - st * 4) * 128
                    for hf in range(2):
                        bank = 1 + hf
                        for j in range(8):
                            op("pe", lambda e, j=j, ts=ts, hf=hf, bank=bank: e.matmul(
                                PB[bank][:], lhsT=mT[:, j, ts:ts + 128], rhs=wout[:, j, hf * 512:(hf + 1) * 512],
                                start=(j == 0), stop=(j == 7)),
                               reads=["mT", tE[0][1], tE[0][2]], writes=[tPB[bank]])
                        op("dve", lambda e, i=i, hf=hf, bank=bank: e.tensor_tensor(
                            out=H[:, i, hf * 512:(hf + 1) * 512], in0=PB[bank][:], in1=H[:, i, hf * 512:(hf + 1) * 512],
                            op=ALU.add),
                           reads=[tPB[bank]], writes=[tH[i]])
                    op("sp", lambda e, i=i, tok0=tok0: e.dma_start(out=hS_d[tok0 + i * 128: tok0 + (i + 1) * 128, :],
                                                                   in_=H[:, i, :]),
                       reads=[tH[i]], writes=["hS"], dma=f"H{i}")

                if debug and ps_i == 0:
                    for i in tiles:
                        out_ops.append(op("sp", lambda e, i=i: e.dma_start(out=dbg_d[i * 128:(i + 1) * 128, :], in_=H[:, i, :]),
                                          reads=[tH[i]], dma=f"H{i}"))

            def F_pieces():
                def spill_hn2(i, hb2, hnn):
                    op("sp", lambda e: e.dma_start(out=hn2S_d[tok0 + i * 128: tok0 + (i + 1) * 128, :], in_=hb2),
                       reads=[hnn], writes=["hn2S"], dma="hn2S" + hnn)
                sq, rs, tr = norm_pieces(tiles, gfcol, hn2T, "hn2T", ssF, rsF, "ssF", "rsF", (3, 4), after_hn=spill_hn2)

                def router():
                    for k, i in enumerate(tiles):
                        gi = ps_i * NT + i
                        ts = k * 128
                        for c in range(8):
                            op("pe", lambda e, c=c, ts=ts: e.matmul(PB[7][:, 0:20], lhsT=hn2T[:, c, ts:ts + 128], rhs=wr[:, c, :],
                                                                    start=(c == 0), stop=(c == 7)),
                               reads=["hn2T", "wr"], writes=[tPB[7]])
                        op("dve", lambda e, gi=gi: e.tensor_tensor(out=lg[:, gi, :], in0=PB[7][:, 0:20], in1=rb[:], op=ALU.add),
                           reads=[tPB[7], "rb"], writes=["lg"])
                return sq, rs, tr, router
            return dict(A=A_pieces, B=B, C=C, D=Dg, E=E, F=F_pieces)

        def mk_hook(pieces, base):
            if pieces is None:
                return None
            sq, rs, tr = pieces[0], pieces[1], pieces[2]
            router = pieces[3] if len(pieces) > 3 else None

            def hook(it):
                k = it - base
                if 0 <= k < 4:
                    sq(k)
                if k == 3:
                    rs()
                if 4 <= k < 8:
                    tr(k - 4)
                if k == 8 and router is not None:
                    router()
            return hook

        G = 2 * npass
        sts = [make_st(g) for g in range(G)]
        load_x(0)
        op("pool", lambda e: e.dma_start(out=wpa, in_=wpa_d.rearrange("(c p) n -> p c n", p=128)),
           writes=[tE[0][0]], dma="wg0")
        for a_ in range(2):
            op("pool", lambda e, a_=a_: e.dma_start(
                out=wpb[a_ * 64:(a_ + 1) * 64, :, :],
                in_=wpb_d.rearrange("(a j d) n -> a d j n", a=2, j=4)[a_]),
               writes=[tk(f"wpb{a_}")], dma=f"wpbd{a_}")
        op("pool", lambda e: e.dma_start(out=wout, in_=wout_d.rearrange("(c p) n -> p c n", p=128)),
           writes=[tE[0][1], tE[0][2]], dma="wu0")
        if G > 1:
            load_x(1)
        sq, rs, tr = sts[0]["A"]()
        for k in range(4):
            sq(k)
        rs()
        for k in range(4):
            tr(k)
        for g in range(G):
            fp = sts[g - 1]["F"]() if g > 0 else None
            sts[g]["B"](mk_hook(fp, 0))
            if fp is not None and fp[3] is not None and False:
                pass
            if g >= 1 and g + 1 < G:
                load_x(g + 1)
            sts[g]["C"]()
            ap = sts[g + 1]["A"]() if g + 1 < G else None
            sts[g]["D"](mk_hook(ap, 0))
            sts[g]["E"]()
        sq, rs, tr, router = sts[G - 1]["F"]()
        for k in range(4):
            sq(k)
        rs()
        for k in range(4):
            tr(k)
        router()

        precast_some(100)

        S.barrier("dve", lambda e: e.memset(dummy[:], 0.0))
        NTK = npass * NT

        cdma2 = op("sp", lambda e: e.dma_start(out=gfin[:], in_=gfin_d.partition_broadcast(128)), writes=["gfin"], dma="gfin")

        gl = lg[:, :, 0:4]
        el = lg[:, :, 4:20].rearrange("p t (g x) -> p t g x", g=4)
        bc3 = lambda a: a.unsqueeze(2).to_broadcast([128, NTT, 4])
        if npass < NPASS_FULL:
            op("dve", lambda e: e.memset(lg[:, NTK:, :], 0.0), writes=["lg"])
        dv = lambda fn, r, w: op("dve", fn, reads=r, writes=w)
        dv(lambda e: e.tensor_reduce(out=r_m, in_=gl, axis=AX.X, op=ALU.max), ["lg"], ["r_m"])
        dv(lambda e: e.tensor_tensor(out=r_og, in0=gl, in1=bc3(r_m), op=ALU.is_equal), ["lg", "r_m"], ["r_og"])
        dv(lambda e: e.tensor_tensor(out=r_eg, in0=gl, in1=bc3(r_m), op=ALU.subtract), ["lg", "r_m"], ["r_eg"])
        op("act", lambda e: e.activation(out=r_eg, in_=r_eg, func=AF.Exp), reads=[], writes=["r_eg"])
        dv(lambda e: e.tensor_reduce(out=r_gp, in_=r_eg, axis=AX.X, op=ALU.add), ["r_eg"], ["r_gp"])
        dv(lambda e: e.reciprocal(out=r_gp, in_=r_gp), [], ["r_gp"])
        dv(lambda e: e.tensor_tensor(out=r_sel, in0=el, in1=r_og.unsqueeze(3).to_broadcast([128, NTT, 4, 4]), op=ALU.mult),
           ["lg", "r_og"], ["r_sel"])
        dv(lambda e: e.tensor_reduce(out=r_es, in_=r_sel.rearrange("p t g x -> p t x g"), axis=AX.X, op=ALU.add),
           ["r_sel"], ["r_es"])
        dv(lambda e: e.tensor_reduce(out=r_m1, in_=r_es, axis=AX.X, op=ALU.max), ["r_es"], ["r_m1"])
        dv(lambda e: e.tensor_tensor(out=r_o1, in0=r_es, in1=bc3(r_m1), op=ALU.is_equal), ["r_es", "r_m1"], ["r_o1"])
        dv(lambda e: e.scalar_tensor_tensor(out=r_es2, in0=r_o1, scalar=-1e9, in1=r_es, op0=ALU.mult, op1=ALU.add),
           ["r_o1", "r_es"], ["r_es2"])
        dv(lambda e: e.tensor_reduce(out=r_m2, in_=r_es2, axis=AX.X, op=ALU.max), ["r_es2"], ["r_m2"])
        dv(lambda e: e.tensor_tensor(out=r_o2, in0=r_es2, in1=bc3(r_m2), op=ALU.is_equal), ["r_es2", "r_m2"], ["r_o2"])
        dv(lambda e: e.tensor_tensor(out=r_d, in0=r_m2, in1=r_m1, op=ALU.subtract), ["r_m1", "r_m2"], ["r_d"])
        op("act", lambda e: e.activation(out=r_d, in_=r_d, func=AF.Exp), reads=[], writes=["r_d"])
        dv(lambda e: e.tensor_scalar_add(out=r_w1, in0=r_d, scalar1=1.0), ["r_d"], ["r_w1"])
        dv(lambda e: e.reciprocal(out=r_w1, in_=r_w1), [], ["r_w1"])
        dv(lambda e: e.tensor_tensor(out=wB[:], in0=r_d, in1=r_w1, op=ALU.mult), ["r_d", "r_w1"], ["wB"])
        dv(lambda e: e.tensor_tensor(out=wA[:], in0=r_w1, in1=r_gp, op=ALU.mult), ["r_w1", "r_gp"], ["wA"])
        dv(lambda e: e.tensor_tensor(out=wB[:], in0=wB[:], in1=r_gp, op=ALU.mult), ["r_gp"], ["wB"])
        bg = lambda a: a.unsqueeze(3).to_broadcast([128, NTT, 4, 4])
        bx = lambda a: a.unsqueeze(2).to_broadcast([128, NTT, 4, 4])
        dv(lambda e: e.tensor_tensor(out=M1, in0=bg(r_og), in1=bx(r_o1), op=ALU.mult), ["r_og", "r_o1"], ["M1"])
        dv(lambda e: e.tensor_tensor(out=M2, in0=bg(r_og), in1=bx(r_o2), op=ALU.mult), ["r_og", "r_o2"], ["M2"])
        M1f = M1.rearrange("p t g x -> p t (g x)")
        M2f = M2.rearrange("p t g x -> p t (g x)")
        dv(lambda e: e.tensor_tensor(out=Mb.rearrange("p (t n) -> p t n", n=16), in0=M1f, in1=M2f, op=ALU.add),
           ["M1", "M2"], ["Mb"])
        if npass < NPASS_FULL:
            dv(lambda e: e.memset(Mb[:, NTK * 16:], 0.0), [], ["Mb"])
        op("pe", lambda e: e.matmul(PB[0][:], lhsT=utri[:], rhs=Mb, start=True, stop=True),
           reads=["utri", "Mb"], writes=[tPB[0]])
        op("pe", lambda e: e.matmul(PB[1][:], lhsT=ones128[:], rhs=Mb, start=True, stop=True),
           reads=["ones128", "Mb"], writes=[tPB[1]])
        dv(lambda e: e.tensor_copy(out=R1s.rearrange("p t n -> p (t n)"), in_=PB[0][:]), [tPB[0]], ["R1s"])
        dv(lambda e: e.tensor_copy(out=Tts.rearrange("p t n -> p (t n)"), in_=PB[1][:]), [tPB[1]], ["Tts"])
        dv(lambda e: e.tensor_copy(out=Ca, in_=Tts), ["Tts"], ["Ca"])
        cur, nxt, cn, nn = Ca, Cb, "Ca", "Cb"
        sh = 1
        while sh < NTT:
            dv(lambda e, cur=cur, nxt=nxt, sh=sh: e.tensor_copy(out=nxt[:, 0:sh, :], in_=cur[:, 0:sh, :]), [cn], [nn])
            dv(lambda e, cur=cur, nxt=nxt, sh=sh: e.tensor_tensor(out=nxt[:, sh:, :], in0=cur[:, sh:, :],
                                                                  in1=cur[:, 0:NTT - sh, :], op=ALU.add), [cn], [nn])
            cur, nxt, cn, nn = nxt, cur, nn, cn
            sh *= 2
        Cin, cin_n, Cex, cex_n = cur, cn, nxt, nn
        dv(lambda e: e.tensor_tensor(out=Cex, in0=Cin, in1=Tts, op=ALU.subtract), [cin_n, "Tts"], [cex_n])
        ntot = Cin[:, NTT - 1, :]
        thr = cst[:, 0:8]
        tri = cst[:, 8:264].rearrange("p (a b) -> p a b", a=16)
        sio = cst[:, 264:264 + NSLOT]
        cmp8 = r_cmp.rearrange("p a b -> p (a b)")[:, 0:128].rearrange("p (a b) -> p a b", b=8)
        dv(lambda e: e.tensor_tensor(out=cmp8, in0=ntot.unsqueeze(2).to_broadcast([128, 16, 8]),
                                     in1=thr.unsqueeze(1).to_broadcast([128, 16, 8]), op=ALU.is_gt),
           [cin_n, "cst"], ["r_cmp"])
        dv(lambda e: e.tensor_reduce(out=r_nb, in_=cmp8, axis=AX.X, op=ALU.add), ["r_cmp"], ["r_nb"])
        dv(lambda e: e.tensor_tensor(out=r_cmp, in0=r_nb.unsqueeze(1).to_broadcast([128, 16, 16]), in1=tri, op=ALU.mult),
           ["r_nb", "cst"], ["r_cmp"])
        dv(lambda e: e.tensor_reduce(out=r_ob, in_=r_cmp, axis=AX.X, op=ALU.add), ["r_cmp"], ["r_ob"])
        dv(lambda e: e.tensor_tensor(out=r_oe, in0=r_ob, in1=r_nb, op=ALU.add), ["r_ob", "r_nb"], ["r_oe"])
        dv(lambda e: e.tensor_tensor(out=R1s, in0=R1s, in1=Cex, op=ALU.add), [cex_n], ["R1s"])
        dv(lambda e: e.scalar_tensor_tensor(out=R1s, in0=r_ob.unsqueeze(1).to_broadcast([128, NTT, 16]), scalar=512.0,
                                            in1=R1s, op0=ALU.mult, op1=ALU.add), ["r_ob"], ["R1s"])
        for (Mf, mn, pos_i, pn) in ((M1f, "M1", posA, "posA"), (M2f, "M2", posB, "posB")):
            dv(lambda e, Mf=Mf: e.tensor_tensor(out=Mf, in0=Mf, in1=R1s, op=ALU.mult), ["R1s"], [mn])
            dv(lambda e, Mf=Mf: e.tensor_reduce(out=r_pf, in_=Mf, axis=AX.X, op=ALU.add), [mn], ["r_pf"])
            dv(lambda e: e.tensor_scalar(out=r_pf, in0=r_pf, scalar1=0.0, scalar2=float(NSLOT * 512 - 1),
                                         op0=ALU.max, op1=ALU.min), [], ["r_pf"])
            dv(lambda e, pos_i=pos_i: e.tensor_copy(out=pos_i[:], in_=r_pf), ["r_pf"], [pn])
        dv(lambda e: e.tensor_tensor(out=r_cmp2, in0=sio.unsqueeze(2).to_broadcast([128, NSLOT, 16]),
                                     in1=r_oe.unsqueeze(1).to_broadcast([128, NSLOT, 16]), op=ALU.is_ge),
           ["cst", "r_oe"], ["r_cmp2"])
        dv(lambda e: e.tensor_reduce(out=r_eid, in_=r_cmp2, axis=AX.X, op=ALU.add), ["r_cmp2"], ["r_eid"])
        dv(lambda e: e.tensor_scalar(out=r_eid, in0=r_eid, scalar1=15.0, scalar2=128.0, op0=ALU.min, op1=ALU.mult),
           [], ["r_eid"])
        dv(lambda e: e.tensor_tensor(out=r_eid, in0=r_eid, in1=iop[:].to_broadcast([128, NSLOT]), op=ALU.add),
           ["iop"], ["r_eid"])
        dv(lambda e: e.tensor_scalar(out=r_eid, in0=r_eid, scalar1=0.0, scalar2=2047.0, op0=ALU.max, op1=ALU.min),
           [], ["r_eid"])
        dv(lambda e: e.tensor_copy(out=idxW[:], in_=r_eid), ["r_eid"], ["idxW"])
        if debug:
            dr = sb("dbgr_s", [128, 6, NTT], F32)
            dv(lambda e: e.tensor_copy(out=dr[:, 0, :], in_=posA[:]), ["posA"], ["dr"])
            dv(lambda e: e.tensor_copy(out=dr[:, 1, :], in_=posB[:]), ["posB"], ["dr"])
            dv(lambda e: e.tensor_copy(out=dr[:, 2, :], in_=wA[:]), ["wA"], ["dr"])
            dv(lambda e: e.tensor_copy(out=dr[:, 3, :], in_=wB[:]), ["wB"], ["dr"])
            dv(lambda e: e.tensor_copy(out=dr[:, 4, 0:NSLOT], in_=idxW[:]), ["idxW"], ["dr"])
            dv(lambda e: e.tensor_copy(out=dr[:, 5, 0:16], in_=ntot), [cin_n], ["dr"])
            out_ops.append(op("sp", lambda e: e.dma_start(out=dbgr_d, in_=dr[:]), reads=["dr"], dma="dbgr"))

        for gi in range(NTK if stages >= 3 else 0):
            xb = xin[gi % NXIN]
            xn = f"xin{gi % NXIN}"
            op("sp", lambda e, gi=gi, xb=xb: e.dma_start(out=xb, in_=hn2S_d[gi * 128:(gi + 1) * 128, :]),
               reads=["hn2S"], writes=[xn], dma=xn)
            for (pos_i, pn) in ((posA, "posA"), (posB, "posB")):
                op("pool", lambda e, gi=gi, xb=xb, pos_i=pos_i: e.indirect_dma_start(
                    out=Xs_d, out_offset=bass.IndirectOffsetOnAxis(pos_i[:, gi:gi + 1], 0), in_=xb, in_offset=None),
                   reads=[xn, pn], writes=[f"Xs{gi % NXIN}"], dma=f"scat{gi % NXIN}")

        NS3 = NSLOT if stages >= 3 else 0

        def slot_loads(s):
            b = s % NEB
            for m in range(3):
                op("pool", lambda e, s=s, b=b, m=m: e.indirect_dma_start(
                    out=wflat[b][m], out_offset=None, in_=wscr_d[m],
                    in_offset=bass.IndirectOffsetOnAxis(idxW[:, s:s + 1], 0)),
                   reads=["idxW", "wscr"], writes=[tE[b][m]], dma=f"w{'gud'[m]}{b}")
            xb = xs[s % 2]
            xn = f"xs{s % 2}"
            op("sp", lambda e, s=s, xb=xb: e.dma_start(out=xb, in_=Xs_d[s * 512:(s + 1) * 512, :].rearrange("(j p) d -> p j d", p=128)),
               reads=[f"Xs{q}" for q in range(NXIN)], writes=[xn], dma=xn)

        def slot_transposes(s):
            xb = xs[s % 2]
            xn = f"xs{s % 2}"
            xt = xT[s % 2]
            xtn = f"xT{s % 2}"
            for j in range(4):
                bank = j % 2
                pbT = PB[bank][:].bitcast(BF16).rearrange("p (c t) -> p c t", c=8)
                for c in range(8):
                    op("pe", lambda e, c=c, j=j, pbT=pbT, xb=xb: e.transpose(out=pbT[:, c, :], in_=xb[:, j, c * 128:(c + 1) * 128],
                                                                            identity=ident[:]),
                       reads=[xn, "ident"], writes=[tPB[bank]])
                op("dve", lambda e, j=j, pbT=pbT, xt=xt: e.tensor_tensor(
                    out=xt[:, :, j * 128:(j + 1) * 128], in0=pbT,
                    in1=gfcol[:].unsqueeze(2).to_broadcast([128, 8, 128]), op=ALU.mult),
                   reads=[tPB[bank], "gfcol"], writes=[xtn])

        def slot_gateup(s):
            b = s % NEB
            xt = xT[s % 2]
            xtn = f"xT{s % 2}"
            hb_ = s % 2
            for j in range(4):
                bg_, bu_ = 2 + (j % 2) * 2, 3 + (j % 2) * 2
                for c in range(8):
                    op("pe", lambda e, c=c, j=j, b=b, bg_=bg_, xt=xt: e.matmul(
                        PB[bg_][:], lhsT=wgs[b][:, c, j * 128:(j + 1) * 128], rhs=xt[:, c, :],
                        start=(c == 0), stop=(c == 7)),
                       reads=[tE[b][0], xtn], writes=[tPB[bg_]])
                for c in range(8):
                    op("pe", lambda e, c=c, j=j, b=b, bu_=bu_, xt=xt: e.matmul(
                        PB[bu_][:], lhsT=wus[b][:, c, j * 128:(j + 1) * 128], rhs=xt[:, c, :],
                        start=(c == 0), stop=(c == 7)),
                       reads=[tE[b][1], xtn], writes=[tPB[bu_]])
                op("act", lambda e, j=j, bg_=bg_: e.activation(out=sgs[j % 2], in_=PB[bg_][:], func=AF.Silu),
                   reads=[tPB[bg_]], writes=[f"sgs{j % 2}"])
                op("dve", lambda e, j=j, bu_=bu_, hb_=hb_: e.tensor_tensor(out=hid[hb_][:, j, :], in0=PB[bu_][:],
                                                                         in1=sgs[j % 2], op=ALU.mult),
                   reads=[tPB[bu_], f"sgs{j % 2}"], writes=[f"hid{hb_}"])

        def slot_down(s):
            b = s % NEB
            hb_ = s % 2
            for tl in range(4):
                yi = (s * 4 + tl) % NYS
                yb = ys[yi]
                yn = f"ys{yi}"
                for hf in range(2):
                    bank = 6 + hf
                    for j in range(4):
                        op("pe", lambda e, j=j, tl=tl, hf=hf, b=b, hb_=hb_, bank=bank: e.matmul(
                            PB[bank][:], lhsT=hid[hb_][:, j, tl * 128:(tl + 1) * 128],
                            rhs=wds[b][:, j, hf * 512:(hf + 1) * 512], start=(j == 0), stop=(j == 3)),
                           reads=[f"hid{hb_}", tE[b][2]], writes=[tPB[bank]])
                    op("act", lambda e, hf=hf, bank=bank, yb=yb: e.copy(out=yb[:, hf * 512:(hf + 1) * 512], in_=PB[bank][:]),
                       reads=[tPB[bank]], writes=[yn])
                op("sp", lambda e, s=s, tl=tl, yb=yb: e.dma_start(out=Ys_d[s * 512 + tl * 128: s * 512 + (tl + 1) * 128, :], in_=yb),
                   reads=[yn], writes=[f"Ys{yi}"], dma=f"ysd{yi}")

        if NS3:
            slot_loads(0)
            slot_transposes(0)
            slot_loads(1)
        for s in range(NS3):
            slot_gateup(s)
            if s + 1 < NS3:
                slot_transposes(s + 1)
            slot_down(s)
            if s + 2 < NS3:
                slot_loads(s + 2)

        if stages >= 4:
            S.barrier("dve", lambda e: e.memset(dummy[:], 0.0))
        NT4 = NTK if stages >= 4 else 0

        def loads4(gi):
            k2 = gi % N4
            op("sp", lambda e, gi=gi, k2=k2: e.dma_start(out=hb[k2], in_=hS_d[gi * 128:(gi + 1) * 128, :]),
               reads=["hS"], writes=[f"hb{k2}"], dma=f"hb{k2}")
            for (Yb, ynm, pos_i, pn) in ((YA, "YA", posA, "posA"), (YB, "YB", posB, "posB")):
                op("pool", lambda e, gi=gi, k2=k2, Yb=Yb, pos_i=pos_i: e.indirect_dma_start(
                    out=Yb[k2], out_offset=None, in_=Ys_d, in_offset=bass.IndirectOffsetOnAxis(pos_i[:, gi:gi + 1], 0)),
                   reads=[pn], writes=[f"{ynm}{k2}"], dma=f"{ynm}{k2}")

        for gi in range(min(N4, NT4)):
            loads4(gi)
        for gi in range(NT4):
            k2 = gi % N4
            op("dve", lambda e, gi=gi, k2=k2: e.scalar_tensor_tensor(out=hb[k2], in0=YA[k2], scalar=wA[:, gi:gi + 1], in1=hb[k2],
                                                                     op0=ALU.mult, op1=ALU.add),
               reads=[f"YA{k2}", "wA"], writes=[f"hb{k2}"])
            op("dve", lambda e, gi=gi, k2=k2: e.scalar_tensor_tensor(out=hb[k2], in0=YB[k2], scalar=wB[:, gi:gi + 1], in1=hb[k2],
                                                                     op0=ALU.mult, op1=ALU.add),
               reads=[f"YB{k2}", "wB"], writes=[f"hb{k2}"])
            op("act", lambda e, k2=k2: e.activation(out=hn4, in_=hb[k2], func=AF.Square, accum_out=ss4[k2]),
               reads=[f"hb{k2}"], writes=["hn4", f"ss4{k2}"])
            op("dve", lambda e, k2=k2: e.tensor_scalar(out=rs4[k2], in0=ss4[k2], scalar1=1.0 / D, scalar2=EPS, op0=ALU.mult, op1=ALU.add),
               reads=[f"ss4{k2}"], writes=[f"rs4{k2}"])
            op("act", lambda e, k2=k2: e.sqrt(out=rs4[k2], in_=rs4[k2]), reads=[], writes=[f"rs4{k2}"])
            op("dve", lambda e, k2=k2: e.reciprocal(out=rs4[k2], in_=rs4[k2]), reads=[], writes=[f"rs4{k2}"])
            op("dve", lambda e, k2=k2: e.scalar_tensor_tensor(out=hb[k2], in0=hb[k2], scalar=rs4[k2], in1=gfin[:],
                                                              op0=ALU.mult, op1=ALU.mult),
               reads=[f"rs4{k2}", "gfin"], writes=[f"hb{k2}"])
            out_ops.append(op("sp", lambda e, gi=gi, k2=k2: e.dma_start(out=y_d[gi * 128:(gi + 1) * 128, :], in_=hb[k2]),
                              reads=[f"hb{k2}"], dma=f"hb{k2}"))
            if gi + N4 < NT4:
                loads4(gi + N4)

        S.emit(nc, final_wait_ops=out_ops)
    return nc


def _t5_bucket(dist):
    dist = np.asarray(dist, dtype=np.int64)
    nf = np.maximum(dist, 1).astype(np.float32)
    large = 16 + (np.log(nf / np.float32(16)) / np.float32(math.log(128 / 16)) * np.float32(16)).astype(np.int32)
    large = np.minimum(large, 31)
    return np.where(dist < 16, dist, large)


def prep_shared(inp):
    f = lambda a: np.ascontiguousarray(np.asarray(a, dtype=np.float32))
    w_in = f(inp["w_in"])[0]
    perm = np.arange(3840)
    qcols = []
    for j in range(4):
        qcols += list(range(1024 + j * 64, 1024 + (j + 1) * 64))
        qcols += list(range(1024 + (4 + j) * 64, 1024 + (5 + j) * 64))
    perm[1024:1536] = np.array(qcols)
    w_in = np.ascontiguousarray(w_in[:, perm])
    wr = np.concatenate([f(inp["router_group_w"])[0]] + [f(inp["router_expert_w"])[0, g] for g in range(4)], axis=1)
    rb = np.concatenate([f(inp["router_group_b"])[0], f(inp["router_expert_b"])[0].reshape(-1)])[None, :]
    col = lambda v, c: np.ascontiguousarray(f(v).reshape(c, 128).T)
    ws = f(inp["gm_w_spatial"])[0]
    wst = np.ascontiguousarray(ws.transpose(2, 0, 1))
    s_i = np.arange(128)[:, None, None]
    t_i = np.arange(128)[None, None, :]
    wmask = np.broadcast_to((s_i <= t_i), (128, 8, 128)).astype(np.float32)
    bs = f(inp["gm_b_spatial"])[0]
    bst = np.zeros((128, 4, 128), np.float32)
    for g in range(8):
        bst[(g % 2) * 64:(g % 2) * 64 + 64, g // 2, :] = bs[g][None, :]
    rel = f(inp["rel_bias"])
    s_ = np.arange(128)[:, None]
    q_ = np.arange(128)[None, :]
    d_own = q_ - s_
    d_prev = q_ + 128 - s_
    bias = np.zeros((128, 2, 2, 4, 128), np.float32)
    maskc = np.zeros((128, 2, 2, 4, 128), np.float32)
    b_own = _t5_bucket(np.clip(d_own, 0, 127))
    b_prev = _t5_bucket(np.clip(d_prev, 0, 127))
    for kh in range(2):
        for h4 in range(4):
            h = kh * 4 + h4
            bias[:, kh, 0, h4, :] = rel[b_own, h]
            bias[:, kh, 1, h4, :] = rel[b_prev, h]
            maskc[:, kh, 0, h4, :] = np.where(d_own >= 0, 0.0, NEG)
            maskc[:, kh, 1, h4, :] = np.where(d_prev < 128, 0.0, NEG)
    def pmaj(w, c):
        e_, r_, n_ = w.shape
        return np.ascontiguousarray(w.reshape(e_, c, 128, n_).transpose(0, 2, 1, 3).reshape(e_, 128, c * n_))

    return {
        "w_in": w_in,
        "wpa": f(inp["w_proj_a"])[0], "wpb": f(inp["w_proj_b"])[0], "wout": f(inp["w_out"])[0],
        "wgp": pmaj(f(inp["expert_w_gate"])[0], 8), "wup": pmaj(f(inp["expert_w_up"])[0], 8),
        "wdp": pmaj(f(inp["expert_w_down"])[0], 4),
        "utri": np.triu(np.ones((128, 128), np.float32), 1),
        "cst": np.concatenate([np.arange(8, dtype=np.float32) * 512.0,
                               np.tril(np.ones((16, 16), np.float32), -1).reshape(-1),
                               np.arange(32, dtype=np.float32)])[None, :],
        "iop": np.arange(128, dtype=np.float32)[:, None],
        "wr": np.ascontiguousarray(wr), "rb": np.ascontiguousarray(rb),
        "gacol": col(inp["attn_norm_g"], 8), "gfcol": col(inp["ffn_norm_g"], 8), "gvcol": col(inp["gm_v_norm_g"], 4),
        "gfin": f(inp["final_norm_g"]).reshape(1, D),
        "wst": wst, "wmask": wmask, "bst": bst,
        "sinks": np.ascontiguousarray(np.repeat(f(inp["attn_sinks"]).reshape(2, 4), 64, axis=0)),
        "bias": bias, "maskc": maskc,
        "idn": np.eye(128, dtype=np.float32),
    }


def kernel(**inputs):
    shared = prep_shared(inputs)
    x = np.asarray(inputs["x"], dtype=np.float32)
    nc = build()
    in_maps = []
    for c in range(NCORES):
        m = dict(shared)
        m["x"] = np.ascontiguousarray(x[c])
        in_maps.append(m)
    res = run_bass_kernel_spmd(nc, in_maps, core_ids=list(range(NCORES)))
    return np.stack([np.asarray(r["y"], dtype=np.float32) for r in res.results], axis=0)
```

```python
import math
from contextlib import ExitStack

import numpy as np
import concourse.bass as bass
import concourse.mybir as mybir
from concourse.bass_utils import run_bass_kernel_spmd

F32 = mybir.dt.float32
BF16 = mybir.dt.bfloat16
AF = mybir.ActivationFunctionType
ALU = mybir.AluOpType
AX = mybir.AxisListType

SEQ = 4096
D = 1024
NCORES = 8
PT = 1024
NT = PT // 128
NST = PT // 512
NPASS_FULL = SEQ // PT
EPS = 1e-6
NEG = -30000.0
NSLOT = 31
NTT = SEQ // 128
I32 = mybir.dt.int32

U_OFF, V_OFF, Q_OFF, K_OFF, VA_OFF, GA_OFF, GB_OFF = 0, 512, 1024, 1536, 1664, 1792, 2816

ENG = ("pe", "act", "dve", "pool", "sp")


class Tok:
    __slots__ = ("name", "last_w", "readers")

    def __init__(self, name):
        self.name = name
        self.last_w = None
        self.readers = []


class Op:
    __slots__ = ("eng", "fn", "deps", "sig", "dma_sem", "dma_cnt", "seq")

    def __init__(self, eng, fn):
        self.eng = eng
        self.fn = fn
        self.deps = set()
        self.sig = False
        self.dma_sem = None
        self.dma_cnt = 0
        self.seq = 0


class Sched:
    def __init__(self):
        self.ops = []
        self.per = {e: [] for e in ENG}
        self.dma_counts = {}
        self.total_keys = set()
        self.epoch_op = None
        self.last_dma = {}

    def op(self, eng, fn, reads=(), writes=(), dma=None):
        o = Op(eng, fn)
        for t in list(reads) + list(writes):
            if t.last_w is not None:
                o.deps.add(t.last_w)
        for t in writes:
            for r in t.readers:
                o.deps.add(r)
        if self.epoch_op is not None:
            o.deps.add(self.epoch_op)
        o.deps.discard(o)
        for t in reads:
            t.readers.append(o)
        for t in writes:
            t.last_w = o
            t.readers = []
        if dma is not None:
            o.dma_sem = dma
            self.dma_counts[dma] = self.dma_counts.get(dma, 0) + 1
            o.dma_cnt = self.dma_counts[dma]
            self.last_dma[dma] = o
        self.ops.append(o)
        self.per[eng].append(o)
        return o

    def barrier(self, eng, fn):
        o = Op(eng, fn)
        for e in ENG:
            seen_c = False
            for p in reversed(self.per[e]):
                if p.dma_sem is None:
                    o.deps.add(p)
                    break
        for k, p in self.last_dma.items():
            o.deps.add(p)
        if self.epoch_op is not None:
            o.deps.add(self.epoch_op)
        self.ops.append(o)
        self.per[eng].append(o)
        self.epoch_op = o
        return o

    def emit(self, nc, final_wait_ops=()):
        def skip(d, o):
            return d.dma_sem is None and o.dma_sem is None and d.eng == "pe" and o.eng == "pe"

        for o in self.ops:
            for d in o.deps:
                if d.dma_sem is None and not skip(d, o):
                    d.sig = True
        for o in final_wait_ops:
            if o.dma_sem is None:
                o.sig = True
        for e in ENG:
            c = 0
            for o in self.per[e]:
                if o.dma_sem is None and o.sig:
                    c += 1
                    o.seq = c
        with ExitStack() as es:
            esem = {e: es.enter_context(nc.semaphore(f"s_{e}")) for e in ENG}
            dsem = {k: es.enter_context(nc.semaphore(f"d_{k}")) for k in self.dma_counts}
            block = es.enter_context(nc.Block())

            def dval(d):
                if d.dma_sem in self.total_keys:
                    return 16 * self.dma_counts[d.dma_sem]
                return 16 * d.dma_cnt

            def need(o):
                w = {}
                for d in o.deps:
                    if d.dma_sem is not None:
                        key, val = ("d", d.dma_sem), dval(d)
                    else:
                        if skip(d, o):
                            continue
                        key, val = ("e", d.eng), d.seq
                    if w.get(key, 0) < val:
                        w[key] = val
                return w

            def run(ename):
                def body(eng):
                    waited = {}
                    for o in self.per[ename]:
                        for key, val in need(o).items():
                            if waited.get(key, 0) >= val:
                                continue
                            waited[key] = val
                            sem = dsem[key[1]] if key[0] == "d" else esem[key[1]]
                            eng.wait_ge(sem, val)
                        ins = o.fn(eng)
                        if o.dma_sem is not None:
                            ins.then_inc(dsem[o.dma_sem], 16)
                        elif o.sig:
                            ins.then_inc(esem[ename], 1)
                    if ename == "sp":
                        for o in final_wait_ops:
                            if o.dma_sem is not None:
                                eng.wait_ge(dsem[o.dma_sem], dval(o))
                            else:
                                eng.wait_ge(esem[o.eng], o.seq)
                return body

            block.tensor(run("pe"))
            block.scalar(run("act"))
            block.vector(run("dve"))
            block.gpsimd(run("pool"))
            block.sync(run("sp"))


class Arena:
    def __init__(self, nc, es, name, nbytes):
        self.t = es.enter_context(nc.sbuf_tensor(name, [128, nbytes // 2], BF16))
        self.cap = nbytes
        self.off = 0

    def alloc(self, shape, dt):
        n = 1
        for s_ in shape[1:]:
            n *= s_
        esz = 2 if dt == BF16 else 4
        o = self.off
        self.off += (n * esz + 63) // 64 * 64
        assert self.off <= self.cap, (self.off, self.cap)
        ap = self.t[0:shape[0], o // 2:(o + n * esz) // 2]
        if dt != BF16:
            ap = ap.bitcast(dt)
        if len(shape) > 2:
            names = " ".join(f"d{k}" for k in range(len(shape) - 1))
            ap = ap.rearrange(f"p ({names}) -> p {names}", **{f"d{k}": shape[k + 1] for k in range(len(shape) - 1)})
        return ap


def build(npass=NPASS_FULL, debug=False, stages=4):
    nc = bass.Bass("TRN2", target_bir_lowering=False)

    def din(name, shape):
        return nc.dram_tensor(name, list(shape), F32, kind="ExternalInput").ap()

    x_d = din("x", [SEQ, D])
    win_d = din("w_in", [D, 3840])
    wpa_d = din("wpa", [512, D])
    wpb_d = din("wpb", [512, D])
    wout_d = din("wout", [D, D])
    wexp_d = [din("wgp", [16, 128, 4096]), din("wup", [16, 128, 4096]), din("wdp", [16, 128, 4096])]
    wr_d = din("wr", [D, 20])
    rb_d = din("rb", [1, 20])
    gacol_d = din("gacol", [128, 8])
    gfcol_d = din("gfcol", [128, 8])
    gvcol_d = din("gvcol", [128, 4])
    gfin_d = din("gfin", [1, D])
    wst_d = din("wst", [128, 8, 128])
    wmask_d = din("wmask", [128, 8, 128])
    bst_d = din("bst", [128, 4, 128])
    sinks_d = din("sinks", [128, 4])
    bias_d = din("bias", [128, 2, 2, 4, 128])
    mask_d = din("maskc", [128, 2, 2, 4, 128])
    idn_d = din("idn", [128, 128])
    utri_d = din("utri", [128, 128])
    cst_d = din("cst", [1, 8 + 256 + 32])
    iop_d = din("iop", [128, 1])
    y_d = nc.dram_tensor("y", [SEQ, D], F32, kind="ExternalOutput").ap()
    hS_d = nc.dram_tensor("hS", [SEQ, D], F32, kind="Internal").ap()
    hn2S_d = nc.dram_tensor("hn2S", [SEQ, D], BF16, kind="Internal").ap()
    Xs_d = nc.dram_tensor("Xs", [NSLOT * 512, D], BF16, kind="Internal").ap()
    Ys_d = nc.dram_tensor("Ys", [NSLOT * 512, D], F32, kind="Internal").ap()
    winS_d = nc.dram_tensor("winS", [25, 128, 1024], BF16, kind="Internal").ap()
    wscr_d = [nc.dram_tensor(f"wscr{m}", [16 * 128, 4096], BF16, kind="Internal").ap() for m in range(3)]
    if debug:
        dbg_d = nc.dram_tensor("dbg", [PT, D], F32, kind="ExternalOutput").ap()
        dbgr_d = nc.dram_tensor("dbgr", [128, 6, NTT], F32, kind="ExternalOutput").ap()

    S = Sched()
    S.total_keys.update(["const"])

    with ExitStack() as es:
        def sb(name, shape, dt):
            return es.enter_context(nc.sbuf_tensor(name, list(shape), dt))

        ident = sb("ident", [128, 128], BF16)
        gacol = sb("gacol_s", [128, 8], F32)
        gfcol = sb("gfcol_s", [128, 8], F32)
        gvcol = sb("gvcol_s", [128, 4], F32)
        wstb = sb("wstb", [128, 8, 128], BF16)
        bst = sb("bst_s", [128, 4, 128], F32)
        sinkexp = sb("sinkexp", [128, 4], F32)
        BMhi = sb("BMhi", [128, 2048], BF16)
        BMlo = sb("BMlo", [128, 2048], BF16)
        ones64 = sb("ones64", [128, 64], BF16)
        ones128 = sb("ones128", [128, 128], BF16)
        utri = sb("utri_s", [128, 128], BF16)
        cst = sb("cst_s", [128, 8 + 256 + 32], F32)
        iop = sb("iop_s", [128, 1], F32)
        wr = sb("wr_s", [128, 8, 20], BF16)
        rb = sb("rb_s", [128, 20], F32)
        lg = sb("lg_all", [128, NTT, 20], F32)
        posA = sb("posA", [128, NTT], I32)
        posB = sb("posB", [128, NTT], I32)
        wA = sb("wA", [128, NTT], F32)
        wB = sb("wB", [128, NTT], F32)
        idxW = sb("idxW", [128, NSLOT], I32)
        dummy = sb("bar_dummy", [128, 1], F32)
        gfin = sb("gfin_s", [128, D], F32)
        NEB = 2
        wreg = sb("wreg", [128, NEB * 12288], BF16)
        wgs = [wreg[:, b * 12288:b * 12288 + 4096].rearrange("p (c n) -> p c n", c=8) for b in range(NEB)]
        wus = [wreg[:, b * 12288 + 4096:b * 12288 + 8192].rearrange("p (c n) -> p c n", c=8) for b in range(NEB)]
        wds = [wreg[:, b * 12288 + 8192:b * 12288 + 12288].rearrange("p (c n) -> p c n", c=4) for b in range(NEB)]
        wflat = [[wreg[:, b * 12288 + m * 4096:b * 12288 + (m + 1) * 4096] for m in range(3)] for b in range(NEB)]
        wpa = wreg[:, 0:4096].rearrange("p (c n) -> p c n", c=4)
        wout = wreg[:, 4096:12288].rearrange("p (c n) -> p c n", c=8)
        wpb = wreg[:, 12288:16384].rearrange("p (c n) -> p c n", c=4)
        stgs = [wreg[:, 20480:24576], wreg[:, 16384:20480]]
        AR = Arena(nc, es, "arena", 133 * 1024)
        H = AR.alloc([128, NT, D], F32)
        BM = H[:, 6:8, :].rearrange("p a (k v j t) -> p (a k) v j t", k=1, v=2, j=4)
        hn2T = AR.alloc([128, 8, 512], BF16)
        wv = AR.alloc([128, 8, 512], BF16)
        wva = AR.alloc([128, 8, 128], BF16)
        NRING = 6
        ring = [AR.alloc([128, 8, 128], BF16) for _ in range(NRING)]
        hn2 = [AR.alloc([128, D], BF16) for _ in range(2)]
        hn = hn2[0]
        hnT = AR.alloc([128, 8, 512], BF16)
        hnTb = AR.alloc([128, 8, 512], BF16)
        ssA = AR.alloc([128, 4], F32)
        rsA = AR.alloc([128, 4], F32)
        ssF = AR.alloc([128, 4], F32)
        rsF = AR.alloc([128, 4], F32)
        wst = hnT[:, 0:2, :].rearrange("p a (b t) -> p (a b) t", b=4)
        wmask = hnT[:, 2:4, :].rearrange("p a (b t) -> p (a b) t", b=4)
        ss = AR.alloc([128, 8], F32)
        rstd = AR.alloc([128, 8], F32)
        uT = AR.alloc([128, 4, 512], BF16)
        qT = AR.alloc([128, 4, 512], BF16)
        kT = AR.alloc([128, (NT + 1) * 128], BF16)
        vatt = AR.alloc([128, NT + 1, 128], BF16)
        gv2 = [AR.alloc([128, 512], F32) for _ in range(2)]
        vnh2 = [AR.alloc([128, 512], BF16) for _ in range(2)]
        bnst2 = [AR.alloc([128, 6], F32) for _ in range(2)]
        mv2 = [AR.alloc([128, 2], F32) for _ in range(2)]
        rsv2 = [AR.alloc([128, 1], F32) for _ in range(2)]
        nmr2 = [AR.alloc([128, 1], F32) for _ in range(2)]
        gtmp = AR.alloc([128, 4, 128], F32)
        pT = AR.alloc([128, 2048], BF16)
        den = AR.alloc([128, 512], F32)
        aT = AR.alloc([128, 4, 512], BF16)
        bT = AR.alloc([128, 4, 512], BF16)
        sga = AR.alloc([128, 512], F32)
        sgb = AR.alloc([128, 512], F32)
        mT = AR.alloc([128, 8, 512], BF16)
        maskc = mT[:, 0:4, :].rearrange("p a (b c t) -> p (a b c) t", b=2, c=2).rearrange("p (v k h) t -> p v k h t", v=2, k=2)
        side1_end = AR.off
        AR.off = 0
        NB_ = NTT
        r_m = AR.alloc([128, NB_], F32)
        r_og = AR.alloc([128, NB_, 4], F32)
        r_eg = AR.alloc([128, NB_, 4], F32)
        r_gp = AR.alloc([128, NB_], F32)
        r_sel = AR.alloc([128, NB_, 4, 4], F32)
        r_es = AR.alloc([128, NB_, 4], F32)
        r_m1 = AR.alloc([128, NB_], F32)
        r_o1 = AR.alloc([128, NB_, 4], F32)
        r_es2 = AR.alloc([128, NB_, 4], F32)
        r_m2 = AR.alloc([128, NB_], F32)
        r_o2 = AR.alloc([128, NB_, 4], F32)
        r_d = AR.alloc([128, NB_], F32)
        r_w1 = AR.alloc([128, NB_], F32)
        M1 = AR.alloc([128, NB_, 4, 4], F32)
        M2 = AR.alloc([128, NB_, 4, 4], F32)
        Mb = AR.alloc([128, NB_ * 16], BF16)
        R1s = AR.alloc([128, NB_, 16], F32)
        Ca = AR.alloc([128, NB_, 16], F32)
        Cb = AR.alloc([128, NB_, 16], F32)
        Tts = AR.alloc([128, NB_, 16], F32)
        r_cmp = AR.alloc([128, 16, 16], F32)
        r_cmp2 = AR.alloc([128, NSLOT, 16], F32)
        r_nb = AR.alloc([128, 16], F32)
        r_ob = AR.alloc([128, 16], F32)
        r_oe = AR.alloc([128, 16], F32)
        r_pf = AR.alloc([128, NB_], F32)
        r_eid = AR.alloc([128, NSLOT], F32)
        NXIN = 12
        xin = [AR.alloc([128, D], BF16) for _ in range(NXIN)]
        xs = [AR.alloc([128, 4, D], BF16) for _ in range(2)]
        xT = [AR.alloc([128, 8, 512], BF16) for _ in range(2)]
        sgs = [AR.alloc([128, 512], F32) for _ in range(2)]
        hid = [AR.alloc([128, 4, 512], BF16) for _ in range(2)]
        NYS = 3
        ys = [AR.alloc([128, D], F32) for _ in range(NYS)]
        side2_end = AR.off
        AR.off = 0
        N4 = 6
        YA = [AR.alloc([128, D], F32) for _ in range(N4)]
        YB = [AR.alloc([128, D], F32) for _ in range(N4)]
        hb = [AR.alloc([128, D], F32) for _ in range(N4)]
        ss4 = [AR.alloc([128, 1], F32) for _ in range(N4)]
        rs4 = [AR.alloc([128, 1], F32) for _ in range(N4)]
        hn4 = AR.alloc([128, D], BF16)
        PQ = es.enter_context(nc.psum_tensor("pq", [128, 4096], F32))
        PB = [PQ[:, i * 512:(i + 1) * 512] for i in range(8)]

        T = {}

        def tk(n):
            if n not in T:
                T[n] = Tok(n)
            return T[n]

        tH = [tk(f"H{i}") for i in range(NT)]
        tPB = [tk(f"PB{i}") for i in range(8)]
        tE = [[tk(f"wg{i}"), tk(f"wu{i}"), tk(f"wd{i}")] for i in range(NEB)]
        tRing = [tk(f"ring{i}") for i in range(NRING)]

        def op(eng, fn, reads=(), writes=(), dma=None):
            return S.op(eng, fn, [tk(r) if isinstance(r, str) else r for r in reads],
                        [tk(w) if isinstance(w, str) else w for w in writes], dma)

        def cdma(eng, out, in_, w):
            op(eng, lambda e, out=out, in_=in_: e.dma_start(out=out, in_=in_), writes=[w], dma="const")

        cdma("sp", gacol[:], gacol_d, "gacol")
        cdma("sp", gfcol[:], gfcol_d, "gfcol")
        cdma("sp", gvcol[:], gvcol_d, "gvcol")
        cdma("sp", bst[:], bst_d, "bst")
        cdma("sp", sinkexp[:], sinks_d, "sinkexp")
        cdma("sp", BM, bias_d, "BM")
        cdma("sp", rb[:], rb_d.partition_broadcast(128), "rb")
        cdma("sp", cst[:], cst_d.partition_broadcast(128), "cst")
        cdma("sp", iop[:], iop_d, "iop")
        cdma("pool", ident[:], idn_d, "ident")
        cdma("pool", utri[:], utri_d, "utri")
        cdma("pool", wst, wst_d, "wst_a")
        cdma("pool", wmask, wmask_d, "wmask_a")
        cdma("pool", maskc, mask_d, "maskc_a")
        cdma("pool", wr[:], wr_d.rearrange("(c p) n -> p c n", p=128), "wr")
        cdma("pool", wv, win_d[:, V_OFF:V_OFF + 512].rearrange("(c p) n -> p c n", p=128), "wv")
        cdma("pool", wva, win_d[:, VA_OFF:VA_OFF + 128].rearrange("(c p) n -> p c n", p=128), "wva")
        op("dve", lambda e: e.tensor_tensor(out=wstb[:], in0=wst, in1=wmask, op=ALU.mult),
           reads=["wst_a", "wmask_a"], writes=["wstb", "hnT"])
        BMf = BM.rearrange("p k v j t -> p (k v j t)")
        op("dve", lambda e: e.tensor_tensor(out=BMf, in0=BMf, in1=maskc.rearrange("p k v j t -> p (k v j t)"), op=ALU.add),
           reads=["maskc_a"], writes=["BM", "mT"])
        op("dve", lambda e: e.tensor_copy(out=BMhi[:], in_=BMf), reads=["BM"], writes=["BMhi"])
        op("dve", lambda e: e.tensor_tensor(out=BMlo[:], in0=BMf, in1=BMhi[:], op=ALU.subtract),
           reads=["BM", "BMhi"], writes=["BMlo", tH[6], tH[7]])
        op("act", lambda e: e.activation(out=sinkexp[:], in_=sinkexp[:], func=AF.Exp), reads=[], writes=["sinkexp"])
        op("dve", lambda e: e.memset(ones64[:], 1.0), writes=["ones64"])
        op("dve", lambda e: e.memset(ones128[:], 1.0), writes=["ones128"])

        out_ops = []

        def rstd_ops(n):
            op("dve", lambda e: e.tensor_scalar(out=rstd[:, 0:n], in0=ss[:, 0:n], scalar1=1.0 / D, scalar2=EPS,
                                                op0=ALU.mult, op1=ALU.add), reads=["ss"], writes=["rstd"])
            op("act", lambda e: e.sqrt(out=rstd[:, 0:n], in_=rstd[:, 0:n]), reads=[], writes=["rstd"])
            op("dve", lambda e: e.reciprocal(out=rstd[:, 0:n], in_=rstd[:, 0:n]), reads=[], writes=["rstd"])

        def norm_transpose(ti_list, gcol, dstT, dst_col0, dst_tok, after_hn=None):
            n = len(ti_list)
            for k, i in enumerate(ti_list):
                hb2 = hn2[k % 2]
                op("act", lambda e, i=i, k=k, hb2=hb2: e.activation(out=hb2, in_=H[:, i, :], func=AF.Square,
                                                                   accum_out=ss[:, k:k + 1]),
                   reads=[tH[i]], writes=[f"hn{k % 2}", "ss"])
            rstd_ops(n)
            for k, i in enumerate(ti_list):
                hb2 = hn2[k % 2]
                hnn = f"hn{k % 2}"
                op("act", lambda e, i=i, k=k, hb2=hb2: e.activation(out=hb2, in_=H[:, i, :], func=AF.Identity,
                                                                   scale=rstd[:, k:k + 1]),
                   reads=[tH[i], "rstd"], writes=[hnn])
                if after_hn is not None:
                    after_hn(i, hb2, hnn)
                bank = k % 2
                pbT = PB[bank].bitcast(BF16).rearrange("p (c t) -> p c t", c=8)
                for c in range(8):
                    op("pe", lambda e, c=c, pbT=pbT, hb2=hb2: e.transpose(out=pbT[:, c, :], in_=hb2[:, c * 128:(c + 1) * 128],
                                                                         identity=ident[:]),
                       reads=[hnn, "ident"], writes=[tPB[bank]])
                c0 = dst_col0(i)
                op("dve", lambda e, pbT=pbT, c0=c0: e.tensor_tensor(
                    out=dstT[:, :, c0:c0 + 128], in0=pbT,
                    in1=gcol[:].unsqueeze(2).to_broadcast([128, 8, 128]), op=ALU.mult),
                   reads=[tPB[bank], "gacol", "gfcol"], writes=[dst_tok(i)])

        precast = [(m, ex) for ex in range(16) for m in range(3)]
        pc_ctr = [0]

        def precast_some(n):
            if stages == 1:
                return
            for _ in range(n):
                if pc_ctr[0] >= len(precast):
                    return
                m, ex = precast[pc_ctr[0]]
                q = pc_ctr[0] % 2
                pc_ctr[0] += 1
                op("pool", lambda e, m=m, ex=ex, q=q: e.dma_start(out=stgs[q], in_=wexp_d[m][ex]),
                   writes=[f"stg{q}"], dma=f"stg{q}")
                op("sp", lambda e, m=m, ex=ex, q=q: e.dma_start(out=wscr_d[m][ex * 128:(ex + 1) * 128, :], in_=stgs[q]),
                   reads=[f"stg{q}"], writes=[f"wscr{q}"], dma=f"wscr{q}")

        hnTs = [hnT, hnTb]
        hnTn = ["hnT", "hnTb"]
        ring_ctr = [0]

        blk_ids = {}

        def stream_block(col0):
            r = ring_ctr[0] % NRING
            ring_ctr[0] += 1
            first = col0 not in blk_ids
            if first:
                blk_ids[col0] = len(blk_ids)
            bid = blk_ids[col0]
            if first:
                op("pool", lambda e, r=r, col0=col0: e.dma_start(
                    out=ring[r], in_=win_d[:, col0:col0 + 128].rearrange("(c p) n -> p c n", p=128)),
                   writes=[tRing[r]], dma=f"ring{r}")
                op("sp", lambda e, r=r, bid=bid: e.dma_start(out=winS_d[bid], in_=ring[r].rearrange("p c n -> p (c n)")),
                   reads=[tRing[r]], writes=[f"winS{bid}"], dma=f"ring{r}")
            else:
                op("pool", lambda e, r=r, bid=bid: e.dma_start(out=ring[r].rearrange("p c n -> p (c n)"), in_=winS_d[bid]),
                   reads=[f"winS{bid}"], writes=[tRing[r]], dma=f"ring{r}")
            return ring[r], tRing[r]

        def load_x(g):
            for k in range(4):
                i = (g % 2) * 4 + k
                t0_ = g * 512 + k * 128
                op("sp", lambda e, i=i, t0_=t0_: e.dma_start(out=H[:, i, :], in_=x_d[t0_:t0_ + 128, :]),
                   writes=[tH[i]], dma=f"H{i}")

        def norm_pieces(tiles, gcol, dstT, dst_tok, ssb, rsb, ssn, rsn, banks, after_hn=None):
            def sq(k):
                i = tiles[k]
                hb2 = hn2[k % 2]
                op("act", lambda e: e.activation(out=hb2, in_=H[:, i, :], func=AF.Square, accum_out=ssb[:, k:k + 1]),
                   reads=[tH[i]], writes=[f"hn{k % 2}", ssn])

            def rs():
                op("dve", lambda e: e.tensor_scalar(out=rsb, in0=ssb, scalar1=1.0 / D, scalar2=EPS,
                                                    op0=ALU.mult, op1=ALU.add), reads=[ssn], writes=[rsn])
                op("act", lambda e: e.sqrt(out=rsb, in_=rsb), reads=[], writes=[rsn])
                op("dve", lambda e: e.reciprocal(out=rsb, in_=rsb), reads=[], writes=[rsn])

            def tr(k):
                i = tiles[k]
                hb2 = hn2[k % 2]
                hnn = f"hn{k % 2}"
                op("act", lambda e: e.activation(out=hb2, in_=H[:, i, :], func=AF.Identity, scale=rsb[:, k:k + 1]),
                   reads=[tH[i], rsn], writes=[hnn])
                if after_hn is not None:
                    after_hn(i, hb2, hnn)
                bank = banks[k % 2]
                pbT = PB[bank].bitcast(BF16).rearrange("p (c t) -> p c t", c=8)
                for c in range(8):
                    op("pe", lambda e, c=c: e.transpose(out=pbT[:, c, :], in_=hb2[:, c * 128:(c + 1) * 128], identity=ident[:]),
                       reads=[hnn, "ident"], writes=[tPB[bank]])
                op("dve", lambda e: e.tensor_tensor(out=dstT[:, :, k * 128:(k + 1) * 128], in0=pbT,
                                                    in1=gcol[:].unsqueeze(2).to_broadcast([128, 8, 128]), op=ALU.mult),
                   reads=[tPB[bank], "gacol", "gfcol"], writes=[dst_tok])
            return sq, rs, tr

        def make_st(g):
            ps_i, st = g // 2, g % 2
            tok0 = ps_i * PT
            tiles = [st * 4 + k for k in range(4)]
            hT, hTn = hnTs[g % 2], hnTn[g % 2]

            def A_pieces():
                return norm_pieces(tiles, gacol, hT, hTn, ssA, rsA, "ssA", "rsA", (0, 1))

            def proj_fm(col0, bank, evac):
                wb_, wt_ = stream_block(col0)
                for c in range(8):
                    op("pe", lambda e, c=c: e.matmul(PB[bank], lhsT=wb_[:, c, :], rhs=hT[:, c, :], start=(c == 0), stop=(c == 7)),
                       reads=[wt_, hTn], writes=[tPB[bank]])
                evac(bank)

            def B(hook):
                if st == 0 and ps_i > 0:
                    op("dve", lambda e: e.tensor_copy(out=kT[:, 0:128], in_=kT[:, NT * 128:(NT + 1) * 128]),
                       reads=[], writes=["kT"])
                    op("dve", lambda e: e.tensor_copy(out=vatt[:, 0, :], in_=vatt[:, NT, :]), reads=[], writes=["vatt"])
                blk = 0
                for j in range(4):
                    proj_fm(U_OFF + j * 128, 1 + (j % 2),
                            lambda bank, j=j: op("act", lambda e: e.activation(out=uT[:, j, :], in_=PB[bank],
                                                                              func=AF.Gelu_apprx_tanh),
                                                 reads=[tPB[bank]], writes=["uT"]))
                    if hook is not None:
                        hook(blk)
                    blk += 1
                for j in range(4):
                    proj_fm(Q_OFF + j * 128, 1 + (j % 2),
                            lambda bank, j=j: op("act", lambda e: e.activation(out=qT[:, j, :], in_=PB[bank],
                                                                              func=AF.Identity, scale=0.125),
                                                 reads=[tPB[bank]], writes=["qT"]))
                    if hook is not None:
                        hook(blk)
                    blk += 1
                kc0 = (1 + st * 4) * 128
                proj_fm(K_OFF, 1,
                        lambda bank: op("dve", lambda e: e.tensor_copy(out=kT[:, kc0:kc0 + 512], in_=PB[bank]),
                                        reads=[tPB[bank]], writes=["kT"]))
                if hook is not None:
                    hook(blk)

            def t_v(i):
                ts = (i - st * 4) * 128
                slot = 1 + i
                q2 = i % 2
                gv, vnh, bnst, mv, rsv, nmr = gv2[q2], vnh2[q2], bnst2[q2], mv2[q2], rsv2[q2], nmr2[q2]
                nm = lambda x: f"{x}{q2}"
                for c in range(8):
                    op("pe", lambda e, c=c, ts=ts: e.matmul(PB[0], lhsT=hT[:, c, ts:ts + 128], rhs=wv[:, c, :],
                                                            start=(c == 0), stop=(c == 7)),
                       reads=[hTn, "wv"], writes=[tPB[0]])
                for c in range(8):
                    op("pe", lambda e, c=c, ts=ts: e.matmul(PB[1][:, 0:128], lhsT=hT[:, c, ts:ts + 128],
                                                            rhs=wva[:, c, :], start=(c == 0), stop=(c == 7)),
                       reads=[hTn, "wva"], writes=[tPB[1]])
                op("act", lambda e, gv=gv: e.activation(out=gv, in_=PB[0], func=AF.Gelu_apprx_tanh),
                   reads=[tPB[0]], writes=[nm("gv")])
                op("dve", lambda e, slot=slot: e.tensor_copy(out=vatt[:, slot, :], in_=PB[1][:, 0:128]),
                   reads=[tPB[1]], writes=["vatt"])
                op("dve", lambda e, gv=gv, bnst=bnst: e.bn_stats(out=bnst, in_=gv), reads=[nm("gv")], writes=[nm("bnst")])
                op("dve", lambda e, mv=mv, bnst=bnst: e.bn_aggr(out=mv, in_=bnst), reads=[nm("bnst")], writes=[nm("mv")])
                op("dve", lambda e, mv=mv, rsv=rsv: e.tensor_scalar_add(out=rsv, in0=mv[:, 1:2], scalar1=EPS),
                   reads=[nm("mv")], writes=[nm("rsv")])
                op("act", lambda e, rsv=rsv: e.sqrt(out=rsv, in_=rsv), reads=[], writes=[nm("rsv")])
                op("dve", lambda e, rsv=rsv: e.reciprocal(out=rsv, in_=rsv), reads=[], writes=[nm("rsv")])
                op("dve", lambda e, mv=mv, rsv=rsv, nmr=nmr: e.tensor_scalar(out=nmr, in0=mv[:, 0:1], scalar1=rsv, scalar2=-1.0,
                                                                           op0=ALU.mult, op1=ALU.mult),
                   reads=[nm("mv"), nm("rsv")], writes=[nm("nmr")])
                op("act", lambda e, gv=gv, vnh=vnh, rsv=rsv, nmr=nmr: e.activation(out=vnh, in_=gv, func=AF.Identity,
                                                                                 scale=rsv, bias=nmr),
                   reads=[nm("gv"), nm("rsv"), nm("nmr")], writes=[nm("vnh")])

            def t_sc(i):
                ts = (i - st * 4) * 128
                slot = 1 + i
                gblk = ps_i * NT + i
                for kh in range(2):
                    pr = slice(kh * 64, (kh + 1) * 64)
                    for vi in range(2 if gblk > 0 else 1):
                        bank = 4 + kh * 2 + vi
                        ksl = slot - vi
                        pc = (kh * 2 + vi) * 512
                        op("pe", lambda e, pr=pr, ksl=ksl, ts=ts, bank=bank: e.matmul(
                            PB[bank].rearrange("p (j t) -> p j t", j=4),
                            lhsT=kT[pr, ksl * 128:(ksl + 1) * 128], rhs=qT[pr, :, ts:ts + 128],
                            start=True, stop=False),
                           reads=["kT", "qT"], writes=[tPB[bank]])
                        op("pe", lambda e, bank=bank, pc=pc: e.matmul(PB[bank], lhsT=ident[:], rhs=BMhi[:, pc:pc + 512],
                                                                      start=False, stop=False),
                           reads=["ident", "BMhi"], writes=[tPB[bank]])
                        op("pe", lambda e, bank=bank, pc=pc: e.matmul(PB[bank], lhsT=ident[:], rhs=BMlo[:, pc:pc + 512],
                                                                      start=False, stop=True),
                           reads=["ident", "BMlo"], writes=[tPB[bank]])
                scb = PQ[:, 2048:4096]
                op("act", lambda e, scb=scb: e.activation(out=pT, in_=scb, func=AF.Exp),
                   reads=[tPB[4], tPB[5], tPB[6], tPB[7]], writes=["pT"])

            def t_pv(i):
                ts = (i - st * 4) * 128
                slot = 1 + i
                gblk = ps_i * NT + i
                nv = 2 if gblk > 0 else 1
                for (bank, use_v) in ((3, True), (0, False)):
                    for kh in range(2):
                        kw = {"tile_position": (0, 64)} if kh else {}
                        for vi in range(nv):
                            ksl = slot - vi
                            lhs = vatt[:, ksl, kh * 64:(kh + 1) * 64] if use_v else ones64[:]
                            pc = (kh * 2 + vi) * 512
                            op("pe", lambda e, bank=bank, kh=kh, vi=vi, lhs=lhs, pc=pc, kw=kw, nv=nv: e.matmul(
                                PB[bank][kh * 64:(kh + 1) * 64, :], lhsT=lhs, rhs=pT[:, pc:pc + 512],
                                start=(vi == 0), stop=(vi == nv - 1), **kw),
                               reads=["vatt", "ones64", "pT"], writes=[tPB[bank]])
                op("dve", lambda e: e.tensor_tensor(
                    out=den.rearrange("p (j t) -> p j t", j=4), in0=PB[0].rearrange("p (j t) -> p j t", j=4),
                    in1=sinkexp[:].unsqueeze(2).to_broadcast([128, 4, 128]), op=ALU.add),
                   reads=[tPB[0], "sinkexp"], writes=["den"])
                op("dve", lambda e: e.reciprocal(out=den, in_=den), reads=[], writes=["den"])
                op("dve", lambda e, ts=ts: e.tensor_tensor(
                    out=bT[:, :, ts:ts + 128], in0=PB[3].rearrange("p (j t) -> p j t", j=4),
                    in1=den.rearrange("p (j t) -> p j t", j=4), op=ALU.mult),
                   reads=[tPB[3], "den"], writes=["bT"])

            def t_sp(i):
                ts = (i - st * 4) * 128
                q2 = i % 2
                vnh = vnh2[q2]
                pbs = PB[2].rearrange("p (j t) -> p j t", j=4)
                for g in range(8):
                    lo = (g % 2) * 64
                    kw = {"tile_position": (0, 64)} if g % 2 else {}
                    op("pe", lambda e, g=g, lo=lo, kw=kw, vnh=vnh: e.matmul(
                        pbs[lo:lo + 64, g // 2, :], lhsT=vnh[:, g * 64:(g + 1) * 64], rhs=wstb[:, g, :],
                        start=True, stop=True, **kw),
                       reads=[f"vnh{q2}", "wstb"], writes=[tPB[2]])
                op("dve", lambda e: e.tensor_tensor(out=gtmp, in0=pbs,
                                                    in1=gvcol[:].unsqueeze(2).to_broadcast([128, 4, 128]), op=ALU.mult),
                   reads=[tPB[2], "gvcol"], writes=["gtmp"])
                op("dve", lambda e: e.tensor_tensor(out=gtmp, in0=gtmp, in1=bst[:], op=ALU.add),
                   reads=["bst"], writes=["gtmp"])
                op("dve", lambda e, ts=ts: e.tensor_tensor(out=aT[:, :, ts:ts + 128], in0=gtmp,
                                                           in1=uT[:, :, ts:ts + 128], op=ALU.mult),
                   reads=["gtmp", "uT"], writes=["aT"])

            def C():
                precast_some(2)
                t_v(tiles[0])
                t_sc(tiles[0])
                for k in range(1, 4):
                    precast_some(1 if k < 3 else 2)
                    t_v(tiles[k])
                    t_pv(tiles[k - 1])
                    t_sc(tiles[k])
                    t_sp(tiles[k - 1])
                t_pv(tiles[3])
                t_sp(tiles[3])

            def Dg(hook):
                for j in range(8):
                    b0 = (j % 2) * 4
                    wga, tga = stream_block(GA_OFF + j * 128)
                    for c in range(8):
                        op("pe", lambda e, c=c, wga=wga, b0=b0: e.matmul(PB[b0], lhsT=wga[:, c, :], rhs=hT[:, c, :],
                                                                         start=(c == 0), stop=(c == 7)),
                           reads=[tga, hTn], writes=[tPB[b0]])
                    wgb, tgb = stream_block(GB_OFF + j * 128)
                    for c in range(8):
                        op("pe", lambda e, c=c, wgb=wgb, b0=b0: e.matmul(PB[b0 + 1], lhsT=wgb[:, c, :], rhs=hT[:, c, :],
                                                                         start=(c == 0), stop=(c == 7)),
                           reads=[tgb, hTn], writes=[tPB[b0 + 1]])
                    for c in range(4):
                        op("pe", lambda e, c=c, j=j, b0=b0: e.matmul(PB[b0 + 2], lhsT=wpa[:, c, j * 128:(j + 1) * 128], rhs=aT[:, c, :],
                                                                     start=(c == 0), stop=(c == 3)),
                           reads=[tE[0][0], "aT"], writes=[tPB[b0 + 2]])
                    for c in range(4):
                        op("pe", lambda e, c=c, j=j, b0=b0: e.matmul(PB[b0 + 3], lhsT=wpb[:, c, j * 128:(j + 1) * 128], rhs=bT[:, c, :],
                                                                     start=(c == 0), stop=(c == 3)),
                           reads=["wpb0", "wpb1", "bT"], writes=[tPB[b0 + 3]])
                    op("act", lambda e, b0=b0: e.activation(out=sga, in_=PB[b0], func=AF.Sigmoid),
                       reads=[tPB[b0]], writes=["sga"])
                    op("act", lambda e, b0=b0: e.activation(out=sgb, in_=PB[b0 + 1], func=AF.Sigmoid),
                       reads=[tPB[b0 + 1]], writes=["sgb"])
                    op("dve", lambda e, b0=b0: e.tensor_tensor(out=sga, in0=PB[b0 + 2], in1=sga, op=ALU.mult),
                       reads=[tPB[b0 + 2]], writes=["sga"])
                    op("dve", lambda e, b0=b0: e.tensor_tensor(out=sgb, in0=PB[b0 + 3], in1=sgb, op=ALU.mult),
                       reads=[tPB[b0 + 3]], writes=["sgb"])
                    op("dve", lambda e, j=j: e.tensor_tensor(out=mT[:, j, :], in0=sga, in1=sgb, op=ALU.add),
                       reads=["sga", "sgb"], writes=["mT"])
                    if hook is not None:
                        hook(j)

            def E():
                for i in tiles:
                    ts = (i - st * 4) * 128
                    for hf in range(2):
                        bank = 1 + hf
                        for j in range(8):
                            op("pe", lambda e, j=j, ts=ts, hf=hf, bank=bank: e.matmul(
                                PB[bank][:], lhsT=mT[:, j, ts:ts + 128], rhs=wout[:, j, hf * 512:(hf + 1) * 512],
                                start=(j == 0), stop=(j == 7)),
                               reads=["mT", tE[0][1], tE[0][2]], writes=[tPB[bank]])
                        op("dve", lambda e, i=i, hf=hf, bank=bank: e.tensor_tensor(
                            out=H[:, i, hf * 512:(hf + 1) * 512], in0=PB[bank][:], in1=H[:, i, hf * 512:(hf + 1) * 512],
                            op=ALU.add),
                           reads=[tPB[bank]], writes=[tH[i]])
                    op("sp", lambda e, i=i, tok0=tok0: e.dma_start(out=hS_d[tok0 + i * 128: tok0 + (i + 1) * 128, :],
                                                                   in_=H[:, i, :]),
                       reads=[tH[i]], writes=["hS"], dma=f"H{i}")

                if debug and ps_i == 0:
                    for i in tiles:
                        out_ops.append(op("sp", lambda e, i=i: e.dma_start(out=dbg_d[i * 128:(i + 1) * 128, :], in_=H[:, i, :]),
                                          reads=[tH[i]], dma=f"H{i}"))

            def F_pieces():
                def spill_hn2(i, hb2, hnn):
                    op("sp", lambda e: e.dma_start(out=hn2S_d[tok0 + i * 128: tok0 + (i + 1) * 128, :], in_=hb2),
                       reads=[hnn], writes=["hn2S"], dma="hn2S" + hnn)
                sq, rs, tr = norm_pieces(tiles, gfcol, hn2T, "hn2T", ssF, rsF, "ssF", "rsF", (3, 4), after_hn=spill_hn2)

                def router():
                    for k, i in enumerate(tiles):
                        gi = ps_i * NT + i
                        ts = k * 128
                        for c in range(8):
                            op("pe", lambda e, c=c, ts=ts: e.matmul(PB[7][:, 0:20], lhsT=hn2T[:, c, ts:ts + 128], rhs=wr[:, c, :],
                                                                    start=(c == 0), stop=(c == 7)),
                               reads=["hn2T", "wr"], writes=[tPB[7]])
                        op("dve", lambda e, gi=gi: e.tensor_tensor(out=lg[:, gi, :], in0=PB[7][:, 0:20], in1=rb[:], op=ALU.add),
                           reads=[tPB[7], "rb"], writes=["lg"])
                return sq, rs, tr, router
            return dict(A=A_pieces, B=B, C=C, D=Dg, E=E, F=F_pieces)

        def mk_hook(pieces, base):
            if pieces is None:
                return None
            sq, rs, tr = pieces[0], pieces[1], pieces[2]
            router = pieces[3] if len(pieces) > 3 else None

            def hook(it):
                k = it - base
                if 0 <= k < 4:
                    sq(k)
                if k == 3:
                    rs()
                if 4 <= k < 8:
                    tr(k - 4)
                if k == 8 and router is not None:
                    router()
            return hook

        G = 2 * npass
        sts = [make_st(g) for g in range(G)]
        load_x(0)
        op("pool", lambda e: e.dma_start(out=wpa, in_=wpa_d.rearrange("(c p) n -> p c n", p=128)),
           writes=[tE[0][0]], dma="wg0")
        for a_ in range(2):
            op("pool", lambda e, a_=a_: e.dma_start(
                out=wpb[a_ * 64:(a_ + 1) * 64, :, :],
                in_=wpb_d.rearrange("(a j d) n -> a d j n", a=2, j=4)[a_]),
               writes=[tk(f"wpb{a_}")], dma=f"wpbd{a_}")
        op("pool", lambda e: e.dma_start(out=wout, in_=wout_d.rearrange("(c p) n -> p c n", p=128)),
           writes=[tE[0][1], tE[0][2]], dma="wu0")
        if G > 1:
            load_x(1)
        sq, rs, tr = sts[0]["A"]()
        for k in range(4):
            sq(k)
        rs()
        for k in range(4):
            tr(k)
        for g in range(G):
            fp = sts[g - 1]["F"]() if g > 0 else None
            sts[g]["B"](mk_hook(fp, 0))
            if fp is not None and fp[3] is not None and False:
                pass
            if g >= 1 and g + 1 < G:
                load_x(g + 1)
            sts[g]["C"]()
            ap = sts[g + 1]["A"]() if g + 1 < G else None
            sts[g]["D"](mk_hook(ap, 0))
            sts[g]["E"]()
        sq, rs, tr, router = sts[G - 1]["F"]()
        for k in range(4):
            sq(k)
        rs()
        for k in range(4):
            tr(k)
        router()

        precast_some(100)

        S.barrier("dve", lambda e: e.memset(dummy[:], 0.0))
        NTK = npass * NT

        cdma2 = op("sp", lambda e: e.dma_start(out=gfin[:], in_=gfin_d.partition_broadcast(128)), writes=["gfin"], dma="gfin")

        gl = lg[:, :, 0:4]
        el = lg[:, :, 4:20].rearrange("p t (g x) -> p t g x", g=4)
        bc3 = lambda a: a.unsqueeze(2).to_broadcast([128, NTT, 4])
        if npass < NPASS_FULL:
            op("dve", lambda e: e.memset(lg[:, NTK:, :], 0.0), writes=["lg"])
        dv = lambda fn, r, w: op("dve", fn, reads=r, writes=w)
        dv(lambda e: e.tensor_reduce(out=r_m, in_=gl, axis=AX.X, op=ALU.max), ["lg"], ["r_m"])
        dv(lambda e: e.tensor_tensor(out=r_og, in0=gl, in1=bc3(r_m), op=ALU.is_equal), ["lg", "r_m"], ["r_og"])
        dv(lambda e: e.tensor_tensor(out=r_eg, in0=gl, in1=bc3(r_m), op=ALU.subtract), ["lg", "r_m"], ["r_eg"])
        op("act", lambda e: e.activation(out=r_eg, in_=r_eg, func=AF.Exp), reads=[], writes=["r_eg"])
        dv(lambda e: e.tensor_reduce(out=r_gp, in_=r_eg, axis=AX.X, op=ALU.add), ["r_eg"], ["r_gp"])
        dv(lambda e: e.reciprocal(out=r_gp, in_=r_gp), [], ["r_gp"])
        dv(lambda e: e.tensor_tensor(out=r_sel, in0=el, in1=r_og.unsqueeze(3).to_broadcast([128, NTT, 4, 4]), op=ALU.mult),
           ["lg", "r_og"], ["r_sel"])
        dv(lambda e: e.tensor_reduce(out=r_es, in_=r_sel.rearrange("p t g x -> p t x g"), axis=AX.X, op=ALU.add),
           ["r_sel"], ["r_es"])
        dv(lambda e: e.tensor_reduce(out=r_m1, in_=r_es, axis=AX.X, op=ALU.max), ["r_es"], ["r_m1"])
        dv(lambda e: e.tensor_tensor(out=r_o1, in0=r_es, in1=bc3(r_m1), op=ALU.is_equal), ["r_es", "r_m1"], ["r_o1"])
        dv(lambda e: e.scalar_tensor_tensor(out=r_es2, in0=r_o1, scalar=-1e9, in1=r_es, op0=ALU.mult, op1=ALU.add),
           ["r_o1", "r_es"], ["r_es2"])
        dv(lambda e: e.tensor_reduce(out=r_m2, in_=r_es2, axis=AX.X, op=ALU.max), ["r_es2"], ["r_m2"])
        dv(lambda e: e.tensor_tensor(out=r_o2, in0=r_es2, in1=bc3(r_m2), op=ALU.is_equal), ["r_es2", "r_m2"], ["r_o2"])
        dv(lambda e: e.tensor_tensor(out=r_d, in0=r_m2, in1=r_m1, op=ALU.subtract), ["r_m1", "r_m2"], ["r_d"])
        op("act", lambda e: e.activation(out=r_d, in_=r_d, func=AF.Exp), reads=[], writes=["r_d"])
        dv(lambda e: e.tensor_scalar_add(out=r_w1, in0=r_d, scalar1=1.0), ["r_d"], ["r_w1"])
        dv(lambda e: e.reciprocal(out=r_w1, in_=r_w1), [], ["r_w1"])
        dv(lambda e: e.tensor_tensor(out=wB[:], in0=r_d, in1=r_w1, op=ALU.mult), ["r_d", "r_w1"], ["wB"])
        dv(lambda e: e.tensor_tensor(out=wA[:], in0=r_w1, in1=r_gp, op=ALU.mult), ["r_w1", "r_gp"], ["wA"])
        dv(lambda e: e.tensor_tensor(out=wB[:], in0=wB[:], in1=r_gp, op=ALU.mult), ["r_gp"], ["wB"])
        bg = lambda a: a.unsqueeze(3).to_broadcast([128, NTT, 4, 4])
        bx = lambda a: a.unsqueeze(2).to_broadcast([128, NTT, 4, 4])
        dv(lambda e: e.tensor_tensor(out=M1, in0=bg(r_og), in1=bx(r_o1), op=ALU.mult), ["r_og", "r_o1"], ["M1"])
        dv(lambda e: e.tensor_tensor(out=M2, in0=bg(r_og), in1=bx(r_o2), op=ALU.mult), ["r_og", "r_o2"], ["M2"])
        M1f = M1.rearrange("p t g x -> p t (g x)")
        M2f = M2.rearrange("p t g x -> p t (g x)")
        dv(lambda e: e.tensor_tensor(out=Mb.rearrange("p (t n) -> p t n", n=16), in0=M1f, in1=M2f, op=ALU.add),
           ["M1", "M2"], ["Mb"])
        if npass < NPASS_FULL:
            dv(lambda e: e.memset(Mb[:, NTK * 16:], 0.0), [], ["Mb"])
        op("pe", lambda e: e.matmul(PB[0][:], lhsT=utri[:], rhs=Mb, start=True, stop=True),
           reads=["utri", "Mb"], writes=[tPB[0]])
        op("pe", lambda e: e.matmul(PB[1][:], lhsT=ones128[:], rhs=Mb, start=True, stop=True),
           reads=["ones128", "Mb"], writes=[tPB[1]])
        dv(lambda e: e.tensor_copy(out=R1s.rearrange("p t n -> p (t n)"), in_=PB[0][:]), [tPB[0]], ["R1s"])
        dv(lambda e: e.tensor_copy(out=Tts.rearrange("p t n -> p (t n)"), in_=PB[1][:]), [tPB[1]], ["Tts"])
        dv(lambda e: e.tensor_copy(out=Ca, in_=Tts), ["Tts"], ["Ca"])
        cur, nxt, cn, nn = Ca, Cb, "Ca", "Cb"
        sh = 1
        while sh < NTT:
            dv(lambda e, cur=cur, nxt=nxt, sh=sh: e.tensor_copy(out=nxt[:, 0:sh, :], in_=cur[:, 0:sh, :]), [cn], [nn])
            dv(lambda e, cur=cur, nxt=nxt, sh=sh: e.tensor_tensor(out=nxt[:, sh:, :], in0=cur[:, sh:, :],
                                                                  in1=cur[:, 0:NTT - sh, :], op=ALU.add), [cn], [nn])
            cur, nxt, cn, nn = nxt, cur, nn, cn
            sh *= 2
        Cin, cin_n, Cex, cex_n = cur, cn, nxt, nn
        dv(lambda e: e.tensor_tensor(out=Cex, in0=Cin, in1=Tts, op=ALU.subtract), [cin_n, "Tts"], [cex_n])
        ntot = Cin[:, NTT - 1, :]
        thr = cst[:, 0:8]
        tri = cst[:, 8:264].rearrange("p (a b) -> p a b", a=16)
        sio = cst[:, 264:264 + NSLOT]
        cmp8 = r_cmp.rearrange("p a b -> p (a b)")[:, 0:128].rearrange("p (a b) -> p a b", b=8)
        dv(lambda e: e.tensor_tensor(out=cmp8, in0=ntot.unsqueeze(2).to_broadcast([128, 16, 8]),
                                     in1=thr.unsqueeze(1).to_broadcast([128, 16, 8]), op=ALU.is_gt),
           [cin_n, "cst"], ["r_cmp"])
        dv(lambda e: e.tensor_reduce(out=r_nb, in_=cmp8, axis=AX.X, op=ALU.add), ["r_cmp"], ["r_nb"])
        dv(lambda e: e.tensor_tensor(out=r_cmp, in0=r_nb.unsqueeze(1).to_broadcast([128, 16, 16]), in1=tri, op=ALU.mult),
           ["r_nb", "cst"], ["r_cmp"])
        dv(lambda e: e.tensor_reduce(out=r_ob, in_=r_cmp, axis=AX.X, op=ALU.add), ["r_cmp"], ["r_ob"])
        dv(lambda e: e.tensor_tensor(out=r_oe, in0=r_ob, in1=r_nb, op=ALU.add), ["r_ob", "r_nb"], ["r_oe"])
        dv(lambda e: e.tensor_tensor(out=R1s, in0=R1s, in1=Cex, op=ALU.add), [cex_n], ["R1s"])
        dv(lambda e: e.scalar_tensor_tensor(out=R1s, in0=r_ob.unsqueeze(1).to_broadcast([128, NTT, 16]), scalar=512.0,
                                            in1=R1s, op0=ALU.mult, op1=ALU.add), ["r_ob"], ["R1s"])
        for (Mf, mn, pos_i, pn) in ((M1f, "M1", posA, "posA"), (M2f, "M2", posB, "posB")):
            dv(lambda e, Mf=Mf: e.tensor_tensor(out=Mf, in0=Mf, in1=R1s, op=ALU.mult), ["R1s"], [mn])
            dv(lambda e, Mf=Mf: e.tensor_reduce(out=r_pf, in_=Mf, axis=AX.X, op=ALU.add), [mn], ["r_pf"])
            dv(lambda e: e.tensor_scalar(out=r_pf, in0=r_pf, scalar1=0.0, scalar2=float(NSLOT * 512 - 1),
                                         op0=ALU.max, op1=ALU.min), [], ["r_pf"])
            dv(lambda e, pos_i=pos_i: e.tensor_copy(out=pos_i[:], in_=r_pf), ["r_pf"], [pn])
        dv(lambda e: e.tensor_tensor(out=r_cmp2, in0=sio.unsqueeze(2).to_broadcast([128, NSLOT, 16]),
                                     in1=r_oe.unsqueeze(1).to_broadcast([128, NSLOT, 16]), op=ALU.is_ge),
           ["cst", "r_oe"], ["r_cmp2"])
        dv(lambda e: e.tensor_reduce(out=r_eid, in_=r_cmp2, axis=AX.X, op=ALU.add), ["r_cmp2"], ["r_eid"])
        dv(lambda e: e.tensor_scalar(out=r_eid, in0=r_eid, scalar1=15.0, scalar2=128.0, op0=ALU.min, op1=ALU.mult),
           [], ["r_eid"])
        dv(lambda e: e.tensor_tensor(out=r_eid, in0=r_eid, in1=iop[:].to_broadcast([128, NSLOT]), op=ALU.add),
           ["iop"], ["r_eid"])
        dv(lambda e: e.tensor_scalar(out=r_eid, in0=r_eid, scalar1=0.0, scalar2=2047.0, op0=ALU.max, op1=ALU.min),
           [], ["r_eid"])
        dv(lambda e: e.tensor_copy(out=idxW[:], in_=r_eid), ["r_eid"], ["idxW"])
        if debug:
            dr = sb("dbgr_s", [128, 6, NTT], F32)
            dv(lambda e: e.tensor_copy(out=dr[:, 0, :], in_=posA[:]), ["posA"], ["dr"])
            dv(lambda e: e.tensor_copy(out=dr[:, 1, :], in_=posB[:]), ["posB"], ["dr"])
            dv(lambda e: e.tensor_copy(out=dr[:, 2, :], in_=wA[:]), ["wA"], ["dr"])
            dv(lambda e: e.tensor_copy(out=dr[:, 3, :], in_=wB[:]), ["wB"], ["dr"])
            dv(lambda e: e.tensor_copy(out=dr[:, 4, 0:NSLOT], in_=idxW[:]), ["idxW"], ["dr"])
            dv(lambda e: e.tensor_copy(out=dr[:, 5, 0:16], in_=ntot), [cin_n], ["dr"])
            out_ops.append(op("sp", lambda e: e.dma_start(out=dbgr_d, in_=dr[:]), reads=["dr"], dma="dbgr"))

        for gi in range(NTK if stages >= 3 else 0):
            xb = xin[gi % NXIN]
            xn = f"xin{gi % NXIN}"
            op("sp", lambda e, gi=gi, xb=xb: e.dma_start(out=xb, in_=hn2S_d[gi * 128:(gi + 1) * 128, :]),
               reads=["hn2S"], writes=[xn], dma=xn)
            for (pos_i, pn) in ((posA, "posA"), (posB, "posB")):
                op("pool", lambda e, gi=gi, xb=xb, pos_i=pos_i: e.indirect_dma_start(
                    out=Xs_d, out_offset=bass.IndirectOffsetOnAxis(pos_i[:, gi:gi + 1], 0), in_=xb, in_offset=None),
                   reads=[xn, pn], writes=[f"Xs{gi % NXIN}"], dma=f"scat{gi % NXIN}")

        NS3 = NSLOT if stages >= 3 else 0

        def slot_loads(s):
            b = s % NEB
            for m in range(3):
                op("pool", lambda e, s=s, b=b, m=m: e.indirect_dma_start(
                    out=wflat[b][m], out_offset=None, in_=wscr_d[m],
                    in_offset=bass.IndirectOffsetOnAxis(idxW[:, s:s + 1], 0)),
                   reads=["idxW", "wscr0", "wscr1"], writes=[tE[b][m]], dma=f"w{'gud'[m]}{b}")
            xb = xs[s % 2]
            xn = f"xs{s % 2}"
            op("sp", lambda e, s=s, xb=xb: e.dma_start(out=xb, in_=Xs_d[s * 512:(s + 1) * 512, :].rearrange("(j p) d -> p j d", p=128)),
               reads=[f"Xs{q}" for q in range(NXIN)], writes=[xn], dma=xn)

        def slot_transposes(s):
            xb = xs[s % 2]
            xn = f"xs{s % 2}"
            xt = xT[s % 2]
            xtn = f"xT{s % 2}"
            for j in range(4):
                bank = j % 2
                pbT = PB[bank][:].bitcast(BF16).rearrange("p (c t) -> p c t", c=8)
                for c in range(8):
                    op("pe", lambda e, c=c, j=j, pbT=pbT, xb=xb: e.transpose(out=pbT[:, c, :], in_=xb[:, j, c * 128:(c + 1) * 128],
                                                                            identity=ident[:]),
                       reads=[xn, "ident"], writes=[tPB[bank]])
                op("dve", lambda e, j=j, pbT=pbT, xt=xt: e.tensor_tensor(
                    out=xt[:, :, j * 128:(j + 1) * 128], in0=pbT,
                    in1=gfcol[:].unsqueeze(2).to_broadcast([128, 8, 128]), op=ALU.mult),
                   reads=[tPB[bank], "gfcol"], writes=[xtn])

        def slot_gateup(s):
            b = s % NEB
            xt = xT[s % 2]
            xtn = f"xT{s % 2}"
            hb_ = s % 2
            for j in range(4):
                bg_, bu_ = 2 + (j % 2) * 2, 3 + (j % 2) * 2
                for c in range(8):
                    op("pe", lambda e, c=c, j=j, b=b, bg_=bg_, xt=xt: e.matmul(
                        PB[bg_][:], lhsT=wgs[b][:, c, j * 128:(j + 1) * 128], rhs=xt[:, c, :],
                        start=(c == 0), stop=(c == 7)),
                       reads=[tE[b][0], xtn], writes=[tPB[bg_]])
                for c in range(8):
                    op("pe", lambda e, c=c, j=j, b=b, bu_=bu_, xt=xt: e.matmul(
                        PB[bu_][:], lhsT=wus[b][:, c, j * 128:(j + 1) * 128], rhs=xt[:, c, :],
                        start=(c == 0), stop=(c == 7)),
                       reads=[tE[b][1], xtn], writes=[tPB[bu_]])
                op("act", lambda e, j=j, bg_=bg_: e.activation(out=sgs[j % 2], in_=PB[bg_][:], func=AF.Silu),
                   reads=[tPB[bg_]], writes=[f"sgs{j % 2}"])
                op("dve", lambda e, j=j, bu_=bu_, hb_=hb_: e.tensor_tensor(out=hid[hb_][:, j, :], in0=PB[bu_][:],
                                                                         in1=sgs[j % 2], op=ALU.mult),
                   reads=[tPB[bu_], f"sgs{j % 2}"], writes=[f"hid{hb_}"])

        def slot_down(s):
            b = s % NEB
            hb_ = s % 2
            for tl in range(4):
                yi = (s * 4 + tl) % NYS
                yb = ys[yi]
                yn = f"ys{yi}"
                for hf in range(2):
                    bank = 6 + hf
                    for j in range(4):
                        op("pe", lambda e, j=j, tl=tl, hf=hf, b=b, hb_=hb_, bank=bank: e.matmul(
                            PB[bank][:], lhsT=hid[hb_][:, j, tl * 128:(tl + 1) * 128],
                            rhs=wds[b][:, j, hf * 512:(hf + 1) * 512], start=(j == 0), stop=(j == 3)),
                           reads=[f"hid{hb_}", tE[b][2]], writes=[tPB[bank]])
                    op("act", lambda e, hf=hf, bank=bank, yb=yb: e.copy(out=yb[:, hf * 512:(hf + 1) * 512], in_=PB[bank][:]),
                       reads=[tPB[bank]], writes=[yn])
                op("sp", lambda e, s=s, tl=tl, yb=yb: e.dma_start(out=Ys_d[s * 512 + tl * 128: s * 512 + (tl + 1) * 128, :], in_=yb),
                   reads=[yn], writes=[f"Ys{yi}"], dma=f"ysd{yi}")

        if NS3:
            slot_loads(0)
            slot_transposes(0)
            slot_loads(1)
        for s in range(NS3):
            slot_gateup(s)
            if s + 1 < NS3:
                slot_transposes(s + 1)
            slot_down(s)
            if s + 2 < NS3:
                slot_loads(s + 2)

        if stages >= 4:
            S.barrier("dve", lambda e: e.memset(dummy[:], 0.0))
        NT4 = NTK if stages >= 4 else 0

        def loads4(gi):
            k2 = gi % N4
            op("sp", lambda e, gi=gi, k2=k2: e.dma_start(out=hb[k2], in_=hS_d[gi * 128:(gi + 1) * 128, :]),
               reads=["hS"], writes=[f"hb{k2}"], dma=f"hb{k2}")
            for (Yb, ynm, pos_i, pn) in ((YA, "YA", posA, "posA"), (YB, "YB", posB, "posB")):
                op("pool", lambda e, gi=gi, k2=k2, Yb=Yb, pos_i=pos_i: e.indirect_dma_start(
                    out=Yb[k2], out_offset=None, in_=Ys_d, in_offset=bass.IndirectOffsetOnAxis(pos_i[:, gi:gi + 1], 0)),
                   reads=[pn], writes=[f"{ynm}{k2}"], dma=f"{ynm}{k2}")

        for gi in range(min(N4, NT4)):
            loads4(gi)
        for gi in range(NT4):
            k2 = gi % N4
            op("dve", lambda e, gi=gi, k2=k2: e.scalar_tensor_tensor(out=hb[k2], in0=YA[k2], scalar=wA[:, gi:gi + 1], in1=hb[k2],
                                                                     op0=ALU.mult, op1=ALU.add),
               reads=[f"YA{k2}", "wA"], writes=[f"hb{k2}"])
            op("dve", lambda e, gi=gi, k2=k2: e.scalar_tensor_tensor(out=hb[k2], in0=YB[k2], scalar=wB[:, gi:gi + 1], in1=hb[k2],
                                                                     op0=ALU.mult, op1=ALU.add),
               reads=[f"YB{k2}", "wB"], writes=[f"hb{k2}"])
            op("act", lambda e, k2=k2: e.activation(out=hn4, in_=hb[k2], func=AF.Square, accum_out=ss4[k2]),
               reads=[f"hb{k2}"], writes=["hn4", f"ss4{k2}"])
            op("dve", lambda e, k2=k2: e.tensor_scalar(out=rs4[k2], in0=ss4[k2], scalar1=1.0 / D, scalar2=EPS, op0=ALU.mult, op1=ALU.add),
               reads=[f"ss4{k2}"], writes=[f"rs4{k2}"])
            op("act", lambda e, k2=k2: e.sqrt(out=rs4[k2], in_=rs4[k2]), reads=[], writes=[f"rs4{k2}"])
            op("dve", lambda e, k2=k2: e.reciprocal(out=rs4[k2], in_=rs4[k2]), reads=[], writes=[f"rs4{k2}"])
            op("dve", lambda e, k2=k2: e.scalar_tensor_tensor(out=hb[k2], in0=hb[k2], scalar=rs4[k2], in1=gfin[:],
                                                              op0=ALU.mult, op1=ALU.mult),
               reads=[f"rs4{k2}", "gfin"], writes=[f"hb{k2}"])
            out_ops.append(op("sp", lambda e, gi=gi, k2=k2: e.dma_start(out=y_d[gi * 128:(gi + 1) * 128, :], in_=hb[k2]),
                              reads=[f"hb{k2}"], dma=f"hb{k2}"))
            if gi + N4 < NT4:
                loads4(gi + N4)

        S.emit(nc, final_wait_ops=out_ops)
    return nc


def _t5_bucket(dist):
    dist = np.asarray(dist, dtype=np.int64)
    nf = np.maximum(dist, 1).astype(np.float32)
    large = 16 + (np.log(nf / np.float32(16)) / np.float32(math.log(128 / 16)) * np.float32(16)).astype(np.int32)
    large = np.minimum(large, 31)
    return np.where(dist < 16, dist, large)


def prep_shared(inp):
    f = lambda a: np.ascontiguousarray(np.asarray(a, dtype=np.float32))
    w_in = f(inp["w_in"])[0]
    perm = np.arange(3840)
    qcols = []
    for j in range(4):
        qcols += list(range(1024 + j * 64, 1024 + (j + 1) * 64))
        qcols += list(range(1024 + (4 + j) * 64, 1024 + (5 + j) * 64))
    perm[1024:1536] = np.array(qcols)
    w_in = np.ascontiguousarray(w_in[:, perm])
    wr = np.concatenate([f(inp["router_group_w"])[0]] + [f(inp["router_expert_w"])[0, g] for g in range(4)], axis=1)
    rb = np.concatenate([f(inp["router_group_b"])[0], f(inp["router_expert_b"])[0].reshape(-1)])[None, :]
    col = lambda v, c: np.ascontiguousarray(f(v).reshape(c, 128).T)
    ws = f(inp["gm_w_spatial"])[0]
    wst = np.ascontiguousarray(ws.transpose(2, 0, 1))
    s_i = np.arange(128)[:, None, None]
    t_i = np.arange(128)[None, None, :]
    wmask = np.broadcast_to((s_i <= t_i), (128, 8, 128)).astype(np.float32)
    bs = f(inp["gm_b_spatial"])[0]
    bst = np.zeros((128, 4, 128), np.float32)
    for g in range(8):
        bst[(g % 2) * 64:(g % 2) * 64 + 64, g // 2, :] = bs[g][None, :]
    rel = f(inp["rel_bias"])
    s_ = np.arange(128)[:, None]
    q_ = np.arange(128)[None, :]
    d_own = q_ - s_
    d_prev = q_ + 128 - s_
    bias = np.zeros((128, 2, 2, 4, 128), np.float32)
    maskc = np.zeros((128, 2, 2, 4, 128), np.float32)
    b_own = _t5_bucket(np.clip(d_own, 0, 127))
    b_prev = _t5_bucket(np.clip(d_prev, 0, 127))
    for kh in range(2):
        for h4 in range(4):
            h = kh * 4 + h4
            bias[:, kh, 0, h4, :] = rel[b_own, h]
            bias[:, kh, 1, h4, :] = rel[b_prev, h]
            maskc[:, kh, 0, h4, :] = np.where(d_own >= 0, 0.0, NEG)
            maskc[:, kh, 1, h4, :] = np.where(d_prev < 128, 0.0, NEG)
    def pmaj(w, c):
        e_, r_, n_ = w.shape
        return np.ascontiguousarray(w.reshape(e_, c, 128, n_).transpose(0, 2, 1, 3).reshape(e_, 128, c * n_))

    return {
        "w_in": w_in,
        "wpa": f(inp["w_proj_a"])[0], "wpb": f(inp["w_proj_b"])[0], "wout": f(inp["w_out"])[0],
        "wgp": pmaj(f(inp["expert_w_gate"])[0], 8), "wup": pmaj(f(inp["expert_w_up"])[0], 8),
        "wdp": pmaj(f(inp["expert_w_down"])[0], 4),
        "utri": np.triu(np.ones((128, 128), np.float32), 1),
        "cst": np.concatenate([np.arange(8, dtype=np.float32) * 512.0,
                               np.tril(np.ones((16, 16), np.float32), -1).reshape(-1),
                               np.arange(32, dtype=np.float32)])[None, :],
        "iop": np.arange(128, dtype=np.float32)[:, None],
        "wr": np.ascontiguousarray(wr), "rb": np.ascontiguousarray(rb),
        "gacol": col(inp["attn_norm_g"], 8), "gfcol": col(inp["ffn_norm_g"], 8), "gvcol": col(inp["gm_v_norm_g"], 4),
        "gfin": f(inp["final_norm_g"]).reshape(1, D),
        "wst": wst, "wmask": wmask, "bst": bst,
        "sinks": np.ascontiguousarray(np.repeat(f(inp["attn_sinks"]).reshape(2, 4), 64, axis=0)),
        "bias": bias, "maskc": maskc,
        "idn": np.eye(128, dtype=np.float32),
    }


def kernel(**inputs):
    shared = prep_shared(inputs)
    x = np.asarray(inputs["x"], dtype=np.float32)
    nc = build()
    in_maps = []
    for c in range(NCORES):
        m = dict(shared)
        m["x"] = np.ascontiguousarray(x[c])
        in_maps.append(m)
    res = run_bass_kernel_spmd(nc, in_maps, core_ids=list(range(NCORES)))
    return np.stack([np.asarray(r["y"], dtype=np.float32) for r in res.results], axis=0)
```

```python
import math
from contextlib import ExitStack

import numpy as np
import concourse.bass as bass
import concourse.mybir as mybir
from concourse.bass_utils import run_bass_kernel_spmd

F32 = mybir.dt.float32
BF16 = mybir.dt.bfloat16
AF = mybir.ActivationFunctionType
ALU = mybir.AluOpType
AX = mybir.AxisListType

SEQ = 4096
D = 1024
NCORES = 8
PT = 1024
NT = PT // 128
NST = PT // 512
NPASS_FULL = SEQ // PT
EPS = 1e-6
NEG = -30000.0
NSLOT = 31
NTT = SEQ // 128
I32 = mybir.dt.int32

U_OFF, V_OFF, Q_OFF, K_OFF, VA_OFF, GA_OFF, GB_OFF = 0, 512, 1024, 1536, 1664, 1792, 2816

ENG = ("pe", "act", "dve", "pool", "sp")


class Tok:
    __slots__ = ("name", "last_w", "readers")

    def __init__(self, name):
        self.name = name
        self.last_w = None
        self.readers = []


class Op:
    __slots__ = ("eng", "fn", "deps", "sig", "dma_sem", "dma_cnt", "seq")

    def __init__(self, eng, fn):
        self.eng = eng
        self.fn = fn
        self.deps = set()
        self.sig = False
        self.dma_sem = None
        self.dma_cnt = 0
        self.seq = 0


class Sched:
    def __init__(self):
        self.ops = []
        self.per = {e: [] for e in ENG}
        self.dma_counts = {}
        self.total_keys = set()
        self.epoch_op = None
        self.last_dma = {}

    def op(self, eng, fn, reads=(), writes=(), dma=None):
        o = Op(eng, fn)
        for t in list(reads) + list(writes):
            if t.last_w is not None:
                o.deps.add(t.last_w)
        for t in writes:
            for r in t.readers:
                o.deps.add(r)
        if self.epoch_op is not None:
            o.deps.add(self.epoch_op)
        o.deps.discard(o)
        for t in reads:
            t.readers.append(o)
        for t in writes:
            t.last_w = o
            t.readers = []
        if dma is not None:
            o.dma_sem = dma
            self.dma_counts[dma] = self.dma_counts.get(dma, 0) + 1
            o.dma_cnt = self.dma_counts[dma]
            self.last_dma[dma] = o
        self.ops.append(o)
        self.per[eng].append(o)
        return o

    def barrier(self, eng, fn):
        o = Op(eng, fn)
        for e in ENG:
            seen_c = False
            for p in reversed(self.per[e]):
                if p.dma_sem is None:
                    o.deps.add(p)
                    break
        for k, p in self.last_dma.items():
            o.deps.add(p)
        if self.epoch_op is not None:
            o.deps.add(self.epoch_op)
        self.ops.append(o)
        self.per[eng].append(o)
        self.epoch_op = o
        return o

    def emit(self, nc, final_wait_ops=()):
        def skip(d, o):
            return d.dma_sem is None and o.dma_sem is None and d.eng == "pe" and o.eng == "pe"

        for o in self.ops:
            for d in o.deps:
                if d.dma_sem is None and not skip(d, o):
                    d.sig = True
        for o in final_wait_ops:
            if o.dma_sem is None:
                o.sig = True
        for e in ENG:
            c = 0
            for o in self.per[e]:
                if o.dma_sem is None and o.sig:
                    c += 1
                    o.seq = c
        with ExitStack() as es:
            esem = {e: es.enter_context(nc.semaphore(f"s_{e}")) for e in ENG}
            dsem = {k: es.enter_context(nc.semaphore(f"d_{k}")) for k in self.dma_counts}
            block = es.enter_context(nc.Block())

            def dval(d):
                if d.dma_sem in self.total_keys:
                    return 16 * self.dma_counts[d.dma_sem]
                return 16 * d.dma_cnt

            def need(o):
                w = {}
                for d in o.deps:
                    if d.dma_sem is not None:
                        key, val = ("d", d.dma_sem), dval(d)
                    else:
                        if skip(d, o):
                            continue
                        key, val = ("e", d.eng), d.seq
                    if w.get(key, 0) < val:
                        w[key] = val
                return w

            def run(ename):
                def body(eng):
                    waited = {}
                    for o in self.per[ename]:
                        for key, val in need(o).items():
                            if waited.get(key, 0) >= val:
                                continue
                            waited[key] = val
                            sem = dsem[key[1]] if key[0] == "d" else esem[key[1]]
                            eng.wait_ge(sem, val)
                        ins = o.fn(eng)
                        if o.dma_sem is not None:
                            ins.then_inc(dsem[o.dma_sem], 16)
                        elif o.sig:
                            ins.then_inc(esem[ename], 1)
                    if ename == "sp":
                        for o in final_wait_ops:
                            if o.dma_sem is not None:
                                eng.wait_ge(dsem[o.dma_sem], dval(o))
                            else:
                                eng.wait_ge(esem[o.eng], o.seq)
                return body

            block.tensor(run("pe"))
            block.scalar(run("act"))
            block.vector(run("dve"))
            block.gpsimd(run("pool"))
            block.sync(run("sp"))


class Arena:
    def __init__(self, nc, es, name, nbytes):
        self.t = es.enter_context(nc.sbuf_tensor(name, [128, nbytes // 2], BF16))
        self.cap = nbytes
        self.off = 0

    def alloc(self, shape, dt):
        n = 1
        for s_ in shape[1:]:
            n *= s_
        esz = 2 if dt == BF16 else 4
        o = self.off
        self.off += (n * esz + 63) // 64 * 64
        assert self.off <= self.cap, (self.off, self.cap)
        ap = self.t[0:shape[0], o // 2:(o + n * esz) // 2]
        if dt != BF16:
            ap = ap.bitcast(dt)
        if len(shape) > 2:
            names = " ".join(f"d{k}" for k in range(len(shape) - 1))
            ap = ap.rearrange(f"p ({names}) -> p {names}", **{f"d{k}": shape[k + 1] for k in range(len(shape) - 1)})
        return ap


def build(npass=NPASS_FULL, debug=False, stages=4):
    nc = bass.Bass("TRN2", target_bir_lowering=False)

    def din(name, shape):
        return nc.dram_tensor(name, list(shape), F32, kind="ExternalInput").ap()

    x_d = din("x", [SEQ, D])
    win_d = din("w_in", [D, 3840])
    wpa_d = din("wpa", [512, D])
    wpb_d = din("wpb", [512, D])
    wout_d = din("wout", [D, D])
    wexp_d = [din("wgp", [16, 128, 4096]), din("wup", [16, 128, 4096]), din("wdp", [16, 128, 4096])]
    wr_d = din("wr", [D, 20])
    rb_d = din("rb", [1, 20])
    gacol_d = din("gacol", [128, 8])
    gfcol_d = din("gfcol", [128, 8])
    gvcol_d = din("gvcol", [128, 4])
    gfin_d = din("gfin", [1, D])
    wst_d = din("wst", [128, 8, 128])
    wmask_d = din("wmask", [128, 8, 128])
    bst_d = din("bst", [128, 4, 128])
    sinks_d = din("sinks", [128, 4])
    bias_d = din("bias", [128, 2, 2, 4, 128])
    mask_d = din("maskc", [128, 2, 2, 4, 128])
    idn_d = din("idn", [128, 128])
    utri_d = din("utri", [128, 128])
    cst_d = din("cst", [1, 8 + 256 + 32])
    iop_d = din("iop", [128, 1])
    y_d = nc.dram_tensor("y", [SEQ, D], F32, kind="ExternalOutput").ap()
    hS_d = nc.dram_tensor("hS", [SEQ, D], F32, kind="Internal").ap()
    hn2S_d = nc.dram_tensor("hn2S", [SEQ, D], BF16, kind="Internal").ap()
    Xs_d = nc.dram_tensor("Xs", [NSLOT * 512, D], BF16, kind="Internal").ap()
    Ys_d = nc.dram_tensor("Ys", [NSLOT * 512, D], F32, kind="Internal").ap()
    winS_d = nc.dram_tensor("winS", [25, 128, 1024], BF16, kind="Internal").ap()
    wscr_d = [nc.dram_tensor(f"wscr{m}", [16 * 128, 4096], BF16, kind="Internal").ap() for m in range(3)]
    if debug:
        dbg_d = nc.dram_tensor("dbg", [PT, D], F32, kind="ExternalOutput").ap()
        dbgr_d = nc.dram_tensor("dbgr", [128, 6, NTT], F32, kind="ExternalOutput").ap()

    S = Sched()
    S.total_keys.update(["const"])

    with ExitStack() as es:
        def sb(name, shape, dt):
            return es.enter_context(nc.sbuf_tensor(name, list(shape), dt))

        ident = sb("ident", [128, 128], BF16)
        gacol = sb("gacol_s", [128, 8], F32)
        gfcol = sb("gfcol_s", [128, 8], F32)
        gvcol = sb("gvcol_s", [128, 4], F32)
        wstb = sb("wstb", [128, 8, 128], BF16)
        bst = sb("bst_s", [128, 4, 128], F32)
        sinkexp = sb("sinkexp", [128, 4], F32)
        BMhi = sb("BMhi", [128, 2048], BF16)
        BMlo = sb("BMlo", [128, 2048], BF16)
        ones64 = sb("ones64", [128, 64], BF16)
        ones128 = sb("ones128", [128, 128], BF16)
        utri = sb("utri_s", [128, 128], BF16)
        cst = sb("cst_s", [128, 8 + 256 + 32], F32)
        iop = sb("iop_s", [128, 1], F32)
        wr = sb("wr_s", [128, 8, 20], BF16)
        rb = sb("rb_s", [128, 20], F32)
        lg = sb("lg_all", [128, NTT, 20], F32)
        posA = sb("posA", [128, NTT], I32)
        posB = sb("posB", [128, NTT], I32)
        wA = sb("wA", [128, NTT], F32)
        wB = sb("wB", [128, NTT], F32)
        idxW = sb("idxW", [128, NSLOT], I32)
        dummy = sb("bar_dummy", [128, 1], F32)
        gfin = sb("gfin_s", [128, D], F32)
        NEB = 2
        wreg = sb("wreg", [128, NEB * 12288], BF16)
        wgs = [wreg[:, b * 12288:b * 12288 + 4096].rearrange("p (c n) -> p c n", c=8) for b in range(NEB)]
        wus = [wreg[:, b * 12288 + 4096:b * 12288 + 8192].rearrange("p (c n) -> p c n", c=8) for b in range(NEB)]
        wds = [wreg[:, b * 12288 + 8192:b * 12288 + 12288].rearrange("p (c n) -> p c n", c=4) for b in range(NEB)]
        wflat = [[wreg[:, b * 12288 + m * 4096:b * 12288 + (m + 1) * 4096] for m in range(3)] for b in range(NEB)]
        wpa = wreg[:, 0:4096].rearrange("p (c n) -> p c n", c=4)
        wout = wreg[:, 4096:12288].rearrange("p (c n) -> p c n", c=8)
        wpb = wreg[:, 12288:16384].rearrange("p (c n) -> p c n", c=4)
        stgs = [wreg[:, 20480:24576], wreg[:, 16384:20480]]
        AR = Arena(nc, es, "arena", 133 * 1024)
        H = AR.alloc([128, NT, D], F32)
        BM = H[:, 6:8, :].rearrange("p a (k v j t) -> p (a k) v j t", k=1, v=2, j=4)
        hn2T = AR.alloc([128, 8, 512], BF16)
        wv = AR.alloc([128, 8, 512], BF16)
        wva = AR.alloc([128, 8, 128], BF16)
        NRING = 6
        ring = [AR.alloc([128, 8, 128], BF16) for _ in range(NRING)]
        hn2 = [AR.alloc([128, D], BF16) for _ in range(2)]
        hn = hn2[0]
        hnT = AR.alloc([128, 8, 512], BF16)
        hnTb = AR.alloc([128, 8, 512], BF16)
        ssA = AR.alloc([128, 4], F32)
        rsA = AR.alloc([128, 4], F32)
        ssF = AR.alloc([128, 4], F32)
        rsF = AR.alloc([128, 4], F32)
        wst = hnT[:, 0:2, :].rearrange("p a (b t) -> p (a b) t", b=4)
        wmask = hnT[:, 2:4, :].rearrange("p a (b t) -> p (a b) t", b=4)
        ss = AR.alloc([128, 8], F32)
        rstd = AR.alloc([128, 8], F32)
        uT = AR.alloc([128, 4, 512], BF16)
        qT = AR.alloc([128, 4, 512], BF16)
        kT = AR.alloc([128, (NT + 1) * 128], BF16)
        vatt = AR.alloc([128, NT + 1, 128], BF16)
        gv2 = [AR.alloc([128, 512], F32) for _ in range(2)]
        vnh2 = [AR.alloc([128, 512], BF16) for _ in range(2)]
        bnst2 = [AR.alloc([128, 6], F32) for _ in range(2)]
        mv2 = [AR.alloc([128, 2], F32) for _ in range(2)]
        rsv2 = [AR.alloc([128, 1], F32) for _ in range(2)]
        nmr2 = [AR.alloc([128, 1], F32) for _ in range(2)]
        gtmp = AR.alloc([128, 4, 128], F32)
        pT = AR.alloc([128, 2048], BF16)
        den = AR.alloc([128, 512], F32)
        aT = AR.alloc([128, 4, 512], BF16)
        bT = AR.alloc([128, 4, 512], BF16)
        sga = AR.alloc([128, 512], F32)
        sgb = AR.alloc([128, 512], F32)
        mT = AR.alloc([128, 8, 512], BF16)
        maskc = mT[:, 0:4, :].rearrange("p a (b c t) -> p (a b c) t", b=2, c=2).rearrange("p (v k h) t -> p v k h t", v=2, k=2)
        side1_end = AR.off
        AR.off = 0
        NB_ = NTT
        r_m = AR.alloc([128, NB_], F32)
        r_og = AR.alloc([128, NB_, 4], F32)
        r_eg = AR.alloc([128, NB_, 4], F32)
        r_gp = AR.alloc([128, NB_], F32)
        r_sel = AR.alloc([128, NB_, 4, 4], F32)
        r_es = AR.alloc([128, NB_, 4], F32)
        r_m1 = AR.alloc([128, NB_], F32)
        r_o1 = AR.alloc([128, NB_, 4], F32)
        r_es2 = AR.alloc([128, NB_, 4], F32)
        r_m2 = AR.alloc([128, NB_], F32)
        r_o2 = AR.alloc([128, NB_, 4], F32)
        r_d = AR.alloc([128, NB_], F32)
        r_w1 = AR.alloc([128, NB_], F32)
        M1 = AR.alloc([128, NB_, 4, 4], F32)
        M2 = AR.alloc([128, NB_, 4, 4], F32)
        Mb = AR.alloc([128, NB_ * 16], BF16)
        R1s = AR.alloc([128, NB_, 16], F32)
        Ca = AR.alloc([128, NB_, 16], F32)
        Cb = AR.alloc([128, NB_, 16], F32)
        Tts = AR.alloc([128, NB_, 16], F32)
        r_cmp = AR.alloc([128, 16, 16], F32)
        r_cmp2 = AR.alloc([128, NSLOT, 16], F32)
        r_nb = AR.alloc([128, 16], F32)
        r_ob = AR.alloc([128, 16], F32)
        r_oe = AR.alloc([128, 16], F32)
        r_pf = AR.alloc([128, NB_], F32)
        r_eid = AR.alloc([128, NSLOT], F32)
        NXIN = 24
        xin = [AR.alloc([128, D], BF16) for _ in range(NXIN)]
        xs = [AR.alloc([128, 4, D], BF16) for _ in range(2)]
        xT = [AR.alloc([128, 8, 512], BF16) for _ in range(2)]
        sgs = [AR.alloc([128, 512], F32) for _ in range(2)]
        hid = [AR.alloc([128, 4, 512], BF16) for _ in range(2)]
        NYS = 3
        ys = [AR.alloc([128, D], F32) for _ in range(NYS)]
        side2_end = AR.off
        AR.off = 0
        N4 = 6
        YA = [AR.alloc([128, D], F32) for _ in range(N4)]
        YB = [AR.alloc([128, D], F32) for _ in range(N4)]
        hb = [AR.alloc([128, D], F32) for _ in range(N4)]
        ss4 = [AR.alloc([128, 1], F32) for _ in range(N4)]
        rs4 = [AR.alloc([128, 1], F32) for _ in range(N4)]
        hn4 = AR.alloc([128, D], BF16)
        PQ = es.enter_context(nc.psum_tensor("pq", [128, 4096], F32))
        PB = [PQ[:, i * 512:(i + 1) * 512] for i in range(8)]

        T = {}

        def tk(n):
            if n not in T:
                T[n] = Tok(n)
            return T[n]

        tH = [tk(f"H{i}") for i in range(NT)]
        tPB = [tk(f"PB{i}") for i in range(8)]
        tE = [[tk(f"wg{i}"), tk(f"wu{i}"), tk(f"wd{i}")] for i in range(NEB)]
        tRing = [tk(f"ring{i}") for i in range(NRING)]

        def op(eng, fn, reads=(), writes=(), dma=None):
            return S.op(eng, fn, [tk(r) if isinstance(r, str) else r for r in reads],
                        [tk(w) if isinstance(w, str) else w for w in writes], dma)

        def cdma(eng, out, in_, w):
            op(eng, lambda e, out=out, in_=in_: e.dma_start(out=out, in_=in_), writes=[w], dma="const")

        cdma("sp", gacol[:], gacol_d, "gacol")
        cdma("sp", gfcol[:], gfcol_d, "gfcol")
        cdma("sp", gvcol[:], gvcol_d, "gvcol")
        cdma("sp", bst[:], bst_d, "bst")
        cdma("sp", sinkexp[:], sinks_d, "sinkexp")
        cdma("sp", BM, bias_d, "BM")
        cdma("sp", rb[:], rb_d.partition_broadcast(128), "rb")
        cdma("sp", cst[:], cst_d.partition_broadcast(128), "cst")
        cdma("sp", iop[:], iop_d, "iop")
        cdma("pool", ident[:], idn_d, "ident")
        cdma("pool", utri[:], utri_d, "utri")
        cdma("pool", wst, wst_d, "wst_a")
        cdma("pool", wmask, wmask_d, "wmask_a")
        cdma("pool", maskc, mask_d, "maskc_a")
        cdma("pool", wr[:], wr_d.rearrange("(c p) n -> p c n", p=128), "wr")
        cdma("pool", wv, win_d[:, V_OFF:V_OFF + 512].rearrange("(c p) n -> p c n", p=128), "wv")
        cdma("pool", wva, win_d[:, VA_OFF:VA_OFF + 128].rearrange("(c p) n -> p c n", p=128), "wva")
        op("dve", lambda e: e.tensor_tensor(out=wstb[:], in0=wst, in1=wmask, op=ALU.mult),
           reads=["wst_a", "wmask_a"], writes=["wstb", "hnT"])
        BMf = BM.rearrange("p k v j t -> p (k v j t)")
        op("dve", lambda e: e.tensor_tensor(out=BMf, in0=BMf, in1=maskc.rearrange("p k v j t -> p (k v j t)"), op=ALU.add),
           reads=["maskc_a"], writes=["BM", "mT"])
        op("dve", lambda e: e.tensor_copy(out=BMhi[:], in_=BMf), reads=["BM"], writes=["BMhi"])
        op("dve", lambda e: e.tensor_tensor(out=BMlo[:], in0=BMf, in1=BMhi[:], op=ALU.subtract),
           reads=["BM", "BMhi"], writes=["BMlo", tH[6], tH[7]])
        op("act", lambda e: e.activation(out=sinkexp[:], in_=sinkexp[:], func=AF.Exp), reads=[], writes=["sinkexp"])
        op("dve", lambda e: e.memset(ones64[:], 1.0), writes=["ones64"])
        op("dve", lambda e: e.memset(ones128[:], 1.0), writes=["ones128"])

        out_ops = []

        def rstd_ops(n):
            op("dve", lambda e: e.tensor_scalar(out=rstd[:, 0:n], in0=ss[:, 0:n], scalar1=1.0 / D, scalar2=EPS,
                                                op0=ALU.mult, op1=ALU.add), reads=["ss"], writes=["rstd"])
            op("act", lambda e: e.sqrt(out=rstd[:, 0:n], in_=rstd[:, 0:n]), reads=[], writes=["rstd"])
            op("dve", lambda e: e.reciprocal(out=rstd[:, 0:n], in_=rstd[:, 0:n]), reads=[], writes=["rstd"])

        def norm_transpose(ti_list, gcol, dstT, dst_col0, dst_tok, after_hn=None):
            n = len(ti_list)
            for k, i in enumerate(ti_list):
                hb2 = hn2[k % 2]
                op("act", lambda e, i=i, k=k, hb2=hb2: e.activation(out=hb2, in_=H[:, i, :], func=AF.Square,
                                                                   accum_out=ss[:, k:k + 1]),
                   reads=[tH[i]], writes=[f"hn{k % 2}", "ss"])
            rstd_ops(n)
            for k, i in enumerate(ti_list):
                hb2 = hn2[k % 2]
                hnn = f"hn{k % 2}"
                op("act", lambda e, i=i, k=k, hb2=hb2: e.activation(out=hb2, in_=H[:, i, :], func=AF.Identity,
                                                                   scale=rstd[:, k:k + 1]),
                   reads=[tH[i], "rstd"], writes=[hnn])
                if after_hn is not None:
                    after_hn(i, hb2, hnn)
                bank = k % 2
                pbT = PB[bank].bitcast(BF16).rearrange("p (c t) -> p c t", c=8)
                for c in range(8):
                    op("pe", lambda e, c=c, pbT=pbT, hb2=hb2: e.transpose(out=pbT[:, c, :], in_=hb2[:, c * 128:(c + 1) * 128],
                                                                         identity=ident[:]),
                       reads=[hnn, "ident"], writes=[tPB[bank]])
                c0 = dst_col0(i)
                op("dve", lambda e, pbT=pbT, c0=c0: e.tensor_tensor(
                    out=dstT[:, :, c0:c0 + 128], in0=pbT,
                    in1=gcol[:].unsqueeze(2).to_broadcast([128, 8, 128]), op=ALU.mult),
                   reads=[tPB[bank], "gacol", "gfcol"], writes=[dst_tok(i)])

        precast = [(m, ex) for ex in range(16) for m in range(3)]
        pc_ctr = [0]

        def precast_some(n):
            if stages == 1:
                return
            for _ in range(n):
                if pc_ctr[0] >= len(precast):
                    return
                m, ex = precast[pc_ctr[0]]
                q = pc_ctr[0] % 2
                pc_ctr[0] += 1
                op("pool", lambda e, m=m, ex=ex, q=q: e.dma_start(out=stgs[q], in_=wexp_d[m][ex]),
                   writes=[f"stg{q}"], dma=f"stg{q}")
                op("sp", lambda e, m=m, ex=ex, q=q: e.dma_start(out=wscr_d[m][ex * 128:(ex + 1) * 128, :], in_=stgs[q]),
                   reads=[f"stg{q}"], writes=[f"wscr{q}"], dma=f"wscr{q}")

        hnTs = [hnT, hnTb]
        hnTn = ["hnT", "hnTb"]
        ring_ctr = [0]

        blk_ids = {}

        def stream_block(col0):
            r = ring_ctr[0] % NRING
            ring_ctr[0] += 1
            first = col0 not in blk_ids
            if first:
                blk_ids[col0] = len(blk_ids)
            bid = blk_ids[col0]
            if first:
                op("pool", lambda e, r=r, col0=col0: e.dma_start(
                    out=ring[r], in_=win_d[:, col0:col0 + 128].rearrange("(c p) n -> p c n", p=128)),
                   writes=[tRing[r]], dma=f"ring{r}")
                op("sp", lambda e, r=r, bid=bid: e.dma_start(out=winS_d[bid], in_=ring[r].rearrange("p c n -> p (c n)")),
                   reads=[tRing[r]], writes=[f"winS{bid}"], dma=f"ring{r}")
            else:
                op("pool", lambda e, r=r, bid=bid: e.dma_start(out=ring[r].rearrange("p c n -> p (c n)"), in_=winS_d[bid]),
                   reads=[f"winS{bid}"], writes=[tRing[r]], dma=f"ring{r}")
            return ring[r], tRing[r]

        def load_x(g):
            for k in range(4):
                i = (g % 2) * 4 + k
                t0_ = g * 512 + k * 128
                op("sp", lambda e, i=i, t0_=t0_: e.dma_start(out=H[:, i, :], in_=x_d[t0_:t0_ + 128, :]),
                   writes=[tH[i]], dma=f"H{i}")

        def norm_pieces(tiles, gcol, dstT, dst_tok, ssb, rsb, ssn, rsn, banks, after_hn=None):
            def sq(k):
                i = tiles[k]
                hb2 = hn2[k % 2]
                op("act", lambda e: e.activation(out=hb2, in_=H[:, i, :], func=AF.Square, accum_out=ssb[:, k:k + 1]),
                   reads=[tH[i]], writes=[f"hn{k % 2}", ssn])

            def rs():
                op("dve", lambda e: e.tensor_scalar(out=rsb, in0=ssb, scalar1=1.0 / D, scalar2=EPS,
                                                    op0=ALU.mult, op1=ALU.add), reads=[ssn], writes=[rsn])
                op("act", lambda e: e.sqrt(out=rsb, in_=rsb), reads=[], writes=[rsn])
                op("dve", lambda e: e.reciprocal(out=rsb, in_=rsb), reads=[], writes=[rsn])

            def tr(k):
                i = tiles[k]
                hb2 = hn2[k % 2]
                hnn = f"hn{k % 2}"
                op("act", lambda e: e.activation(out=hb2, in_=H[:, i, :], func=AF.Identity, scale=rsb[:, k:k + 1]),
                   reads=[tH[i], rsn], writes=[hnn])
                if after_hn is not None:
                    after_hn(i, hb2, hnn)
                bank = banks[k % 2]
                pbT = PB[bank].bitcast(BF16).rearrange("p (c t) -> p c t", c=8)
                for c in range(8):
                    op("pe", lambda e, c=c: e.transpose(out=pbT[:, c, :], in_=hb2[:, c * 128:(c + 1) * 128], identity=ident[:]),
                       reads=[hnn, "ident"], writes=[tPB[bank]])
                op("dve", lambda e: e.tensor_tensor(out=dstT[:, :, k * 128:(k + 1) * 128], in0=pbT,
                                                    in1=gcol[:].unsqueeze(2).to_broadcast([128, 8, 128]), op=ALU.mult),
                   reads=[tPB[bank], "gacol", "gfcol"], writes=[dst_tok])
            return sq, rs, tr

        def make_st(g):
            ps_i, st = g // 2, g % 2
            tok0 = ps_i * PT
            tiles = [st * 4 + k for k in range(4)]
            hT, hTn = hnTs[g % 2], hnTn[g % 2]

            def A_pieces():
                return norm_pieces(tiles, gacol, hT, hTn, ssA, rsA, "ssA", "rsA", (0, 1))

            def proj_fm(col0, bank, evac):
                wb_, wt_ = stream_block(col0)
                for c in range(8):
                    op("pe", lambda e, c=c: e.matmul(PB[bank], lhsT=wb_[:, c, :], rhs=hT[:, c, :], start=(c == 0), stop=(c == 7)),
                       reads=[wt_, hTn], writes=[tPB[bank]])
                evac(bank)

            def B(hook):
                if st == 0 and ps_i > 0:
                    op("dve", lambda e: e.tensor_copy(out=kT[:, 0:128], in_=kT[:, NT * 128:(NT + 1) * 128]),
                       reads=[], writes=["kT"])
                    op("dve", lambda e: e.tensor_copy(out=vatt[:, 0, :], in_=vatt[:, NT, :]), reads=[], writes=["vatt"])
                blk = 0
                for j in range(4):
                    proj_fm(U_OFF + j * 128, 1 + (j % 2),
                            lambda bank, j=j: op("act", lambda e: e.activation(out=uT[:, j, :], in_=PB[bank],
                                                                              func=AF.Gelu_apprx_tanh),
                                                 reads=[tPB[bank]], writes=["uT"]))
                    if hook is not None:
                        hook(blk)
                    blk += 1
                for j in range(4):
                    proj_fm(Q_OFF + j * 128, 1 + (j % 2),
                            lambda bank, j=j: op("act", lambda e: e.activation(out=qT[:, j, :], in_=PB[bank],
                                                                              func=AF.Identity, scale=0.125),
                                                 reads=[tPB[bank]], writes=["qT"]))
                    if hook is not None:
                        hook(blk)
                    blk += 1
                kc0 = (1 + st * 4) * 128
                proj_fm(K_OFF, 1,
                        lambda bank: op("dve", lambda e: e.tensor_copy(out=kT[:, kc0:kc0 + 512], in_=PB[bank]),
                                        reads=[tPB[bank]], writes=["kT"]))
                if hook is not None:
                    hook(blk)

            def t_v(i):
                ts = (i - st * 4) * 128
                slot = 1 + i
                q2 = i % 2
                gv, vnh, bnst, mv, rsv, nmr = gv2[q2], vnh2[q2], bnst2[q2], mv2[q2], rsv2[q2], nmr2[q2]
                nm = lambda x: f"{x}{q2}"
                for c in range(8):
                    op("pe", lambda e, c=c, ts=ts: e.matmul(PB[0], lhsT=hT[:, c, ts:ts + 128], rhs=wv[:, c, :],
                                                            start=(c == 0), stop=(c == 7)),
                       reads=[hTn, "wv"], writes=[tPB[0]])
                for c in range(8):
                    op("pe", lambda e, c=c, ts=ts: e.matmul(PB[1][:, 0:128], lhsT=hT[:, c, ts:ts + 128],
                                                            rhs=wva[:, c, :], start=(c == 0), stop=(c == 7)),
                       reads=[hTn, "wva"], writes=[tPB[1]])
                op("act", lambda e, gv=gv: e.activation(out=gv, in_=PB[0], func=AF.Gelu_apprx_tanh),
                   reads=[tPB[0]], writes=[nm("gv")])
                op("dve", lambda e, slot=slot: e.tensor_copy(out=vatt[:, slot, :], in_=PB[1][:, 0:128]),
                   reads=[tPB[1]], writes=["vatt"])
                op("dve", lambda e, gv=gv, bnst=bnst: e.bn_stats(out=bnst, in_=gv), reads=[nm("gv")], writes=[nm("bnst")])
                op("dve", lambda e, mv=mv, bnst=bnst: e.bn_aggr(out=mv, in_=bnst), reads=[nm("bnst")], writes=[nm("mv")])
                op("dve", lambda e, mv=mv, rsv=rsv: e.tensor_scalar_add(out=rsv, in0=mv[:, 1:2], scalar1=EPS),
                   reads=[nm("mv")], writes=[nm("rsv")])
                op("act", lambda e, rsv=rsv: e.sqrt(out=rsv, in_=rsv), reads=[], writes=[nm("rsv")])
                op("dve", lambda e, rsv=rsv: e.reciprocal(out=rsv, in_=rsv), reads=[], writes=[nm("rsv")])
                op("dve", lambda e, mv=mv, rsv=rsv, nmr=nmr: e.tensor_scalar(out=nmr, in0=mv[:, 0:1], scalar1=rsv, scalar2=-1.0,
                                                                           op0=ALU.mult, op1=ALU.mult),
                   reads=[nm("mv"), nm("rsv")], writes=[nm("nmr")])
                op("act", lambda e, gv=gv, vnh=vnh, rsv=rsv, nmr=nmr: e.activation(out=vnh, in_=gv, func=AF.Identity,
                                                                                 scale=rsv, bias=nmr),
                   reads=[nm("gv"), nm("rsv"), nm("nmr")], writes=[nm("vnh")])

            def t_sc(i):
                ts = (i - st * 4) * 128
                slot = 1 + i
                gblk = ps_i * NT + i
                for kh in range(2):
                    pr = slice(kh * 64, (kh + 1) * 64)
                    for vi in range(2 if gblk > 0 else 1):
                        bank = 4 + kh * 2 + vi
                        ksl = slot - vi
                        pc = (kh * 2 + vi) * 512
                        op("pe", lambda e, pr=pr, ksl=ksl, ts=ts, bank=bank: e.matmul(
                            PB[bank].rearrange("p (j t) -> p j t", j=4),
                            lhsT=kT[pr, ksl * 128:(ksl + 1) * 128], rhs=qT[pr, :, ts:ts + 128],
                            start=True, stop=False),
                           reads=["kT", "qT"], writes=[tPB[bank]])
                        op("pe", lambda e, bank=bank, pc=pc: e.matmul(PB[bank], lhsT=ident[:], rhs=BMhi[:, pc:pc + 512],
                                                                      start=False, stop=False),
                           reads=["ident", "BMhi"], writes=[tPB[bank]])
                        op("pe", lambda e, bank=bank, pc=pc: e.matmul(PB[bank], lhsT=ident[:], rhs=BMlo[:, pc:pc + 512],
                                                                      start=False, stop=True),
                           reads=["ident", "BMlo"], writes=[tPB[bank]])
                scb = PQ[:, 2048:4096]
                op("act", lambda e, scb=scb: e.activation(out=pT, in_=scb, func=AF.Exp),
                   reads=[tPB[4], tPB[5], tPB[6], tPB[7]], writes=["pT"])

            def t_pv(i):
                ts = (i - st * 4) * 128
                slot = 1 + i
                gblk = ps_i * NT + i
                nv = 2 if gblk > 0 else 1
                for (bank, use_v) in ((3, True), (0, False)):
                    for kh in range(2):
                        kw = {"tile_position": (0, 64)} if kh else {}
                        for vi in range(nv):
                            ksl = slot - vi
                            lhs = vatt[:, ksl, kh * 64:(kh + 1) * 64] if use_v else ones64[:]
                            pc = (kh * 2 + vi) * 512
                            op("pe", lambda e, bank=bank, kh=kh, vi=vi, lhs=lhs, pc=pc, kw=kw, nv=nv: e.matmul(
                                PB[bank][kh * 64:(kh + 1) * 64, :], lhsT=lhs, rhs=pT[:, pc:pc + 512],
                                start=(vi == 0), stop=(vi == nv - 1), **kw),
                               reads=["vatt", "ones64", "pT"], writes=[tPB[bank]])
                op("dve", lambda e: e.tensor_tensor(
                    out=den.rearrange("p (j t) -> p j t", j=4), in0=PB[0].rearrange("p (j t) -> p j t", j=4),
                    in1=sinkexp[:].unsqueeze(2).to_broadcast([128, 4, 128]), op=ALU.add),
                   reads=[tPB[0], "sinkexp"], writes=["den"])
                op("dve", lambda e: e.reciprocal(out=den, in_=den), reads=[], writes=["den"])
                op("dve", lambda e, ts=ts: e.tensor_tensor(
                    out=bT[:, :, ts:ts + 128], in0=PB[3].rearrange("p (j t) -> p j t", j=4),
                    in1=den.rearrange("p (j t) -> p j t", j=4), op=ALU.mult),
                   reads=[tPB[3], "den"], writes=["bT"])

            def t_sp(i):
                ts = (i - st * 4) * 128
                q2 = i % 2
                vnh = vnh2[q2]
                pbs = PB[2].rearrange("p (j t) -> p j t", j=4)
                for g in range(8):
                    lo = (g % 2) * 64
                    kw = {"tile_position": (0, 64)} if g % 2 else {}
                    op("pe", lambda e, g=g, lo=lo, kw=kw, vnh=vnh: e.matmul(
                        pbs[lo:lo + 64, g // 2, :], lhsT=vnh[:, g * 64:(g + 1) * 64], rhs=wstb[:, g, :],
                        start=True, stop=True, **kw),
                       reads=[f"vnh{q2}", "wstb"], writes=[tPB[2]])
                op("dve", lambda e: e.tensor_tensor(out=gtmp, in0=pbs,
                                                    in1=gvcol[:].unsqueeze(2).to_broadcast([128, 4, 128]), op=ALU.mult),
                   reads=[tPB[2], "gvcol"], writes=["gtmp"])
                op("dve", lambda e: e.tensor_tensor(out=gtmp, in0=gtmp, in1=bst[:], op=ALU.add),
                   reads=["bst"], writes=["gtmp"])
                op("dve", lambda e, ts=ts: e.tensor_tensor(out=aT[:, :, ts:ts + 128], in0=gtmp,
                                                           in1=uT[:, :, ts:ts + 128], op=ALU.mult),
                   reads=["gtmp", "uT"], writes=["aT"])

            def C():
                precast_some(2)
                t_v(tiles[0])
                t_sc(tiles[0])
                for k in range(1, 4):
                    precast_some(1 if k < 3 else 2)
                    t_v(tiles[k])
                    t_pv(tiles[k - 1])
                    t_sc(tiles[k])
                    t_sp(tiles[k - 1])
                t_pv(tiles[3])
                t_sp(tiles[3])

            def Dg(hook):
                for j in range(8):
                    b0 = (j % 2) * 4
                    wga, tga = stream_block(GA_OFF + j * 128)
                    for c in range(8):
                        op("pe", lambda e, c=c, wga=wga, b0=b0: e.matmul(PB[b0], lhsT=wga[:, c, :], rhs=hT[:, c, :],
                                                                         start=(c == 0), stop=(c == 7)),
                           reads=[tga, hTn], writes=[tPB[b0]])
                    wgb, tgb = stream_block(GB_OFF + j * 128)
                    for c in range(8):
                        op("pe", lambda e, c=c, wgb=wgb, b0=b0: e.matmul(PB[b0 + 1], lhsT=wgb[:, c, :], rhs=hT[:, c, :],
                                                                         start=(c == 0), stop=(c == 7)),
                           reads=[tgb, hTn], writes=[tPB[b0 + 1]])
                    for c in range(4):
                        op("pe", lambda e, c=c, j=j, b0=b0: e.matmul(PB[b0 + 2], lhsT=wpa[:, c, j * 128:(j + 1) * 128], rhs=aT[:, c, :],
                                                                     start=(c == 0), stop=(c == 3)),
                           reads=[tE[0][0], "aT"], writes=[tPB[b0 + 2]])
                    for c in range(4):
                        op("pe", lambda e, c=c, j=j, b0=b0: e.matmul(PB[b0 + 3], lhsT=wpb[:, c, j * 128:(j + 1) * 128], rhs=bT[:, c, :],
                                                                     start=(c == 0), stop=(c == 3)),
                           reads=["wpb0", "wpb1", "bT"], writes=[tPB[b0 + 3]])
                    op("act", lambda e, b0=b0: e.activation(out=sga, in_=PB[b0], func=AF.Sigmoid),
                       reads=[tPB[b0]], writes=["sga"])
                    op("act", lambda e, b0=b0: e.activation(out=sgb, in_=PB[b0 + 1], func=AF.Sigmoid),
                       reads=[tPB[b0 + 1]], writes=["sgb"])
                    op("dve", lambda e, b0=b0: e.tensor_tensor(out=sga, in0=PB[b0 + 2], in1=sga, op=ALU.mult),
                       reads=[tPB[b0 + 2]], writes=["sga"])
                    op("dve", lambda e, b0=b0: e.tensor_tensor(out=sgb, in0=PB[b0 + 3], in1=sgb, op=ALU.mult),
                       reads=[tPB[b0 + 3]], writes=["sgb"])
                    op("dve", lambda e, j=j: e.tensor_tensor(out=mT[:, j, :], in0=sga, in1=sgb, op=ALU.add),
                       reads=["sga", "sgb"], writes=["mT"])
                    if hook is not None:
                        hook(j)

            def E():
                for i in tiles:
                    ts = (i - st * 4) * 128
                    for hf in range(2):
                        bank = 1 + hf
                        for j in range(8):
                            op("pe", lambda e, j=j, ts=ts, hf=hf, bank=bank: e.matmul(
                                PB[bank][:], lhsT=mT[:, j, ts:ts + 128], rhs=wout[:, j, hf * 512:(hf + 1) * 512],
                                start=(j == 0), stop=(j == 7)),
                               reads=["mT", tE[0][1], tE[0][2]], writes=[tPB[bank]])
                        op("dve", lambda e, i=i, hf=hf, bank=bank: e.tensor_tensor(
                            out=H[:, i, hf * 512:(hf + 1) * 512], in0=PB[bank][:], in1=H[:, i, hf * 512:(hf + 1) * 512],
                            op=ALU.add),
                           reads=[tPB[bank]], writes=[tH[i]])
                    op("sp", lambda e, i=i, tok0=tok0: e.dma_start(out=hS_d[tok0 + i * 128: tok0 + (i + 1) * 128, :],
                                                                   in_=H[:, i, :]),
                       reads=[tH[i]], writes=["hS"], dma=f"H{i}")

                if debug and ps_i == 0:
                    for i in tiles:
                        out_ops.append(op("sp", lambda e, i=i: e.dma_start(out=dbg_d[i * 128:(i + 1) * 128, :], in_=H[:, i, :]),
                                          reads=[tH[i]], dma=f"H{i}"))

            def F_pieces():
                def spill_hn2(i, hb2, hnn):
                    op("sp", lambda e: e.dma_start(out=hn2S_d[tok0 + i * 128: tok0 + (i + 1) * 128, :], in_=hb2),
                       reads=[hnn], writes=["hn2S"], dma="hn2S" + hnn)
                sq, rs, tr = norm_pieces(tiles, gfcol, hn2T, "hn2T", ssF, rsF, "ssF", "rsF", (3, 4), after_hn=spill_hn2)

                def router():
                    for k, i in enumerate(tiles):
                        gi = ps_i * NT + i
                        ts = k * 128
                        for c in range(8):
                            op("pe", lambda e, c=c, ts=ts: e.matmul(PB[7][:, 0:20], lhsT=hn2T[:, c, ts:ts + 128], rhs=wr[:, c, :],
                                                                    start=(c == 0), stop=(c == 7)),
                               reads=["hn2T", "wr"], writes=[tPB[7]])
                        op("dve", lambda e, gi=gi: e.tensor_tensor(out=lg[:, gi, :], in0=PB[7][:, 0:20], in1=rb[:], op=ALU.add),
                           reads=[tPB[7], "rb"], writes=["lg"])
                return sq, rs, tr, router
            return dict(A=A_pieces, B=B, C=C, D=Dg, E=E, F=F_pieces)

        def mk_hook(pieces, base):
            if pieces is None:
                return None
            sq, rs, tr = pieces[0], pieces[1], pieces[2]
            router = pieces[3] if len(pieces) > 3 else None

            def hook(it):
                k = it - base
                if 0 <= k < 4:
                    sq(k)
                if k == 3:
                    rs()
                if 4 <= k < 8:
                    tr(k - 4)
                if k == 8 and router is not None:
                    router()
            return hook

        G = 2 * npass
        sts = [make_st(g) for g in range(G)]
        load_x(0)
        op("pool", lambda e: e.dma_start(out=wpa, in_=wpa_d.rearrange("(c p) n -> p c n", p=128)),
           writes=[tE[0][0]], dma="wg0")
        for a_ in range(2):
            op("pool", lambda e, a_=a_: e.dma_start(
                out=wpb[a_ * 64:(a_ + 1) * 64, :, :],
                in_=wpb_d.rearrange("(a j d) n -> a d j n", a=2, j=4)[a_]),
               writes=[tk(f"wpb{a_}")], dma=f"wpbd{a_}")
        op("pool", lambda e: e.dma_start(out=wout, in_=wout_d.rearrange("(c p) n -> p c n", p=128)),
           writes=[tE[0][1], tE[0][2]], dma="wu0")
        if G > 1:
            load_x(1)
        sq, rs, tr = sts[0]["A"]()
        for k in range(4):
            sq(k)
        rs()
        for k in range(4):
            tr(k)
        for g in range(G):
            fp = sts[g - 1]["F"]() if g > 0 else None
            sts[g]["B"](mk_hook(fp, 0))
            if fp is not None and fp[3] is not None and False:
                pass
            if g >= 1 and g + 1 < G:
                load_x(g + 1)
            sts[g]["C"]()
            ap = sts[g + 1]["A"]() if g + 1 < G else None
            sts[g]["D"](mk_hook(ap, 0))
            sts[g]["E"]()
        sq, rs, tr, router = sts[G - 1]["F"]()
        for k in range(4):
            sq(k)
        rs()
        for k in range(4):
            tr(k)
        router()

        precast_some(100)

        S.barrier("dve", lambda e: e.memset(dummy[:], 0.0))
        NTK = npass * NT

        cdma2 = op("sp", lambda e: e.dma_start(out=gfin[:], in_=gfin_d.partition_broadcast(128)), writes=["gfin"], dma="gfin")

        ND = NTK if stages >= 3 else 0

        def xin_load(gi):
            xb = xin[gi % NXIN]
            xn = f"xin{gi % NXIN}"
            op("sp", lambda e, gi=gi, xb=xb: e.dma_start(out=xb, in_=hn2S_d[gi * 128:(gi + 1) * 128, :]),
               reads=["hn2S"], writes=[xn], dma=xn)

        for gi in range(min(NXIN, ND)):
            xin_load(gi)

        gl = lg[:, :, 0:4]
        el = lg[:, :, 4:20].rearrange("p t (g x) -> p t g x", g=4)
        bc3 = lambda a: a.unsqueeze(2).to_broadcast([128, NTT, 4])
        if npass < NPASS_FULL:
            op("dve", lambda e: e.memset(lg[:, NTK:, :], 0.0), writes=["lg"])
        dv = lambda fn, r, w: op("dve", fn, reads=r, writes=w)
        dv(lambda e: e.tensor_reduce(out=r_m, in_=gl, axis=AX.X, op=ALU.max), ["lg"], ["r_m"])
        dv(lambda e: e.tensor_tensor(out=r_og, in0=gl, in1=bc3(r_m), op=ALU.is_equal), ["lg", "r_m"], ["r_og"])
        dv(lambda e: e.tensor_tensor(out=r_eg, in0=gl, in1=bc3(r_m), op=ALU.subtract), ["lg", "r_m"], ["r_eg"])
        op("act", lambda e: e.activation(out=r_eg, in_=r_eg, func=AF.Exp), reads=[], writes=["r_eg"])
        dv(lambda e: e.tensor_reduce(out=r_gp, in_=r_eg, axis=AX.X, op=ALU.add), ["r_eg"], ["r_gp"])
        dv(lambda e: e.reciprocal(out=r_gp, in_=r_gp), [], ["r_gp"])
        dv(lambda e: e.tensor_tensor(out=r_sel, in0=el, in1=r_og.unsqueeze(3).to_broadcast([128, NTT, 4, 4]), op=ALU.mult),
           ["lg", "r_og"], ["r_sel"])
        dv(lambda e: e.tensor_reduce(out=r_es, in_=r_sel.rearrange("p t g x -> p t x g"), axis=AX.X, op=ALU.add),
           ["r_sel"], ["r_es"])
        dv(lambda e: e.tensor_reduce(out=r_m1, in_=r_es, axis=AX.X, op=ALU.max), ["r_es"], ["r_m1"])
        dv(lambda e: e.tensor_tensor(out=r_o1, in0=r_es, in1=bc3(r_m1), op=ALU.is_equal), ["r_es", "r_m1"], ["r_o1"])
        dv(lambda e: e.scalar_tensor_tensor(out=r_es2, in0=r_o1, scalar=-1e9, in1=r_es, op0=ALU.mult, op1=ALU.add),
           ["r_o1", "r_es"], ["r_es2"])
        dv(lambda e: e.tensor_reduce(out=r_m2, in_=r_es2, axis=AX.X, op=ALU.max), ["r_es2"], ["r_m2"])
        dv(lambda e: e.tensor_tensor(out=r_o2, in0=r_es2, in1=bc3(r_m2), op=ALU.is_equal), ["r_es2", "r_m2"], ["r_o2"])
        dv(lambda e: e.tensor_tensor(out=r_d, in0=r_m2, in1=r_m1, op=ALU.subtract), ["r_m1", "r_m2"], ["r_d"])
        op("act", lambda e: e.activation(out=r_d, in_=r_d, func=AF.Exp), reads=[], writes=["r_d"])
        dv(lambda e: e.tensor_scalar_add(out=r_w1, in0=r_d, scalar1=1.0), ["r_d"], ["r_w1"])
        dv(lambda e: e.reciprocal(out=r_w1, in_=r_w1), [], ["r_w1"])
        dv(lambda e: e.tensor_tensor(out=wB[:], in0=r_d, in1=r_w1, op=ALU.mult), ["r_d", "r_w1"], ["wB"])
        dv(lambda e: e.tensor_tensor(out=wA[:], in0=r_w1, in1=r_gp, op=ALU.mult), ["r_w1", "r_gp"], ["wA"])
        dv(lambda e: e.tensor_tensor(out=wB[:], in0=wB[:], in1=r_gp, op=ALU.mult), ["r_gp"], ["wB"])
        bg = lambda a: a.unsqueeze(3).to_broadcast([128, NTT, 4, 4])
        bx = lambda a: a.unsqueeze(2).to_broadcast([128, NTT, 4, 4])
        dv(lambda e: e.tensor_tensor(out=M1, in0=bg(r_og), in1=bx(r_o1), op=ALU.mult), ["r_og", "r_o1"], ["M1"])
        dv(lambda e: e.tensor_tensor(out=M2, in0=bg(r_og), in1=bx(r_o2), op=ALU.mult), ["r_og", "r_o2"], ["M2"])
        M1f = M1.rearrange("p t g x -> p t (g x)")
        M2f = M2.rearrange("p t g x -> p t (g x)")
        dv(lambda e: e.tensor_tensor(out=Mb.rearrange("p (t n) -> p t n", n=16), in0=M1f, in1=M2f, op=ALU.add),
           ["M1", "M2"], ["Mb"])
        if npass < NPASS_FULL:
            dv(lambda e: e.memset(Mb[:, NTK * 16:], 0.0), [], ["Mb"])
        op("pe", lambda e: e.matmul(PB[0][:], lhsT=utri[:], rhs=Mb, start=True, stop=True),
           reads=["utri", "Mb"], writes=[tPB[0]])
        op("pe", lambda e: e.matmul(PB[1][:], lhsT=ones128[:], rhs=Mb, start=True, stop=True),
           reads=["ones128", "Mb"], writes=[tPB[1]])
        dv(lambda e: e.tensor_copy(out=R1s.rearrange("p t n -> p (t n)"), in_=PB[0][:]), [tPB[0]], ["R1s"])
        dv(lambda e: e.tensor_copy(out=Tts.rearrange("p t n -> p (t n)"), in_=PB[1][:]), [tPB[1]], ["Tts"])
        dv(lambda e: e.tensor_copy(out=Ca, in_=Tts), ["Tts"], ["Ca"])
        cur, nxt, cn, nn = Ca, Cb, "Ca", "Cb"
        sh = 1
        while sh < NTT:
            dv(lambda e, cur=cur, nxt=nxt, sh=sh: e.tensor_copy(out=nxt[:, 0:sh, :], in_=cur[:, 0:sh, :]), [cn], [nn])
            dv(lambda e, cur=cur, nxt=nxt, sh=sh: e.tensor_tensor(out=nxt[:, sh:, :], in0=cur[:, sh:, :],
                                                                  in1=cur[:, 0:NTT - sh, :], op=ALU.add), [cn], [nn])
            cur, nxt, cn, nn = nxt, cur, nn, cn
            sh *= 2
        Cin, cin_n, Cex, cex_n = cur, cn, nxt, nn
        dv(lambda e: e.tensor_tensor(out=Cex, in0=Cin, in1=Tts, op=ALU.subtract), [cin_n, "Tts"], [cex_n])
        ntot = Cin[:, NTT - 1, :]
        thr = cst[:, 0:8]
        tri = cst[:, 8:264].rearrange("p (a b) -> p a b", a=16)
        sio = cst[:, 264:264 + NSLOT]
        cmp8 = r_cmp.rearrange("p a b -> p (a b)")[:, 0:128].rearrange("p (a b) -> p a b", b=8)
        dv(lambda e: e.tensor_tensor(out=cmp8, in0=ntot.unsqueeze(2).to_broadcast([128, 16, 8]),
                                     in1=thr.unsqueeze(1).to_broadcast([128, 16, 8]), op=ALU.is_gt),
           [cin_n, "cst"], ["r_cmp"])
        dv(lambda e: e.tensor_reduce(out=r_nb, in_=cmp8, axis=AX.X, op=ALU.add), ["r_cmp"], ["r_nb"])
        dv(lambda e: e.tensor_tensor(out=r_cmp, in0=r_nb.unsqueeze(1).to_broadcast([128, 16, 16]), in1=tri, op=ALU.mult),
           ["r_nb", "cst"], ["r_cmp"])
        dv(lambda e: e.tensor_reduce(out=r_ob, in_=r_cmp, axis=AX.X, op=ALU.add), ["r_cmp"], ["r_ob"])
        dv(lambda e: e.tensor_tensor(out=r_oe, in0=r_ob, in1=r_nb, op=ALU.add), ["r_ob", "r_nb"], ["r_oe"])
        dv(lambda e: e.tensor_tensor(out=R1s, in0=R1s, in1=Cex, op=ALU.add), [cex_n], ["R1s"])
        dv(lambda e: e.scalar_tensor_tensor(out=R1s, in0=r_ob.unsqueeze(1).to_broadcast([128, NTT, 16]), scalar=512.0,
                                            in1=R1s, op0=ALU.mult, op1=ALU.add), ["r_ob"], ["R1s"])
        for (Mf, mn, pos_i, pn) in ((M1f, "M1", posA, "posA"), (M2f, "M2", posB, "posB")):
            dv(lambda e, Mf=Mf: e.tensor_tensor(out=Mf, in0=Mf, in1=R1s, op=ALU.mult), ["R1s"], [mn])
            dv(lambda e, Mf=Mf: e.tensor_reduce(out=r_pf, in_=Mf, axis=AX.X, op=ALU.add), [mn], ["r_pf"])
            dv(lambda e: e.tensor_scalar(out=r_pf, in0=r_pf, scalar1=0.0, scalar2=float(NSLOT * 512 - 1),
                                         op0=ALU.max, op1=ALU.min), [], ["r_pf"])
            dv(lambda e, pos_i=pos_i: e.tensor_copy(out=pos_i[:], in_=r_pf), ["r_pf"], [pn])
        dv(lambda e: e.tensor_tensor(out=r_cmp2, in0=sio.unsqueeze(2).to_broadcast([128, NSLOT, 16]),
                                     in1=r_oe.unsqueeze(1).to_broadcast([128, NSLOT, 16]), op=ALU.is_ge),
           ["cst", "r_oe"], ["r_cmp2"])
        dv(lambda e: e.tensor_reduce(out=r_eid, in_=r_cmp2, axis=AX.X, op=ALU.add), ["r_cmp2"], ["r_eid"])
        dv(lambda e: e.tensor_scalar(out=r_eid, in0=r_eid, scalar1=15.0, scalar2=128.0, op0=ALU.min, op1=ALU.mult),
           [], ["r_eid"])
        dv(lambda e: e.tensor_tensor(out=r_eid, in0=r_eid, in1=iop[:].to_broadcast([128, NSLOT]), op=ALU.add),
           ["iop"], ["r_eid"])
        dv(lambda e: e.tensor_scalar(out=r_eid, in0=r_eid, scalar1=0.0, scalar2=2047.0, op0=ALU.max, op1=ALU.min),
           [], ["r_eid"])
        dv(lambda e: e.tensor_copy(out=idxW[:], in_=r_eid), ["r_eid"], ["idxW"])
        if debug:
            dr = sb("dbgr_s", [128, 6, NTT], F32)
            dv(lambda e: e.tensor_copy(out=dr[:, 0, :], in_=posA[:]), ["posA"], ["dr"])
            dv(lambda e: e.tensor_copy(out=dr[:, 1, :], in_=posB[:]), ["posB"], ["dr"])
            dv(lambda e: e.tensor_copy(out=dr[:, 2, :], in_=wA[:]), ["wA"], ["dr"])
            dv(lambda e: e.tensor_copy(out=dr[:, 3, :], in_=wB[:]), ["wB"], ["dr"])
            dv(lambda e: e.tensor_copy(out=dr[:, 4, 0:NSLOT], in_=idxW[:]), ["idxW"], ["dr"])
            dv(lambda e: e.tensor_copy(out=dr[:, 5, 0:16], in_=ntot), [cin_n], ["dr"])
            out_ops.append(op("sp", lambda e: e.dma_start(out=dbgr_d, in_=dr[:]), reads=["dr"], dma="dbgr"))

        for gi in range(ND):
            xb = xin[gi % NXIN]
            xn = f"xin{gi % NXIN}"
            for (pos_i, pn) in ((posA, "posA"), (posB, "posB")):
                op("pool", lambda e, gi=gi, xb=xb, pos_i=pos_i: e.indirect_dma_start(
                    out=Xs_d, out_offset=bass.IndirectOffsetOnAxis(pos_i[:, gi:gi + 1], 0), in_=xb, in_offset=None),
                   reads=[xn, pn], writes=[f"Xs{gi % NXIN}"], dma=xn)
            if gi + NXIN < ND:
                xin_load(gi + NXIN)

        NS3 = NSLOT if stages >= 3 else 0

        def slot_loads(s):
            b = s % NEB
            for m in range(3):
                op("pool", lambda e, s=s, b=b, m=m: e.indirect_dma_start(
                    out=wflat[b][m], out_offset=None, in_=wscr_d[m],
                    in_offset=bass.IndirectOffsetOnAxis(idxW[:, s:s + 1], 0)),
                   reads=["idxW", "wscr0", "wscr1"], writes=[tE[b][m]], dma=f"w{'gud'[m]}{b}")
            xb = xs[s % 2]
            xn = f"xs{s % 2}"
            op("sp", lambda e, s=s, xb=xb: e.dma_start(out=xb, in_=Xs_d[s * 512:(s + 1) * 512, :].rearrange("(j p) d -> p j d", p=128)),
               reads=[f"Xs{q}" for q in range(NXIN)], writes=[xn], dma=xn)

        def slot_transposes(s):
            xb = xs[s % 2]
            xn = f"xs{s % 2}"
            xt = xT[s % 2]
            xtn = f"xT{s % 2}"
            for j in range(4):
                bank = j % 2
                pbT = PB[bank][:].bitcast(BF16).rearrange("p (c t) -> p c t", c=8)
                for c in range(8):
                    op("pe", lambda e, c=c, j=j, pbT=pbT, xb=xb: e.transpose(out=pbT[:, c, :], in_=xb[:, j, c * 128:(c + 1) * 128],
                                                                            identity=ident[:]),
                       reads=[xn, "ident"], writes=[tPB[bank]])
                op("dve", lambda e, j=j, pbT=pbT, xt=xt: e.tensor_tensor(
                    out=xt[:, :, j * 128:(j + 1) * 128], in0=pbT,
                    in1=gfcol[:].unsqueeze(2).to_broadcast([128, 8, 128]), op=ALU.mult),
                   reads=[tPB[bank], "gfcol"], writes=[xtn])

        def slot_gateup(s):
            b = s % NEB
            xt = xT[s % 2]
            xtn = f"xT{s % 2}"
            hb_ = s % 2
            for j in range(4):
                bg_, bu_ = 2 + (j % 2) * 2, 3 + (j % 2) * 2
                for c in range(8):
                    op("pe", lambda e, c=c, j=j, b=b, bg_=bg_, xt=xt: e.matmul(
                        PB[bg_][:], lhsT=wgs[b][:, c, j * 128:(j + 1) * 128], rhs=xt[:, c, :],
                        start=(c == 0), stop=(c == 7)),
                       reads=[tE[b][0], xtn], writes=[tPB[bg_]])
                for c in range(8):
                    op("pe", lambda e, c=c, j=j, b=b, bu_=bu_, xt=xt: e.matmul(
                        PB[bu_][:], lhsT=wus[b][:, c, j * 128:(j + 1) * 128], rhs=xt[:, c, :],
                        start=(c == 0), stop=(c == 7)),
                       reads=[tE[b][1], xtn], writes=[tPB[bu_]])
                op("act", lambda e, j=j, bg_=bg_: e.activation(out=sgs[j % 2], in_=PB[bg_][:], func=AF.Silu),
                   reads=[tPB[bg_]], writes=[f"sgs{j % 2}"])
                op("dve", lambda e, j=j, bu_=bu_, hb_=hb_: e.tensor_tensor(out=hid[hb_][:, j, :], in0=PB[bu_][:],
                                                                         in1=sgs[j % 2], op=ALU.mult),
                   reads=[tPB[bu_], f"sgs{j % 2}"], writes=[f"hid{hb_}"])

        def slot_down(s):
            b = s % NEB
            hb_ = s % 2
            for tl in range(4):
                yi = (s * 4 + tl) % NYS
                yb = ys[yi]
                yn = f"ys{yi}"
                for hf in range(2):
                    bank = 6 + hf
                    for j in range(4):
                        op("pe", lambda e, j=j, tl=tl, hf=hf, b=b, hb_=hb_, bank=bank: e.matmul(
                            PB[bank][:], lhsT=hid[hb_][:, j, tl * 128:(tl + 1) * 128],
                            rhs=wds[b][:, j, hf * 512:(hf + 1) * 512], start=(j == 0), stop=(j == 3)),
                           reads=[f"hid{hb_}", tE[b][2]], writes=[tPB[bank]])
                    op("act", lambda e, hf=hf, bank=bank, yb=yb: e.copy(out=yb[:, hf * 512:(hf + 1) * 512], in_=PB[bank][:]),
                       reads=[tPB[bank]], writes=[yn])
                op("sp", lambda e, s=s, tl=tl, yb=yb: e.dma_start(out=Ys_d[s * 512 + tl * 128: s * 512 + (tl + 1) * 128, :], in_=yb),
                   reads=[yn], writes=[f"Ys{yi}"], dma=f"ysd{yi}")

        if NS3:
            slot_loads(0)
            slot_transposes(0)
            slot_loads(1)
        for s in range(NS3):
            slot_gateup(s)
            if s + 1 < NS3:
                slot_transposes(s + 1)
            slot_down(s)
            if s + 2 < NS3:
                slot_loads(s + 2)

        if stages >= 4:
            S.barrier("dve", lambda e: e.memset(dummy[:], 0.0))
        NT4 = NTK if stages >= 4 else 0

        def loads4(gi):
            k2 = gi % N4
            op("sp", lambda e, gi=gi, k2=k2: e.dma_start(out=hb[k2], in_=hS_d[gi * 128:(gi + 1) * 128, :]),
               reads=["hS"], writes=[f"hb{k2}"], dma=f"hb{k2}")
            for (Yb, ynm, pos_i, pn) in ((YA, "YA", posA, "posA"), (YB, "YB", posB, "posB")):
                op("pool", lambda e, gi=gi, k2=k2, Yb=Yb, pos_i=pos_i: e.indirect_dma_start(
                    out=Yb[k2], out_offset=None, in_=Ys_d, in_offset=bass.IndirectOffsetOnAxis(pos_i[:, gi:gi + 1], 0)),
                   reads=[pn], writes=[f"{ynm}{k2}"], dma=f"{ynm}{k2}")

        for gi in range(min(N4, NT4)):
            loads4(gi)
        for gi in range(NT4):
            k2 = gi % N4
            op("dve", lambda e, gi=gi, k2=k2: e.scalar_tensor_tensor(out=hb[k2], in0=YA[k2], scalar=wA[:, gi:gi + 1], in1=hb[k2],
                                                                     op0=ALU.mult, op1=ALU.add),
               reads=[f"YA{k2}", "wA"], writes=[f"hb{k2}"])
            op("dve", lambda e, gi=gi, k2=k2: e.scalar_tensor_tensor(out=hb[k2], in0=YB[k2], scalar=wB[:, gi:gi + 1], in1=hb[k2],
                                                                     op0=ALU.mult, op1=ALU.add),
               reads=[f"YB{k2}", "wB"], writes=[f"hb{k2}"])
            op("act", lambda e, k2=k2: e.activation(out=hn4, in_=hb[k2], func=AF.Square, accum_out=ss4[k2]),
               reads=[f"hb{k2}"], writes=["hn4", f"ss4{k2}"])
            op("dve", lambda e, k2=k2: e.tensor_scalar(out=rs4[k2], in0=ss4[k2], scalar1=1.0 / D, scalar2=EPS, op0=ALU.mult, op1=ALU.add),
               reads=[f"ss4{k2}"], writes=[f"rs4{k2}"])
            op("act", lambda e, k2=k2: e.sqrt(out=rs4[k2], in_=rs4[k2]), reads=[], writes=[f"rs4{k2}"])
            op("dve", lambda e, k2=k2: e.reciprocal(out=rs4[k2], in_=rs4[k2]), reads=[], writes=[f"rs4{k2}"])
            op("dve", lambda e, k2=k2: e.scalar_tensor_tensor(out=hb[k2], in0=hb[k2], scalar=rs4[k2], in1=gfin[:],
                                                              op0=ALU.mult, op1=ALU.mult),
               reads=[f"rs4{k2}", "gfin"], writes=[f"hb{k2}"])
            out_ops.append(op("sp", lambda e, gi=gi, k2=k2: e.dma_start(out=y_d[gi * 128:(gi + 1) * 128, :], in_=hb[k2]),
                              reads=[f"hb{k2}"], dma=f"hb{k2}"))
            if gi + N4 < NT4:
                loads4(gi + N4)

        S.emit(nc, final_wait_ops=out_ops)
    return nc


def _t5_bucket(dist):
    dist = np.asarray(dist, dtype=np.int64)
    nf = np.maximum(dist, 1).astype(np.float32)
    large = 16 + (np.log(nf / np.float32(16)) / np.float32(math.log(128 / 16)) * np.float32(16)).astype(np.int32)
    large = np.minimum(large, 31)
    return np.where(dist < 16, dist, large)


def prep_shared(inp):
    f = lambda a: np.ascontiguousarray(np.asarray(a, dtype=np.float32))
    w_in = f(inp["w_in"])[0]
    perm = np.arange(3840)
    qcols = []
    for j in range(4):
        qcols += list(range(1024 + j * 64, 1024 + (j + 1) * 64))
        qcols += list(range(1024 + (4 + j) * 64, 1024 + (5 + j) * 64))
    perm[1024:1536] = np.array(qcols)
    w_in = np.ascontiguousarray(w_in[:, perm])
    wr = np.concatenate([f(inp["router_group_w"])[0]] + [f(inp["router_expert_w"])[0, g] for g in range(4)], axis=1)
    rb = np.concatenate([f(inp["router_group_b"])[0], f(inp["router_expert_b"])[0].reshape(-1)])[None, :]
    col = lambda v, c: np.ascontiguousarray(f(v).reshape(c, 128).T)
    ws = f(inp["gm_w_spatial"])[0]
    wst = np.ascontiguousarray(ws.transpose(2, 0, 1))
    s_i = np.arange(128)[:, None, None]
    t_i = np.arange(128)[None, None, :]
    wmask = np.broadcast_to((s_i <= t_i), (128, 8, 128)).astype(np.float32)
    bs = f(inp["gm_b_spatial"])[0]
    bst = np.zeros((128, 4, 128), np.float32)
    for g in range(8):
        bst[(g % 2) * 64:(g % 2) * 64 + 64, g // 2, :] = bs[g][None, :]
    rel = f(inp["rel_bias"])
    s_ = np.arange(128)[:, None]
    q_ = np.arange(128)[None, :]
    d_own = q_ - s_
    d_prev = q_ + 128 - s_
    bias = np.zeros((128, 2, 2, 4, 128), np.float32)
    maskc = np.zeros((128, 2, 2, 4, 128), np.float32)
    b_own = _t5_bucket(np.clip(d_own, 0, 127))
    b_prev = _t5_bucket(np.clip(d_prev, 0, 127))
    for kh in range(2):
        for h4 in range(4):
            h = kh * 4 + h4
            bias[:, kh, 0, h4, :] = rel[b_own, h]
            bias[:, kh, 1, h4, :] = rel[b_prev, h]
            maskc[:, kh, 0, h4, :] = np.where(d_own >= 0, 0.0, NEG)
            maskc[:, kh, 1, h4, :] = np.where(d_prev < 128, 0.0, NEG)
    def pmaj(w, c):
        e_, r_, n_ = w.shape
        return np.ascontiguousarray(w.reshape(e_, c, 128, n_).transpose(0, 2, 1, 3).reshape(e_, 128, c * n_))

    return {
        "w_in": w_in,
        "wpa": f(inp["w_proj_a"])[0], "wpb": f(inp["w_proj_b"])[0], "wout": f(inp["w_out"])[0],
        "wgp": pmaj(f(inp["expert_w_gate"])[0], 8), "wup": pmaj(f(inp["expert_w_up"])[0], 8),
        "wdp": pmaj(f(inp["expert_w_down"])[0], 4),
        "utri": np.triu(np.ones((128, 128), np.float32), 1),
        "cst": np.concatenate([np.arange(8, dtype=np.float32) * 512.0,
                               np.tril(np.ones((16, 16), np.float32), -1).reshape(-1),
                               np.arange(32, dtype=np.float32)])[None, :],
        "iop": np.arange(128, dtype=np.float32)[:, None],
        "wr": np.ascontiguousarray(wr), "rb": np.ascontiguousarray(rb),
        "gacol": col(inp["attn_norm_g"], 8), "gfcol": col(inp["ffn_norm_g"], 8), "gvcol": col(inp["gm_v_norm_g"], 4),
        "gfin": f(inp["final_norm_g"]).reshape(1, D),
        "wst": wst, "wmask": wmask, "bst": bst,
        "sinks": np.ascontiguousarray(np.repeat(f(inp["attn_sinks"]).reshape(2, 4), 64, axis=0)),
        "bias": bias, "maskc": maskc,
        "idn": np.eye(128, dtype=np.float32),
    }


def kernel(**inputs):
    shared = prep_shared(inputs)
    x = np.asarray(inputs["x"], dtype=np.float32)
    nc = build()
    in_maps = []
    for c in range(NCORES):
        m = dict(shared)
        m["x"] = np.ascontiguousarray(x[c])
        in_maps.append(m)
    res = run_bass_kernel_spmd(nc, in_maps, core_ids=list(range(NCORES)))
    return np.stack([np.asarray(r["y"], dtype=np.float32) for r in res.results], axis=0)
```

```python
import math
from contextlib import ExitStack

import numpy as np
import concourse.bass as bass
import concourse.mybir as mybir
from concourse.bass_utils import run_bass_kernel_spmd

F32 = mybir.dt.float32
BF16 = mybir.dt.bfloat16
AF = mybir.ActivationFunctionType
ALU = mybir.AluOpType
AX = mybir.AxisListType

SEQ = 4096
D = 1024
NCORES = 8
PT = 1024
NT = PT // 128
NST = PT // 512
NPASS_FULL = SEQ // PT
EPS = 1e-6
NEG = -30000.0
NSLOT = 31
NTT = SEQ // 128
I32 = mybir.dt.int32

U_OFF, V_OFF, Q_OFF, K_OFF, VA_OFF, GA_OFF, GB_OFF = 0, 512, 1024, 1536, 1664, 1792, 2816

ENG = ("pe", "act", "dve", "pool", "sp")


class Tok:
    __slots__ = ("name", "last_w", "readers")

    def __init__(self, name):
        self.name = name
        self.last_w = None
        self.readers = []


class Op:
    __slots__ = ("eng", "fn", "deps", "sig", "dma_sem", "dma_cnt", "seq")

    def __init__(self, eng, fn):
        self.eng = eng
        self.fn = fn
        self.deps = set()
        self.sig = False
        self.dma_sem = None
        self.dma_cnt = 0
        self.seq = 0


class Sched:
    def __init__(self):
        self.ops = []
        self.per = {e: [] for e in ENG}
        self.dma_counts = {}
        self.total_keys = set()
        self.epoch_op = None
        self.last_dma = {}

    def op(self, eng, fn, reads=(), writes=(), dma=None):
        o = Op(eng, fn)
        for t in list(reads) + list(writes):
            if t.last_w is not None:
                o.deps.add(t.last_w)
        for t in writes:
            for r in t.readers:
                o.deps.add(r)
        if self.epoch_op is not None:
            o.deps.add(self.epoch_op)
        o.deps.discard(o)
        for t in reads:
            t.readers.append(o)
        for t in writes:
            t.last_w = o
            t.readers = []
        if dma is not None:
            o.dma_sem = dma
            self.dma_counts[dma] = self.dma_counts.get(dma, 0) + 1
            o.dma_cnt = self.dma_counts[dma]
            self.last_dma[dma] = o
        self.ops.append(o)
        self.per[eng].append(o)
        return o

    def barrier(self, eng, fn):
        o = Op(eng, fn)
        for e in ENG:
            seen_c = False
            for p in reversed(self.per[e]):
                if p.dma_sem is None:
                    o.deps.add(p)
                    break
        for k, p in self.last_dma.items():
            o.deps.add(p)
        if self.epoch_op is not None:
            o.deps.add(self.epoch_op)
        self.ops.append(o)
        self.per[eng].append(o)
        self.epoch_op = o
        return o

    def emit(self, nc, final_wait_ops=()):
        def skip(d, o):
            return d.dma_sem is None and o.dma_sem is None and d.eng == "pe" and o.eng == "pe"

        for o in self.ops:
            for d in o.deps:
                if d.dma_sem is None and not skip(d, o):
                    d.sig = True
        for o in final_wait_ops:
            if o.dma_sem is None:
                o.sig = True
        for e in ENG:
            c = 0
            for o in self.per[e]:
                if o.dma_sem is None and o.sig:
                    c += 1
                    o.seq = c
        with ExitStack() as es:
            esem = {e: es.enter_context(nc.semaphore(f"s_{e}")) for e in ENG}
            dsem = {k: es.enter_context(nc.semaphore(f"d_{k}")) for k in self.dma_counts}
            block = es.enter_context(nc.Block())

            def dval(d):
                if d.dma_sem in self.total_keys:
                    return 16 * self.dma_counts[d.dma_sem]
                return 16 * d.dma_cnt

            def need(o):
                w = {}
                for d in o.deps:
                    if d.dma_sem is not None:
                        key, val = ("d", d.dma_sem), dval(d)
                    else:
                        if skip(d, o):
                            continue
                        key, val = ("e", d.eng), d.seq
                    if w.get(key, 0) < val:
                        w[key] = val
                return w

            def run(ename):
                def body(eng):
                    waited = {}
                    for o in self.per[ename]:
                        for key, val in need(o).items():
                            if waited.get(key, 0) >= val:
                                continue
                            waited[key] = val
                            sem = dsem[key[1]] if key[0] == "d" else esem[key[1]]
                            eng.wait_ge(sem, val)
                        ins = o.fn(eng)
                        if o.dma_sem is not None:
                            ins.then_inc(dsem[o.dma_sem], 16)
                        elif o.sig:
                            ins.then_inc(esem[ename], 1)
                    if ename == "sp":
                        for o in final_wait_ops:
                            if o.dma_sem is not None:
                                eng.wait_ge(dsem[o.dma_sem], dval(o))
                            else:
                                eng.wait_ge(esem[o.eng], o.seq)
                return body

            block.tensor(run("pe"))
            block.scalar(run("act"))
            block.vector(run("dve"))
            block.gpsimd(run("pool"))
            block.sync(run("sp"))


class Arena:
    def __init__(self, nc, es, name, nbytes):
        self.t = es.enter_context(nc.sbuf_tensor(name, [128, nbytes // 2], BF16))
        self.cap = nbytes
        self.off = 0

    def alloc(self, shape, dt):
        n = 1
        for s_ in shape[1:]:
            n *= s_
        esz = 2 if dt == BF16 else 4
        o = self.off
        self.off += (n * esz + 63) // 64 * 64
        assert self.off <= self.cap, (self.off, self.cap)
        ap = self.t[0:shape[0], o // 2:(o + n * esz) // 2]
        if dt != BF16:
            ap = ap.bitcast(dt)
        if len(shape) > 2:
            names = " ".join(f"d{k}" for k in range(len(shape) - 1))
            ap = ap.rearrange(f"p ({names}) -> p {names}", **{f"d{k}": shape[k + 1] for k in range(len(shape) - 1)})
        return ap


def build(npass=NPASS_FULL, debug=False, stages=4):
    nc = bass.Bass("TRN2", target_bir_lowering=False)

    def din(name, shape):
        return nc.dram_tensor(name, list(shape), F32, kind="ExternalInput").ap()

    x_d = din("x", [SEQ, D])
    win_d = din("w_in", [D, 3840])
    wpa_d = din("wpa", [512, D])
    wpb_d = din("wpb", [512, D])
    wout_d = din("wout", [D, D])
    wexp_d = [din("wgp", [16, 128, 4096]), din("wup", [16, 128, 4096]), din("wdp", [16, 128, 4096])]
    wr_d = din("wr", [D, 20])
    rb_d = din("rb", [1, 20])
    gacol_d = din("gacol", [128, 8])
    gfcol_d = din("gfcol", [128, 8])
    gvcol_d = din("gvcol", [128, 4])
    gfin_d = din("gfin", [1, D])
    wst_d = din("wst", [128, 8, 128])
    wmask_d = din("wmask", [128, 8, 128])
    bst_d = din("bst", [128, 4, 128])
    sinks_d = din("sinks", [128, 4])
    bias_d = din("bias", [128, 2, 2, 4, 128])
    mask_d = din("maskc", [128, 2, 2, 4, 128])
    idn_d = din("idn", [128, 128])
    utri_d = din("utri", [128, 128])
    cst_d = din("cst", [1, 8 + 256 + 32])
    iop_d = din("iop", [128, 1])
    y_d = nc.dram_tensor("y", [SEQ, D], F32, kind="ExternalOutput").ap()
    hS_d = nc.dram_tensor("hS", [SEQ, D], F32, kind="Internal").ap()
    hn2S_d = nc.dram_tensor("hn2S", [SEQ, D], BF16, kind="Internal").ap()
    Xs_d = nc.dram_tensor("Xs", [NSLOT * 512, D], BF16, kind="Internal").ap()
    Ys_d = nc.dram_tensor("Ys", [NSLOT * 512, D], F32, kind="Internal").ap()
    winS_d = nc.dram_tensor("winS", [25, 128, 1024], BF16, kind="Internal").ap()
    wscr_d = [nc.dram_tensor(f"wscr{m}", [16 * 128, 4096], BF16, kind="Internal").ap() for m in range(3)]
    if debug:
        dbg_d = nc.dram_tensor("dbg", [PT, D], F32, kind="ExternalOutput").ap()
        dbgr_d = nc.dram_tensor("dbgr", [128, 6, NTT], F32, kind="ExternalOutput").ap()

    S = Sched()
    S.total_keys.update(["const"])

    with ExitStack() as es:
        def sb(name, shape, dt):
            return es.enter_context(nc.sbuf_tensor(name, list(shape), dt))

        ident = sb("ident", [128, 128], BF16)
        gacol = sb("gacol_s", [128, 8], F32)
        gfcol = sb("gfcol_s", [128, 8], F32)
        gvcol = sb("gvcol_s", [128, 4], F32)
        wstb = sb("wstb", [128, 8, 128], BF16)
        bst = sb("bst_s", [128, 4, 128], F32)
        sinkexp = sb("sinkexp", [128, 4], F32)
        BMhi = sb("BMhi", [128, 2048], BF16)
        BMlo = sb("BMlo", [128, 2048], BF16)
        ones64 = sb("ones64", [128, 64], BF16)
        ones128 = sb("ones128", [128, 128], BF16)
        utri = sb("utri_s", [128, 128], BF16)
        cst = sb("cst_s", [128, 8 + 256 + 32], F32)
        iop = sb("iop_s", [128, 1], F32)
        wr = sb("wr_s", [128, 8, 20], BF16)
        rb = sb("rb_s", [128, 20], F32)
        lg = sb("lg_all", [128, NTT, 20], F32)
        posA = sb("posA", [128, NTT], I32)
        posB = sb("posB", [128, NTT], I32)
        wA = sb("wA", [128, NTT], F32)
        wB = sb("wB", [128, NTT], F32)
        idxW = sb("idxW", [128, NSLOT], I32)
        dummy = sb("bar_dummy", [128, 1], F32)
        gfin = sb("gfin_s", [128, D], F32)
        NEB = 2
        wreg = sb("wreg", [128, NEB * 12288], BF16)
        wgs = [wreg[:, b * 12288:b * 12288 + 4096].rearrange("p (c n) -> p c n", c=8) for b in range(NEB)]
        wus = [wreg[:, b * 12288 + 4096:b * 12288 + 8192].rearrange("p (c n) -> p c n", c=8) for b in range(NEB)]
        wds = [wreg[:, b * 12288 + 8192:b * 12288 + 12288].rearrange("p (c n) -> p c n", c=4) for b in range(NEB)]
        wflat = [[wreg[:, b * 12288 + m * 4096:b * 12288 + (m + 1) * 4096] for m in range(3)] for b in range(NEB)]
        wpa = wreg[:, 0:4096].rearrange("p (c n) -> p c n", c=4)
        wout = wreg[:, 4096:12288].rearrange("p (c n) -> p c n", c=8)
        wpb = wreg[:, 12288:16384].rearrange("p (c n) -> p c n", c=4)
        stgs = [wreg[:, 20480:24576], wreg[:, 16384:20480]]
        AR = Arena(nc, es, "arena", 133 * 1024)
        H = AR.alloc([128, NT, D], F32)
        BM = H[:, 6:8, :].rearrange("p a (k v j t) -> p (a k) v j t", k=1, v=2, j=4)
        hn2T = AR.alloc([128, 8, 512], BF16)
        wv = AR.alloc([128, 8, 512], BF16)
        wva = AR.alloc([128, 8, 128], BF16)
        NRING = 6
        ring = [AR.alloc([128, 8, 128], BF16) for _ in range(NRING)]
        hn2 = [AR.alloc([128, D], BF16) for _ in range(2)]
        hn = hn2[0]
        hnT = AR.alloc([128, 8, 512], BF16)
        hnTb = AR.alloc([128, 8, 512], BF16)
        ssA = AR.alloc([128, 4], F32)
        rsA = AR.alloc([128, 4], F32)
        ssF = AR.alloc([128, 4], F32)
        rsF = AR.alloc([128, 4], F32)
        wst = hnT[:, 0:2, :].rearrange("p a (b t) -> p (a b) t", b=4)
        wmask = hnT[:, 2:4, :].rearrange("p a (b t) -> p (a b) t", b=4)
        ss = AR.alloc([128, 8], F32)
        rstd = AR.alloc([128, 8], F32)
        uT = AR.alloc([128, 4, 512], BF16)
        qT = AR.alloc([128, 4, 512], BF16)
        kT = AR.alloc([128, (NT + 1) * 128], BF16)
        vatt = AR.alloc([128, NT + 1, 128], BF16)
        gv2 = [AR.alloc([128, 512], F32) for _ in range(2)]
        vnh2 = [AR.alloc([128, 512], BF16) for _ in range(2)]
        bnst2 = [AR.alloc([128, 6], F32) for _ in range(2)]
        mv2 = [AR.alloc([128, 2], F32) for _ in range(2)]
        rsv2 = [AR.alloc([128, 1], F32) for _ in range(2)]
        nmr2 = [AR.alloc([128, 1], F32) for _ in range(2)]
        gtmp = AR.alloc([128, 4, 128], F32)
        pT = AR.alloc([128, 2048], BF16)
        den = AR.alloc([128, 512], F32)
        aT = AR.alloc([128, 4, 512], BF16)
        bT = AR.alloc([128, 4, 512], BF16)
        sga = AR.alloc([128, 512], F32)
        sgb = AR.alloc([128, 512], F32)
        mT = AR.alloc([128, 8, 512], BF16)
        maskc = mT[:, 0:4, :].rearrange("p a (b c t) -> p (a b c) t", b=2, c=2).rearrange("p (v k h) t -> p v k h t", v=2, k=2)
        side1_end = AR.off
        AR.off = 0
        NB_ = NTT
        r_m = AR.alloc([128, NB_], F32)
        r_og = AR.alloc([128, NB_, 4], F32)
        r_eg = AR.alloc([128, NB_, 4], F32)
        r_gp = AR.alloc([128, NB_], F32)
        r_sel = AR.alloc([128, NB_, 4, 4], F32)
        r_es = AR.alloc([128, NB_, 4], F32)
        r_m1 = AR.alloc([128, NB_], F32)
        r_o1 = AR.alloc([128, NB_, 4], F32)
        r_es2 = AR.alloc([128, NB_, 4], F32)
        r_m2 = AR.alloc([128, NB_], F32)
        r_o2 = AR.alloc([128, NB_, 4], F32)
        r_d = AR.alloc([128, NB_], F32)
        r_w1 = AR.alloc([128, NB_], F32)
        M1 = AR.alloc([128, NB_, 4, 4], F32)
        M2 = AR.alloc([128, NB_, 4, 4], F32)
        Mb = AR.alloc([128, NB_ * 16], BF16)
        R1s = AR.alloc([128, NB_, 16], F32)
        Ca = AR.alloc([128, NB_, 16], F32)
        Cb = AR.alloc([128, NB_, 16], F32)
        Tts = AR.alloc([128, NB_, 16], F32)
        r_cmp = AR.alloc([128, 16, 16], F32)
        r_cmp2 = AR.alloc([128, NSLOT, 16], F32)
        r_nb = AR.alloc([128, 16], F32)
        r_ob = AR.alloc([128, 16], F32)
        r_oe = AR.alloc([128, 16], F32)
        r_pf = AR.alloc([128, NB_], F32)
        r_eid = AR.alloc([128, NSLOT], F32)
        NXIN = 24
        xin = [AR.alloc([128, D], BF16) for _ in range(NXIN)]
        xs = [AR.alloc([128, 4, D], BF16) for _ in range(2)]
        xT = [AR.alloc([128, 8, 512], BF16) for _ in range(2)]
        sgs = [AR.alloc([128, 512], F32) for _ in range(2)]
        hid = [AR.alloc([128, 4, 512], BF16) for _ in range(2)]
        NYS = 3
        ys = [AR.alloc([128, D], F32) for _ in range(NYS)]
        side2_end = AR.off
        AR.off = 0
        N4 = 6
        YA = [AR.alloc([128, D], F32) for _ in range(N4)]
        YB = [AR.alloc([128, D], F32) for _ in range(N4)]
        hb = [AR.alloc([128, D], F32) for _ in range(N4)]
        ss4 = [AR.alloc([128, 1], F32) for _ in range(N4)]
        rs4 = [AR.alloc([128, 1], F32) for _ in range(N4)]
        hn4 = AR.alloc([128, D], BF16)
        PQ = es.enter_context(nc.psum_tensor("pq", [128, 4096], F32))
        PB = [PQ[:, i * 512:(i + 1) * 512] for i in range(8)]

        T = {}

        def tk(n):
            if n not in T:
                T[n] = Tok(n)
            return T[n]

        tH = [tk(f"H{i}") for i in range(NT)]
        tPB = [tk(f"PB{i}") for i in range(8)]
        tE = [[tk(f"wg{i}"), tk(f"wu{i}"), tk(f"wd{i}")] for i in range(NEB)]
        tRing = [tk(f"ring{i}") for i in range(NRING)]

        def op(eng, fn, reads=(), writes=(), dma=None):
            return S.op(eng, fn, [tk(r) if isinstance(r, str) else r for r in reads],
                        [tk(w) if isinstance(w, str) else w for w in writes], dma)

        def cdma(eng, out, in_, w):
            op(eng, lambda e, out=out, in_=in_: e.dma_start(out=out, in_=in_), writes=[w], dma="const")

        cdma("sp", gacol[:], gacol_d, "gacol")
        cdma("sp", gfcol[:], gfcol_d, "gfcol")
        cdma("sp", gvcol[:], gvcol_d, "gvcol")
        cdma("sp", bst[:], bst_d, "bst")
        cdma("sp", sinkexp[:], sinks_d, "sinkexp")
        cdma("sp", BM, bias_d, "BM")
        cdma("sp", rb[:], rb_d.partition_broadcast(128), "rb")
        cdma("sp", cst[:], cst_d.partition_broadcast(128), "cst")
        cdma("sp", iop[:], iop_d, "iop")
        cdma("pool", ident[:], idn_d, "ident")
        cdma("pool", utri[:], utri_d, "utri")
        cdma("pool", wst, wst_d, "wst_a")
        cdma("pool", wmask, wmask_d, "wmask_a")
        cdma("pool", maskc, mask_d, "maskc_a")
        cdma("pool", wr[:], wr_d.rearrange("(c p) n -> p c n", p=128), "wr")
        cdma("pool", wv, win_d[:, V_OFF:V_OFF + 512].rearrange("(c p) n -> p c n", p=128), "wv")
        cdma("pool", wva, win_d[:, VA_OFF:VA_OFF + 128].rearrange("(c p) n -> p c n", p=128), "wva")
        op("dve", lambda e: e.tensor_tensor(out=wstb[:], in0=wst, in1=wmask, op=ALU.mult),
           reads=["wst_a", "wmask_a"], writes=["wstb", "hnT"])
        BMf = BM.rearrange("p k v j t -> p (k v j t)")
        op("dve", lambda e: e.tensor_tensor(out=BMf, in0=BMf, in1=maskc.rearrange("p k v j t -> p (k v j t)"), op=ALU.add),
           reads=["maskc_a"], writes=["BM", "mT"])
        op("dve", lambda e: e.tensor_copy(out=BMhi[:], in_=BMf), reads=["BM"], writes=["BMhi"])
        op("dve", lambda e: e.tensor_tensor(out=BMlo[:], in0=BMf, in1=BMhi[:], op=ALU.subtract),
           reads=["BM", "BMhi"], writes=["BMlo", tH[6], tH[7]])
        op("act", lambda e: e.activation(out=sinkexp[:], in_=sinkexp[:], func=AF.Exp), reads=[], writes=["sinkexp"])
        op("dve", lambda e: e.memset(ones64[:], 1.0), writes=["ones64"])
        op("dve", lambda e: e.memset(ones128[:], 1.0), writes=["ones128"])

        out_ops = []

        precast = [(m, ex) for ex in range(16) for m in range(3)]
        pc_ctr = [0]

        def precast_some(n):
            if stages == 1:
                return
            for _ in range(n):
                if pc_ctr[0] >= len(precast):
                    return
                m, ex = precast[pc_ctr[0]]
                q = pc_ctr[0] % 2
                pc_ctr[0] += 1
                op("pool", lambda e, m=m, ex=ex, q=q: e.dma_start(out=stgs[q], in_=wexp_d[m][ex]),
                   writes=[f"stg{q}"], dma=f"stg{q}")
                op("sp", lambda e, m=m, ex=ex, q=q: e.dma_start(out=wscr_d[m][ex * 128:(ex + 1) * 128, :], in_=stgs[q]),
                   reads=[f"stg{q}"], writes=[f"wscr{q}"], dma=f"wscr{q}")

        hnTs = [hnT, hnTb]
        hnTn = ["hnT", "hnTb"]
        ring_ctr = [0]

        blk_ids = {}

        def stream_block(col0):
            r = ring_ctr[0] % NRING
            ring_ctr[0] += 1
            first = col0 not in blk_ids
            if first:
                blk_ids[col0] = len(blk_ids)
            bid = blk_ids[col0]
            if first:
                op("pool", lambda e, r=r, col0=col0: e.dma_start(
                    out=ring[r], in_=win_d[:, col0:col0 + 128].rearrange("(c p) n -> p c n", p=128)),
                   writes=[tRing[r]], dma=f"ring{r}")
                op("sp", lambda e, r=r, bid=bid: e.dma_start(out=winS_d[bid], in_=ring[r].rearrange("p c n -> p (c n)")),
                   reads=[tRing[r]], writes=[f"winS{bid}"], dma=f"ring{r}")
            else:
                op("pool", lambda e, r=r, bid=bid: e.dma_start(out=ring[r].rearrange("p c n -> p (c n)"), in_=winS_d[bid]),
                   reads=[f"winS{bid}"], writes=[tRing[r]], dma=f"ring{r}")
            return ring[r], tRing[r]

        def load_x(g):
            for k in range(4):
                i = (g % 2) * 4 + k
                t0_ = g * 512 + k * 128
                op("sp", lambda e, i=i, t0_=t0_: e.dma_start(out=H[:, i, :], in_=x_d[t0_:t0_ + 128, :]),
                   writes=[tH[i]], dma=f"H{i}")

        def norm_pieces(tiles, gcol, dstT, dst_tok, ssb, rsb, ssn, rsn, banks, after_hn=None):
            def sq(k):
                i = tiles[k]
                hb2 = hn2[k % 2]
                op("act", lambda e: e.activation(out=hb2, in_=H[:, i, :], func=AF.Square, accum_out=ssb[:, k:k + 1]),
                   reads=[tH[i]], writes=[f"hn{k % 2}", ssn])

            def rs():
                op("dve", lambda e: e.tensor_scalar(out=rsb, in0=ssb, scalar1=1.0 / D, scalar2=EPS,
                                                    op0=ALU.mult, op1=ALU.add), reads=[ssn], writes=[rsn])
                op("act", lambda e: e.sqrt(out=rsb, in_=rsb), reads=[], writes=[rsn])
                op("dve", lambda e: e.reciprocal(out=rsb, in_=rsb), reads=[], writes=[rsn])

            def tr_a(k):
                i = tiles[k]
                hb2 = hn2[k % 2]
                hnn = f"hn{k % 2}"
                op("act", lambda e: e.activation(out=hb2, in_=H[:, i, :], func=AF.Identity, scale=rsb[:, k:k + 1]),
                   reads=[tH[i], rsn], writes=[hnn])
                if after_hn is not None:
                    after_hn(i, hb2, hnn)

            def tr_b(k):
                hb2 = hn2[k % 2]
                hnn = f"hn{k % 2}"
                bank = banks[k % 2]
                pbT = PB[bank].bitcast(BF16).rearrange("p (c t) -> p c t", c=8)
                for c in range(8):
                    op("pe", lambda e, c=c: e.transpose(out=pbT[:, c, :], in_=hb2[:, c * 128:(c + 1) * 128], identity=ident[:]),
                       reads=[hnn, "ident"], writes=[tPB[bank]])
                op("dve", lambda e: e.tensor_tensor(out=dstT[:, :, k * 128:(k + 1) * 128], in0=pbT,
                                                    in1=gcol[:].unsqueeze(2).to_broadcast([128, 8, 128]), op=ALU.mult),
                   reads=[tPB[bank], "gacol", "gfcol"], writes=[dst_tok])

            def tr(k):
                tr_a(k)
                tr_b(k)
            tr.a = tr_a
            tr.b = tr_b
            return sq, rs, tr

        def make_st(g):
            ps_i, st = g // 2, g % 2
            tok0 = ps_i * PT
            tiles = [st * 4 + k for k in range(4)]
            hT, hTn = hnTs[g % 2], hnTn[g % 2]

            def A_pieces():
                return norm_pieces(tiles, gacol, hT, hTn, ssA, rsA, "ssA", "rsA", (0, 1))

            def proj_fm(col0, bank, evac):
                wb_, wt_ = stream_block(col0)
                for c in range(8):
                    op("pe", lambda e, c=c: e.matmul(PB[bank], lhsT=wb_[:, c, :], rhs=hT[:, c, :], start=(c == 0), stop=(c == 7)),
                       reads=[wt_, hTn], writes=[tPB[bank]])
                evac(bank)

            def B(hook):
                if st == 0 and ps_i > 0:
                    op("dve", lambda e: e.tensor_copy(out=kT[:, 0:128], in_=kT[:, NT * 128:(NT + 1) * 128]),
                       reads=[], writes=["kT"])
                    op("dve", lambda e: e.tensor_copy(out=vatt[:, 0, :], in_=vatt[:, NT, :]), reads=[], writes=["vatt"])
                blk = 0
                for j in range(4):
                    proj_fm(U_OFF + j * 128, 1 + (j % 2),
                            lambda bank, j=j: op("act", lambda e: e.activation(out=uT[:, j, :], in_=PB[bank],
                                                                              func=AF.Gelu_apprx_tanh),
                                                 reads=[tPB[bank]], writes=["uT"]))
                    if hook is not None:
                        hook(blk)
                    blk += 1
                for j in range(4):
                    proj_fm(Q_OFF + j * 128, 1 + (j % 2),
                            lambda bank, j=j: op("act", lambda e: e.activation(out=qT[:, j, :], in_=PB[bank],
                                                                              func=AF.Identity, scale=0.125),
                                                 reads=[tPB[bank]], writes=["qT"]))
                    if hook is not None:
                        hook(blk)
                    blk += 1
                kc0 = (1 + st * 4) * 128
                proj_fm(K_OFF, 1,
                        lambda bank: op("dve", lambda e: e.tensor_copy(out=kT[:, kc0:kc0 + 512], in_=PB[bank]),
                                        reads=[tPB[bank]], writes=["kT"]))
                if hook is not None:
                    hook(blk)
                    hook(blk + 1)

            def t_v(i):
                ts = (i - st * 4) * 128
                slot = 1 + i
                q2 = i % 2
                gv, vnh, bnst, mv, rsv, nmr = gv2[q2], vnh2[q2], bnst2[q2], mv2[q2], rsv2[q2], nmr2[q2]
                nm = lambda x: f"{x}{q2}"
                for c in range(8):
                    op("pe", lambda e, c=c, ts=ts: e.matmul(PB[0], lhsT=hT[:, c, ts:ts + 128], rhs=wv[:, c, :],
                                                            start=(c == 0), stop=(c == 7)),
                       reads=[hTn, "wv"], writes=[tPB[0]])
                for c in range(8):
                    op("pe", lambda e, c=c, ts=ts: e.matmul(PB[1][:, 0:128], lhsT=hT[:, c, ts:ts + 128],
                                                            rhs=wva[:, c, :], start=(c == 0), stop=(c == 7)),
                       reads=[hTn, "wva"], writes=[tPB[1]])
                op("act", lambda e, gv=gv: e.activation(out=gv, in_=PB[0], func=AF.Gelu_apprx_tanh),
                   reads=[tPB[0]], writes=[nm("gv")])
                op("dve", lambda e, slot=slot: e.tensor_copy(out=vatt[:, slot, :], in_=PB[1][:, 0:128]),
                   reads=[tPB[1]], writes=["vatt"])
                op("dve", lambda e, gv=gv, bnst=bnst: e.bn_stats(out=bnst, in_=gv), reads=[nm("gv")], writes=[nm("bnst")])
                op("dve", lambda e, mv=mv, bnst=bnst: e.bn_aggr(out=mv, in_=bnst), reads=[nm("bnst")], writes=[nm("mv")])
                op("dve", lambda e, mv=mv, rsv=rsv: e.tensor_scalar_add(out=rsv, in0=mv[:, 1:2], scalar1=EPS),
                   reads=[nm("mv")], writes=[nm("rsv")])
                op("act", lambda e, rsv=rsv: e.sqrt(out=rsv, in_=rsv), reads=[], writes=[nm("rsv")])
                op("dve", lambda e, rsv=rsv: e.reciprocal(out=rsv, in_=rsv), reads=[], writes=[nm("rsv")])
                op("dve", lambda e, mv=mv, rsv=rsv, nmr=nmr: e.tensor_scalar(out=nmr, in0=mv[:, 0:1], scalar1=rsv, scalar2=-1.0,
                                                                           op0=ALU.mult, op1=ALU.mult),
                   reads=[nm("mv"), nm("rsv")], writes=[nm("nmr")])
                op("act", lambda e, gv=gv, vnh=vnh, rsv=rsv, nmr=nmr: e.activation(out=vnh, in_=gv, func=AF.Identity,
                                                                                 scale=rsv, bias=nmr),
                   reads=[nm("gv"), nm("rsv"), nm("nmr")], writes=[nm("vnh")])

            def t_sc(i):
                ts = (i - st * 4) * 128
                slot = 1 + i
                gblk = ps_i * NT + i
                for kh in range(2):
                    pr = slice(kh * 64, (kh + 1) * 64)
                    for vi in range(2 if gblk > 0 else 1):
                        bank = 4 + kh * 2 + vi
                        ksl = slot - vi
                        pc = (kh * 2 + vi) * 512
                        op("pe", lambda e, pr=pr, ksl=ksl, ts=ts, bank=bank: e.matmul(
                            PB[bank].rearrange("p (j t) -> p j t", j=4),
                            lhsT=kT[pr, ksl * 128:(ksl + 1) * 128], rhs=qT[pr, :, ts:ts + 128],
                            start=True, stop=False),
                           reads=["kT", "qT"], writes=[tPB[bank]])
                        op("pe", lambda e, bank=bank, pc=pc: e.matmul(PB[bank], lhsT=ident[:], rhs=BMhi[:, pc:pc + 512],
                                                                      start=False, stop=False),
                           reads=["ident", "BMhi"], writes=[tPB[bank]])
                        op("pe", lambda e, bank=bank, pc=pc: e.matmul(PB[bank], lhsT=ident[:], rhs=BMlo[:, pc:pc + 512],
                                                                      start=False, stop=True),
                           reads=["ident", "BMlo"], writes=[tPB[bank]])
                scb = PQ[:, 2048:4096]
                op("act", lambda e, scb=scb: e.activation(out=pT, in_=scb, func=AF.Exp),
                   reads=[tPB[4], tPB[5], tPB[6], tPB[7]], writes=["pT"])

            def t_pv(i):
                ts = (i - st * 4) * 128
                slot = 1 + i
                gblk = ps_i * NT + i
                nv = 2 if gblk > 0 else 1
                for (bank, use_v) in ((3, True), (0, False)):
                    for kh in range(2):
                        kw = {"tile_position": (0, 64)} if kh else {}
                        for vi in range(nv):
                            ksl = slot - vi
                            lhs = vatt[:, ksl, kh * 64:(kh + 1) * 64] if use_v else ones64[:]
                            pc = (kh * 2 + vi) * 512
                            op("pe", lambda e, bank=bank, kh=kh, vi=vi, lhs=lhs, pc=pc, kw=kw, nv=nv: e.matmul(
                                PB[bank][kh * 64:(kh + 1) * 64, :], lhsT=lhs, rhs=pT[:, pc:pc + 512],
                                start=(vi == 0), stop=(vi == nv - 1), **kw),
                               reads=["vatt", "ones64", "pT"], writes=[tPB[bank]])
                op("dve", lambda e: e.tensor_tensor(
                    out=den.rearrange("p (j t) -> p j t", j=4), in0=PB[0].rearrange("p (j t) -> p j t", j=4),
                    in1=sinkexp[:].unsqueeze(2).to_broadcast([128, 4, 128]), op=ALU.add),
                   reads=[tPB[0], "sinkexp"], writes=["den"])
                op("dve", lambda e: e.reciprocal(out=den, in_=den), reads=[], writes=["den"])
                op("dve", lambda e, ts=ts: e.tensor_tensor(
                    out=bT[:, :, ts:ts + 128], in0=PB[3].rearrange("p (j t) -> p j t", j=4),
                    in1=den.rearrange("p (j t) -> p j t", j=4), op=ALU.mult),
                   reads=[tPB[3], "den"], writes=["bT"])

            def t_sp(i):
                ts = (i - st * 4) * 128
                q2 = i % 2
                vnh = vnh2[q2]
                pbs = PB[2].rearrange("p (j t) -> p j t", j=4)
                for g in range(8):
                    lo = (g % 2) * 64
                    kw = {"tile_position": (0, 64)} if g % 2 else {}
                    op("pe", lambda e, g=g, lo=lo, kw=kw, vnh=vnh: e.matmul(
                        pbs[lo:lo + 64, g // 2, :], lhsT=vnh[:, g * 64:(g + 1) * 64], rhs=wstb[:, g, :],
                        start=True, stop=True, **kw),
                       reads=[f"vnh{q2}", "wstb"], writes=[tPB[2]])
                op("dve", lambda e: e.tensor_tensor(out=gtmp, in0=pbs,
                                                    in1=gvcol[:].unsqueeze(2).to_broadcast([128, 4, 128]), op=ALU.mult),
                   reads=[tPB[2], "gvcol"], writes=["gtmp"])
                op("dve", lambda e: e.tensor_tensor(out=gtmp, in0=gtmp, in1=bst[:], op=ALU.add),
                   reads=["bst"], writes=["gtmp"])
                op("dve", lambda e, ts=ts: e.tensor_tensor(out=aT[:, :, ts:ts + 128], in0=gtmp,
                                                           in1=uT[:, :, ts:ts + 128], op=ALU.mult),
                   reads=["gtmp", "uT"], writes=["aT"])

            def C():
                precast_some(2)
                t_v(tiles[0])
                t_sc(tiles[0])
                for k in range(1, 4):
                    precast_some(1 if k < 3 else 2)
                    t_v(tiles[k])
                    t_pv(tiles[k - 1])
                    t_sc(tiles[k])
                    t_sp(tiles[k - 1])
                t_pv(tiles[3])
                t_sp(tiles[3])

            def Dg(hook):
                for j in range(8):
                    b0 = (j % 2) * 4
                    wga, tga = stream_block(GA_OFF + j * 128)
                    for c in range(8):
                        op("pe", lambda e, c=c, wga=wga, b0=b0: e.matmul(PB[b0], lhsT=wga[:, c, :], rhs=hT[:, c, :],
                                                                         start=(c == 0), stop=(c == 7)),
                           reads=[tga, hTn], writes=[tPB[b0]])
                    wgb, tgb = stream_block(GB_OFF + j * 128)
                    for c in range(8):
                        op("pe", lambda e, c=c, wgb=wgb, b0=b0: e.matmul(PB[b0 + 1], lhsT=wgb[:, c, :], rhs=hT[:, c, :],
                                                                         start=(c == 0), stop=(c == 7)),
                           reads=[tgb, hTn], writes=[tPB[b0 + 1]])
                    for c in range(4):
                        op("pe", lambda e, c=c, j=j, b0=b0: e.matmul(PB[b0 + 2], lhsT=wpa[:, c, j * 128:(j + 1) * 128], rhs=aT[:, c, :],
                                                                     start=(c == 0), stop=(c == 3)),
                           reads=[tE[0][0], "aT"], writes=[tPB[b0 + 2]])
                    for c in range(4):
                        op("pe", lambda e, c=c, j=j, b0=b0: e.matmul(PB[b0 + 3], lhsT=wpb[:, c, j * 128:(j + 1) * 128], rhs=bT[:, c, :],
                                                                     start=(c == 0), stop=(c == 3)),
                           reads=["wpb0", "wpb1", "bT"], writes=[tPB[b0 + 3]])
                    op("act", lambda e, b0=b0: e.activation(out=sga, in_=PB[b0], func=AF.Sigmoid),
                       reads=[tPB[b0]], writes=["sga"])
                    op("act", lambda e, b0=b0: e.activation(out=sgb, in_=PB[b0 + 1], func=AF.Sigmoid),
                       reads=[tPB[b0 + 1]], writes=["sgb"])
                    op("dve", lambda e, b0=b0: e.tensor_tensor(out=sga, in0=PB[b0 + 2], in1=sga, op=ALU.mult),
                       reads=[tPB[b0 + 2]], writes=["sga"])
                    op("dve", lambda e, b0=b0: e.tensor_tensor(out=sgb, in0=PB[b0 + 3], in1=sgb, op=ALU.mult),
                       reads=[tPB[b0 + 3]], writes=["sgb"])
                    op("dve", lambda e, j=j: e.tensor_tensor(out=mT[:, j, :], in0=sga, in1=sgb, op=ALU.add),
                       reads=["sga", "sgb"], writes=["mT"])
                    if hook is not None:
                        hook(j)
                if hook is not None:
                    hook(8)

            def E():
                for i in tiles:
                    ts = (i - st * 4) * 128
                    for hf in range(2):
                        bank = 1 + hf
                        for j in range(8):
                            op("pe", lambda e, j=j, ts=ts, hf=hf, bank=bank: e.matmul(
                                PB[bank][:], lhsT=mT[:, j, ts:ts + 128], rhs=wout[:, j, hf * 512:(hf + 1) * 512],
                                start=(j == 0), stop=(j == 7)),
                               reads=["mT", tE[0][1], tE[0][2]], writes=[tPB[bank]])
                        op("dve", lambda e, i=i, hf=hf, bank=bank: e.tensor_tensor(
                            out=H[:, i, hf * 512:(hf + 1) * 512], in0=PB[bank][:], in1=H[:, i, hf * 512:(hf + 1) * 512],
                            op=ALU.add),
                           reads=[tPB[bank]], writes=[tH[i]])
                    op("sp", lambda e, i=i, tok0=tok0: e.dma_start(out=hS_d[tok0 + i * 128: tok0 + (i + 1) * 128, :],
                                                                   in_=H[:, i, :]),
                       reads=[tH[i]], writes=["hS"], dma=f"H{i}")

                if debug and ps_i == 0:
                    for i in tiles:
                        out_ops.append(op("sp", lambda e, i=i: e.dma_start(out=dbg_d[i * 128:(i + 1) * 128, :], in_=H[:, i, :]),
                                          reads=[tH[i]], dma=f"H{i}"))

            def F_pieces():
                def spill_hn2(i, hb2, hnn):
                    op("sp", lambda e: e.dma_start(out=hn2S_d[tok0 + i * 128: tok0 + (i + 1) * 128, :], in_=hb2),
                       reads=[hnn], writes=["hn2S"], dma="hn2S" + hnn)
                sq, rs, tr = norm_pieces(tiles, gfcol, hn2T, "hn2T", ssF, rsF, "ssF", "rsF", (3, 4), after_hn=spill_hn2)

                def router():
                    for k, i in enumerate(tiles):
                        gi = ps_i * NT + i
                        ts = k * 128
                        for c in range(8):
                            op("pe", lambda e, c=c, ts=ts: e.matmul(PB[7][:, 0:20], lhsT=hn2T[:, c, ts:ts + 128], rhs=wr[:, c, :],
                                                                    start=(c == 0), stop=(c == 7)),
                               reads=["hn2T", "wr"], writes=[tPB[7]])
                        op("dve", lambda e, gi=gi: e.tensor_tensor(out=lg[:, gi, :], in0=PB[7][:, 0:20], in1=rb[:], op=ALU.add),
                           reads=[tPB[7], "rb"], writes=["lg"])
                return sq, rs, tr, router
            return dict(A=A_pieces, B=B, C=C, D=Dg, E=E, F=F_pieces)

        def mk_hook(pieces, base):
            if pieces is None:
                return None
            sq, rs, tr = pieces[0], pieces[1], pieces[2]
            router = pieces[3] if len(pieces) > 3 else None

            def hook(it):
                k = it - base
                if 0 <= k < 4:
                    sq(k)
                if k == 3:
                    rs()
                if 5 <= k < 9:
                    tr.b(k - 5)
                if 4 <= k < 8:
                    tr.a(k - 4)
                if k == 9 and router is not None:
                    router()
            return hook

        G = 2 * npass
        sts = [make_st(g) for g in range(G)]
        load_x(0)
        op("pool", lambda e: e.dma_start(out=wpa, in_=wpa_d.rearrange("(c p) n -> p c n", p=128)),
           writes=[tE[0][0]], dma="wg0")
        for a_ in range(2):
            op("pool", lambda e, a_=a_: e.dma_start(
                out=wpb[a_ * 64:(a_ + 1) * 64, :, :],
                in_=wpb_d.rearrange("(a j d) n -> a d j n", a=2, j=4)[a_]),
               writes=[tk(f"wpb{a_}")], dma=f"wpbd{a_}")
        op("pool", lambda e: e.dma_start(out=wout, in_=wout_d.rearrange("(c p) n -> p c n", p=128)),
           writes=[tE[0][1], tE[0][2]], dma="wu0")
        if G > 1:
            load_x(1)
        sq, rs, tr = sts[0]["A"]()
        for k in range(4):
            sq(k)
        rs()
        for k in range(4):
            tr(k)
        for g in range(G):
            fp = sts[g - 1]["F"]() if g > 0 else None
            sts[g]["B"](mk_hook(fp, 0))
            if g >= 1 and g + 1 < G:
                load_x(g + 1)
            sts[g]["C"]()
            ap = sts[g + 1]["A"]() if g + 1 < G else None
            sts[g]["D"](mk_hook(ap, 0))
            sts[g]["E"]()
        sq, rs, tr, router = sts[G - 1]["F"]()
        for k in range(4):
            sq(k)
        rs()
        for k in range(4):
            tr(k)
        router()

        precast_some(100)

        S.barrier("dve", lambda e: e.memset(dummy[:], 0.0))
        NTK = npass * NT

        cdma2 = op("sp", lambda e: e.dma_start(out=gfin[:], in_=gfin_d.partition_broadcast(128)), writes=["gfin"], dma="gfin")

        ND = NTK if stages >= 3 else 0

        def xin_load(gi):
            xb = xin[gi % NXIN]
            xn = f"xin{gi % NXIN}"
            op("sp", lambda e, gi=gi, xb=xb: e.dma_start(out=xb, in_=hn2S_d[gi * 128:(gi + 1) * 128, :]),
               reads=["hn2S"], writes=[xn], dma=xn)

        for gi in range(min(NXIN, ND)):
            xin_load(gi)

        gl = lg[:, :, 0:4]
        el = lg[:, :, 4:20].rearrange("p t (g x) -> p t g x", g=4)
        bc3 = lambda a: a.unsqueeze(2).to_broadcast([128, NTT, 4])
        if npass < NPASS_FULL:
            op("dve", lambda e: e.memset(lg[:, NTK:, :], 0.0), writes=["lg"])
        dv = lambda fn, r, w: op("dve", fn, reads=r, writes=w)
        dv(lambda e: e.tensor_reduce(out=r_m, in_=gl, axis=AX.X, op=ALU.max), ["lg"], ["r_m"])
        dv(lambda e: e.tensor_tensor(out=r_og, in0=gl, in1=bc3(r_m), op=ALU.is_equal), ["lg", "r_m"], ["r_og"])
        dv(lambda e: e.tensor_tensor(out=r_eg, in0=gl, in1=bc3(r_m), op=ALU.subtract), ["lg", "r_m"], ["r_eg"])
        op("act", lambda e: e.activation(out=r_eg, in_=r_eg, func=AF.Exp), reads=[], writes=["r_eg"])
        dv(lambda e: e.tensor_reduce(out=r_gp, in_=r_eg, axis=AX.X, op=ALU.add), ["r_eg"], ["r_gp"])
        dv(lambda e: e.reciprocal(out=r_gp, in_=r_gp), [], ["r_gp"])
        dv(lambda e: e.tensor_tensor(out=r_sel, in0=el, in1=r_og.unsqueeze(3).to_broadcast([128, NTT, 4, 4]), op=ALU.mult),
           ["lg", "r_og"], ["r_sel"])
        dv(lambda e: e.tensor_reduce(out=r_es, in_=r_sel.rearrange("p t g x -> p t x g"), axis=AX.X, op=ALU.add),
           ["r_sel"], ["r_es"])
        dv(lambda e: e.tensor_reduce(out=r_m1, in_=r_es, axis=AX.X, op=ALU.max), ["r_es"], ["r_m1"])
        dv(lambda e: e.tensor_tensor(out=r_o1, in0=r_es, in1=bc3(r_m1), op=ALU.is_equal), ["r_es", "r_m1"], ["r_o1"])
        dv(lambda e: e.scalar_tensor_tensor(out=r_es2, in0=r_o1, scalar=-1e9, in1=r_es, op0=ALU.mult, op1=ALU.add),
           ["r_o1", "r_es"], ["r_es2"])
        dv(lambda e: e.tensor_reduce(out=r_m2, in_=r_es2, axis=AX.X, op=ALU.max), ["r_es2"], ["r_m2"])
        dv(lambda e: e.tensor_tensor(out=r_o2, in0=r_es2, in1=bc3(r_m2), op=ALU.is_equal), ["r_es2", "r_m2"], ["r_o2"])
        dv(lambda e: e.tensor_tensor(out=r_d, in0=r_m2, in1=r_m1, op=ALU.subtract), ["r_m1", "r_m2"], ["r_d"])
        op("act", lambda e: e.activation(out=r_d, in_=r_d, func=AF.Exp), reads=[], writes=["r_d"])
        dv(lambda e: e.tensor_scalar_add(out=r_w1, in0=r_d, scalar1=1.0), ["r_d"], ["r_w1"])
        dv(lambda e: e.reciprocal(out=r_w1, in_=r_w1), [], ["r_w1"])
        dv(lambda e: e.tensor_tensor(out=wB[:], in0=r_d, in1=r_w1, op=ALU.mult), ["r_d", "r_w1"], ["wB"])
        dv(lambda e: e.tensor_tensor(out=wA[:], in0=r_w1, in1=r_gp, op=ALU.mult), ["r_w1", "r_gp"], ["wA"])
        dv(lambda e: e.tensor_tensor(out=wB[:], in0=wB[:], in1=r_gp, op=ALU.mult), ["r_gp"], ["wB"])
        bg = lambda a: a.unsqueeze(3).to_broadcast([128, NTT, 4, 4])
        bx = lambda a: a.unsqueeze(2).to_broadcast([128, NTT, 4, 4])
        dv(lambda e: e.tensor_tensor(out=M1, in0=bg(r_og), in1=bx(r_o1), op=ALU.mult), ["r_og", "r_o1"], ["M1"])
        dv(lambda e: e.tensor_tensor(out=M2, in0=bg(r_og), in1=bx(r_o2), op=ALU.mult), ["r_og", "r_o2"], ["M2"])
        M1f = M1.rearrange("p t g x -> p t (g x)")
        M2f = M2.rearrange("p t g x -> p t (g x)")
        dv(lambda e: e.tensor_tensor(out=Mb.rearrange("p (t n) -> p t n", n=16), in0=M1f, in1=M2f, op=ALU.add),
           ["M1", "M2"], ["Mb"])
        if npass < NPASS_FULL:
            dv(lambda e: e.memset(Mb[:, NTK * 16:], 0.0), [], ["Mb"])
        op("pe", lambda e: e.matmul(PB[0][:], lhsT=utri[:], rhs=Mb, start=True, stop=True),
           reads=["utri", "Mb"], writes=[tPB[0]])
        op("pe", lambda e: e.matmul(PB[1][:], lhsT=ones128[:], rhs=Mb, start=True, stop=True),
           reads=["ones128", "Mb"], writes=[tPB[1]])
        dv(lambda e: e.tensor_copy(out=R1s.rearrange("p t n -> p (t n)"), in_=PB[0][:]), [tPB[0]], ["R1s"])
        dv(lambda e: e.tensor_copy(out=Tts.rearrange("p t n -> p (t n)"), in_=PB[1][:]), [tPB[1]], ["Tts"])
        dv(lambda e: e.tensor_copy(out=Ca, in_=Tts), ["Tts"], ["Ca"])
        cur, nxt, cn, nn = Ca, Cb, "Ca", "Cb"
        sh = 1
        while sh < NTT:
            dv(lambda e, cur=cur, nxt=nxt, sh=sh: e.tensor_copy(out=nxt[:, 0:sh, :], in_=cur[:, 0:sh, :]), [cn], [nn])
            dv(lambda e, cur=cur, nxt=nxt, sh=sh: e.tensor_tensor(out=nxt[:, sh:, :], in0=cur[:, sh:, :],
                                                                  in1=cur[:, 0:NTT - sh, :], op=ALU.add), [cn], [nn])
            cur, nxt, cn, nn = nxt, cur, nn, cn
            sh *= 2
        Cin, cin_n, Cex, cex_n = cur, cn, nxt, nn
        dv(lambda e: e.tensor_tensor(out=Cex, in0=Cin, in1=Tts, op=ALU.subtract), [cin_n, "Tts"], [cex_n])
        ntot = Cin[:, NTT - 1, :]
        thr = cst[:, 0:8]
        tri = cst[:, 8:264].rearrange("p (a b) -> p a b", a=16)
        sio = cst[:, 264:264 + NSLOT]
        cmp8 = r_cmp.rearrange("p a b -> p (a b)")[:, 0:128].rearrange("p (a b) -> p a b", b=8)
        dv(lambda e: e.tensor_tensor(out=cmp8, in0=ntot.unsqueeze(2).to_broadcast([128, 16, 8]),
                                     in1=thr.unsqueeze(1).to_broadcast([128, 16, 8]), op=ALU.is_gt),
           [cin_n, "cst"], ["r_cmp"])
        dv(lambda e: e.tensor_reduce(out=r_nb, in_=cmp8, axis=AX.X, op=ALU.add), ["r_cmp"], ["r_nb"])
        dv(lambda e: e.tensor_tensor(out=r_cmp, in0=r_nb.unsqueeze(1).to_broadcast([128, 16, 16]), in1=tri, op=ALU.mult),
           ["r_nb", "cst"], ["r_cmp"])
        dv(lambda e: e.tensor_reduce(out=r_ob, in_=r_cmp, axis=AX.X, op=ALU.add), ["r_cmp"], ["r_ob"])
        dv(lambda e: e.tensor_tensor(out=r_oe, in0=r_ob, in1=r_nb, op=ALU.add), ["r_ob", "r_nb"], ["r_oe"])
        dv(lambda e: e.tensor_tensor(out=R1s, in0=R1s, in1=Cex, op=ALU.add), [cex_n], ["R1s"])
        dv(lambda e: e.scalar_tensor_tensor(out=R1s, in0=r_ob.unsqueeze(1).to_broadcast([128, NTT, 16]), scalar=512.0,
                                            in1=R1s, op0=ALU.mult, op1=ALU.add), ["r_ob"], ["R1s"])
        for (Mf, mn, pos_i, pn) in ((M1f, "M1", posA, "posA"), (M2f, "M2", posB, "posB")):
            dv(lambda e, Mf=Mf: e.tensor_tensor(out=Mf, in0=Mf, in1=R1s, op=ALU.mult), ["R1s"], [mn])
            dv(lambda e, Mf=Mf: e.tensor_reduce(out=r_pf, in_=Mf, axis=AX.X, op=ALU.add), [mn], ["r_pf"])
            dv(lambda e: e.tensor_scalar(out=r_pf, in0=r_pf, scalar1=0.0, scalar2=float(NSLOT * 512 - 1),
                                         op0=ALU.max, op1=ALU.min), [], ["r_pf"])
            dv(lambda e, pos_i=pos_i: e.tensor_copy(out=pos_i[:], in_=r_pf), ["r_pf"], [pn])
        dv(lambda e: e.tensor_tensor(out=r_cmp2, in0=sio.unsqueeze(2).to_broadcast([128, NSLOT, 16]),
                                     in1=r_oe.unsqueeze(1).to_broadcast([128, NSLOT, 16]), op=ALU.is_ge),
           ["cst", "r_oe"], ["r_cmp2"])
        dv(lambda e: e.tensor_reduce(out=r_eid, in_=r_cmp2, axis=AX.X, op=ALU.add), ["r_cmp2"], ["r_eid"])
        dv(lambda e: e.tensor_scalar(out=r_eid, in0=r_eid, scalar1=15.0, scalar2=128.0, op0=ALU.min, op1=ALU.mult),
           [], ["r_eid"])
        dv(lambda e: e.tensor_tensor(out=r_eid, in0=r_eid, in1=iop[:].to_broadcast([128, NSLOT]), op=ALU.add),
           ["iop"], ["r_eid"])
        dv(lambda e: e.tensor_scalar(out=r_eid, in0=r_eid, scalar1=0.0, scalar2=2047.0, op0=ALU.max, op1=ALU.min),
           [], ["r_eid"])
        dv(lambda e: e.tensor_copy(out=idxW[:], in_=r_eid), ["r_eid"], ["idxW"])
        if debug:
            dr = sb("dbgr_s", [128, 6, NTT], F32)
            dv(lambda e: e.tensor_copy(out=dr[:, 0, :], in_=posA[:]), ["posA"], ["dr"])
            dv(lambda e: e.tensor_copy(out=dr[:, 1, :], in_=posB[:]), ["posB"], ["dr"])
            dv(lambda e: e.tensor_copy(out=dr[:, 2, :], in_=wA[:]), ["wA"], ["dr"])
            dv(lambda e: e.tensor_copy(out=dr[:, 3, :], in_=wB[:]), ["wB"], ["dr"])
            dv(lambda e: e.tensor_copy(out=dr[:, 4, 0:NSLOT], in_=idxW[:]), ["idxW"], ["dr"])
            dv(lambda e: e.tensor_copy(out=dr[:, 5, 0:16], in_=ntot), [cin_n], ["dr"])
            out_ops.append(op("sp", lambda e: e.dma_start(out=dbgr_d, in_=dr[:]), reads=["dr"], dma="dbgr"))

        for gi in range(ND):
            xb = xin[gi % NXIN]
            xn = f"xin{gi % NXIN}"
            for (pos_i, pn) in ((posA, "posA"), (posB, "posB")):
                op("pool", lambda e, gi=gi, xb=xb, pos_i=pos_i: e.indirect_dma_start(
                    out=Xs_d, out_offset=bass.IndirectOffsetOnAxis(pos_i[:, gi:gi + 1], 0), in_=xb, in_offset=None),
                   reads=[xn, pn], writes=[f"Xs{gi % NXIN}"], dma=xn)
            if gi + NXIN < ND:
                xin_load(gi + NXIN)

        NS3 = NSLOT if stages >= 3 else 0

        def slot_loads(s):
            b = s % NEB
            for m in range(3):
                op("pool", lambda e, s=s, b=b, m=m: e.indirect_dma_start(
                    out=wflat[b][m], out_offset=None, in_=wscr_d[m],
                    in_offset=bass.IndirectOffsetOnAxis(idxW[:, s:s + 1], 0)),
                   reads=["idxW", "wscr0", "wscr1"], writes=[tE[b][m]], dma=f"w{'gud'[m]}{b}")
            xb = xs[s % 2]
            xn = f"xs{s % 2}"
            op("sp", lambda e, s=s, xb=xb: e.dma_start(out=xb, in_=Xs_d[s * 512:(s + 1) * 512, :].rearrange("(j p) d -> p j d", p=128)),
               reads=[f"Xs{q}" for q in range(NXIN)], writes=[xn], dma=xn)

        def slot_transposes(s):
            xb = xs[s % 2]
            xn = f"xs{s % 2}"
            xt = xT[s % 2]
            xtn = f"xT{s % 2}"
            for j in range(4):
                bank = j % 2
                pbT = PB[bank][:].bitcast(BF16).rearrange("p (c t) -> p c t", c=8)
                for c in range(8):
                    op("pe", lambda e, c=c, j=j, pbT=pbT, xb=xb: e.transpose(out=pbT[:, c, :], in_=xb[:, j, c * 128:(c + 1) * 128],
                                                                            identity=ident[:]),
                       reads=[xn, "ident"], writes=[tPB[bank]])
                op("dve", lambda e, j=j, pbT=pbT, xt=xt: e.tensor_tensor(
                    out=xt[:, :, j * 128:(j + 1) * 128], in0=pbT,
                    in1=gfcol[:].unsqueeze(2).to_broadcast([128, 8, 128]), op=ALU.mult),
                   reads=[tPB[bank], "gfcol"], writes=[xtn])

        def slot_gateup(s):
            b = s % NEB
            xt = xT[s % 2]
            xtn = f"xT{s % 2}"
            hb_ = s % 2
            for j in range(4):
                bg_, bu_ = 2 + (j % 2) * 2, 3 + (j % 2) * 2
                for c in range(8):
                    op("pe", lambda e, c=c, j=j, b=b, bg_=bg_, xt=xt: e.matmul(
                        PB[bg_][:], lhsT=wgs[b][:, c, j * 128:(j + 1) * 128], rhs=xt[:, c, :],
                        start=(c == 0), stop=(c == 7)),
                       reads=[tE[b][0], xtn], writes=[tPB[bg_]])
                for c in range(8):
                    op("pe", lambda e, c=c, j=j, b=b, bu_=bu_, xt=xt: e.matmul(
                        PB[bu_][:], lhsT=wus[b][:, c, j * 128:(j + 1) * 128], rhs=xt[:, c, :],
                        start=(c == 0), stop=(c == 7)),
                       reads=[tE[b][1], xtn], writes=[tPB[bu_]])
                op("act", lambda e, j=j, bg_=bg_: e.activation(out=sgs[j % 2], in_=PB[bg_][:], func=AF.Silu),
                   reads=[tPB[bg_]], writes=[f"sgs{j % 2}"])
                op("dve", lambda e, j=j, bu_=bu_, hb_=hb_: e.tensor_tensor(out=hid[hb_][:, j, :], in0=PB[bu_][:],
                                                                         in1=sgs[j % 2], op=ALU.mult),
                   reads=[tPB[bu_], f"sgs{j % 2}"], writes=[f"hid{hb_}"])

        def slot_down(s):
            b = s % NEB
            hb_ = s % 2
            for tl in range(4):
                yi = (s * 4 + tl) % NYS
                yb = ys[yi]
                yn = f"ys{yi}"
                for hf in range(2):
                    bank = 6 + hf
                    for j in range(4):
                        op("pe", lambda e, j=j, tl=tl, hf=hf, b=b, hb_=hb_, bank=bank: e.matmul(
                            PB[bank][:], lhsT=hid[hb_][:, j, tl * 128:(tl + 1) * 128],
                            rhs=wds[b][:, j, hf * 512:(hf + 1) * 512], start=(j == 0), stop=(j == 3)),
                           reads=[f"hid{hb_}", tE[b][2]], writes=[tPB[bank]])
                    op("act", lambda e, hf=hf, bank=bank, yb=yb: e.copy(out=yb[:, hf * 512:(hf + 1) * 512], in_=PB[bank][:]),
                       reads=[tPB[bank]], writes=[yn])
                op("sp", lambda e, s=s, tl=tl, yb=yb: e.dma_start(out=Ys_d[s * 512 + tl * 128: s * 512 + (tl + 1) * 128, :], in_=yb),
                   reads=[yn], writes=[f"Ys{yi}"], dma=f"ysd{yi}")

        if NS3:
            slot_loads(0)
            slot_transposes(0)
            slot_loads(1)
        for s in range(NS3):
            slot_gateup(s)
            if s + 1 < NS3:
                slot_transposes(s + 1)
            slot_down(s)
            if s + 2 < NS3:
                slot_loads(s + 2)

        if stages >= 4:
            S.barrier("dve", lambda e: e.memset(dummy[:], 0.0))
        NT4 = NTK if stages >= 4 else 0

        def loads4(gi):
            k2 = gi % N4
            op("sp", lambda e, gi=gi, k2=k2: e.dma_start(out=hb[k2], in_=hS_d[gi * 128:(gi + 1) * 128, :]),
               reads=["hS"], writes=[f"hb{k2}"], dma=f"hb{k2}")
            for (Yb, ynm, pos_i, pn) in ((YA, "YA", posA, "posA"), (YB, "YB", posB, "posB")):
                op("pool", lambda e, gi=gi, k2=k2, Yb=Yb, pos_i=pos_i: e.indirect_dma_start(
                    out=Yb[k2], out_offset=None, in_=Ys_d, in_offset=bass.IndirectOffsetOnAxis(pos_i[:, gi:gi + 1], 0)),
                   reads=[pn], writes=[f"{ynm}{k2}"], dma=f"{ynm}{k2}")

        for gi in range(min(N4, NT4)):
            loads4(gi)
        for gi in range(NT4):
            k2 = gi % N4
            op("dve", lambda e, gi=gi, k2=k2: e.scalar_tensor_tensor(out=hb[k2], in0=YA[k2], scalar=wA[:, gi:gi + 1], in1=hb[k2],
                                                                     op0=ALU.mult, op1=ALU.add),
               reads=[f"YA{k2}", "wA"], writes=[f"hb{k2}"])
            op("dve", lambda e, gi=gi, k2=k2: e.scalar_tensor_tensor(out=hb[k2], in0=YB[k2], scalar=wB[:, gi:gi + 1], in1=hb[k2],
                                                                     op0=ALU.mult, op1=ALU.add),
               reads=[f"YB{k2}", "wB"], writes=[f"hb{k2}"])
            op("act", lambda e, k2=k2: e.activation(out=hn4, in_=hb[k2], func=AF.Square, accum_out=ss4[k2]),
               reads=[f"hb{k2}"], writes=["hn4", f"ss4{k2}"])
            op("dve", lambda e, k2=k2: e.tensor_scalar(out=rs4[k2], in0=ss4[k2], scalar1=1.0 / D, scalar2=EPS, op0=ALU.mult, op1=ALU.add),
               reads=[f"ss4{k2}"], writes=[f"rs4{k2}"])
            op("act", lambda e, k2=k2: e.sqrt(out=rs4[k2], in_=rs4[k2]), reads=[], writes=[f"rs4{k2}"])
            op("dve", lambda e, k2=k2: e.reciprocal(out=rs4[k2], in_=rs4[k2]), reads=[], writes=[f"rs4{k2}"])
            op("dve", lambda e, k2=k2: e.scalar_tensor_tensor(out=hb[k2], in0=hb[k2], scalar=rs4[k2], in1=gfin[:],
                                                              op0=ALU.mult, op1=ALU.mult),
               reads=[f"rs4{k2}", "gfin"], writes=[f"hb{k2}"])
            out_ops.append(op("sp", lambda e, gi=gi, k2=k2: e.dma_start(out=y_d[gi * 128:(gi + 1) * 128, :], in_=hb[k2]),
                              reads=[f"hb{k2}"], dma=f"hb{k2}"))
            if gi + N4 < NT4:
                loads4(gi + N4)

        S.emit(nc, final_wait_ops=out_ops)
    return nc


def _t5_bucket(dist):
    dist = np.asarray(dist, dtype=np.int64)
    nf = np.maximum(dist, 1).astype(np.float32)
    large = 16 + (np.log(nf / np.float32(16)) / np.float32(math.log(128 / 16)) * np.float32(16)).astype(np.int32)
    large = np.minimum(large, 31)
    return np.where(dist < 16, dist, large)


def prep_shared(inp):
    f = lambda a: np.ascontiguousarray(np.asarray(a, dtype=np.float32))
    w_in = f(inp["w_in"])[0]
    perm = np.arange(3840)
    qcols = []
    for j in range(4):
        qcols += list(range(1024 + j * 64, 1024 + (j + 1) * 64))
        qcols += list(range(1024 + (4 + j) * 64, 1024 + (5 + j) * 64))
    perm[1024:1536] = np.array(qcols)
    w_in = np.ascontiguousarray(w_in[:, perm])
    wr = np.concatenate([f(inp["router_group_w"])[0]] + [f(inp["router_expert_w"])[0, g] for g in range(4)], axis=1)
    rb = np.concatenate([f(inp["router_group_b"])[0], f(inp["router_expert_b"])[0].reshape(-1)])[None, :]
    col = lambda v, c: np.ascontiguousarray(f(v).reshape(c, 128).T)
    ws = f(inp["gm_w_spatial"])[0]
    wst = np.ascontiguousarray(ws.transpose(2, 0, 1))
    s_i = np.arange(128)[:, None, None]
    t_i = np.arange(128)[None, None, :]
    wmask = np.broadcast_to((s_i <= t_i), (128, 8, 128)).astype(np.float32)
    bs = f(inp["gm_b_spatial"])[0]
    bst = np.zeros((128, 4, 128), np.float32)
    for g in range(8):
        bst[(g % 2) * 64:(g % 2) * 64 + 64, g // 2, :] = bs[g][None, :]
    rel = f(inp["rel_bias"])
    s_ = np.arange(128)[:, None]
    q_ = np.arange(128)[None, :]
    d_own = q_ - s_
    d_prev = q_ + 128 - s_
    bias = np.zeros((128, 2, 2, 4, 128), np.float32)
    maskc = np.zeros((128, 2, 2, 4, 128), np.float32)
    b_own = _t5_bucket(np.clip(d_own, 0, 127))
    b_prev = _t5_bucket(np.clip(d_prev, 0, 127))
    for kh in range(2):
        for h4 in range(4):
            h = kh * 4 + h4
            bias[:, kh, 0, h4, :] = rel[b_own, h]
            bias[:, kh, 1, h4, :] = rel[b_prev, h]
            maskc[:, kh, 0, h4, :] = np.where(d_own >= 0, 0.0, NEG)
            maskc[:, kh, 1, h4, :] = np.where(d_prev < 128, 0.0, NEG)
    def pmaj(w, c):
        e_, r_, n_ = w.shape
        return np.ascontiguousarray(w.reshape(e_, c, 128, n_).transpose(0, 2, 1, 3).reshape(e_, 128, c * n_))

    return {
        "w_in": w_in,
        "wpa": f(inp["w_proj_a"])[0], "wpb": f(inp["w_proj_b"])[0], "wout": f(inp["w_out"])[0],
        "wgp": pmaj(f(inp["expert_w_gate"])[0], 8), "wup": pmaj(f(inp["expert_w_up"])[0], 8),
        "wdp": pmaj(f(inp["expert_w_down"])[0], 4),
        "utri": np.triu(np.ones((128, 128), np.float32), 1),
        "cst": np.concatenate([np.arange(8, dtype=np.float32) * 512.0,
                               np.tril(np.ones((16, 16), np.float32), -1).reshape(-1),
                               np.arange(32, dtype=np.float32)])[None, :],
        "iop": np.arange(128, dtype=np.float32)[:, None],
        "wr": np.ascontiguousarray(wr), "rb": np.ascontiguousarray(rb),
        "gacol": col(inp["attn_norm_g"], 8), "gfcol": col(inp["ffn_norm_g"], 8), "gvcol": col(inp["gm_v_norm_g"], 4),
        "gfin": f(inp["final_norm_g"]).reshape(1, D),
        "wst": wst, "wmask": wmask, "bst": bst,
        "sinks": np.ascontiguousarray(np.repeat(f(inp["attn_sinks"]).reshape(2, 4), 64, axis=0)),
        "bias": bias, "maskc": maskc,
        "idn": np.eye(128, dtype=np.float32),
    }


def kernel(**inputs):
    shared = prep_shared(inputs)
    x = np.asarray(inputs["x"], dtype=np.float32)
    nc = build()
    in_maps = []
    for c in range(NCORES):
        m = dict(shared)
        m["x"] = np.ascontiguousarray(x[c])
        in_maps.append(m)
    res = run_bass_kernel_spmd(nc, in_maps, core_ids=list(range(NCORES)))
    return np.stack([np.asarray(r["y"], dtype=np.float32) for r in res.results], axis=0)
```
